# Optimizing a Trainium2 kernel written in Bass

```python
import math
import jax, jax.numpy as jnp
from jax import lax
import numpy as np

D_MODEL = 1024
BATCH = 8
SEQ = 2048
DEPTH = 1

CHUNK = 64
Q_BLOCK = 128
EPS = 1e-6
N_HEADS = 16
N_KV_HEADS = 4
HEAD_DIM = 64
ROPE_DIM = HEAD_DIM // 4
ROPE_THETA = 500000.0
IDX_HEADS = 8
IDX_DIM = 64
TOPK_MAX = 256
SSD_D_INNER = 2 * D_MODEL
SSD_HEAD_DIM = 64
SSD_HEADS = SSD_D_INNER // SSD_HEAD_DIM
SSD_GROUPS = 4
SSD_STATE = 128
SSD_CONV = 4
SSD_XBC = SSD_D_INNER + 2 * SSD_GROUPS * SSD_STATE
N_EXPERT_GROUPS = 4
EXPERTS_PER_GROUP = 8
EXPERT_TOPK = 2
EXPERT_HIDDEN = 256
Q_W = N_HEADS * HEAD_DIM
KV_W = N_KV_HEADS * HEAD_DIM
IQ_W = IDX_HEADS * IDX_DIM
SPLITS = (Q_W, KV_W, KV_W, IQ_W, IDX_DIM, IDX_HEADS, SSD_D_INNER, SSD_XBC, SSD_HEADS, D_MODEL, D_MODEL)
IN_PROJ_W = sum(SPLITS)

kernel_name = "hybrid_dsa_ssd_hiermoe_block"


def rmsnorm(x, g):
    xf = x.astype(jnp.float32)
    y = xf * lax.rsqrt(jnp.mean(xf * xf, axis=-1, keepdims=True) + EPS)
    return (y * g.astype(jnp.float32)).astype(x.dtype)


def rope_partial(x, pos):
    half = ROPE_DIM // 2
    inv = ROPE_THETA ** (-jnp.arange(half, dtype=jnp.float32) * 2.0 / ROPE_DIM)
    ang = pos.astype(jnp.float32)[:, None] * inv[None, :]
    cos = jnp.cos(ang)[:, None, :]
    sin = jnp.sin(ang)[:, None, :]
    xr = x[..., :ROPE_DIM].astype(jnp.float32)
    x1, x2 = xr[..., :half], xr[..., half:]
    rot = jnp.concatenate([x1 * cos - x2 * sin, x2 * cos + x1 * sin], axis=-1)
    return jnp.concatenate([rot.astype(x.dtype), x[..., ROPE_DIM:]], axis=-1)


def dsa_attention(q, k, v, iq, ik, iw):
    Bsz, T = q.shape[0], q.shape[1]
    topk = min(TOPK_MAX, T // 4)
    nblk = T // Q_BLOCK
    grp = N_HEADS // N_KV_HEADS
    key_chunk = jnp.arange(T) // CHUNK
    ikf = ik.astype(jnp.float32)

    def to_blocks(a):
        return a.reshape(Bsz, nblk, Q_BLOCK, *a.shape[2:]).swapaxes(0, 1)

    def one_block(args):
        qb, iqb, iwb, start = args
        q_chunk = (start + jnp.arange(Q_BLOCK)) // CHUNK
        admissible = key_chunk[None, :] <= q_chunk[:, None]
        rel = jax.nn.relu(jnp.einsum('bqhd,bsd->bqhs', iqb.astype(jnp.float32), ikf) * IDX_DIM ** -0.5)
        score = jnp.einsum('bqhs,bqh->bqs', rel, iwb.astype(jnp.float32) * IDX_HEADS ** -0.5)
        score = jnp.where(admissible[None], score, -jnp.inf)
        top_s, sel = lax.top_k(score, topk)
        valid = jnp.isfinite(top_s)
        k_sel = jax.vmap(lambda kk, ii: kk[ii])(k, sel)
        v_sel = jax.vmap(lambda vv, ii: vv[ii])(v, sel)
        qg = qb.reshape(Bsz, Q_BLOCK, N_KV_HEADS, grp, HEAD_DIM).astype(jnp.float32)
        logits = jnp.einsum('bqhgd,bqkhd->bqhgk', qg, k_sel.astype(jnp.float32)) * HEAD_DIM ** -0.5
        logits = jnp.where(valid[:, :, None, None, :], logits, -jnp.inf)
        p = jax.nn.softmax(logits, axis=-1)
        o = jnp.einsum('bqhgk,bqkhd->bqhgd', p, v_sel.astype(jnp.float32))
        return o.reshape(Bsz, Q_BLOCK, N_HEADS * HEAD_DIM).astype(q.dtype)

    starts = jnp.arange(nblk, dtype=jnp.int32) * Q_BLOCK
    out = lax.map(one_block, (to_blocks(q), to_blocks(iq), to_blocks(iw), starts))
    return out.swapaxes(0, 1).reshape(Bsz, T, N_HEADS * HEAD_DIM)


def causal_depthwise_conv(x, w, b):
    y = lax.conv_general_dilated(x, w[:, None, :], window_strides=(1,),
                                 padding=[(w.shape[0] - 1, 0)],
                                 dimension_numbers=('NWC', 'WIO', 'NWC'),
                                 feature_group_count=x.shape[-1])
    return y + b


def ssd_chunked(xh, dt, A, Bm, Cm):
    Bsz, T, H, P = xh.shape
    nc = T // CHUNK
    R = H // SSD_GROUPS
    x = (xh * dt[..., None]).reshape(Bsz, nc, CHUNK, SSD_GROUPS, R, P)
    a_cs = jnp.cumsum((dt * A).reshape(Bsz, nc, CHUNK, SSD_GROUPS, R), axis=2)
    Bc = Bm.reshape(Bsz, nc, CHUNK, SSD_GROUPS, SSD_STATE)
    Cc = Cm.reshape(Bsz, nc, CHUNK, SSD_GROUPS, SSD_STATE)
    seg = a_cs[:, :, :, None] - a_cs[:, :, None, :]
    causal = jnp.tril(jnp.ones((CHUNK, CHUNK), dtype=bool))[None, None, :, :, None, None]
    decay = jnp.exp(jnp.where(causal, seg, -jnp.inf))
    cb = jnp.einsum('bclgn,bcsgn->bclsg', Cc, Bc)
    y_diag = jnp.einsum('bclsg,bclsgr,bcsgrp->bclgrp', cb, decay, x)
    decay_to_end = jnp.exp(a_cs[:, :, -1:] - a_cs)
    states = jnp.einsum('bclgn,bclgr,bclgrp->bcgrpn', Bc, decay_to_end, x)
    chunk_decay = jnp.exp(a_cs[:, :, -1])

    def step(h, inp):
        dec, st = inp
        return dec[..., None, None] * h + st, h

    h0 = jnp.zeros((Bsz, SSD_GROUPS, R, P, SSD_STATE), jnp.float32)
    _, prev = lax.scan(step, h0, (chunk_decay.swapaxes(0, 1), states.swapaxes(0, 1)))
    prev = prev.swapaxes(0, 1)
    y_off = jnp.einsum('bclgn,bcgrpn,bclgr->bclgrp', Cc, prev, jnp.exp(a_cs))
    return (y_diag + y_off).reshape(Bsz, T, H, P)


def gated_group_rmsnorm(y, z, g):
    yf = y.astype(jnp.float32) * jax.nn.silu(z.astype(jnp.float32))
    yg = yf.reshape(*yf.shape[:-1], SSD_GROUPS, -1)
    yg = yg * lax.rsqrt(jnp.mean(yg * yg, axis=-1, keepdims=True) + EPS)
    return yg.reshape(yf.shape) * g.astype(jnp.float32)


def hier_moe(h, w_rg, b_rg, w_re, b_re, w_gate, w_up, w_down):
    Bsz, T, D = h.shape
    hf = h.reshape(-1, D)
    g_logits = (hf @ w_rg + b_rg).astype(jnp.float32)
    g_prob = jax.nn.softmax(g_logits, axis=-1)
    g_idx = jnp.argmax(g_logits, axis=-1)
    g_w = jnp.take_along_axis(g_prob, g_idx[:, None], axis=-1)
    e_logits = (hf @ w_re + b_re).astype(jnp.float32).reshape(-1, N_EXPERT_GROUPS, EXPERTS_PER_GROUP)
    e_in = jnp.take_along_axis(e_logits, g_idx[:, None, None], axis=1)[:, 0]
    top_v, top_i = lax.top_k(e_in, EXPERT_TOPK)
    top_p = jax.nn.softmax(top_v, axis=-1)
    within = jnp.sum(jax.nn.one_hot(top_i, EXPERTS_PER_GROUP, dtype=jnp.float32) * top_p[..., None], axis=1)
    combine = jax.nn.one_hot(g_idx, N_EXPERT_GROUPS, dtype=jnp.float32)[:, :, None] * (g_w * within)[:, None, :]
    combine = combine.astype(hf.dtype)
    out = jnp.zeros_like(hf)
    for g in range(N_EXPERT_GROUPS):
        a = jnp.einsum('nd,edf->nef', hf, w_gate[g])
        u = jnp.einsum('nd,edf->nef', hf, w_up[g])
        act = jax.nn.silu(a) * u * combine[:, g, :, None]
        out = out + jnp.einsum('nef,efd->nd', act, w_down[g])
    return out.reshape(Bsz, T, D)


def setup_inputs(seed: int = 0) -> dict:
    key = jax.random.key(seed)
    ks = jax.random.split(key, 24)
    L = DEPTH
    f32 = jnp.float32

    def nrm(k, shape, scale):
        return jax.random.normal(k, shape, f32) * scale

    def gain(k, n):
        return 1.0 + 0.02 * jax.random.normal(k, (L, n), f32)

    dt0 = jnp.exp(jax.random.uniform(ks[8], (L, SSD_HEADS), f32, math.log(1e-3), math.log(1e-1)))
    return {
        "x": nrm(ks[0], (BATCH, SEQ, D_MODEL), 1.0),
        "attn_norm": gain(ks[1], D_MODEL),
        "w_in": nrm(ks[2], (L, D_MODEL, IN_PROJ_W), D_MODEL ** -0.5),
        "q_norm": gain(ks[3], HEAD_DIM),
        "k_norm": gain(ks[4], HEAD_DIM),
        "idx_k_norm": gain(ks[5], IDX_DIM),
        "conv_w": nrm(ks[6], (L, SSD_CONV, SSD_XBC), SSD_CONV ** -0.5),
        "conv_b": nrm(ks[7], (L, SSD_XBC), 0.02),
        "dt_bias": dt0 + jnp.log(-jnp.expm1(-dt0)),
        "a_log": jnp.log(jax.random.uniform(ks[9], (L, SSD_HEADS), f32, 1.0, 16.0)),
        "d_skip": 1.0 + 0.1 * jax.random.normal(ks[10], (L, SSD_HEADS), f32),
        "ssd_norm": gain(ks[11], SSD_D_INNER),
        "w_attn_branch": nrm(ks[12], (L, Q_W, D_MODEL), Q_W ** -0.5),
        "w_ssd_branch": nrm(ks[13], (L, SSD_D_INNER, D_MODEL), SSD_D_INNER ** -0.5),
        "w_out": nrm(ks[14], (L, D_MODEL, D_MODEL), D_MODEL ** -0.5),
        "ffn_norm": gain(ks[15], D_MODEL),
        "w_route_group": nrm(ks[16], (L, D_MODEL, N_EXPERT_GROUPS), D_MODEL ** -0.5),
        "b_route_group": nrm(ks[17], (L, N_EXPERT_GROUPS), 0.01),
        "w_route_expert": nrm(ks[18], (L, D_MODEL, N_EXPERT_GROUPS * EXPERTS_PER_GROUP), D_MODEL ** -0.5),
        "b_route_expert": nrm(ks[19], (L, N_EXPERT_GROUPS * EXPERTS_PER_GROUP), 0.01),
        "w_gate": nrm(ks[20], (L, N_EXPERT_GROUPS, EXPERTS_PER_GROUP, D_MODEL, EXPERT_HIDDEN), D_MODEL ** -0.5),
        "w_up": nrm(ks[21], (L, N_EXPERT_GROUPS, EXPERTS_PER_GROUP, D_MODEL, EXPERT_HIDDEN), D_MODEL ** -0.5),
        "w_down": nrm(ks[22], (L, N_EXPERT_GROUPS, EXPERTS_PER_GROUP, EXPERT_HIDDEN, D_MODEL), EXPERT_HIDDEN ** -0.5),
    }


def reference(x, attn_norm, w_in, q_norm, k_norm, idx_k_norm, conv_w, conv_b, dt_bias, a_log, d_skip,
              ssd_norm, w_attn_branch, w_ssd_branch, w_out, ffn_norm, w_route_group, b_route_group,
              w_route_expert, b_route_expert, w_gate, w_up, w_down):
    Bsz, T, _ = x.shape
    pos = jnp.arange(T)
    offs = np.cumsum(SPLITS)[:-1].tolist()
    for l in range(DEPTH):
        h = rmsnorm(x, attn_norm[l])
        proj = h @ w_in[l]
        (q, k, v, iq, ik, iw, z, xbc, dt_raw, gate_a, gate_b) = jnp.split(proj, offs, axis=-1)
        q = rope_partial(rmsnorm(q.reshape(Bsz, T, N_HEADS, HEAD_DIM), q_norm[l]), pos)
        k = rope_partial(rmsnorm(k.reshape(Bsz, T, N_KV_HEADS, HEAD_DIM), k_norm[l]), pos)
        v = v.reshape(Bsz, T, N_KV_HEADS, HEAD_DIM)
        iq = rope_partial(iq.reshape(Bsz, T, IDX_HEADS, IDX_DIM), pos)
        ik = rope_partial(rmsnorm(ik, idx_k_norm[l])[:, :, None, :], pos)[:, :, 0, :]
        y_attn = dsa_attention(q, k, v, iq, ik, iw)
        xbc = jax.nn.silu(causal_depthwise_conv(xbc, conv_w[l], conv_b[l]))
        xs, bs, cs = jnp.split(xbc, [SSD_D_INNER, SSD_D_INNER + SSD_GROUPS * SSD_STATE], axis=-1)
        xh = xs.reshape(Bsz, T, SSD_HEADS, SSD_HEAD_DIM).astype(jnp.float32)
        dt = jax.nn.softplus(dt_raw.astype(jnp.float32) + dt_bias[l].astype(jnp.float32))
        A = -jnp.exp(a_log[l].astype(jnp.float32))
        y_ssd = ssd_chunked(xh, dt, A,
                            bs.reshape(Bsz, T, SSD_GROUPS, SSD_STATE).astype(jnp.float32),
                            cs.reshape(Bsz, T, SSD_GROUPS, SSD_STATE).astype(jnp.float32))
        y_ssd = y_ssd + d_skip[l].astype(jnp.float32)[:, None] * xh
        y_ssd = gated_group_rmsnorm(y_ssd.reshape(Bsz, T, SSD_D_INNER), z, ssd_norm[l]).astype(x.dtype)
        merged = jax.nn.sigmoid(gate_a) * (y_attn @ w_attn_branch[l]) + jax.nn.sigmoid(gate_b) * (y_ssd @ w_ssd_branch[l])
        x = x + merged @ w_out[l]
        x = x + hier_moe(rmsnorm(x, ffn_norm[l]), w_route_group[l], b_route_group[l], w_route_expert[l],
                         b_route_expert[l], w_gate[l], w_up[l], w_down[l])
    return x
```

```python
import contextlib
import math
import numpy as np
import concourse.bass as bass
import concourse.mybir as mybir
from concourse.bass_utils import run_bass_kernel_spmd

F32 = mybir.dt.float32
BF16 = mybir.dt.bfloat16
U32 = mybir.dt.uint32
AF = mybir.ActivationFunctionType
ALU = mybir.AluOpType
AX = mybir.AxisListType

T = 2048
D = 1024
NT = 16
EPS = 1e-6
NBIS = 16
SPL = (1024, 256, 256, 512, 64, 8, 2048, 3072, 32, 1024, 1024)
OFF = [0]
for _s in SPL:
    OFF.append(OFF[-1] + _s)
(O_Q, O_K, O_V, O_IQ, O_IK, O_IW, O_Z, O_XBC, O_DT, O_GA, O_GB, O_END) = OFF
NW = O_END

C_ID = 0
C_DM = 128
C_NB = 256
C_CT = 384
C_ONE = 512
C_BIS = 640
CW = 672


class Prog:
    ENG = ("pe", "act", "dve", "pool", "sp")

    def __init__(self, nc, es):
        self.nc = nc
        self.es = es
        self.e = {"pe": nc.tensor, "act": nc.scalar, "dve": nc.vector, "pool": nc.gpsimd, "sp": nc.sync}
        self.sem = {}
        self.cnt = {k: 0 for k in self.ENG}
        self.epoch = {k: 0 for k in self.ENG}
        for k in self.ENG:
            self.sem[("e", k, 0)] = es.enter_context(nc.semaphore(f"s_{k}_0"))
        self.dcnt = {}
        self.waited = {k: {} for k in self.ENG}
        self.res = {}
        self.nins = 0
        self.pending = {k: [] for k in self.ENG}

    def _deps(self, eng, reads, writes):
        deps = []
        for r in reads:
            st = self.res.get(r)
            if st and st[0] is not None:
                deps.append((st[0], True))
        for w in writes:
            st = self.res.get(w)
            if st:
                if st[0] is not None:
                    deps.append((st[0], True))
                for t in st[1].values():
                    deps.append((t, False))
        for (tok, strong) in deps:
            key, val = tok
            if key[0] == "e" and key[1] == eng:
                if eng == "pe" or (not strong and eng != "pool"):
                    continue
            if self.waited[eng].get(key, -1) >= val:
                continue
            self.e[eng].wait_ge(self.sem[key], val)
            self.waited[eng][key] = val

    def _commit(self, tok, reads, writes, rkey):
        for r in reads:
            st = self.res.setdefault(r, [None, {}])
            st[1][rkey] = tok
        for w in writes:
            self.res[w] = [tok, {}]

    def op(self, eng, fn, reads=(), writes=()):
        self._deps(eng, reads, writes)
        ins = fn()
        if self.cnt[eng] >= 30000:
            self.epoch[eng] += 1
            self.cnt[eng] = 0
            self.sem[("e", eng, self.epoch[eng])] = self.es.enter_context(
                self.nc.semaphore(f"s_{eng}_{self.epoch[eng]}"))
        key = ("e", eng, self.epoch[eng])
        self.cnt[eng] += 1
        ins.then_inc(self.sem[key], 1)
        self.nins += 1
        for (r_, w_) in self.pending[eng]:
            self._commit((key, self.cnt[eng]), r_, w_, key)
        self.pending[eng] = []
        self._commit((key, self.cnt[eng]), reads, writes, key)
        return ins

    def quiet(self, eng, fn, reads=(), writes=()):
        self._deps(eng, reads, writes)
        self.nins += 1
        self.pending[eng].append((tuple(reads), tuple(writes)))
        return fn()

    def dma(self, q, key, pairs, reads=(), writes=()):
        self._deps(q, reads, writes)
        k = ("d", key)
        if k not in self.sem:
            self.sem[k] = self.es.enter_context(self.nc.semaphore(f"d_{key}"))
            self.dcnt[k] = 0
        for (o, i) in pairs:
            self.e[q].dma_start(out=o, in_=i).then_inc(self.sem[k], 16)
            self.dcnt[k] += 16
            self.nins += 1
        self._commit((k, self.dcnt[k]), reads, writes, k)

    def barrier(self):
        toks = []
        for k in self.ENG:
            if self.cnt[k] > 0:
                toks.append((("e", k, self.epoch[k]), self.cnt[k]))
        for k, v in self.dcnt.items():
            if v > 0:
                toks.append((k, v))
        for eng in self.ENG:
            for (key, val) in toks:
                if key[0] == "e" and key[1] == eng:
                    continue
                if self.waited[eng].get(key, -1) >= val:
                    continue
                self.e[eng].wait_ge(self.sem[key], val)
                self.waited[eng][key] = val
        self.res = {}


def bview(ap, h):
    return ap.rearrange("p (h d) -> p h d", h=h)


def build(stage=99, sub=99):
    nc = bass.Bass("TRN2", target_bir_lowering=False)
    dr = {}

    def din(name, shape):
        dr[name] = nc.dram_tensor(name, list(shape), F32, kind="ExternalInput").ap()
        return dr[name]

    x_d = din("x", [T, D])
    win_d = din("w_in", [D, NW])
    gains = {n: din(n, [1, s]) for n, s in [("attn_norm", D), ("q_norm", 64), ("k_norm", 64), ("idx_k_norm", 64),
                                             ("ffn_norm", D), ("ssd_norm", 2048), ("conv_b", 3072), ("dt_bias", 32),
                                             ("a_log", 32), ("d_skip", 32), ("b_route_group", 4),
                                             ("b_route_expert", 32)]}
    convw_d = din("conv_w", [4, 3072])
    wa_d = din("w_attn_branch", [1024, 1024])
    wb_d = din("w_ssd_branch", [2048, 1024])
    wo_d = din("w_out", [1024, 1024])
    wrg_d = din("w_route_group", [1024, 4])
    wre_d = din("w_route_expert", [1024, 32])
    wg_d = din("w_gate", [32, 1024, 256])
    wu_d = din("w_up", [32, 1024, 256])
    wd_d = din("w_down", [32, 256, 1024])
    cst_d = din("cst", [128, CW])
    rope_d = din("rope", [T, 32])
    out_d = nc.dram_tensor("out", [T, D], F32, kind="ExternalOutput").ap()
    dbg = {}

    def dout(name, shape, dt=F32):
        dbg[name] = nc.dram_tensor(name, list(shape), dt, kind="ExternalOutput").ap()
        return dbg[name]

    es = contextlib.ExitStack()
    with es:
        P = Prog(nc, es)

        uid = [0]

        def sb(name, shape, dt, stack=es):
            uid[0] += 1
            return stack.enter_context(nc.sbuf_tensor(f"sb{uid[0]}_{name}", list(shape), dt))

        ps = [es.enter_context(nc.psum_tensor(f"ps{i}", [128, 512], F32)) for i in range(7)]
        psb = es.enter_context(nc.psum_tensor("psb", [128, 1024], BF16))

        cst = sb("cst", [128, CW], F32)
        cstb = sb("cstb", [128, CW], BF16)
        P.dma("sp", "cst", [(cst[:], cst_d)], writes=["cst"])
        P.op("pool", lambda: nc.gpsimd.tensor_copy(out=cstb[:], in_=cst[:]), reads=["cst"], writes=["cstb"])
        ident = cstb[:, C_ID:C_ID + 128]
        gb = {}

        def load_gain(n, width, st, c0=0):
            gb[n] = sb("g_" + n, [128, width], F32, st)
            P.dma("sp", "gain_" + n, [(gb[n][:], gains[n][:, c0:c0 + width].partition_broadcast(128))], writes=["g_" + n])

        hT = sb("hT", [128, 8, T], BF16)
        ya_spill = nc.dram_tensor("ya_spill", [128, 8, T], BF16, kind="Internal").ap()

        def rmsnorm_T(src_fn, gname, dst, st):
            xb = [sb(f"rn_x{i}", [128, D], F32, st) for i in range(2)]
            xn = [sb(f"rn_xn{i}", [128, D], BF16, st) for i in range(2)]
            junk = sb("rn_junk", [128, D], BF16, st)
            ss = sb("rn_ss", [128, NT], F32, st)
            ms = sb("rn_ms", [128, NT], F32, st)
            sd = sb("rn_sd", [128, NT], F32, st)
            rs = sb("rn_rs", [128, NT], F32, st)
            for c in range(NT):
                b = c % 2
                src_fn(c, xb[b], f"rn_x{b}")
                P.op("act", lambda: nc.scalar.activation(out=junk[:], in_=xb[b][:], func=AF.Square,
                                                         accum_out=ss[:, c:c + 1]),
                     reads=[f"rn_x{b}"], writes=["rn_junk", ("rn_ss", c)])
                P.op("dve", lambda: nc.vector.tensor_scalar(out=ms[:, c:c + 1], in0=ss[:, c:c + 1], scalar1=1.0 / D,
                                                            scalar2=EPS, op0=ALU.mult, op1=ALU.add),
                     reads=[("rn_ss", c)], writes=[("rn_ms", c)])
                P.op("act", lambda: nc.scalar.activation(out=sd[:, c:c + 1], in_=ms[:, c:c + 1], func=AF.Sqrt),
                     reads=[("rn_ms", c)], writes=[("rn_sd", c)])
                P.op("dve", lambda: nc.vector.reciprocal(out=rs[:, c:c + 1], in_=sd[:, c:c + 1]),
                     reads=[("rn_sd", c)], writes=[("rn_rs", c)])
                P.op("dve", lambda: nc.vector.scalar_tensor_tensor(out=xn[b][:], in0=xb[b][:], scalar=rs[:, c:c + 1],
                                                                   in1=gb[gname][:], op0=ALU.mult, op1=ALU.mult),
                     reads=[f"rn_x{b}", ("rn_rs", c), "g_" + gname], writes=[f"rn_xn{b}"])
                for kt in range(8):
                    f = lambda: nc.tensor.transpose(out=psb[:, kt * 128:(kt + 1) * 128],
                                                    in_=xn[b][:, kt * 128:(kt + 1) * 128], identity=ident)
                    if kt < 7:
                        P.quiet("pe", f, reads=[f"rn_xn{b}", "cstb"], writes=["psb"])
                    else:
                        P.op("pe", f, reads=[f"rn_xn{b}", "cstb"], writes=["psb"])
                P.op("act", lambda: nc.scalar.copy(out=dst[:, :, c * 128:(c + 1) * 128], in_=bview(psb[:], 8)),
                     reads=["psb"], writes=[("hT", c)])

        def load_x(c, buf, rname):
            P.dma("sp", rname, [(buf[:], x_d[c * 128:(c + 1) * 128, :])], writes=[rname])

        with contextlib.ExitStack() as st:
            load_gain("attn_norm", D, st)
            rmsnorm_T(load_x, "attn_norm", hT, st)
            P.barrier()

        if stage == 0:
            o = dout("hT_dbg", [128, 8, T], BF16)
            P.dma("sp", "out", [(o, hT[:])])
            P.barrier()
            return nc, dbg

        wst = [None]
        wbf = [None, None]
        wctr = [0]
        win_v = win_d.rearrange("(kt p) n -> p kt n", p=128)

        def alloc_w(st):
            wst[0] = sb("wst", [128, 8, 512], F32, st)
            wbf[0] = sb("wbf0", [128, 8, 512], BF16, st)
            wbf[1] = sb("wbf1", [128, 8, 512], BF16, st)

        def load_w(c0, ncols):
            i = wctr[0] % 2
            wctr[0] += 1
            P.dma("sp", "wst", [(wst[0][:, :, 0:ncols], win_v[:, :, c0:c0 + ncols])], writes=["wst"])
            P.op("pool", lambda: nc.gpsimd.tensor_copy(out=wbf[i][:, :, 0:ncols], in_=wst[0][:, :, 0:ncols]),
                 reads=["wst"], writes=[f"wbf{i}"])
            return i

        def proj_tm(c, wi, ncols, bank):
            for kt in range(8):
                f = lambda: nc.tensor.matmul(ps[bank][:, 0:ncols], lhsT=hT[:, kt, c * 128:(c + 1) * 128],
                                             rhs=wbf[wi][:, kt, 0:ncols], start=(kt == 0), stop=(kt == 7))
                if kt < 7:
                    P.quiet("pe", f, reads=[("hT", c), f"wbf{wi}"], writes=[f"ps{bank}"])
                else:
                    P.op("pe", f, reads=[("hT", c), f"wbf{wi}"], writes=[f"ps{bank}"])

        with contextlib.ExitStack() as st:
            yaT = sb("yaT", [128, 8, T], BF16, st)
            rope = sb("rope", [128, NT, 32], F32, st)
            P.dma("sp", "rope", [(rope[:], rope_d.rearrange("(c p) f -> p c f", p=128))], writes=["rope"])
            for n_ in ("q_norm", "k_norm", "idx_k_norm"):
                load_gain(n_, 64, st)
            qT = sb("qT", [128, 8, T], BF16, st)
            kT2 = sb("kT2", [128, 4, T], BF16, st)
            iqT = sb("iqT", [128, 4, T], BF16, st)
            ikT2 = sb("ikT2", [128, T], BF16, st)
            vaug = sb("vaug", [128, NT, 4, 66], BF16, st)
            iwa = sb("iwa", [128, NT, 8], F32, st)
            iws = sb("iws", [128, NT, 8], F32, st)
            P.op("pool", lambda: nc.gpsimd.memset(vaug[:], 1.0), writes=["vaug"])

            with contextlib.ExitStack() as st2:
                alloc_w(st2)
                sq = sb("e_sq", [128, 512], F32, st2)
                xn = sb("e_xn", [128, 512], F32, st2)
                ra = sb("e_ra", [128, 8, 16], F32, st2)
                rb = sb("e_rb", [128, 8, 16], F32, st2)
                s8 = [sb(f"e_s8{i}", [128, 8], F32, st2) for i in range(4)]
                tmb = sb("e_tmb", [128, 512], BF16, st2)

                def epilogue(c, bank, nh, gname, prescale, dst_fn, dup):
                    pv = bview(ps[bank][:, 0:nh * 64], nh)
                    xv = bview(xn[:, 0:nh * 64], nh)
                    pr = f"ps{bank}"
                    if gname is not None:
                        P.op("act", lambda: nc.scalar.activation(out=sq[:, 0:nh * 64], in_=ps[bank][:, 0:nh * 64],
                                                                 func=AF.Square), reads=[pr], writes=["e_sq"])
                        P.op("dve", lambda: nc.vector.tensor_reduce(out=s8[0][:, 0:nh], in_=bview(sq[:, 0:nh * 64], nh),
                                                                    axis=AX.X, op=ALU.add), reads=["e_sq"], writes=["e_s80"])
                        P.op("dve", lambda: nc.vector.tensor_scalar(out=s8[1][:, 0:nh], in0=s8[0][:, 0:nh], scalar1=1.0 / 64,
                                                                    scalar2=EPS, op0=ALU.mult, op1=ALU.add),
                             reads=["e_s80"], writes=["e_s81"])
                        P.op("act", lambda: nc.scalar.activation(out=s8[2][:, 0:nh], in_=s8[1][:, 0:nh], func=AF.Sqrt),
                             reads=["e_s81"], writes=["e_s82"])
                        P.op("dve", lambda: nc.vector.reciprocal(out=s8[3][:, 0:nh], in_=s8[2][:, 0:nh]),
                             reads=["e_s82"], writes=["e_s83"])
                        P.op("dve", lambda: nc.vector.tensor_tensor(out=xv, in0=pv,
                                                                    in1=s8[3][:, 0:nh].unsqueeze(2).to_broadcast([128, nh, 64]),
                                                                    op=ALU.mult), reads=[pr, "e_s83"], writes=["e_xn"])
                        P.op("dve", lambda: nc.vector.tensor_tensor(out=xv, in0=xv,
                                                                    in1=gb[gname][:].unsqueeze(1).to_broadcast([128, nh, 64]),
                                                                    op=ALU.mult), reads=["e_xn", "g_" + gname], writes=["e_xn"])
                    elif prescale is not None:
                        P.op("dve", lambda: nc.vector.tensor_tensor(out=xv, in0=pv,
                                                                    in1=prescale.unsqueeze(2).to_broadcast([128, nh, 64]),
                                                                    op=ALU.mult), reads=[pr, ("iw", c)], writes=["e_xn"])
                    else:
                        P.op("dve", lambda: nc.vector.tensor_copy(out=xv, in_=pv), reads=[pr], writes=["e_xn"])
                    c16 = rope[:, c, 0:16].unsqueeze(1).to_broadcast([128, nh, 16])
                    nsn = rope[:, c, 16:24].unsqueeze(1).to_broadcast([128, nh, 8])
                    psn = rope[:, c, 24:32].unsqueeze(1).to_broadcast([128, nh, 8])
                    P.op("dve", lambda: nc.vector.tensor_tensor(out=ra[:, 0:nh, :], in0=xv[:, :, 0:16], in1=c16, op=ALU.mult),
                         reads=["e_xn", "rope"], writes=["e_ra"])
                    P.op("dve", lambda: nc.vector.tensor_tensor(out=rb[:, 0:nh, 0:8], in0=xv[:, :, 8:16], in1=nsn, op=ALU.mult),
                         reads=["e_xn", "rope"], writes=["e_rb0"])
                    P.op("dve", lambda: nc.vector.tensor_tensor(out=rb[:, 0:nh, 8:16], in0=xv[:, :, 0:8], in1=psn, op=ALU.mult),
                         reads=["e_xn", "rope"], writes=["e_rb1"])
                    P.op("dve", lambda: nc.vector.tensor_tensor(out=xv[:, :, 0:16], in0=ra[:, 0:nh, :], in1=rb[:, 0:nh, :],
                                                                op=ALU.add), reads=["e_ra", "e_rb0", "e_rb1"], writes=["e_xn"])
                    if dup:
                        tv = tmb[:, 0:nh * 128].rearrange("p (h t d) -> p h t d", h=nh, t=2)
                        P.op("act", lambda: nc.scalar.copy(out=tv[:, :, 0, :], in_=xv), reads=["e_xn"], writes=["e_tmb0"])
                        P.op("act", lambda: nc.scalar.copy(out=tv[:, :, 1, :], in_=xv), reads=["e_xn"], writes=["e_tmb1"])
                        nblk = nh
                    else:
                        P.op("act", lambda: nc.scalar.copy(out=tmb[:, 0:nh * 64], in_=xn[:, 0:nh * 64]), reads=["e_xn"],
                             writes=["e_tmb0", "e_tmb1"])
                        nblk = nh // 2
                    for j in range(nblk):
                        f = lambda: nc.tensor.transpose(out=psb[:, j * 128:(j + 1) * 128], in_=tmb[:, j * 128:(j + 1) * 128],
                                                        identity=ident)
                        if j < nblk - 1:
                            P.quiet("pe", f, reads=["e_tmb0", "e_tmb1", "cstb"], writes=["psb"])
                        else:
                            P.op("pe", f, reads=["e_tmb0", "e_tmb1", "cstb"], writes=["psb"])
                    dst_fn(nblk)

                wi = load_w(O_IK, 72)
                for c in range(NT):
                    bank = c % 2
                    proj_tm(c, wi, 72, bank)
                    P.op("act", lambda: nc.scalar.activation(out=iwa[:, c, :], in_=ps[bank][:, 64:72], func=AF.Abs),
                         reads=[f"ps{bank}"], writes=[("iw", c)])
                    P.op("act", lambda: nc.scalar.activation(out=iws[:, c, :], in_=ps[bank][:, 64:72], func=AF.Sign),
                         reads=[f"ps{bank}"], writes=[("iws", c)])
                    epilogue(c, bank, 1, "idx_k_norm", None,
                             lambda nblk: P.op("act", lambda: nc.scalar.copy(out=ikT2[:, c * 128:(c + 1) * 128],
                                                                             in_=psb[:, 0:128]),
                                               reads=["psb"], writes=[("ikT2", c)]), True)
                wi = load_w(O_IQ, 512)
                for c in range(NT if sub >= 2 else 0):
                    bank = c % 2
                    proj_tm(c, wi, 512, bank)
                    epilogue(c, bank, 8, None, iwa[:, c, :],
                             lambda nblk: P.op("act", lambda: nc.scalar.copy(out=iqT[:, :, c * 128:(c + 1) * 128],
                                                                             in_=bview(psb[:, 0:512], 4)),
                                               reads=["psb"], writes=[("iqT", c)]), False)
                for half in range(2):
                    wi = load_w(O_Q + half * 512, 512)
                    for c in range(NT if sub >= 3 else 0):
                        bank = c % 2
                        proj_tm(c, wi, 512, bank)
                        epilogue(c, bank, 8, "q_norm", None,
                                 lambda nblk: P.op("act", lambda: nc.scalar.copy(
                                     out=qT[:, half * 4:half * 4 + 4, c * 128:(c + 1) * 128], in_=bview(psb[:, 0:512], 4)),
                                     reads=["psb"], writes=[("qT", c, half)]), False)
                wi = load_w(O_K, 512)
                for c in range(NT if sub >= 4 else 0):
                    bank = c % 2
                    proj_tm(c, wi, 512, bank)
                    if sub != 5:
                        P.op("act", lambda: nc.scalar.copy(out=vaug[:, c, :, 0:64], in_=bview(ps[bank][:, 256:512], 4)),
                             reads=[f"ps{bank}", "vaug"], writes=[("vaug", c)])
                    epilogue(c, bank, 4, "k_norm", None,
                             lambda nblk: P.op("act", lambda: nc.scalar.copy(out=kT2[:, :, c * 128:(c + 1) * 128],
                                                                             in_=bview(psb[:, 0:512], 4)),
                                               reads=["psb"], writes=[("kT2", c)]), True)
                P.barrier()

            if stage == 1:
                for nm, t_, shp in [("qT", qT, [128, 8, T]), ("kT2", kT2, [128, 4, T]), ("iqT", iqT, [128, 4, T]),
                                    ("ikT2", ikT2, [128, T])]:
                    o = dout(nm + "_dbg", shp, BF16)
                    P.dma("sp", "out", [(o, t_[:])])
                o = dout("vaug_dbg", [128, NT, 4, 66], BF16)
                P.dma("sp", "out", [(o, vaug[:])])
                o = dout("iwa_dbg", [128, NT, 8], F32)
                P.dma("sp", "out", [(o, iwa[:])])
                o = dout("iws_dbg", [128, NT, 8], F32)
                P.dma("sp", "out", [(o, iws[:])])
                P.barrier()
                return nc, dbg

            with contextlib.ExitStack() as st3:
                score = sb("score", [128, T], F32, st3)
                maskb = sb("maskb", [128, T], BF16, st3)
                junk = sb("ajunk", [128, T], BF16, st3)
                maskT = sb("maskT", [128, NT, 128], BF16, st3)
                relu = [sb(f"relu{i}", [128, 512], BF16, st3) for i in range(2)]
                diag = sb("diag", [128, 8, 128], BF16, st3)
                PT = [sb(f"PT{i}", [128, 512], BF16, st3) for i in range(2)]
                PTm = [sb(f"PTm{i}", [128, 512], BF16, st3) for i in range(2)]
                ytm = sb("ytm", [128, 1024], BF16, st3)
                hi = sb("b_hi", [128, 1], F32, st3)
                lo = sb("b_lo", [128, 1], F32, st3)
                w0 = sb("b_w0", [128, 1], F32, st3)
                wtab = sb("b_wtab", [128, NBIS], F32, st3)
                tt = sb("b_t", [128, 1], F32, st3)
                cnt = sb("b_cnt", [128, 1], F32, st3)
                uu = sb("b_u", [128, 1], F32, st3)
                thr = sb("b_thr", [128, 1], F32, st3)
                rcp = sb("b_rcp", [128, 4], F32, st3)
                pvc = [0]
                for qi in range(NT if sub >= 30 else max(0, sub - 10)):
                    nk = qi + 1
                    nkeys = 128 * nk
                    qs = slice(qi * 128, (qi + 1) * 128)
                    if qi >= 2:
                        for h in range(8):
                            P.op("pool", lambda: nc.gpsimd.tensor_scalar(out=diag[:, h, :], in0=ident, scalar1=iws[:, qi, h:h + 1],
                                                                         scalar2=None, op0=ALU.mult),
                                 reads=["cstb"], writes=[("diag", h)])
                        nkb = (nkeys + 511) // 512
                        for kb in range(nkb):
                            kw = min(512, nkeys - kb * 512)
                            for h in range(8):
                                hf, pr_ = h % 2, h // 2
                                rb = h % 2
                                P.op("pe", lambda: nc.tensor.matmul(ps[rb][:, 0:kw], lhsT=iqT[64 * hf:64 * hf + 64, pr_, qs],
                                                                    rhs=ikT2[64 * hf:64 * hf + 64, kb * 512:kb * 512 + kw],
                                                                    start=True, stop=True),
                                     writes=[f"ps{rb}"])
                                P.op("act", lambda: nc.scalar.activation(out=relu[rb][:, 0:kw], in_=ps[rb][:, 0:kw], func=AF.Relu),
                                     reads=[f"ps{rb}"], writes=[f"relu{rb}"])
                                P.op("pe", lambda: nc.tensor.matmul(ps[2][:, 0:kw], lhsT=diag[:, h, :], rhs=relu[rb][:, 0:kw],
                                                                    start=(h == 0), stop=(h == 7)),
                                     reads=[f"relu{rb}", ("diag", h)], writes=["ps2"])
                            c0 = kb * 512
                            last = (kb == nkb - 1)
                            nd = kw - 128 if last else kw
                            if nd > 0:
                                P.op("dve", lambda: nc.vector.tensor_copy(out=score[:, c0:c0 + nd], in_=ps[2][:, 0:nd]),
                                     reads=["ps2"], writes=[("score", kb)])
                            if last:
                                P.op("dve", lambda: nc.vector.tensor_tensor(out=score[:, nkeys - 128:nkeys], in0=ps[2][:, nd:nd + 128],
                                                                            in1=cst[:, C_DM:C_DM + 128], op=ALU.mult),
                                     reads=["ps2", "cst"], writes=[("score", "d")])
                                P.op("dve", lambda: nc.vector.tensor_tensor(out=score[:, nkeys - 128:nkeys],
                                                                            in0=score[:, nkeys - 128:nkeys],
                                                                            in1=cst[:, C_NB:C_NB + 128], op=ALU.add),
                                     reads=[("score", "d"), "cst"], writes=[("score", "d")])
                        sres = [("score", kb) for kb in range(nkb)] + [("score", "d")]
                        P.op("dve", lambda: nc.vector.tensor_reduce(out=hi[:], in_=score[:, 0:nkeys], axis=AX.X, op=ALU.max),
                             reads=sres, writes=["b_hi"])
                        P.op("dve", lambda: nc.vector.tensor_reduce(out=lo[:], in_=score[:, 0:nkeys - 128], axis=AX.X, op=ALU.min),
                             reads=sres, writes=["b_lo"])
                        P.op("dve", lambda: nc.vector.tensor_tensor(out=w0[:], in0=hi[:], in1=lo[:], op=ALU.subtract),
                             reads=["b_hi", "b_lo"], writes=["b_w0"])
                        P.op("dve", lambda: nc.vector.tensor_scalar(out=wtab[:], in0=cst[:, C_BIS:C_BIS + NBIS], scalar1=w0[:, 0:1],
                                                                    scalar2=None, op0=ALU.mult),
                             reads=["b_w0", "cst"], writes=["b_wtab"])
                        P.op("dve", lambda: nc.vector.tensor_tensor(out=tt[:], in0=lo[:], in1=wtab[:, 0:1], op=ALU.add),
                             reads=["b_lo", "b_wtab"], writes=["b_t"])
                        for it in range(NBIS):
                            P.op("dve", lambda: nc.vector.tensor_scalar(out=junk[:, 0:nkeys], in0=score[:, 0:nkeys], scalar1=tt[:, 0:1],
                                                                        scalar2=None, op0=ALU.is_ge, op1=ALU.add, accum_out=cnt[:]),
                                 reads=sres + ["b_t"], writes=["ajunk", "b_cnt"])
                            P.op("dve", lambda: nc.vector.tensor_scalar(out=uu[:], in0=cnt[:], scalar1=256.0, scalar2=-0.5,
                                                                        op0=ALU.is_ge, op1=ALU.add),
                                 reads=["b_cnt"], writes=["b_u"])
                            P.op("dve", lambda: nc.vector.scalar_tensor_tensor(out=tt[:], in0=uu[:], scalar=wtab[:, it:it + 1],
                                                                               in1=tt[:], op0=ALU.mult, op1=ALU.add),
                                 reads=["b_u", "b_wtab", "b_t"], writes=["b_t"])
                        P.op("dve", lambda: nc.vector.scalar_tensor_tensor(out=thr[:], in0=wtab[:, NBIS - 1:NBIS], scalar=-0.5,
                                                                           in1=tt[:], op0=ALU.mult, op1=ALU.add),
                             reads=["b_wtab", "b_t"], writes=["b_thr"])
                        P.op("dve", lambda: nc.vector.tensor_scalar(out=maskb[:, 0:nkeys], in0=score[:, 0:nkeys], scalar1=thr[:, 0:1],
                                                                    scalar2=None, op0=ALU.is_ge),
                             reads=sres + ["b_thr"], writes=["maskb"])
                    else:
                        if qi == 1:
                            P.op("pool", lambda: nc.gpsimd.tensor_copy(out=maskb[:, 0:128], in_=cstb[:, C_ONE:C_ONE + 128]),
                                 reads=["cstb"], writes=["maskb"])
                        P.op("pool", lambda: nc.gpsimd.tensor_copy(out=maskb[:, nkeys - 128:nkeys], in_=cstb[:, C_DM:C_DM + 128]),
                             reads=["cstb"], writes=["maskb"])
                    for k0 in range(0, nk, 8):
                        n = min(8, nk - k0)
                        for j in range(n):
                            f = lambda: nc.tensor.transpose(out=psb[:, j * 128:(j + 1) * 128],
                                                            in_=maskb[:, (k0 + j) * 128:(k0 + j + 1) * 128], identity=ident)
                            if j < n - 1:
                                P.quiet("pe", f, reads=["maskb", "cstb"], writes=["psb"])
                            else:
                                P.op("pe", f, reads=["maskb", "cstb"], writes=["psb"])
                        P.op("act", lambda: nc.scalar.copy(out=maskT[:, k0:k0 + n, :], in_=bview(psb[:, 0:n * 128], n)),
                             reads=["psb"], writes=[("maskT", k0 // 8)])
                    for g in range(4):
                        ob = 3 + g % 2
                        for kj in range(nk):
                            ks = slice(kj * 128, (kj + 1) * 128)
                            par = pvc[0] % 2
                            pvc[0] += 1
                            sa, sb_ = (5, 6) if par == 0 else (0, 1)
                            P.op("pe", lambda: nc.tensor.matmul(bview(ps[sa][:, 0:256], 2), lhsT=kT2[0:64, g, ks],
                                                                rhs=qT[0:64, 2 * g:2 * g + 2, qs], start=True, stop=True),
                                 writes=[f"ps{sa}"])
                            P.op("pe", lambda: nc.tensor.matmul(bview(ps[sb_][:, 0:256], 2), lhsT=kT2[64:128, g, ks],
                                                                rhs=qT[64:128, 2 * g:2 * g + 2, qs], start=True, stop=True),
                                 writes=[f"ps{sb_}"])
                            P.op("act", lambda: nc.scalar.activation(out=PT[par][:, 0:256], in_=ps[sa][:, 0:256], func=AF.Exp,
                                                                     scale=0.125), reads=[f"ps{sa}"], writes=[("PT", par, 0)])
                            P.op("act", lambda: nc.scalar.activation(out=PT[par][:, 256:512], in_=ps[sb_][:, 0:256], func=AF.Exp,
                                                                     scale=0.125), reads=[f"ps{sb_}"], writes=[("PT", par, 1)])
                            P.op("dve", lambda: nc.vector.tensor_tensor(out=bview(PTm[par][:], 4), in0=bview(PT[par][:], 4),
                                                                        in1=maskT[:, kj, :].unsqueeze(1).to_broadcast([128, 4, 128]),
                                                                        op=ALU.mult),
                                 reads=[("PT", par, 0), ("PT", par, 1), ("maskT", kj // 8)], writes=[("PTm", par)])
                            for j in range(4):
                                hl = [0, 2, 1, 3][j]
                                f = lambda: nc.tensor.matmul(ps[ob][:, hl * 65:hl * 65 + 65], lhsT=PTm[par][:, j * 128:(j + 1) * 128],
                                                             rhs=vaug[:, kj, g, 0:65], start=(kj == 0 and j == 0),
                                                             stop=(kj == nk - 1 and j == 3), skip_group_check=True)
                                if j < 3:
                                    P.quiet("pe", f, reads=[("PTm", par)], writes=[f"ps{ob}"])
                                else:
                                    P.op("pe", f, reads=[("PTm", par)], writes=[f"ps{ob}"])
                        ov = ps[ob][:, 0:260].rearrange("p (h d) -> p h d", h=4)
                        P.op("dve", lambda: nc.vector.reciprocal(out=rcp[:], in_=ov[:, :, 64]), reads=[f"ps{ob}"], writes=["b_rcp"])
                        for hl in range(4):
                            hh = 4 * g + hl
                            P.op("act", lambda: nc.scalar.activation(out=ytm[:, hh * 64:(hh + 1) * 64], in_=ps[ob][:, hl * 65:hl * 65 + 64],
                                                                     func=AF.Copy, scale=rcp[:, hl:hl + 1]),
                                 reads=[f"ps{ob}", "b_rcp"], writes=[("ytm", hh)])
                    for j in range(8):
                        f = lambda: nc.tensor.transpose(out=psb[:, j * 128:(j + 1) * 128], in_=ytm[:, j * 128:(j + 1) * 128],
                                                        identity=ident)
                        if j < 7:
                            P.quiet("pe", f, reads=[("ytm", hh_) for hh_ in range(16)] + ["cstb"], writes=["psb"])
                        else:
                            P.op("pe", f, reads=[("ytm", hh_) for hh_ in range(16)] + ["cstb"], writes=["psb"])
                    P.op("act", lambda: nc.scalar.copy(out=yaT[:, :, qs], in_=bview(psb[:], 8)), reads=["psb"], writes=[("yaT", qi)])
                P.barrier()
            if stage == 2:
                o = dout("yaT_dbg", [128, 8, T], BF16)
                P.dma("sp", "out", [(o, yaT[:])])
                P.barrier()
                return nc, dbg
            P.dma("sp", "spill", [(ya_spill, yaT[:])])
            P.barrier()

        stB = contextlib.ExitStack()
        es.enter_context(stB)
        ysT = sb("ysT", [128, 16, T], BF16, stB)
        with contextlib.ExitStack() as sS:
            G8 = lambda t_, c_, g_: t_[:, c_, 8 * g_:8 * g_ + 8].unsqueeze(2).to_broadcast([128, 8, 64])
            wstS = sb("wstS", [128, 8, 256], F32, sS)
            selb = sb("selb", [128, 32, 128], BF16, sS)
            P.op("pool", lambda: nc.gpsimd.memset(selb[:], 0.0), writes=["selb"])
            for r3 in range(3):
                P.op("pool", lambda: nc.gpsimd.tensor_copy(
                    out=selb[32 * r3:32 * r3 + 32, :, :],
                    in_=cstb[32 * r3:32 * r3 + 32, C_ID + 32 * r3:C_ID + 32 * r3 + 32].unsqueeze(2).to_broadcast([32, 32, 128])),
                    reads=["cstb", "selb"], writes=["selb"])
            if sub == 101:
                P.barrier(); return nc, dbg
            for n_ in ("dt_bias", "a_log", "d_skip"):
                load_gain(n_, 32, sS)
            aneg = sb("aneg", [128, 32], F32, sS)
            P.op("act", lambda: nc.scalar.activation(out=aneg[:], in_=gb["a_log"][:], func=AF.Exp), reads=["g_a_log"], writes=["aneg"])
            P.op("dve", lambda: nc.vector.tensor_scalar(out=aneg[:], in0=aneg[:], scalar1=-1.0, scalar2=None, op0=ALU.mult),
                 reads=["aneg"], writes=["aneg"])
            if sub == 102:
                P.barrier(); return nc, dbg
            cwfm = sb("cwfm", [128, 24, 5], F32, sS)
            s0 = contextlib.ExitStack()
            cw5 = sb("cw5", [5, 3072], F32, s0)
            P.dma("sp", "cw5", [(cw5[0:4, :], convw_d), (cw5[4:5, :], gains["conv_b"])], writes=["cw5"])
            for t_ in range(24):
                f = lambda: nc.tensor.transpose(out=ps[0][:, t_ * 5:t_ * 5 + 5], in_=cw5[:, t_ * 128:(t_ + 1) * 128],
                                                identity=cst[0:5, C_ID:C_ID + 5])
                if t_ < 23:
                    P.quiet("pe", f, reads=["cw5", "cst"], writes=["ps0"])
                else:
                    P.op("pe", f, reads=["cw5", "cst"], writes=["ps0"])
            P.op("dve", lambda: nc.vector.tensor_copy(out=cwfm[:], in_=bview(ps[0][:, 0:120], 24)), reads=["ps0"], writes=["cwfm"])
            P.barrier()
            s0.close()
            if sub == 103:
                P.barrier(); return nc, dbg
            dt_all = sb("dt_all", [128, NT, 32], F32, sS)
            acs = sb("acs", [128, NT, 32], F32, sS)
            ea = sb("ea", [128, NT, 32], F32, sS)
            dtw = sb("dtw", [128, NT, 32], F32, sS)
            cdb = sb("cdb", [128, NT, 32], F32, sS)
            A3 = sb("A3", [128, NT, 128], BF16, sS)
            P.op("pool", lambda: nc.gpsimd.memset(A3[:], 0.0), writes=["A3z"])
            with contextlib.ExitStack() as s1:
                wdt_s = sb("wdt_s", [128, 8, 32], F32, s1)
                wdt = sb("wdt", [128, 8, 32], BF16, s1)
                P.dma("sp", "wdt", [(wdt_s[:], win_v[:, :, O_DT:O_DT + 32])], writes=["wdt_s"])
                P.op("pool", lambda: nc.gpsimd.tensor_copy(out=wdt[:], in_=wdt_s[:]), reads=["wdt_s"], writes=["wdt"])
                f32t = [sb(f"s1_{i}", [128, 32], F32, s1) for i in range(6)]
                a3 = sb("s1_a3", [128, 3, 32], F32, s1)
                Hb = sb("s1_Hb", [128, 128], BF16, s1)
                Mb = sb("s1_Mb", [128, 128], BF16, s1)
                r1 = sb("s1_r1", [128, 128], F32, s1)
                r2 = sb("s1_r2", [128, 128], F32, s1)
                ones_f = cst[:, C_ONE:C_ONE + 128]
                uinc = cst[:, C_CT:C_CT + 128]
                for c in range(NT):
                    cs_ = slice(c * 128, (c + 1) * 128)
                    xd, ax, ee, ll, rr, aa = f32t
                    for kt in range(8):
                        f = lambda: nc.tensor.matmul(ps[1][:, 0:32], lhsT=hT[:, kt, cs_], rhs=wdt[:, kt, :], start=(kt == 0), stop=(kt == 7))
                        if kt < 7:
                            P.quiet("pe", f, reads=["wdt"], writes=["ps1"])
                        else:
                            P.op("pe", f, reads=["wdt"], writes=["ps1"])
                    P.op("dve", lambda: nc.vector.tensor_tensor(out=xd[:], in0=ps[1][:, 0:32], in1=gb["dt_bias"][:], op=ALU.add),
                         reads=["ps1", "g_dt_bias"], writes=["s1_xd"])
                    P.op("act", lambda: nc.scalar.activation(out=ax[:], in_=xd[:], func=AF.Abs), reads=["s1_xd"], writes=["s1_ax"])
                    P.op("act", lambda: nc.scalar.activation(out=ee[:], in_=ax[:], func=AF.Exp, scale=-1.0), reads=["s1_ax"], writes=["s1_ee"])
                    P.op("act", lambda: nc.scalar.activation(out=ll[:], in_=ee[:], func=AF.Ln, bias=1.0), reads=["s1_ee"], writes=["s1_ll"])
                    P.op("dve", lambda: nc.vector.tensor_scalar(out=rr[:], in0=xd[:], scalar1=0.0, scalar2=None, op0=ALU.max),
                         reads=["s1_xd"], writes=["s1_rr"])
                    P.op("dve", lambda: nc.vector.tensor_tensor(out=dt_all[:, c, :], in0=rr[:], in1=ll[:], op=ALU.add),
                         reads=["s1_rr", "s1_ll"], writes=[("dt", c)])
                    P.op("dve", lambda: nc.vector.tensor_tensor(out=aa[:], in0=dt_all[:, c, :], in1=aneg[:], op=ALU.mult),
                         reads=[("dt", c), "aneg"], writes=["s1_aa"])
                    if sub == 104:
                        P.barrier(); return nc, dbg
                    P.op("dve", lambda: nc.vector.tensor_copy(out=a3[:], in_=aa[:].unsqueeze(1).to_broadcast([128, 3, 32])),
                         reads=["s1_aa"], writes=["s1_a3"])
                    if sub == 105:
                        P.barrier(); return nc, dbg
                    P.op("pe", lambda: nc.tensor.matmul(ps[2][:, 0:32], lhsT=uinc, rhs=aa[:], start=True, stop=True),
                         reads=["s1_aa", "cst"], writes=["ps2"])
                    P.op("pe", lambda: nc.tensor.matmul(ps[3][:, 0:32], lhsT=ones_f, rhs=aa[:], start=True, stop=True),
                         reads=["s1_aa", "cst"], writes=["ps3"])
                    P.op("pe", lambda: nc.tensor.matmul(ps[4][0:96, 0:128], lhsT=a3[:].rearrange("p a b -> p (a b)"), rhs=uinc,
                                                        start=True, stop=True),
                         reads=["s1_a3", "cst"], writes=["ps4"])
                    if sub == 106:
                        P.barrier(); return nc, dbg
                    P.op("dve", lambda: nc.vector.tensor_copy(out=acs[:, c, :], in_=ps[2][:, 0:32]), reads=["ps2"], writes=[("acs", c)])
                    if sub == 108:
                        P.barrier(); return nc, dbg
                    P.op("act", lambda: nc.scalar.activation(out=ea[:, c, :], in_=acs[:, c, :], func=AF.Exp), reads=[("acs", c)], writes=[("ea", c)])
                    P.op("dve", lambda: nc.vector.tensor_copy(out=rr[:], in_=ps[3][:, 0:32]), reads=["ps3"], writes=["s1_rr"])
                    P.op("act", lambda: nc.scalar.activation(out=cdb[:, c, :], in_=rr[:], func=AF.Exp), reads=["s1_rr"], writes=[("cdb", c)])
                    if sub == 109:
                        P.barrier(); return nc, dbg
                    P.op("dve", lambda: nc.vector.tensor_tensor(out=xd[:], in0=rr[:], in1=acs[:, c, :], op=ALU.subtract),
                         reads=["s1_rr", ("acs", c)], writes=["s1_xd"])
                    P.op("act", lambda: nc.scalar.activation(out=ee[:], in_=xd[:], func=AF.Exp), reads=["s1_xd"], writes=["s1_ee"])
                    P.op("dve", lambda: nc.vector.tensor_tensor(out=dtw[:, c, :], in0=dt_all[:, c, :], in1=ee[:], op=ALU.mult),
                         reads=[("dt", c), "s1_ee"], writes=[("dtw", c)])
                    if sub == 107:
                        P.barrier(); return nc, dbg
                    P.op("act", lambda: nc.scalar.copy(out=Hb[0:96, :], in_=ps[4][0:96, 0:128]), reads=["ps4"], writes=["s1_Hb"])
                    P.op("dve", lambda: nc.vector.tensor_tensor(out=r1[0:96, :], in0=ps[4][0:96, 0:128], in1=Hb[0:96, :], op=ALU.subtract),
                         reads=["ps4", "s1_Hb"], writes=["s1_r1"])
                    P.op("act", lambda: nc.scalar.copy(out=Mb[0:96, :], in_=r1[0:96, :]), reads=["s1_r1"], writes=["s1_Mb"])
                    P.op("dve", lambda: nc.vector.tensor_tensor(out=r2[0:96, :], in0=r1[0:96, :], in1=Mb[0:96, :], op=ALU.subtract),
                         reads=["s1_r1", "s1_Mb"], writes=["s1_r2"])
                    P.op("pool", lambda: nc.gpsimd.tensor_copy(out=A3[0:32, c, :], in_=Hb[0:32, :]), reads=["s1_Hb", "A3z"], writes=[("A3", c, 0)])
                    P.op("pool", lambda: nc.gpsimd.tensor_copy(out=A3[32:64, c, :], in_=Mb[32:64, :]), reads=["s1_Mb", "A3z"], writes=[("A3", c, 1)])
                    P.op("act", lambda: nc.scalar.copy(out=A3[64:96, c, :], in_=r2[64:96, :]), reads=["s1_r2", "A3z"], writes=[("A3", c, 2)])
                P.barrier()
            if stage == 3 and sub == 1:
                for nm, t_ in [("dt_all", dt_all), ("acs", acs), ("ea", ea), ("dtw", dtw), ("cdb", cdb)]:
                    o = dout(nm + "_dbg", [128, NT, 32], F32)
                    P.dma("sp", "out", [(o, t_[:])])
                o = dout("A3_dbg", [128, NT, 128], BF16)
                P.dma("sp", "out", [(o, A3[:])])
                o = dout("cwfm_dbg", [128, 24, 5], F32)
                P.dma("sp", "out", [(o, cwfm[:])])
                o = dout("selb_dbg", [128, 32, 128], BF16)
                P.dma("sp", "out", [(o, selb[:])])
                P.barrier()
                return nc, dbg

            xs_tm = sb("xs_tm", [128, NT, 512], BF16, sS)
            BT = sb("BT", [128, T], BF16, sS)
            CT = sb("CT", [128, T], BF16, sS)
            B_tm = sb("B_tm", [128, NT, 128], BF16, sS)
            rawb = sb("rawb", [128, T + 4], BF16, sS)
            xcf = [sb(f"xcf{i}", [128, 512], BF16, sS) for i in range(2)]
            dg = [sb(f"dg{i}", [128, 4, 128], BF16, sS) for i in range(2)]
            wch = [sb(f"wch{i}", [128, 8, 128], BF16, sS) for i in range(2)]
            wz = sb("wz", [128, 8, 512], BF16, sS)
            ssdg = sb("ssdg", [128, 512], F32, sS)
            hst = sb("hst", [128, 512], F32, sS)
            hstb = sb("hstb", [128, 512], BF16, sS)
            cbm = sb("cbm", [128, 128], F32, sS)
            seg = [sb(f"seg{i}", [128, 512], F32, sS) for i in range(2)]
            Ee = seg
            MT = [sb(f"MT{i}", [128, 512], BF16, sS) for i in range(2)]
            xdt = sb("xdt", [128, 512], BF16, sS)
            xw = sb("xw", [128, 512], BF16, sS)
            t1 = sb("t1", [128, 512], F32, sS)
            t2 = sb("t2", [128, 512], F32, sS)
            t3 = sb("t3", [128, 512], F32, sS)
            yv = t1
            sz = t3
            ynb = sb("ynb", [128, 512], BF16, sS)
            sjunk = ynb
            g1 = [sb(f"g1_{i}", [128, 1], F32, sS) for i in range(4)]
            P.op("pool", lambda: nc.gpsimd.memset(rawb[:, 0:4], 0.0), writes=["rawb_halo"])
            wctr2 = [0]
            for g in range(4 if sub >= 30 else 1):
                for hf in range(2):
                    c0 = O_Z + g * 512 + hf * 256
                    P.dma("sp", "wstS", [(wstS[:], win_v[:, :, c0:c0 + 256])], writes=["wstS"])
                    P.op("pool", lambda: nc.gpsimd.tensor_copy(out=wz[:, :, hf * 256:(hf + 1) * 256], in_=wstS[:]),
                         reads=["wstS"], writes=[("wz", hf)])
                P.dma("sp", "ssdg", [(ssdg[:], gains["ssd_norm"][:, g * 512:(g + 1) * 512].partition_broadcast(128))], writes=["ssdg"])
                chts = [(O_XBC + g * 512 + j * 128, 4 * g + j, "x", j) for j in range(4)]
                chts += [(O_XBC + 2048 + g * 128, 16 + g, "B", 0), (O_XBC + 2560 + g * 128, 20 + g, "C", 0)]
                for (c0, cti, kind, j) in chts:
                    wi = wctr2[0] % 2
                    wctr2[0] += 1
                    P.dma("sp", "wstS", [(wstS[:, :, 0:128], win_v[:, :, c0:c0 + 128])], writes=["wstS"])
                    P.op("pool", lambda: nc.gpsimd.tensor_copy(out=wch[wi][:], in_=wstS[:, :, 0:128]), reads=["wstS"], writes=[f"wch{wi}"])
                    for jj in range(4):
                        P.op("pool", lambda: nc.gpsimd.tensor_scalar(out=dg[wi][:, jj, :], in0=ident, scalar1=cwfm[:, cti, jj:jj + 1],
                                                                     scalar2=None, op0=ALU.mult),
                             reads=["cstb", "cwfm"], writes=[(f"dg{wi}", jj)])
                    for tb in range(4):
                        bank = tb % 2
                        for kt in range(8):
                            f = lambda: nc.tensor.matmul(ps[bank][:], lhsT=wch[wi][:, kt, :], rhs=hT[:, kt, tb * 512:(tb + 1) * 512],
                                                         start=(kt == 0), stop=(kt == 7))
                            if kt < 7:
                                P.quiet("pe", f, reads=[f"wch{wi}"], writes=[f"ps{bank}"])
                            else:
                                P.op("pe", f, reads=[f"wch{wi}"], writes=[f"ps{bank}"])
                        P.op("act", lambda: nc.scalar.copy(out=rawb[:, 4 + tb * 512:4 + (tb + 1) * 512], in_=ps[bank][:]),
                             reads=[f"ps{bank}"], writes=[("rawb", tb)])
                    for tb in range(4):
                        bank = 2 + tb % 2
                        for jj in range(4):
                            f = lambda: nc.tensor.matmul(ps[bank][:], lhsT=dg[wi][:, jj, :],
                                                         rhs=rawb[:, 1 + tb * 512 + jj:1 + tb * 512 + jj + 512],
                                                         start=(jj == 0), stop=(jj == 3))
                            rd = [(f"dg{wi}", jj), ("rawb", tb), "rawb_halo"] + ([("rawb", tb - 1)] if tb > 0 else [])
                            if jj < 3:
                                P.quiet("pe", f, reads=rd, writes=[f"ps{bank}"])
                            else:
                                P.op("pe", f, reads=rd, writes=[f"ps{bank}"])
                        if kind == "x":
                            xb_ = tb % 2
                            P.op("act", lambda: nc.scalar.activation(out=xcf[xb_][:], in_=ps[bank][:], func=AF.Silu,
                                                                     bias=cwfm[:, cti, 4:5]),
                                 reads=[f"ps{bank}", "cwfm"], writes=[f"xcf{xb_}"])
                            for i4 in range(4):
                                f = lambda: nc.tensor.transpose(out=psb[:, i4 * 128:(i4 + 1) * 128], in_=xcf[xb_][:, i4 * 128:(i4 + 1) * 128],
                                                                identity=ident)
                                if i4 < 3:
                                    P.quiet("pe", f, reads=[f"xcf{xb_}", "cstb"], writes=["psb"])
                                else:
                                    P.op("pe", f, reads=[f"xcf{xb_}", "cstb"], writes=["psb"])
                            P.op("act", lambda: nc.scalar.copy(out=xs_tm[:, tb * 4:(tb + 1) * 4, j * 128:(j + 1) * 128],
                                                               in_=bview(psb[:, 0:512], 4)),
                                 reads=["psb"], writes=[("xs_tm", tb, j)])
                        else:
                            dstT = BT if kind == "B" else CT
                            P.op("act", lambda: nc.scalar.activation(out=dstT[:, tb * 512:(tb + 1) * 512], in_=ps[bank][:], func=AF.Silu,
                                                                     bias=cwfm[:, cti, 4:5]),
                                 reads=[f"ps{bank}", "cwfm"], writes=[(kind + "T", tb)])
                if sub == 202:
                    P.barrier(); return nc, dbg
                for k0 in range(0, NT, 8):
                    for jj in range(8):
                        cc = k0 + jj
                        f = lambda: nc.tensor.transpose(out=psb[:, jj * 128:(jj + 1) * 128], in_=BT[:, cc * 128:(cc + 1) * 128], identity=ident)
                        if jj < 7:
                            P.quiet("pe", f, reads=[("BT", cc // 4), "cstb"], writes=["psb"])
                        else:
                            P.op("pe", f, reads=[("BT", cc // 4), "cstb"], writes=["psb"])
                    P.op("act", lambda: nc.scalar.copy(out=B_tm[:, k0:k0 + 8, :], in_=bview(psb[:], 8)), reads=["psb"], writes=[("B_tm", k0 // 8)])
                P.op("pool", lambda: nc.gpsimd.memset(hst[:], 0.0), writes=["hst"])
                P.op("pool", lambda: nc.gpsimd.memset(hstb[:], 0.0), writes=["hstb"])
                if sub == 203:
                    P.barrier(); return nc, dbg
                xsr = lambda c_: [("xs_tm", c_ // 4, j_) for j_ in range(4)]
                for c in range(NT):
                    cs_ = slice(c * 128, (c + 1) * 128)
                    P.op("pe", lambda: nc.tensor.matmul(ps[0][:, 0:128], lhsT=BT[:, cs_], rhs=CT[:, cs_], start=True, stop=True),
                         reads=[("BT", c // 4), ("CT", c // 4)], writes=["ps0"])
                    P.op("dve", lambda: nc.vector.tensor_tensor(out=cbm[:], in0=ps[0][:, 0:128], in1=cst[:, C_CT:C_CT + 128], op=ALU.mult),
                         reads=["ps0", "cst"], writes=["cbm"])
                    P.op("pool", lambda: nc.gpsimd.tensor_tensor(out=bview(xdt[:], 8), in0=bview(xs_tm[:, c, :], 8), in1=G8(dt_all, c, g),
                                                                 op=ALU.mult), reads=xsr(c), writes=["xdt"])
                    P.op("pool", lambda: nc.gpsimd.tensor_tensor(out=bview(xw[:], 8), in0=bview(xs_tm[:, c, :], 8), in1=G8(dtw, c, g),
                                                                 op=ALU.mult), reads=xsr(c), writes=["xw"])
                    if sub == 2041:
                        P.barrier(); return nc, dbg
                    for hb in ([1] if sub == 2046 else ([1, 0] if sub == 2048 else ([0, 0] if sub == 2049 else range(2)))):
                        bcb = 1 + (hb if sub != 2045 else 0)
                        for hh in range(4):
                            h = 8 * g + 4 * hb + hh
                            f = lambda: nc.tensor.matmul(ps[bcb][:, hh * 128:(hh + 1) * 128], lhsT=selb[:, h, :], rhs=A3[:, c, :],
                                                         start=True, stop=True, skip_group_check=True)
                            if hh < 3:
                                P.quiet("pe", f, reads=["selb"], writes=[f"ps{bcb}"])
                            else:
                                P.op("pe", f, reads=["selb"], writes=[f"ps{bcb}"])
                        for hh in range(4):
                            h = 8 * g + 4 * hb + hh
                            P.op("dve", lambda: nc.vector.tensor_scalar(out=seg[hb][:, hh * 128:(hh + 1) * 128],
                                                                        in0=ps[bcb][:, hh * 128:(hh + 1) * 128],
                                                                        scalar1=acs[:, c, h:h + 1], scalar2=0.0, op0=ALU.subtract, op1=ALU.min),
                                 reads=[f"ps{bcb}"], writes=[(f"seg{hb}", hh), f"Ee{hb}"])
                        if sub == 2042:
                            P.barrier(); return nc, dbg
                        P.op("act", lambda: nc.scalar.activation(out=Ee[hb][:], in_=seg[hb][:], func=AF.Exp),
                             reads=[(f"seg{hb}", hh_) for hh_ in range(4)], writes=[f"Ee{hb}"] + [(f"seg{hb}", hh_) for hh_ in range(4)])
                        P.op("dve", lambda: nc.vector.tensor_tensor(out=bview(MT[hb][:], 4), in0=bview(Ee[hb][:], 4),
                                                                    in1=cbm[:].unsqueeze(1).to_broadcast([128, 4, 128]), op=ALU.mult),
                             reads=[f"Ee{hb}", "cbm"], writes=[f"MT{hb}"])
                        if sub == 2043:
                            P.barrier(); return nc, dbg
                        for hh in range(4):
                            hl = 4 * hb + hh
                            P.op("pe", lambda: nc.tensor.matmul(ps[3][:, hl * 64:(hl + 1) * 64], lhsT=MT[hb][:, hh * 128:(hh + 1) * 128],
                                                                rhs=xdt[:, hl * 64:(hl + 1) * 64], start=True, stop=True, skip_group_check=True),
                                 reads=[f"MT{hb}", "xdt"], writes=["ps3"])
                        if sub == 2044:
                            P.barrier(); return nc, dbg
                    if sub in (204, 2045, 2046, 2047, 2048, 2049):
                        P.barrier(); return nc, dbg
                    yres = ["ps3"]
                    if c > 0:
                        P.op("pe", lambda: nc.tensor.matmul(ps[4][:], lhsT=CT[:, cs_], rhs=hstb[:], start=True, stop=True),
                             reads=[("CT", c // 4), "hstb"], writes=["ps4"])
                        P.op("dve", lambda: nc.vector.tensor_tensor(out=bview(t1[:], 8), in0=bview(ps[4][:], 8), in1=G8(ea, c, g), op=ALU.mult),
                             reads=["ps4"], writes=["t1"])
                        P.op("dve", lambda: nc.vector.tensor_tensor(out=t2[:], in0=ps[3][:], in1=t1[:], op=ALU.add),
                             reads=yres + ["t1"], writes=["t2"])
                    else:
                        P.op("dve", lambda: nc.vector.tensor_copy(out=t2[:], in_=ps[3][:]), reads=yres, writes=["t2"])
                    P.op("pool", lambda: nc.gpsimd.tensor_tensor(out=bview(t3[:], 8), in0=bview(xs_tm[:, c, :], 8), in1=G8(gb["d_skip"][:].unsqueeze(1), 0, g),
                                                                 op=ALU.mult), reads=xsr(c) + ["g_d_skip"], writes=["t3"])
                    P.op("pool", lambda: nc.gpsimd.tensor_tensor(out=yv[:], in0=t2[:], in1=t3[:], op=ALU.add), reads=["t2", "t3"], writes=["t1"])
                    if sub == 205 and c == 1:
                        P.barrier(); return nc, dbg
                    P.op("pe", lambda: nc.tensor.matmul(ps[5][:], lhsT=B_tm[:, c, :], rhs=xw[:], start=True, stop=True),
                         reads=[("B_tm", c // 8), "xw"], writes=["ps5"])
                    P.op("dve", lambda: nc.vector.tensor_tensor(out=bview(hst[:], 8), in0=bview(hst[:], 8), in1=G8(cdb, c, g), op=ALU.mult),
                         reads=["hst"], writes=["hst"])
                    P.op("dve", lambda: nc.vector.tensor_tensor(out=hst[:], in0=ps[5][:], in1=hst[:], op=ALU.add), reads=["ps5", "hst"], writes=["hst"])
                    P.op("act", lambda: nc.scalar.copy(out=hstb[:], in_=hst[:]), reads=["hst"], writes=["hstb"])
                    if sub == 206 and c == 1:
                        P.barrier(); return nc, dbg
                    for kt in range(8):
                        f = lambda: nc.tensor.matmul(ps[6][:], lhsT=hT[:, kt, cs_], rhs=wz[:, kt, :], start=(kt == 0), stop=(kt == 7))
                        if kt < 7:
                            P.quiet("pe", f, reads=[("wz", 0), ("wz", 1)], writes=["ps6"])
                        else:
                            P.op("pe", f, reads=[("wz", 0), ("wz", 1)], writes=["ps6"])
                    P.op("act", lambda: nc.scalar.activation(out=sz[:], in_=ps[6][:], func=AF.Silu), reads=["ps6"], writes=["t3"])
                    P.op("dve", lambda: nc.vector.tensor_tensor(out=yv[:], in0=yv[:], in1=sz[:], op=ALU.mult), reads=["t1", "t3"], writes=["t1"])
                    P.op("act", lambda: nc.scalar.activation(out=sjunk[:], in_=yv[:], func=AF.Square, accum_out=g1[0][:]),
                         reads=["t1"], writes=["ynb", "g1_0"])
                    P.op("dve", lambda: nc.vector.tensor_scalar(out=g1[1][:], in0=g1[0][:], scalar1=1.0 / 512, scalar2=EPS, op0=ALU.mult, op1=ALU.add),
                         reads=["g1_0"], writes=["g1_1"])
                    P.op("act", lambda: nc.scalar.activation(out=g1[2][:], in_=g1[1][:], func=AF.Sqrt), reads=["g1_1"], writes=["g1_2"])
                    P.op("dve", lambda: nc.vector.reciprocal(out=g1[3][:], in_=g1[2][:]), reads=["g1_2"], writes=["g1_3"])
                    P.op("dve", lambda: nc.vector.scalar_tensor_tensor(out=ynb[:], in0=yv[:], scalar=g1[3][:, 0:1], in1=ssdg[:], op0=ALU.mult, op1=ALU.mult),
                         reads=["t1", "g1_3", "ssdg"], writes=["ynb"])
                    for i4 in range(4):
                        f = lambda: nc.tensor.transpose(out=psb[:, i4 * 128:(i4 + 1) * 128], in_=ynb[:, i4 * 128:(i4 + 1) * 128], identity=ident)
                        if i4 < 3:
                            P.quiet("pe", f, reads=["ynb", "cstb"], writes=["psb"])
                        else:
                            P.op("pe", f, reads=["ynb", "cstb"], writes=["psb"])
                    P.op("act", lambda: nc.scalar.copy(out=ysT[:, 4 * g:4 * g + 4, cs_], in_=bview(psb[:, 0:512], 4)), reads=["psb"], writes=[("ysT", g, c)])
            P.barrier()
        if stage == 3:
            o = dout("ysT_dbg", [128, 16, T], BF16)
            P.dma("sp", "out", [(o, ysT[:])])
            P.barrier()
            return nc, dbg

        stM = contextlib.ExitStack()
        es.enter_context(stM)
        mgT = sb("mgT", [128, 8, T], BF16, stM)
        with contextlib.ExitStack() as sM:
            yaT2 = sb("yaT2", [128, 8, T], BF16, sM)
            P.dma("sp", "ya_reload", [(yaT2[:], ya_spill)], writes=["yaT2"])
            wstM = sb("wstM", [128, 16, 128], F32, sM)
            wac = sb("wac", [128, 8, 128], BF16, sM)
            wbc = sb("wbc", [128, 16, 128], BF16, sM)
            wgac = sb("wgac", [128, 8, 128], BF16, sM)
            wgbc = sb("wgbc", [128, 8, 128], BF16, sM)
            sga = [sb(f"sga{i}", [128, 512], F32, sM) for i in range(2)]
            sgb = [sb(f"sgb{i}", [128, 512], F32, sM) for i in range(2)]
            m1 = [sb(f"m1_{i}", [128, 512], F32, sM) for i in range(2)]
            m2 = [sb(f"m2_{i}", [128, 512], F32, sM) for i in range(2)]
            wa_v = wa_d.rearrange("(kt p) n -> p kt n", p=128)
            wb_v = wb_d.rearrange("(kt p) n -> p kt n", p=128)
            it = [0]
            for nt in range(8):
                ns = slice(nt * 128, (nt + 1) * 128)
                for (dst, src, nk_, nm) in [(wac, wa_v[:, :, ns], 8, "wac"), (wbc, wb_v[:, :, ns], 16, "wbc"),
                                            (wgac, win_v[:, :, O_GA + nt * 128:O_GA + (nt + 1) * 128], 8, "wgac"),
                                            (wgbc, win_v[:, :, O_GB + nt * 128:O_GB + (nt + 1) * 128], 8, "wgbc")]:
                    P.dma("sp", "wstM", [(wstM[:, 0:nk_, :], src)], writes=["wstM"])
                    P.op("pool", lambda: nc.gpsimd.tensor_copy(out=dst[:], in_=wstM[:, 0:nk_, :]), reads=["wstM"], writes=[nm])
                for tb in range(4):
                    ts_ = slice(tb * 512, (tb + 1) * 512)
                    par = it[0] % 2
                    it[0] += 1
                    bA, bB, bGA = (0, 1, 2) if par == 0 else (4, 5, 6)
                    bGB = 3

                    def acc(bank, wt, nk_, rhsT, nm, rd):
                        for kt in range(nk_):
                            f = lambda: nc.tensor.matmul(ps[bank][:], lhsT=wt[:, kt, :], rhs=rhsT[:, kt, ts_], start=(kt == 0), stop=(kt == nk_ - 1))
                            if kt < nk_ - 1:
                                P.quiet("pe", f, reads=[nm] + rd, writes=[f"ps{bank}"])
                            else:
                                P.op("pe", f, reads=[nm] + rd, writes=[f"ps{bank}"])
                    acc(bGA, wgac, 8, hT, "wgac", [])
                    acc(bGB, wgbc, 8, hT, "wgbc", [])
                    acc(bA, wac, 8, yaT2, "wac", ["yaT2"])
                    acc(bB, wbc, 16, ysT, "wbc", [])
                    P.op("act", lambda: nc.scalar.activation(out=sga[par][:], in_=ps[bGA][:], func=AF.Sigmoid), reads=[f"ps{bGA}"], writes=[f"sga{par}"])
                    P.op("act", lambda: nc.scalar.activation(out=sgb[par][:], in_=ps[bGB][:], func=AF.Sigmoid), reads=[f"ps{bGB}"], writes=[f"sgb{par}"])
                    P.op("dve", lambda: nc.vector.tensor_tensor(out=m1[par][:], in0=ps[bA][:], in1=sga[par][:], op=ALU.mult),
                         reads=[f"ps{bA}", f"sga{par}"], writes=[f"m1_{par}"])
                    P.op("dve", lambda: nc.vector.tensor_tensor(out=m2[par][:], in0=ps[bB][:], in1=sgb[par][:], op=ALU.mult),
                         reads=[f"ps{bB}", f"sgb{par}"], writes=[f"m2_{par}"])
                    P.op("pool", lambda: nc.gpsimd.tensor_tensor(out=mgT[:, nt, ts_], in0=m1[par][:], in1=m2[par][:], op=ALU.add),
                         reads=[f"m1_{par}", f"m2_{par}"], writes=[("mgT", nt, tb)])
            P.barrier()
        if stage == 4:
            o = dout("mgT_dbg", [128, 8, T], BF16)
            P.dma("sp", "out", [(o, mgT[:])])
            P.barrier()
            return nc, dbg

        x1 = ysT[:].bitcast(F32)
        assert list(x1.shape) == [128, NT, D], x1.shape
        with contextlib.ExitStack() as sO:
            wstO = sb("wstO", [128, 8, 256], F32, sO)
            wo = sb("wo", [128, 8, D], BF16, sO)
            wo_v = wo_d.rearrange("(kt p) n -> p kt n", p=128)
            for q4 in range(4):
                P.dma("sp", "wstO", [(wstO[:], wo_v[:, :, q4 * 256:(q4 + 1) * 256])], writes=["wstO"])
                P.op("pool", lambda: nc.gpsimd.tensor_copy(out=wo[:, :, q4 * 256:(q4 + 1) * 256], in_=wstO[:]), reads=["wstO"], writes=[("wo", q4)])
            for c in range(NT):
                cs_ = slice(c * 128, (c + 1) * 128)
                P.dma("sp", f"x1ld{c % 2}", [(x1[:, c, :], x_d[cs_, :])], writes=[("x1", c)])
                for hf in range(2):
                    bank = (2 * c + hf) % 4
                    for kt in range(8):
                        f = lambda: nc.tensor.matmul(ps[bank][:], lhsT=mgT[:, kt, cs_], rhs=wo[:, kt, hf * 512:(hf + 1) * 512],
                                                     start=(kt == 0), stop=(kt == 7))
                        rd = [("wo", 2 * hf), ("wo", 2 * hf + 1)]
                        if kt < 7:
                            P.quiet("pe", f, reads=rd, writes=[f"ps{bank}"])
                        else:
                            P.op("pe", f, reads=rd, writes=[f"ps{bank}"])
                    P.op("dve", lambda: nc.vector.tensor_tensor(out=x1[:, c, hf * 512:(hf + 1) * 512], in0=ps[bank][:],
                                                                in1=x1[:, c, hf * 512:(hf + 1) * 512], op=ALU.add),
                         reads=[f"ps{bank}", ("x1", c)], writes=[("x1", c)])
            P.barrier()
        stM.close()
        if stage == 5:
            o = dout("x1_dbg", [128, NT, D], F32)
            P.dma("sp", "out", [(o, x1)])
            P.barrier()
            return nc, dbg

        with contextlib.ExitStack() as sN:
            load_gain("ffn_norm", D, sN)

            def from_x1(c, buf, rname):
                P.op("pool", lambda: nc.gpsimd.tensor_copy(out=buf[:], in_=x1[:, c, :]), reads=[("x1", c)], writes=[rname])
            rmsnorm_T(from_x1, "ffn_norm", hT, sN)
            P.barrier()

        with contextlib.ExitStack() as sE:
            selm = sb("selm", [128, 32, 128], BF16, sE)
            P.op("pool", lambda: nc.gpsimd.memset(selm[:], 0.0), writes=["selm"])
            for r3 in range(3):
                P.op("pool", lambda: nc.gpsimd.tensor_copy(
                    out=selm[32 * r3:32 * r3 + 32, :, :],
                    in_=cstb[32 * r3:32 * r3 + 32, C_ID + 32 * r3:C_ID + 32 * r3 + 32].unsqueeze(2).to_broadcast([32, 32, 128])),
                    reads=["cstb", "selm"], writes=["selm"])
            cT3 = sb("cT3", [128, T], BF16, sE)
            P.op("pool", lambda: nc.gpsimd.memset(cT3[:], 0.0), writes=["cT3z"])
            with contextlib.ExitStack() as sR:
                wr_s = sb("wr_s", [128, 8, 36], F32, sR)
                wr = sb("wr", [128, 8, 36], BF16, sR)
                P.dma("sp", "wr_s", [(wr_s[:, :, 0:4], wrg_d.rearrange("(kt p) n -> p kt n", p=128)),
                                     (wr_s[:, :, 4:36], wre_d.rearrange("(kt p) n -> p kt n", p=128))], writes=["wr_s"])
                P.op("pool", lambda: nc.gpsimd.tensor_copy(out=wr[:], in_=wr_s[:]), reads=["wr_s"], writes=["wr"])
                rb_ = sb("rbias", [128, 36], F32, sR)
                P.dma("sp", "rbias", [(rb_[:, 0:4], gains["b_route_group"].partition_broadcast(128)),
                                      (rb_[:, 4:36], gains["b_route_expert"].partition_broadcast(128))], writes=["rbias"])
                lg = sb("r_lg", [128, 36], F32, sR)
                r1c = [sb(f"r_c{i}", [128, 1], F32, sR) for i in range(8)]
                oh = sb("r_oh", [128, 4], F32, sR)
                ge = sb("r_ge", [128, 4], F32, sR)
                tmp48 = sb("r_t48", [128, 4, 8], F32, sR)
                ein = sb("r_ein", [128, 8], F32, sR)
                top8 = sb("r_top8", [128, 8], F32, sR)
                msel = sb("r_msel", [128, 8], F32, sR)
                wex = sb("r_wex", [128, 8], F32, sR)
                comb = sb("r_comb", [128, 4, 8], F32, sR)
                comb3 = sb("r_comb3", [128, 3, 32], F32, sR)
                Hb2 = sb("r_Hb", [128, 128], BF16, sR)
                Mb2 = sb("r_Mb", [128, 128], BF16, sR)
                q1 = sb("r_q1", [128, 128], F32, sR)
                q2 = sb("r_q2", [128, 128], F32, sR)
                mx, nmx, sme, gw, m21, den, rden, sc_ = r1c
                for c in range(NT):
                    cs_ = slice(c * 128, (c + 1) * 128)
                    for kt in range(8):
                        f = lambda: nc.tensor.matmul(ps[0][:, 0:36], lhsT=hT[:, kt, cs_], rhs=wr[:, kt, :], start=(kt == 0), stop=(kt == 7))
                        if kt < 7:
                            P.quiet("pe", f, reads=["wr"], writes=["ps0"])
                        else:
                            P.op("pe", f, reads=["wr"], writes=["ps0"])
                    P.op("dve", lambda: nc.vector.tensor_tensor(out=lg[:], in0=ps[0][:, 0:36], in1=rb_[:], op=ALU.add), reads=["ps0", "rbias"], writes=["r_lg"])
                    P.op("dve", lambda: nc.vector.tensor_reduce(out=mx[:], in_=lg[:, 0:4], axis=AX.X, op=ALU.max), reads=["r_lg"], writes=["r_mx"])
                    P.op("dve", lambda: nc.vector.tensor_scalar(out=oh[:], in0=lg[:, 0:4], scalar1=mx[:, 0:1], scalar2=None, op0=ALU.is_ge),
                         reads=["r_lg", "r_mx"], writes=["r_oh"])
                    P.op("dve", lambda: nc.vector.tensor_scalar(out=nmx[:], in0=mx[:], scalar1=-1.0, scalar2=None, op0=ALU.mult), reads=["r_mx"], writes=["r_nmx"])
                    P.op("act", lambda: nc.scalar.activation(out=ge[:], in_=lg[:, 0:4], func=AF.Exp, bias=nmx[:, 0:1], accum_out=sme[:]),
                         reads=["r_lg", "r_nmx"], writes=["r_ge", "r_sme"])
                    P.op("dve", lambda: nc.vector.reciprocal(out=gw[:], in_=sme[:]), reads=["r_sme"], writes=["r_gw"])
                    P.op("dve", lambda: nc.vector.tensor_tensor(out=tmp48[:], in0=bview(lg[:, 4:36], 4), in1=oh[:].unsqueeze(2).to_broadcast([128, 4, 8]),
                                                                op=ALU.mult), reads=["r_lg", "r_oh"], writes=["r_t48"])
                    P.op("dve", lambda: nc.vector.tensor_reduce(out=ein[:], in_=tmp48[:].rearrange("p g e -> p e g"), axis=AX.X, op=ALU.add),
                         reads=["r_t48"], writes=["r_ein"])
                    P.op("dve", lambda: nc.vector.max(out=top8[:], in_=ein[:]), reads=["r_ein"], writes=["r_top8"])
                    P.op("dve", lambda: nc.vector.tensor_scalar(out=msel[:], in0=ein[:], scalar1=top8[:, 1:2], scalar2=None, op0=ALU.is_ge),
                         reads=["r_ein", "r_top8"], writes=["r_msel"])
                    P.op("dve", lambda: nc.vector.tensor_scalar(out=nmx[:], in0=top8[:, 0:1], scalar1=-1.0, scalar2=None, op0=ALU.mult),
                         reads=["r_top8"], writes=["r_nmx"])
                    P.op("act", lambda: nc.scalar.activation(out=wex[:], in_=ein[:], func=AF.Exp, bias=nmx[:, 0:1]), reads=["r_ein", "r_nmx"], writes=["r_wex"])
                    P.op("act", lambda: nc.scalar.activation(out=m21[:], in_=top8[:, 1:2], func=AF.Exp, bias=nmx[:, 0:1]), reads=["r_top8", "r_nmx"], writes=["r_m21"])
                    P.op("dve", lambda: nc.vector.tensor_scalar(out=den[:], in0=m21[:], scalar1=1.0, scalar2=None, op0=ALU.add), reads=["r_m21"], writes=["r_den"])
                    P.op("dve", lambda: nc.vector.reciprocal(out=rden[:], in_=den[:]), reads=["r_den"], writes=["r_rden"])
                    P.op("dve", lambda: nc.vector.tensor_tensor(out=sc_[:], in0=rden[:], in1=gw[:], op=ALU.mult), reads=["r_rden", "r_gw"], writes=["r_sc"])
                    P.op("dve", lambda: nc.vector.tensor_tensor(out=wex[:], in0=wex[:], in1=msel[:], op=ALU.mult), reads=["r_wex", "r_msel"], writes=["r_wex"])
                    P.op("dve", lambda: nc.vector.tensor_scalar(out=wex[:], in0=wex[:], scalar1=sc_[:, 0:1], scalar2=None, op0=ALU.mult),
                         reads=["r_wex", "r_sc"], writes=["r_wex"])
                    P.op("dve", lambda: nc.vector.tensor_tensor(out=comb[:], in0=oh[:].unsqueeze(2).to_broadcast([128, 4, 8]),
                                                                in1=wex[:].unsqueeze(1).to_broadcast([128, 4, 8]), op=ALU.mult),
                         reads=["r_oh", "r_wex"], writes=["r_comb"])
                    P.op("dve", lambda: nc.vector.tensor_copy(out=comb3[:], in_=comb[:].rearrange("p g e -> p (g e)").unsqueeze(1).to_broadcast([128, 3, 32])),
                         reads=["r_comb"], writes=["r_comb3"])
                    P.op("pe", lambda: nc.tensor.transpose(out=ps[1][0:96, 0:128], in_=comb3[:].rearrange("p a b -> p (a b)"),
                                                           identity=cst[:, C_ID:C_ID + 128]),
                         reads=["r_comb3", "cst"], writes=["ps1"])
                    P.op("act", lambda: nc.scalar.copy(out=Hb2[0:96, :], in_=ps[1][0:96, 0:128]), reads=["ps1"], writes=["r_Hb"])
                    P.op("dve", lambda: nc.vector.tensor_tensor(out=q1[0:96, :], in0=ps[1][0:96, 0:128], in1=Hb2[0:96, :], op=ALU.subtract),
                         reads=["ps1", "r_Hb"], writes=["r_q1"])
                    P.op("act", lambda: nc.scalar.copy(out=Mb2[0:96, :], in_=q1[0:96, :]), reads=["r_q1"], writes=["r_Mb"])
                    P.op("dve", lambda: nc.vector.tensor_tensor(out=q2[0:96, :], in0=q1[0:96, :], in1=Mb2[0:96, :], op=ALU.subtract),
                         reads=["r_q1", "r_Mb"], writes=["r_q2"])
                    P.op("pool", lambda: nc.gpsimd.tensor_copy(out=cT3[0:32, cs_], in_=Hb2[0:32, :]), reads=["r_Hb", "cT3z"], writes=[("cT3", c, 0)])
                    P.op("pool", lambda: nc.gpsimd.tensor_copy(out=cT3[32:64, cs_], in_=Mb2[32:64, :]), reads=["r_Mb", "cT3z"], writes=[("cT3", c, 1)])
                    P.op("act", lambda: nc.scalar.copy(out=cT3[64:96, cs_], in_=q2[64:96, :]), reads=["r_q2", "cT3z"], writes=[("cT3", c, 2)])
                P.barrier()
            if stage == 6:
                o = dout("cT3_dbg", [128, T], BF16)
                P.dma("sp", "out", [(o, cT3[:])])
                o = dout("h2T_dbg", [128, 8, T], BF16)
                P.dma("sp", "out", [(o, hT[:])])
                P.barrier()
                return nc, dbg

            NE = 32 if sub >= 30 else 2
            wstE = [sb(f"wstE{i}", [128, 8, 256], F32, sE) for i in range(2)]
            wgu = [sb(f"wgu{i}", [128, 8, 512], BF16, sE) for i in range(2)]
            wdn = [sb(f"wdn{i}", [128, 2, D], BF16, sE) for i in range(4)]
            actT = [sb(f"actT{i}", [128, 2, T], BF16, sE) for i in range(2)]
            sgs = [sb(f"sgs{i}", [128, 512], F32, sE) for i in range(2)]
            tms = [sb(f"tms{i}", [128, 512], F32, sE) for i in range(2)]
            stc = [0]
            itc = [0]
            for e in range(NE):
                sl = e % 2
                dsl = e % 4
                for (k_, src) in [(0, wg_d[e].rearrange("(kt p) n -> p kt n", p=128)), (1, wu_d[e].rearrange("(kt p) n -> p kt n", p=128))]:
                    si = stc[0] % 2
                    stc[0] += 1
                    P.dma("sp", f"wstE{si}", [(wstE[si][:], src)], writes=[f"wstE{si}"])
                    P.op("pool", lambda: nc.gpsimd.tensor_copy(out=wgu[sl][:, :, k_ * 256:(k_ + 1) * 256], in_=wstE[si][:]),
                         reads=[f"wstE{si}"], writes=[(f"wgu{sl}", k_)])
                si = stc[0] % 2
                stc[0] += 1
                P.dma("sp", f"wstE{si}", [(wstE[si][:].rearrange("p a b -> p (a b)").rearrange("p (f n) -> p f n", f=2),
                                           wd_d[e].rearrange("(ft p) n -> p ft n", p=128))],
                      writes=[f"wstE{si}"])
                P.op("pool", lambda: nc.gpsimd.tensor_copy(out=wdn[dsl][:].rearrange("p a b -> p (a b)"), in_=wstE[si][:].rearrange("p a b -> p (a b)")),
                     reads=[f"wstE{si}"], writes=[f"wdn{dsl}"])
                for tb in range(4):
                    ts_ = slice(tb * 512, (tb + 1) * 512)
                    P.op("pe", lambda: nc.tensor.matmul(ps[4][:], lhsT=selm[:, e, :], rhs=cT3[:, ts_], start=True, stop=True),
                         reads=["selm"], writes=["ps4"])
                    for ft in range(2):
                        par = itc[0] % 2
                        itc[0] += 1
                        bG, bU = (0, 1) if par == 0 else (2, 3)
                        for (bank, k_) in [(bG, 0), (bU, 1)]:
                            for kt in range(8):
                                f = lambda: nc.tensor.matmul(ps[bank][:], lhsT=wgu[sl][:, kt, k_ * 256 + ft * 128:k_ * 256 + (ft + 1) * 128],
                                                             rhs=hT[:, kt, ts_], start=(kt == 0), stop=(kt == 7))
                                if kt < 7:
                                    P.quiet("pe", f, reads=[(f"wgu{sl}", k_)], writes=[f"ps{bank}"])
                                else:
                                    P.op("pe", f, reads=[(f"wgu{sl}", k_)], writes=[f"ps{bank}"])
                        P.op("act", lambda: nc.scalar.activation(out=sgs[par][:], in_=ps[bG][:], func=AF.Silu), reads=[f"ps{bG}"], writes=[f"sgs{par}"])
                        P.op("dve", lambda: nc.vector.tensor_tensor(out=tms[par][:], in0=ps[bU][:], in1=sgs[par][:], op=ALU.mult),
                             reads=[f"ps{bU}", f"sgs{par}"], writes=[f"tms{par}"])
                        P.op("dve", lambda: nc.vector.tensor_tensor(out=actT[sl][:, ft, ts_], in0=ps[4][:], in1=tms[par][:], op=ALU.mult),
                             reads=["ps4", f"tms{par}"], writes=[(f"actT{sl}", ft, tb)])
                if e % 2 == 1:
                    for c in range(NT):
                        cs_ = slice(c * 128, (c + 1) * 128)
                        for hf in range(2):
                            bank = 5 + (2 * c + hf) % 2
                            n_ = 0
                            for ee in (e - 1, e):
                                for ft in range(2):
                                    f = lambda: nc.tensor.matmul(ps[bank][:], lhsT=actT[ee % 2][:, ft, cs_], rhs=wdn[ee % 4][:, ft, hf * 512:(hf + 1) * 512],
                                                                 start=(n_ == 0), stop=(n_ == 3))
                                    rd = [(f"actT{ee % 2}", ft, c // 4), f"wdn{ee % 4}"]
                                    if n_ < 3:
                                        P.quiet("pe", f, reads=rd, writes=[f"ps{bank}"])
                                    else:
                                        P.op("pe", f, reads=rd, writes=[f"ps{bank}"])
                                    n_ += 1
                            P.op("dve", lambda: nc.vector.tensor_tensor(out=x1[:, c, hf * 512:(hf + 1) * 512], in0=ps[bank][:],
                                                                        in1=x1[:, c, hf * 512:(hf + 1) * 512], op=ALU.add),
                                 reads=[f"ps{bank}", ("x1", c)], writes=[("x1", c)])
            for c in range(NT):
                P.dma("sp", "out", [(out_d[c * 128:(c + 1) * 128, :], x1[:, c, :])], reads=[("x1", c)])
            P.barrier()
    return nc, dbg


def host_consts():
    cst = np.zeros((128, CW), np.float32)
    cst[:, C_ID:C_ID + 128] = np.eye(128, dtype=np.float32)
    dm = np.ones((128, 128), np.float32)
    dm[0:64, 64:128] = 0.0
    cst[:, C_DM:C_DM + 128] = dm
    cst[:, C_NB:C_NB + 128] = (dm - 1.0) * 1e30
    cst[:, C_CT:C_CT + 128] = np.triu(np.ones((128, 128), np.float32))
    cst[:, C_ONE:C_ONE + 128] = 1.0
    for i in range(NBIS):
        cst[:, C_BIS + i] = 2.0 ** (-(i + 1))
    half = 8
    inv = 500000.0 ** (-np.arange(half, dtype=np.float64) * 2.0 / 16)
    ang = np.arange(T, dtype=np.float64)[:, None] * inv[None, :]
    cos, sin = np.cos(ang), np.sin(ang)
    rope = np.concatenate([cos, cos, -sin, sin], axis=1).astype(np.float32)
    return cst, rope


_CACHE = {}


def make_inmaps(inputs):
    cst, rope = host_consts()
    maps = []
    sq = lambda a: np.ascontiguousarray(np.asarray(a, np.float32)[0])
    shared = {
        "w_in": sq(inputs["w_in"]),
        "conv_w": sq(inputs["conv_w"]),
        "w_attn_branch": sq(inputs["w_attn_branch"]), "w_ssd_branch": sq(inputs["w_ssd_branch"]),
        "w_out": sq(inputs["w_out"]), "w_route_group": sq(inputs["w_route_group"]),
        "w_route_expert": sq(inputs["w_route_expert"]),
        "w_gate": sq(inputs["w_gate"]).reshape(32, 1024, 256), "w_up": sq(inputs["w_up"]).reshape(32, 1024, 256),
        "w_down": sq(inputs["w_down"]).reshape(32, 256, 1024),
        "cst": cst, "rope": rope,
    }
    for n in ["attn_norm", "q_norm", "k_norm", "idx_k_norm", "ffn_norm", "ssd_norm", "conv_b", "dt_bias", "a_log",
              "d_skip", "b_route_group", "b_route_expert"]:
        shared[n] = np.ascontiguousarray(np.asarray(inputs[n], np.float32).reshape(1, -1))
    x = np.asarray(inputs["x"], np.float32)
    for b in range(8):
        m = dict(shared)
        m["x"] = np.ascontiguousarray(x[b])
        maps.append(m)
    return maps


def kernel(**inputs):
    if "nc" not in _CACHE:
        _CACHE["nc"] = build()[0]
    nc = _CACHE["nc"]
    maps = make_inmaps(inputs)
    res = run_bass_kernel_spmd(nc, maps, core_ids=list(range(8)))
    return np.stack([np.asarray(r["out"], np.float32) for r in res.results], axis=0)
```

```python
import contextlib
import math
import numpy as np
import concourse.bass as bass
import concourse.mybir as mybir
from concourse.bass_utils import run_bass_kernel_spmd

F32 = mybir.dt.float32
BF16 = mybir.dt.bfloat16
U32 = mybir.dt.uint32
AF = mybir.ActivationFunctionType
ALU = mybir.AluOpType
AX = mybir.AxisListType

T = 2048
D = 1024
NT = 16
EPS = 1e-6
NBIS = 16
SPL = (1024, 256, 256, 512, 64, 8, 2048, 3072, 32, 1024, 1024)
OFF = [0]
for _s in SPL:
    OFF.append(OFF[-1] + _s)
(O_Q, O_K, O_V, O_IQ, O_IK, O_IW, O_Z, O_XBC, O_DT, O_GA, O_GB, O_END) = OFF
NW = O_END

C_ID = 0
C_DM = 128
C_NB = 256
C_CT = 384
C_ONE = 512
C_BIS = 640
CW = 672


class Prog:
    ENG = ("pe", "act", "dve", "pool", "sp")

    def __init__(self, nc, es):
        self.nc = nc
        self.es = es
        self.e = {"pe": nc.tensor, "act": nc.scalar, "dve": nc.vector, "pool": nc.gpsimd, "sp": nc.sync}
        self.sem = {}
        self.cnt = {k: 0 for k in self.ENG}
        self.epoch = {k: 0 for k in self.ENG}
        for k in self.ENG:
            self.sem[("e", k, 0)] = es.enter_context(nc.semaphore(f"s_{k}_0"))
        self.dcnt = {}
        self.waited = {k: {} for k in self.ENG}
        self.res = {}
        self.nins = 0
        self.pending = {k: [] for k in self.ENG}

    def _deps(self, eng, reads, writes):
        deps = []
        for r in reads:
            st = self.res.get(r)
            if st and st[0] is not None:
                deps.append((st[0], True))
        for w in writes:
            st = self.res.get(w)
            if st:
                if st[0] is not None:
                    deps.append((st[0], True))
                for t in st[1].values():
                    deps.append((t, False))
        for (tok, strong) in deps:
            key, val = tok
            if key[0] == "e" and key[1] == eng:
                if eng == "pe" or (not strong and eng != "pool"):
                    continue
            if self.waited[eng].get(key, -1) >= val:
                continue
            self.e[eng].wait_ge(self.sem[key], val)
            self.waited[eng][key] = val

    def _commit(self, tok, reads, writes, rkey):
        for r in reads:
            st = self.res.setdefault(r, [None, {}])
            st[1][rkey] = tok
        for w in writes:
            self.res[w] = [tok, {}]

    def op(self, eng, fn, reads=(), writes=()):
        self._deps(eng, reads, writes)
        ins = fn()
        if self.cnt[eng] >= 30000:
            self.epoch[eng] += 1
            self.cnt[eng] = 0
            self.sem[("e", eng, self.epoch[eng])] = self.es.enter_context(
                self.nc.semaphore(f"s_{eng}_{self.epoch[eng]}"))
        key = ("e", eng, self.epoch[eng])
        self.cnt[eng] += 1
        ins.then_inc(self.sem[key], 1)
        self.nins += 1
        for (r_, w_) in self.pending[eng]:
            self._commit((key, self.cnt[eng]), r_, w_, key)
        self.pending[eng] = []
        self._commit((key, self.cnt[eng]), reads, writes, key)
        return ins

    def quiet(self, eng, fn, reads=(), writes=()):
        self._deps(eng, reads, writes)
        self.nins += 1
        self.pending[eng].append((tuple(reads), tuple(writes)))
        return fn()

    def dma(self, q, key, pairs, reads=(), writes=()):
        self._deps(q, reads, writes)
        k = ("d", key)
        if k not in self.sem:
            self.sem[k] = self.es.enter_context(self.nc.semaphore(f"d_{key}"))
            self.dcnt[k] = 0
        for (o, i) in pairs:
            self.e[q].dma_start(out=o, in_=i).then_inc(self.sem[k], 16)
            self.dcnt[k] += 16
            self.nins += 1
        self._commit((k, self.dcnt[k]), reads, writes, k)

    def barrier(self):
        toks = []
        for k in self.ENG:
            if self.cnt[k] > 0:
                toks.append((("e", k, self.epoch[k]), self.cnt[k]))
        for k, v in self.dcnt.items():
            if v > 0:
                toks.append((k, v))
        for eng in self.ENG:
            for (key, val) in toks:
                if key[0] == "e" and key[1] == eng:
                    continue
                if self.waited[eng].get(key, -1) >= val:
                    continue
                self.e[eng].wait_ge(self.sem[key], val)
                self.waited[eng][key] = val
        self.res = {}


def bview(ap, h):
    return ap.rearrange("p (h d) -> p h d", h=h)


def build(stage=99, sub=99):
    nc = bass.Bass("TRN2", target_bir_lowering=False)
    dr = {}

    def din(name, shape):
        dr[name] = nc.dram_tensor(name, list(shape), F32, kind="ExternalInput").ap()
        return dr[name]

    x_d = din("x", [T, D])
    win_d = din("w_in", [D, NW])
    gains = {n: din(n, [1, s]) for n, s in [("attn_norm", D), ("q_norm", 64), ("k_norm", 64), ("idx_k_norm", 64),
                                             ("ffn_norm", D), ("ssd_norm", 2048), ("conv_b", 3072), ("dt_bias", 32),
                                             ("a_log", 32), ("d_skip", 32), ("b_route_group", 4),
                                             ("b_route_expert", 32)]}
    convw_d = din("conv_w", [4, 3072])
    wa_d = din("w_attn_branch", [1024, 1024])
    wb_d = din("w_ssd_branch", [2048, 1024])
    wo_d = din("w_out", [1024, 1024])
    wrg_d = din("w_route_group", [1024, 4])
    wre_d = din("w_route_expert", [1024, 32])
    wg_d = din("w_gate", [32, 1024, 256])
    wu_d = din("w_up", [32, 1024, 256])
    wd_d = din("w_down", [32, 256, 1024])
    cst_d = din("cst", [128, CW])
    rope_d = din("rope", [T, 32])
    out_d = nc.dram_tensor("out", [T, D], F32, kind="ExternalOutput").ap()
    dbg = {}

    def dout(name, shape, dt=F32):
        dbg[name] = nc.dram_tensor(name, list(shape), dt, kind="ExternalOutput").ap()
        return dbg[name]

    es = contextlib.ExitStack()
    with es:
        P = Prog(nc, es)

        uid = [0]

        def sb(name, shape, dt, stack=es):
            uid[0] += 1
            return stack.enter_context(nc.sbuf_tensor(f"sb{uid[0]}_{name}", list(shape), dt))

        ps = [es.enter_context(nc.psum_tensor(f"ps{i}", [128, 512], F32)) for i in range(7)]
        psb = es.enter_context(nc.psum_tensor("psb", [128, 1024], BF16))

        cst = sb("cst", [128, CW], F32)
        cstb = sb("cstb", [128, CW], BF16)
        P.dma("sp", "cst", [(cst[:], cst_d)], writes=["cst"])
        P.op("pool", lambda: nc.gpsimd.tensor_copy(out=cstb[:], in_=cst[:]), reads=["cst"], writes=["cstb"])
        ident = cstb[:, C_ID:C_ID + 128]
        gb = {}

        def load_gain(n, width, st, c0=0):
            gb[n] = sb("g_" + n, [128, width], F32, st)
            P.dma("sp", "gain_" + n, [(gb[n][:], gains[n][:, c0:c0 + width].partition_broadcast(128))], writes=["g_" + n])

        hT = sb("hT", [128, 8, T], BF16)
        ya_spill = nc.dram_tensor("ya_spill", [128, 8, T], BF16, kind="Internal").ap()

        def rmsnorm_T(src_fn, gname, dst, st):
            xb = [sb(f"rn_x{i}", [128, D], F32, st) for i in range(2)]
            xn = [sb(f"rn_xn{i}", [128, D], BF16, st) for i in range(2)]
            junk = sb("rn_junk", [128, D], BF16, st)
            ss = sb("rn_ss", [128, NT], F32, st)
            ms = sb("rn_ms", [128, NT], F32, st)
            sd = sb("rn_sd", [128, NT], F32, st)
            rs = sb("rn_rs", [128, NT], F32, st)
            for c in range(NT):
                b = c % 2
                src_fn(c, xb[b], f"rn_x{b}")
                P.op("act", lambda: nc.scalar.activation(out=junk[:], in_=xb[b][:], func=AF.Square,
                                                         accum_out=ss[:, c:c + 1]),
                     reads=[f"rn_x{b}"], writes=["rn_junk", ("rn_ss", c)])
                P.op("dve", lambda: nc.vector.tensor_scalar(out=ms[:, c:c + 1], in0=ss[:, c:c + 1], scalar1=1.0 / D,
                                                            scalar2=EPS, op0=ALU.mult, op1=ALU.add),
                     reads=[("rn_ss", c)], writes=[("rn_ms", c)])
                P.op("act", lambda: nc.scalar.activation(out=sd[:, c:c + 1], in_=ms[:, c:c + 1], func=AF.Sqrt),
                     reads=[("rn_ms", c)], writes=[("rn_sd", c)])
                P.op("dve", lambda: nc.vector.reciprocal(out=rs[:, c:c + 1], in_=sd[:, c:c + 1]),
                     reads=[("rn_sd", c)], writes=[("rn_rs", c)])
                P.op("dve", lambda: nc.vector.scalar_tensor_tensor(out=xn[b][:], in0=xb[b][:], scalar=rs[:, c:c + 1],
                                                                   in1=gb[gname][:], op0=ALU.mult, op1=ALU.mult),
                     reads=[f"rn_x{b}", ("rn_rs", c), "g_" + gname], writes=[f"rn_xn{b}"])
                for kt in range(8):
                    f = lambda: nc.tensor.transpose(out=psb[:, kt * 128:(kt + 1) * 128],
                                                    in_=xn[b][:, kt * 128:(kt + 1) * 128], identity=ident)
                    if kt < 7:
                        P.quiet("pe", f, reads=[f"rn_xn{b}", "cstb"], writes=["psb"])
                    else:
                        P.op("pe", f, reads=[f"rn_xn{b}", "cstb"], writes=["psb"])
                P.op("act", lambda: nc.scalar.copy(out=dst[:, :, c * 128:(c + 1) * 128], in_=bview(psb[:], 8)),
                     reads=["psb"], writes=[("hT", c)])

        def load_x(c, buf, rname):
            P.dma("sp", rname, [(buf[:], x_d[c * 128:(c + 1) * 128, :])], writes=[rname])

        with contextlib.ExitStack() as st:
            load_gain("attn_norm", D, st)
            rmsnorm_T(load_x, "attn_norm", hT, st)
            P.barrier()

        if stage == 0:
            o = dout("hT_dbg", [128, 8, T], BF16)
            P.dma("sp", "out", [(o, hT[:])])
            P.barrier()
            return nc, dbg

        wst = [None]
        wbf = [None, None]
        wctr = [0]
        win_v = win_d.rearrange("(kt p) n -> p kt n", p=128)

        def alloc_w(st):
            wst[0] = sb("wst", [128, 8, 512], F32, st)
            wbf[0] = sb("wbf0", [128, 8, 512], BF16, st)
            wbf[1] = sb("wbf1", [128, 8, 512], BF16, st)

        def load_w(c0, ncols):
            i = wctr[0] % 2
            wctr[0] += 1
            P.dma("sp", "wst", [(wst[0][:, :, 0:ncols], win_v[:, :, c0:c0 + ncols])], writes=["wst"])
            P.op("pool", lambda: nc.gpsimd.tensor_copy(out=wbf[i][:, :, 0:ncols], in_=wst[0][:, :, 0:ncols]),
                 reads=["wst"], writes=[f"wbf{i}"])
            return i

        def proj_tm(c, wi, ncols, bank):
            for kt in range(8):
                f = lambda: nc.tensor.matmul(ps[bank][:, 0:ncols], lhsT=hT[:, kt, c * 128:(c + 1) * 128],
                                             rhs=wbf[wi][:, kt, 0:ncols], start=(kt == 0), stop=(kt == 7))
                if kt < 7:
                    P.quiet("pe", f, reads=[("hT", c), f"wbf{wi}"], writes=[f"ps{bank}"])
                else:
                    P.op("pe", f, reads=[("hT", c), f"wbf{wi}"], writes=[f"ps{bank}"])

        with contextlib.ExitStack() as st:
            yaT = sb("yaT", [128, 8, T], BF16, st)
            rope = sb("rope", [128, NT, 32], F32, st)
            P.dma("sp", "rope", [(rope[:], rope_d.rearrange("(c p) f -> p c f", p=128))], writes=["rope"])
            for n_ in ("q_norm", "k_norm", "idx_k_norm"):
                load_gain(n_, 64, st)
            qT = sb("qT", [128, 8, T], BF16, st)
            kT2 = sb("kT2", [128, 4, T], BF16, st)
            iqT = sb("iqT", [128, 4, T], BF16, st)
            ikT2 = sb("ikT2", [128, T], BF16, st)
            vaug = sb("vaug", [128, NT, 4, 66], BF16, st)
            iwa = sb("iwa", [128, NT, 8], F32, st)
            iws = sb("iws", [128, NT, 8], F32, st)
            P.op("pool", lambda: nc.gpsimd.memset(vaug[:], 1.0), writes=["vaug"])

            with contextlib.ExitStack() as st2:
                alloc_w(st2)
                sq = sb("e_sq", [128, 512], F32, st2)
                xn = sb("e_xn", [128, 512], F32, st2)
                ra = sb("e_ra", [128, 8, 16], F32, st2)
                rb = sb("e_rb", [128, 8, 16], F32, st2)
                s8 = [sb(f"e_s8{i}", [128, 8], F32, st2) for i in range(4)]
                tmb = sb("e_tmb", [128, 512], BF16, st2)

                def epilogue(c, bank, nh, gname, prescale, dst_fn, dup):
                    pv = bview(ps[bank][:, 0:nh * 64], nh)
                    xv = bview(xn[:, 0:nh * 64], nh)
                    pr = f"ps{bank}"
                    if gname is not None:
                        P.op("act", lambda: nc.scalar.activation(out=sq[:, 0:nh * 64], in_=ps[bank][:, 0:nh * 64],
                                                                 func=AF.Square), reads=[pr], writes=["e_sq"])
                        P.op("dve", lambda: nc.vector.tensor_reduce(out=s8[0][:, 0:nh], in_=bview(sq[:, 0:nh * 64], nh),
                                                                    axis=AX.X, op=ALU.add), reads=["e_sq"], writes=["e_s80"])
                        P.op("dve", lambda: nc.vector.tensor_scalar(out=s8[1][:, 0:nh], in0=s8[0][:, 0:nh], scalar1=1.0 / 64,
                                                                    scalar2=EPS, op0=ALU.mult, op1=ALU.add),
                             reads=["e_s80"], writes=["e_s81"])
                        P.op("act", lambda: nc.scalar.activation(out=s8[2][:, 0:nh], in_=s8[1][:, 0:nh], func=AF.Sqrt),
                             reads=["e_s81"], writes=["e_s82"])
                        P.op("dve", lambda: nc.vector.reciprocal(out=s8[3][:, 0:nh], in_=s8[2][:, 0:nh]),
                             reads=["e_s82"], writes=["e_s83"])
                        P.op("dve", lambda: nc.vector.tensor_tensor(out=xv, in0=pv,
                                                                    in1=s8[3][:, 0:nh].unsqueeze(2).to_broadcast([128, nh, 64]),
                                                                    op=ALU.mult), reads=[pr, "e_s83"], writes=["e_xn"])
                        P.op("dve", lambda: nc.vector.tensor_tensor(out=xv, in0=xv,
                                                                    in1=gb[gname][:].unsqueeze(1).to_broadcast([128, nh, 64]),
                                                                    op=ALU.mult), reads=["e_xn", "g_" + gname], writes=["e_xn"])
                    elif prescale is not None:
                        P.op("dve", lambda: nc.vector.tensor_tensor(out=xv, in0=pv,
                                                                    in1=prescale.unsqueeze(2).to_broadcast([128, nh, 64]),
                                                                    op=ALU.mult), reads=[pr, ("iw", c)], writes=["e_xn"])
                    else:
                        P.op("dve", lambda: nc.vector.tensor_copy(out=xv, in_=pv), reads=[pr], writes=["e_xn"])
                    c16 = rope[:, c, 0:16].unsqueeze(1).to_broadcast([128, nh, 16])
                    nsn = rope[:, c, 16:24].unsqueeze(1).to_broadcast([128, nh, 8])
                    psn = rope[:, c, 24:32].unsqueeze(1).to_broadcast([128, nh, 8])
                    P.op("dve", lambda: nc.vector.tensor_tensor(out=ra[:, 0:nh, :], in0=xv[:, :, 0:16], in1=c16, op=ALU.mult),
                         reads=["e_xn", "rope"], writes=["e_ra"])
                    P.op("dve", lambda: nc.vector.tensor_tensor(out=rb[:, 0:nh, 0:8], in0=xv[:, :, 8:16], in1=nsn, op=ALU.mult),
                         reads=["e_xn", "rope"], writes=["e_rb0"])
                    P.op("dve", lambda: nc.vector.tensor_tensor(out=rb[:, 0:nh, 8:16], in0=xv[:, :, 0:8], in1=psn, op=ALU.mult),
                         reads=["e_xn", "rope"], writes=["e_rb1"])
                    P.op("dve", lambda: nc.vector.tensor_tensor(out=xv[:, :, 0:16], in0=ra[:, 0:nh, :], in1=rb[:, 0:nh, :],
                                                                op=ALU.add), reads=["e_ra", "e_rb0", "e_rb1"], writes=["e_xn"])
                    if dup:
                        tv = tmb[:, 0:nh * 128].rearrange("p (h t d) -> p h t d", h=nh, t=2)
                        P.op("act", lambda: nc.scalar.copy(out=tv[:, :, 0, :], in_=xv), reads=["e_xn"], writes=["e_tmb0"])
                        P.op("act", lambda: nc.scalar.copy(out=tv[:, :, 1, :], in_=xv), reads=["e_xn"], writes=["e_tmb1"])
                        nblk = nh
                    else:
                        P.op("act", lambda: nc.scalar.copy(out=tmb[:, 0:nh * 64], in_=xn[:, 0:nh * 64]), reads=["e_xn"],
                             writes=["e_tmb0", "e_tmb1"])
                        nblk = nh // 2
                    for j in range(nblk):
                        f = lambda: nc.tensor.transpose(out=psb[:, j * 128:(j + 1) * 128], in_=tmb[:, j * 128:(j + 1) * 128],
                                                        identity=ident)
                        if j < nblk - 1:
                            P.quiet("pe", f, reads=["e_tmb0", "e_tmb1", "cstb"], writes=["psb"])
                        else:
                            P.op("pe", f, reads=["e_tmb0", "e_tmb1", "cstb"], writes=["psb"])
                    dst_fn(nblk)

                wi = load_w(O_IK, 72)
                for c in range(NT):
                    bank = c % 2
                    proj_tm(c, wi, 72, bank)
                    P.op("act", lambda: nc.scalar.activation(out=iwa[:, c, :], in_=ps[bank][:, 64:72], func=AF.Abs),
                         reads=[f"ps{bank}"], writes=[("iw", c)])
                    P.op("act", lambda: nc.scalar.activation(out=iws[:, c, :], in_=ps[bank][:, 64:72], func=AF.Sign),
                         reads=[f"ps{bank}"], writes=[("iws", c)])
                    epilogue(c, bank, 1, "idx_k_norm", None,
                             lambda nblk: P.op("act", lambda: nc.scalar.copy(out=ikT2[:, c * 128:(c + 1) * 128],
                                                                             in_=psb[:, 0:128]),
                                               reads=["psb"], writes=[("ikT2", c)]), True)
                wi = load_w(O_IQ, 512)
                for c in range(NT if sub >= 2 else 0):
                    bank = c % 2
                    proj_tm(c, wi, 512, bank)
                    epilogue(c, bank, 8, None, iwa[:, c, :],
                             lambda nblk: P.op("act", lambda: nc.scalar.copy(out=iqT[:, :, c * 128:(c + 1) * 128],
                                                                             in_=bview(psb[:, 0:512], 4)),
                                               reads=["psb"], writes=[("iqT", c)]), False)
                for half in range(2):
                    wi = load_w(O_Q + half * 512, 512)
                    for c in range(NT if sub >= 3 else 0):
                        bank = c % 2
                        proj_tm(c, wi, 512, bank)
                        epilogue(c, bank, 8, "q_norm", None,
                                 lambda nblk: P.op("act", lambda: nc.scalar.copy(
                                     out=qT[:, half * 4:half * 4 + 4, c * 128:(c + 1) * 128], in_=bview(psb[:, 0:512], 4)),
                                     reads=["psb"], writes=[("qT", c, half)]), False)
                wi = load_w(O_K, 512)
                for c in range(NT if sub >= 4 else 0):
                    bank = c % 2
                    proj_tm(c, wi, 512, bank)
                    if sub != 5:
                        P.op("act", lambda: nc.scalar.copy(out=vaug[:, c, :, 0:64], in_=bview(ps[bank][:, 256:512], 4)),
                             reads=[f"ps{bank}", "vaug"], writes=[("vaug", c)])
                    epilogue(c, bank, 4, "k_norm", None,
                             lambda nblk: P.op("act", lambda: nc.scalar.copy(out=kT2[:, :, c * 128:(c + 1) * 128],
                                                                             in_=bview(psb[:, 0:512], 4)),
                                               reads=["psb"], writes=[("kT2", c)]), True)
                P.barrier()

            if stage == 1:
                for nm, t_, shp in [("qT", qT, [128, 8, T]), ("kT2", kT2, [128, 4, T]), ("iqT", iqT, [128, 4, T]),
                                    ("ikT2", ikT2, [128, T])]:
                    o = dout(nm + "_dbg", shp, BF16)
                    P.dma("sp", "out", [(o, t_[:])])
                o = dout("vaug_dbg", [128, NT, 4, 66], BF16)
                P.dma("sp", "out", [(o, vaug[:])])
                o = dout("iwa_dbg", [128, NT, 8], F32)
                P.dma("sp", "out", [(o, iwa[:])])
                o = dout("iws_dbg", [128, NT, 8], F32)
                P.dma("sp", "out", [(o, iws[:])])
                P.barrier()
                return nc, dbg

            with contextlib.ExitStack() as st3:
                score = sb("score", [128, T], F32, st3)
                junk = sb("ajunk", [128, T], BF16, st3)
                maskb = [sb(f"maskb{i}", [128, T], BF16, st3) for i in range(2)]
                maskT = [sb(f"maskT{i}", [128, NT, 128], BF16, st3) for i in range(2)]
                relu = [sb(f"relu{i}", [128, 512], BF16, st3) for i in range(2)]
                diag = [sb(f"diag{i}", [128, 8, 128], BF16, st3) for i in range(2)]
                PT = [sb(f"PT{i}", [128, 512], BF16, st3) for i in range(3)]
                PTm = [sb(f"PTm{i}", [128, 512], BF16, st3) for i in range(3)]
                ytm = sb("ytm", [128, 1024], BF16, st3)
                hi = sb("b_hi", [128, 1], F32, st3)
                lo = sb("b_lo", [128, 1], F32, st3)
                w0 = sb("b_w0", [128, 1], F32, st3)
                wtab = sb("b_wtab", [128, NBIS], F32, st3)
                tt = sb("b_t", [128, 1], F32, st3)
                cnt = sb("b_cnt", [128, 1], F32, st3)
                uu = sb("b_u", [128, 1], F32, st3)
                thr = sb("b_thr", [128, 1], F32, st3)
                rcp = sb("b_rcp", [128, 8], F32, st3)
                pvc = [0]
                SB3 = [4, 5, 6]
                NQ = NT if sub >= 30 else max(0, sub - 10)

                def emit_scores(qi):
                    nkeys = 128 * (qi + 1)
                    qs = slice(qi * 128, (qi + 1) * 128)
                    dgt = diag[qi % 2]
                    for h in range(8):
                        P.op("dve", lambda: nc.vector.tensor_scalar(out=dgt[:, h, :], in0=ident, scalar1=iws[:, qi, h:h + 1],
                                                                    scalar2=None, op0=ALU.mult),
                             reads=["cstb"], writes=[(f"diag{qi % 2}", h)])
                    nkb = (nkeys + 511) // 512
                    for kb in range(nkb):
                        kw = min(512, nkeys - kb * 512)
                        for h in range(8):
                            hf, pr_ = h % 2, h // 2
                            rb = h % 2
                            P.op("pe", lambda: nc.tensor.matmul(ps[rb][:, 0:kw], lhsT=iqT[64 * hf:64 * hf + 64, pr_, qs],
                                                                rhs=ikT2[64 * hf:64 * hf + 64, kb * 512:kb * 512 + kw],
                                                                start=True, stop=True),
                                 writes=[f"ps{rb}"])
                            P.op("act", lambda: nc.scalar.activation(out=relu[rb][:, 0:kw], in_=ps[rb][:, 0:kw], func=AF.Relu),
                                 reads=[f"ps{rb}"], writes=[f"relu{rb}"])
                            f = lambda: nc.tensor.matmul(ps[2][:, 0:kw], lhsT=dgt[:, h, :], rhs=relu[rb][:, 0:kw],
                                                         start=(h == 0), stop=(h == 7))
                            if h < 7:
                                P.quiet("pe", f, reads=[f"relu{rb}", (f"diag{qi % 2}", h)], writes=["ps2"])
                            else:
                                P.op("pe", f, reads=[f"relu{rb}", (f"diag{qi % 2}", h)], writes=["ps2"])
                        c0 = kb * 512
                        last = (kb == nkb - 1)
                        nd = kw - 128 if last else kw
                        if nd > 0:
                            P.op("dve", lambda: nc.vector.tensor_copy(out=score[:, c0:c0 + nd], in_=ps[2][:, 0:nd]),
                                 reads=["ps2"], writes=[("score", kb)])
                        if last:
                            P.op("dve", lambda: nc.vector.tensor_tensor(out=score[:, nkeys - 128:nkeys], in0=ps[2][:, nd:nd + 128],
                                                                        in1=cst[:, C_DM:C_DM + 128], op=ALU.mult),
                                 reads=["ps2", "cst"], writes=[("score", "d")])
                            P.op("dve", lambda: nc.vector.tensor_tensor(out=score[:, nkeys - 128:nkeys],
                                                                        in0=score[:, nkeys - 128:nkeys],
                                                                        in1=cst[:, C_NB:C_NB + 128], op=ALU.add),
                                 reads=[("score", "d"), "cst"], writes=[("score", "d")])

                def search_steps(qi):
                    nkeys = 128 * (qi + 1)
                    nkb = (nkeys + 511) // 512
                    sres = [("score", kb) for kb in range(nkb)] + [("score", "d")]
                    mb = maskb[qi % 2]
                    steps = []

                    def s0():
                        P.op("dve", lambda: nc.vector.tensor_reduce(out=hi[:], in_=score[:, 0:nkeys], axis=AX.X, op=ALU.max),
                             reads=sres, writes=["b_hi"])
                        P.op("dve", lambda: nc.vector.tensor_reduce(out=lo[:], in_=score[:, 0:nkeys - 128], axis=AX.X, op=ALU.min),
                             reads=sres, writes=["b_lo"])
                        P.op("dve", lambda: nc.vector.tensor_tensor(out=w0[:], in0=hi[:], in1=lo[:], op=ALU.subtract),
                             reads=["b_hi", "b_lo"], writes=["b_w0"])
                        P.op("dve", lambda: nc.vector.tensor_scalar(out=wtab[:], in0=cst[:, C_BIS:C_BIS + NBIS], scalar1=w0[:, 0:1],
                                                                    scalar2=None, op0=ALU.mult),
                             reads=["b_w0", "cst"], writes=["b_wtab"])
                        P.op("dve", lambda: nc.vector.tensor_tensor(out=tt[:], in0=lo[:], in1=wtab[:, 0:1], op=ALU.add),
                             reads=["b_lo", "b_wtab"], writes=["b_t"])
                    steps.append(s0)
                    for it in range(NBIS):
                        def si(it=it):
                            P.op("dve", lambda: nc.vector.tensor_scalar(out=junk[:, 0:nkeys], in0=score[:, 0:nkeys], scalar1=tt[:, 0:1],
                                                                        scalar2=None, op0=ALU.is_ge, op1=ALU.add, accum_out=cnt[:]),
                                 reads=sres + ["b_t"], writes=["ajunk", "b_cnt"])
                            P.op("dve", lambda: nc.vector.tensor_scalar(out=uu[:], in0=cnt[:], scalar1=256.0, scalar2=-0.5,
                                                                        op0=ALU.is_ge, op1=ALU.add),
                                 reads=["b_cnt"], writes=["b_u"])
                            P.op("dve", lambda: nc.vector.scalar_tensor_tensor(out=tt[:], in0=uu[:], scalar=wtab[:, it:it + 1],
                                                                               in1=tt[:], op0=ALU.mult, op1=ALU.add),
                                 reads=["b_u", "b_wtab", "b_t"], writes=["b_t"])
                        steps.append(si)

                    def sf():
                        P.op("dve", lambda: nc.vector.scalar_tensor_tensor(out=thr[:], in0=wtab[:, NBIS - 1:NBIS], scalar=-0.5,
                                                                           in1=tt[:], op0=ALU.mult, op1=ALU.add),
                             reads=["b_wtab", "b_t"], writes=["b_thr"])
                        P.op("dve", lambda: nc.vector.tensor_scalar(out=mb[:, 0:nkeys], in0=score[:, 0:nkeys], scalar1=thr[:, 0:1],
                                                                    scalar2=None, op0=ALU.is_ge),
                             reads=sres + ["b_thr"], writes=[f"maskb{qi % 2}"])
                    steps.append(sf)
                    return steps

                def const_mask(qi):
                    nkeys = 128 * (qi + 1)
                    mb = maskb[qi % 2]
                    if qi == 1:
                        P.op("pool", lambda: nc.gpsimd.tensor_copy(out=mb[:, 0:128], in_=cstb[:, C_ONE:C_ONE + 128]),
                             reads=["cstb"], writes=[f"maskb{qi % 2}"])
                    P.op("pool", lambda: nc.gpsimd.tensor_copy(out=mb[:, nkeys - 128:nkeys], in_=cstb[:, C_DM:C_DM + 128]),
                         reads=["cstb"], writes=[f"maskb{qi % 2}"])

                def emit_maskT(qi):
                    nk = qi + 1
                    mb = maskb[qi % 2]
                    mT = maskT[qi % 2]
                    for k0 in range(0, nk, 8):
                        n = min(8, nk - k0)
                        for j in range(n):
                            f = lambda: nc.tensor.transpose(out=psb[:, j * 128:(j + 1) * 128],
                                                            in_=mb[:, (k0 + j) * 128:(k0 + j + 1) * 128], identity=ident)
                            if j < n - 1:
                                P.quiet("pe", f, reads=[f"maskb{qi % 2}", "cstb"], writes=["psb"])
                            else:
                                P.op("pe", f, reads=[f"maskb{qi % 2}", "cstb"], writes=["psb"])
                        P.op("act", lambda: nc.scalar.copy(out=mT[:, k0:k0 + n, :], in_=bview(psb[:, 0:n * 128], n)),
                             reads=["psb"], writes=[(f"maskT{qi % 2}", k0 // 8)])

                SBK = [4, 5, 6, 0, 1]
                LOOK = 2

                def attention_tile(qi, steps):
                    nk = qi + 1
                    qs = slice(qi * 128, (qi + 1) * 128)
                    mT = maskT[qi % 2]
                    seq = [(g, kj) for g in range(4) for kj in range(nk)]
                    nseq = len(seq)
                    info = {}
                    stq = list(steps)
                    every = max(1, nseq // max(1, len(stq))) if stq else 0

                    def front(i):
                        g, kj = seq[i]
                        ks = slice(kj * 128, (kj + 1) * 128)
                        n_ = pvc[0]
                        pvc[0] += 1
                        par = n_ % 3
                        sa, sb_ = SBK[(2 * n_) % 5], SBK[(2 * n_ + 1) % 5]
                        info[i] = par
                        P.op("pe", lambda: nc.tensor.matmul(bview(ps[sa][:, 0:256], 2), lhsT=kT2[0:64, g, ks],
                                                            rhs=qT[0:64, 2 * g:2 * g + 2, qs], start=True, stop=True),
                             writes=[f"ps{sa}"])
                        P.op("pe", lambda: nc.tensor.matmul(bview(ps[sb_][:, 0:256], 2), lhsT=kT2[64:128, g, ks],
                                                            rhs=qT[64:128, 2 * g:2 * g + 2, qs], start=True, stop=True),
                             writes=[f"ps{sb_}"])
                        P.op("act", lambda: nc.scalar.activation(out=PT[par][:, 0:256], in_=ps[sa][:, 0:256], func=AF.Exp,
                                                                 scale=0.125), reads=[f"ps{sa}"], writes=[("PT", par, 0)])
                        P.op("act", lambda: nc.scalar.activation(out=PT[par][:, 256:512], in_=ps[sb_][:, 0:256], func=AF.Exp,
                                                                 scale=0.125), reads=[f"ps{sb_}"], writes=[("PT", par, 1)])
                        me = "dve" if (n_ % 3 == 2) else "pool"
                        P.op(me, lambda: P.e[me].tensor_tensor(out=bview(PTm[par][:], 4), in0=bview(PT[par][:], 4),
                                                               in1=mT[:, kj, :].unsqueeze(1).to_broadcast([128, 4, 128]),
                                                               op=ALU.mult),
                             reads=[("PT", par, 0), ("PT", par, 1), (f"maskT{qi % 2}", kj // 8)], writes=[("PTm", par)])

                    def back(i):
                        g, kj = seq[i]
                        par = info[i]
                        ob = 2 + g % 2
                        for j in range(4):
                            hl = [0, 2, 1, 3][j]
                            f = lambda: nc.tensor.matmul(ps[ob][:, hl * 65:hl * 65 + 65], lhsT=PTm[par][:, j * 128:(j + 1) * 128],
                                                         rhs=vaug[:, kj, g, 0:65], start=(kj == 0 and j == 0),
                                                         stop=(kj == nk - 1 and j == 3), skip_group_check=True)
                            if j < 3:
                                P.quiet("pe", f, reads=[("PTm", par)], writes=[f"ps{ob}"])
                            else:
                                P.op("pe", f, reads=[("PTm", par)], writes=[f"ps{ob}"])
                        if kj == nk - 1:
                            ov = ps[ob][:, 0:260].rearrange("p (h d) -> p h d", h=4)
                            P.op("dve", lambda: nc.vector.reciprocal(out=rcp[:, 4 * (g % 2):4 * (g % 2) + 4], in_=ov[:, :, 64]),
                                 reads=[f"ps{ob}"], writes=[("b_rcp", g % 2)])
                            for hl in range(4):
                                hh = 4 * g + hl
                                P.op("act", lambda: nc.scalar.activation(out=ytm[:, hh * 64:(hh + 1) * 64],
                                                                         in_=ps[ob][:, hl * 65:hl * 65 + 64], func=AF.Copy,
                                                                         scale=rcp[:, 4 * (g % 2) + hl:4 * (g % 2) + hl + 1]),
                                     reads=[f"ps{ob}", ("b_rcp", g % 2)], writes=[("ytm", hh)])

                    for i in range(nseq + LOOK):
                        if i < nseq:
                            front(i)
                        if i >= LOOK:
                            back(i - LOOK)
                        if stq and (i % every == every - 1):
                            stq.pop(0)()
                    while stq:
                        stq.pop(0)()

                if NQ > 0:
                    const_mask(0)
                    emit_maskT(0)
                for qi in range(NQ):
                    nxt = qi + 1
                    steps = []
                    if nxt < NQ:
                        if nxt >= 2:
                            emit_scores(nxt)
                            steps = search_steps(nxt)
                        else:
                            const_mask(nxt)
                    attention_tile(qi, steps)
                    if nxt < NQ:
                        emit_maskT(nxt)
                    qs = slice(qi * 128, (qi + 1) * 128)
                    for j in range(8):
                        f = lambda: nc.tensor.transpose(out=psb[:, j * 128:(j + 1) * 128], in_=ytm[:, j * 128:(j + 1) * 128],
                                                        identity=ident)
                        if j < 7:
                            P.quiet("pe", f, reads=[("ytm", hh_) for hh_ in range(16)] + ["cstb"], writes=["psb"])
                        else:
                            P.op("pe", f, reads=[("ytm", hh_) for hh_ in range(16)] + ["cstb"], writes=["psb"])
                    P.op("act", lambda: nc.scalar.copy(out=yaT[:, :, qs], in_=bview(psb[:], 8)), reads=["psb"], writes=[("yaT", qi)])
                P.barrier()
            if stage == 2:
                o = dout("yaT_dbg", [128, 8, T], BF16)
                P.dma("sp", "out", [(o, yaT[:])])
                P.barrier()
                return nc, dbg
            P.dma("sp", "spill", [(ya_spill, yaT[:])])
            P.barrier()

        stB = contextlib.ExitStack()
        es.enter_context(stB)
        ysT = sb("ysT", [128, 16, T], BF16, stB)
        with contextlib.ExitStack() as sS:
            G8 = lambda t_, c_, g_: t_[:, c_, 8 * g_:8 * g_ + 8].unsqueeze(2).to_broadcast([128, 8, 64])
            wstS = sb("wstS", [128, 8, 256], F32, sS)
            selb = sb("selb", [128, 32, 128], BF16, sS)
            P.op("pool", lambda: nc.gpsimd.memset(selb[:], 0.0), writes=["selb"])
            for r3 in range(3):
                P.op("pool", lambda: nc.gpsimd.tensor_copy(
                    out=selb[32 * r3:32 * r3 + 32, :, :],
                    in_=cstb[32 * r3:32 * r3 + 32, C_ID + 32 * r3:C_ID + 32 * r3 + 32].unsqueeze(2).to_broadcast([32, 32, 128])),
                    reads=["cstb", "selb"], writes=["selb"])
            if sub == 101:
                P.barrier(); return nc, dbg
            for n_ in ("dt_bias", "a_log", "d_skip"):
                load_gain(n_, 32, sS)
            aneg = sb("aneg", [128, 32], F32, sS)
            P.op("act", lambda: nc.scalar.activation(out=aneg[:], in_=gb["a_log"][:], func=AF.Exp), reads=["g_a_log"], writes=["aneg"])
            P.op("dve", lambda: nc.vector.tensor_scalar(out=aneg[:], in0=aneg[:], scalar1=-1.0, scalar2=None, op0=ALU.mult),
                 reads=["aneg"], writes=["aneg"])
            if sub == 102:
                P.barrier(); return nc, dbg
            cwfm = sb("cwfm", [128, 24, 5], F32, sS)
            s0 = contextlib.ExitStack()
            cw5 = sb("cw5", [5, 3072], F32, s0)
            P.dma("sp", "cw5", [(cw5[0:4, :], convw_d), (cw5[4:5, :], gains["conv_b"])], writes=["cw5"])
            for t_ in range(24):
                f = lambda: nc.tensor.transpose(out=ps[0][:, t_ * 5:t_ * 5 + 5], in_=cw5[:, t_ * 128:(t_ + 1) * 128],
                                                identity=cst[0:5, C_ID:C_ID + 5])
                if t_ < 23:
                    P.quiet("pe", f, reads=["cw5", "cst"], writes=["ps0"])
                else:
                    P.op("pe", f, reads=["cw5", "cst"], writes=["ps0"])
            P.op("dve", lambda: nc.vector.tensor_copy(out=cwfm[:], in_=bview(ps[0][:, 0:120], 24)), reads=["ps0"], writes=["cwfm"])
            P.barrier()
            s0.close()
            if sub == 103:
                P.barrier(); return nc, dbg
            dt_all = sb("dt_all", [128, NT, 32], F32, sS)
            acs = sb("acs", [128, NT, 32], F32, sS)
            ea = sb("ea", [128, NT, 32], F32, sS)
            dtw = sb("dtw", [128, NT, 32], F32, sS)
            cdb = sb("cdb", [128, NT, 32], F32, sS)
            A3 = sb("A3", [128, NT, 128], BF16, sS)
            P.op("pool", lambda: nc.gpsimd.memset(A3[:], 0.0), writes=["A3z"])
            with contextlib.ExitStack() as s1:
                wdt_s = sb("wdt_s", [128, 8, 32], F32, s1)
                wdt = sb("wdt", [128, 8, 32], BF16, s1)
                P.dma("sp", "wdt", [(wdt_s[:], win_v[:, :, O_DT:O_DT + 32])], writes=["wdt_s"])
                P.op("pool", lambda: nc.gpsimd.tensor_copy(out=wdt[:], in_=wdt_s[:]), reads=["wdt_s"], writes=["wdt"])
                f32t = [sb(f"s1_{i}", [128, 32], F32, s1) for i in range(6)]
                a3 = sb("s1_a3", [128, 3, 32], F32, s1)
                Hb = sb("s1_Hb", [128, 128], BF16, s1)
                Mb = sb("s1_Mb", [128, 128], BF16, s1)
                r1 = sb("s1_r1", [128, 128], F32, s1)
                r2 = sb("s1_r2", [128, 128], F32, s1)
                ones_f = cst[:, C_ONE:C_ONE + 128]
                uinc = cst[:, C_CT:C_CT + 128]
                for c in range(NT):
                    cs_ = slice(c * 128, (c + 1) * 128)
                    xd, ax, ee, ll, rr, aa = f32t
                    for kt in range(8):
                        f = lambda: nc.tensor.matmul(ps[1][:, 0:32], lhsT=hT[:, kt, cs_], rhs=wdt[:, kt, :], start=(kt == 0), stop=(kt == 7))
                        if kt < 7:
                            P.quiet("pe", f, reads=["wdt"], writes=["ps1"])
                        else:
                            P.op("pe", f, reads=["wdt"], writes=["ps1"])
                    P.op("dve", lambda: nc.vector.tensor_tensor(out=xd[:], in0=ps[1][:, 0:32], in1=gb["dt_bias"][:], op=ALU.add),
                         reads=["ps1", "g_dt_bias"], writes=["s1_xd"])
                    P.op("act", lambda: nc.scalar.activation(out=ax[:], in_=xd[:], func=AF.Abs), reads=["s1_xd"], writes=["s1_ax"])
                    P.op("act", lambda: nc.scalar.activation(out=ee[:], in_=ax[:], func=AF.Exp, scale=-1.0), reads=["s1_ax"], writes=["s1_ee"])
                    P.op("act", lambda: nc.scalar.activation(out=ll[:], in_=ee[:], func=AF.Ln, bias=1.0), reads=["s1_ee"], writes=["s1_ll"])
                    P.op("dve", lambda: nc.vector.tensor_scalar(out=rr[:], in0=xd[:], scalar1=0.0, scalar2=None, op0=ALU.max),
                         reads=["s1_xd"], writes=["s1_rr"])
                    P.op("dve", lambda: nc.vector.tensor_tensor(out=dt_all[:, c, :], in0=rr[:], in1=ll[:], op=ALU.add),
                         reads=["s1_rr", "s1_ll"], writes=[("dt", c)])
                    P.op("dve", lambda: nc.vector.tensor_tensor(out=aa[:], in0=dt_all[:, c, :], in1=aneg[:], op=ALU.mult),
                         reads=[("dt", c), "aneg"], writes=["s1_aa"])
                    if sub == 104:
                        P.barrier(); return nc, dbg
                    P.op("dve", lambda: nc.vector.tensor_copy(out=a3[:], in_=aa[:].unsqueeze(1).to_broadcast([128, 3, 32])),
                         reads=["s1_aa"], writes=["s1_a3"])
                    if sub == 105:
                        P.barrier(); return nc, dbg
                    P.op("pe", lambda: nc.tensor.matmul(ps[2][:, 0:32], lhsT=uinc, rhs=aa[:], start=True, stop=True),
                         reads=["s1_aa", "cst"], writes=["ps2"])
                    P.op("pe", lambda: nc.tensor.matmul(ps[3][:, 0:32], lhsT=ones_f, rhs=aa[:], start=True, stop=True),
                         reads=["s1_aa", "cst"], writes=["ps3"])
                    P.op("pe", lambda: nc.tensor.matmul(ps[4][0:96, 0:128], lhsT=a3[:].rearrange("p a b -> p (a b)"), rhs=uinc,
                                                        start=True, stop=True),
                         reads=["s1_a3", "cst"], writes=["ps4"])
                    if sub == 106:
                        P.barrier(); return nc, dbg
                    P.op("dve", lambda: nc.vector.tensor_copy(out=acs[:, c, :], in_=ps[2][:, 0:32]), reads=["ps2"], writes=[("acs", c)])
                    if sub == 108:
                        P.barrier(); return nc, dbg
                    P.op("act", lambda: nc.scalar.activation(out=ea[:, c, :], in_=acs[:, c, :], func=AF.Exp), reads=[("acs", c)], writes=[("ea", c)])
                    P.op("dve", lambda: nc.vector.tensor_copy(out=rr[:], in_=ps[3][:, 0:32]), reads=["ps3"], writes=["s1_rr"])
                    P.op("act", lambda: nc.scalar.activation(out=cdb[:, c, :], in_=rr[:], func=AF.Exp), reads=["s1_rr"], writes=[("cdb", c)])
                    if sub == 109:
                        P.barrier(); return nc, dbg
                    P.op("dve", lambda: nc.vector.tensor_tensor(out=xd[:], in0=rr[:], in1=acs[:, c, :], op=ALU.subtract),
                         reads=["s1_rr", ("acs", c)], writes=["s1_xd"])
                    P.op("act", lambda: nc.scalar.activation(out=ee[:], in_=xd[:], func=AF.Exp), reads=["s1_xd"], writes=["s1_ee"])
                    P.op("dve", lambda: nc.vector.tensor_tensor(out=dtw[:, c, :], in0=dt_all[:, c, :], in1=ee[:], op=ALU.mult),
                         reads=[("dt", c), "s1_ee"], writes=[("dtw", c)])
                    if sub == 107:
                        P.barrier(); return nc, dbg
                    P.op("act", lambda: nc.scalar.copy(out=Hb[0:96, :], in_=ps[4][0:96, 0:128]), reads=["ps4"], writes=["s1_Hb"])
                    P.op("dve", lambda: nc.vector.tensor_tensor(out=r1[0:96, :], in0=ps[4][0:96, 0:128], in1=Hb[0:96, :], op=ALU.subtract),
                         reads=["ps4", "s1_Hb"], writes=["s1_r1"])
                    P.op("act", lambda: nc.scalar.copy(out=Mb[0:96, :], in_=r1[0:96, :]), reads=["s1_r1"], writes=["s1_Mb"])
                    P.op("dve", lambda: nc.vector.tensor_tensor(out=r2[0:96, :], in0=r1[0:96, :], in1=Mb[0:96, :], op=ALU.subtract),
                         reads=["s1_r1", "s1_Mb"], writes=["s1_r2"])
                    P.op("pool", lambda: nc.gpsimd.tensor_copy(out=A3[0:32, c, :], in_=Hb[0:32, :]), reads=["s1_Hb", "A3z"], writes=[("A3", c, 0)])
                    P.op("pool", lambda: nc.gpsimd.tensor_copy(out=A3[32:64, c, :], in_=Mb[32:64, :]), reads=["s1_Mb", "A3z"], writes=[("A3", c, 1)])
                    P.op("act", lambda: nc.scalar.copy(out=A3[64:96, c, :], in_=r2[64:96, :]), reads=["s1_r2", "A3z"], writes=[("A3", c, 2)])
                P.barrier()
            if stage == 3 and sub == 1:
                for nm, t_ in [("dt_all", dt_all), ("acs", acs), ("ea", ea), ("dtw", dtw), ("cdb", cdb)]:
                    o = dout(nm + "_dbg", [128, NT, 32], F32)
                    P.dma("sp", "out", [(o, t_[:])])
                o = dout("A3_dbg", [128, NT, 128], BF16)
                P.dma("sp", "out", [(o, A3[:])])
                o = dout("cwfm_dbg", [128, 24, 5], F32)
                P.dma("sp", "out", [(o, cwfm[:])])
                o = dout("selb_dbg", [128, 32, 128], BF16)
                P.dma("sp", "out", [(o, selb[:])])
                P.barrier()
                return nc, dbg

            xs_tm = sb("xs_tm", [128, NT, 512], BF16, sS)
            BT = sb("BT", [128, T], BF16, sS)
            CT = sb("CT", [128, T], BF16, sS)
            B_tm = sb("B_tm", [128, NT, 128], BF16, sS)
            rawb = sb("rawb", [128, T + 4], BF16, sS)
            xcf = [sb(f"xcf{i}", [128, 512], BF16, sS) for i in range(2)]
            dg = [sb(f"dg{i}", [128, 4, 128], BF16, sS) for i in range(2)]
            wch = [sb(f"wch{i}", [128, 8, 128], BF16, sS) for i in range(2)]
            wz = sb("wz", [128, 8, 512], BF16, sS)
            ssdg = sb("ssdg", [128, 512], F32, sS)
            hst = sb("hst", [128, 512], F32, sS)
            hstb = sb("hstb", [128, 512], BF16, sS)
            cbm = sb("cbm", [128, 128], F32, sS)
            seg = [sb(f"seg{i}", [128, 512], F32, sS) for i in range(2)]
            Ee = seg
            MT = [sb(f"MT{i}", [128, 512], BF16, sS) for i in range(2)]
            xdt = sb("xdt", [128, 512], BF16, sS)
            xw = sb("xw", [128, 512], BF16, sS)
            t1 = sb("t1", [128, 512], F32, sS)
            t2 = sb("t2", [128, 512], F32, sS)
            t3 = sb("t3", [128, 512], F32, sS)
            yv = t1
            sz = t3
            ynb = sb("ynb", [128, 512], BF16, sS)
            sjunk = ynb
            g1 = [sb(f"g1_{i}", [128, 1], F32, sS) for i in range(4)]
            P.op("pool", lambda: nc.gpsimd.memset(rawb[:, 0:4], 0.0), writes=["rawb_halo"])
            wctr2 = [0]
            for g in range(4 if sub >= 30 else 1):
                for hf in range(2):
                    c0 = O_Z + g * 512 + hf * 256
                    P.dma("sp", "wstS", [(wstS[:], win_v[:, :, c0:c0 + 256])], writes=["wstS"])
                    P.op("pool", lambda: nc.gpsimd.tensor_copy(out=wz[:, :, hf * 256:(hf + 1) * 256], in_=wstS[:]),
                         reads=["wstS"], writes=[("wz", hf)])
                P.dma("sp", "ssdg", [(ssdg[:], gains["ssd_norm"][:, g * 512:(g + 1) * 512].partition_broadcast(128))], writes=["ssdg"])
                chts = [(O_XBC + g * 512 + j * 128, 4 * g + j, "x", j) for j in range(4)]
                chts += [(O_XBC + 2048 + g * 128, 16 + g, "B", 0), (O_XBC + 2560 + g * 128, 20 + g, "C", 0)]
                for (c0, cti, kind, j) in chts:
                    wi = wctr2[0] % 2
                    wctr2[0] += 1
                    P.dma("sp", "wstS", [(wstS[:, :, 0:128], win_v[:, :, c0:c0 + 128])], writes=["wstS"])
                    P.op("pool", lambda: nc.gpsimd.tensor_copy(out=wch[wi][:], in_=wstS[:, :, 0:128]), reads=["wstS"], writes=[f"wch{wi}"])
                    for jj in range(4):
                        P.op("pool", lambda: nc.gpsimd.tensor_scalar(out=dg[wi][:, jj, :], in0=ident, scalar1=cwfm[:, cti, jj:jj + 1],
                                                                     scalar2=None, op0=ALU.mult),
                             reads=["cstb", "cwfm"], writes=[(f"dg{wi}", jj)])
                    for tb in range(4):
                        bank = tb % 2
                        for kt in range(8):
                            f = lambda: nc.tensor.matmul(ps[bank][:], lhsT=wch[wi][:, kt, :], rhs=hT[:, kt, tb * 512:(tb + 1) * 512],
                                                         start=(kt == 0), stop=(kt == 7))
                            if kt < 7:
                                P.quiet("pe", f, reads=[f"wch{wi}"], writes=[f"ps{bank}"])
                            else:
                                P.op("pe", f, reads=[f"wch{wi}"], writes=[f"ps{bank}"])
                        P.op("act", lambda: nc.scalar.copy(out=rawb[:, 4 + tb * 512:4 + (tb + 1) * 512], in_=ps[bank][:]),
                             reads=[f"ps{bank}"], writes=[("rawb", tb)])
                    for tb in range(4):
                        bank = 2 + tb % 2
                        for jj in range(4):
                            f = lambda: nc.tensor.matmul(ps[bank][:], lhsT=dg[wi][:, jj, :],
                                                         rhs=rawb[:, 1 + tb * 512 + jj:1 + tb * 512 + jj + 512],
                                                         start=(jj == 0), stop=(jj == 3))
                            rd = [(f"dg{wi}", jj), ("rawb", tb), "rawb_halo"] + ([("rawb", tb - 1)] if tb > 0 else [])
                            if jj < 3:
                                P.quiet("pe", f, reads=rd, writes=[f"ps{bank}"])
                            else:
                                P.op("pe", f, reads=rd, writes=[f"ps{bank}"])
                        if kind == "x":
                            xb_ = tb % 2
                            P.op("act", lambda: nc.scalar.activation(out=xcf[xb_][:], in_=ps[bank][:], func=AF.Silu,
                                                                     bias=cwfm[:, cti, 4:5]),
                                 reads=[f"ps{bank}", "cwfm"], writes=[f"xcf{xb_}"])
                            for i4 in range(4):
                                f = lambda: nc.tensor.transpose(out=psb[:, i4 * 128:(i4 + 1) * 128], in_=xcf[xb_][:, i4 * 128:(i4 + 1) * 128],
                                                                identity=ident)
                                if i4 < 3:
                                    P.quiet("pe", f, reads=[f"xcf{xb_}", "cstb"], writes=["psb"])
                                else:
                                    P.op("pe", f, reads=[f"xcf{xb_}", "cstb"], writes=["psb"])
                            P.op("act", lambda: nc.scalar.copy(out=xs_tm[:, tb * 4:(tb + 1) * 4, j * 128:(j + 1) * 128],
                                                               in_=bview(psb[:, 0:512], 4)),
                                 reads=["psb"], writes=[("xs_tm", tb, j)])
                        else:
                            dstT = BT if kind == "B" else CT
                            P.op("act", lambda: nc.scalar.activation(out=dstT[:, tb * 512:(tb + 1) * 512], in_=ps[bank][:], func=AF.Silu,
                                                                     bias=cwfm[:, cti, 4:5]),
                                 reads=[f"ps{bank}", "cwfm"], writes=[(kind + "T", tb)])
                if sub == 202:
                    P.barrier(); return nc, dbg
                for k0 in range(0, NT, 8):
                    for jj in range(8):
                        cc = k0 + jj
                        f = lambda: nc.tensor.transpose(out=psb[:, jj * 128:(jj + 1) * 128], in_=BT[:, cc * 128:(cc + 1) * 128], identity=ident)
                        if jj < 7:
                            P.quiet("pe", f, reads=[("BT", cc // 4), "cstb"], writes=["psb"])
                        else:
                            P.op("pe", f, reads=[("BT", cc // 4), "cstb"], writes=["psb"])
                    P.op("act", lambda: nc.scalar.copy(out=B_tm[:, k0:k0 + 8, :], in_=bview(psb[:], 8)), reads=["psb"], writes=[("B_tm", k0 // 8)])
                P.op("pool", lambda: nc.gpsimd.memset(hst[:], 0.0), writes=["hst"])
                P.op("pool", lambda: nc.gpsimd.memset(hstb[:], 0.0), writes=["hstb"])
                if sub == 203:
                    P.barrier(); return nc, dbg
                xsr = lambda c_: [("xs_tm", c_ // 4, j_) for j_ in range(4)]
                for c in range(NT):
                    cs_ = slice(c * 128, (c + 1) * 128)
                    P.op("pe", lambda: nc.tensor.matmul(ps[0][:, 0:128], lhsT=BT[:, cs_], rhs=CT[:, cs_], start=True, stop=True),
                         reads=[("BT", c // 4), ("CT", c // 4)], writes=["ps0"])
                    P.op("dve", lambda: nc.vector.tensor_tensor(out=cbm[:], in0=ps[0][:, 0:128], in1=cst[:, C_CT:C_CT + 128], op=ALU.mult),
                         reads=["ps0", "cst"], writes=["cbm"])
                    P.op("pool", lambda: nc.gpsimd.tensor_tensor(out=bview(xdt[:], 8), in0=bview(xs_tm[:, c, :], 8), in1=G8(dt_all, c, g),
                                                                 op=ALU.mult), reads=xsr(c), writes=["xdt"])
                    P.op("pool", lambda: nc.gpsimd.tensor_tensor(out=bview(xw[:], 8), in0=bview(xs_tm[:, c, :], 8), in1=G8(dtw, c, g),
                                                                 op=ALU.mult), reads=xsr(c), writes=["xw"])
                    if sub == 2041:
                        P.barrier(); return nc, dbg
                    for hb in ([1] if sub == 2046 else ([1, 0] if sub == 2048 else ([0, 0] if sub == 2049 else range(2)))):
                        bcb = 1 + (hb if sub != 2045 else 0)
                        for hh in range(4):
                            h = 8 * g + 4 * hb + hh
                            f = lambda: nc.tensor.matmul(ps[bcb][:, hh * 128:(hh + 1) * 128], lhsT=selb[:, h, :], rhs=A3[:, c, :],
                                                         start=True, stop=True, skip_group_check=True)
                            if hh < 3:
                                P.quiet("pe", f, reads=["selb"], writes=[f"ps{bcb}"])
                            else:
                                P.op("pe", f, reads=["selb"], writes=[f"ps{bcb}"])
                        for hh in range(4):
                            h = 8 * g + 4 * hb + hh
                            P.op("dve", lambda: nc.vector.tensor_scalar(out=seg[hb][:, hh * 128:(hh + 1) * 128],
                                                                        in0=ps[bcb][:, hh * 128:(hh + 1) * 128],
                                                                        scalar1=acs[:, c, h:h + 1], scalar2=0.0, op0=ALU.subtract, op1=ALU.min),
                                 reads=[f"ps{bcb}"], writes=[(f"seg{hb}", hh), f"Ee{hb}"])
                        if sub == 2042:
                            P.barrier(); return nc, dbg
                        P.op("act", lambda: nc.scalar.activation(out=Ee[hb][:], in_=seg[hb][:], func=AF.Exp),
                             reads=[(f"seg{hb}", hh_) for hh_ in range(4)], writes=[f"Ee{hb}"] + [(f"seg{hb}", hh_) for hh_ in range(4)])
                        P.op("dve", lambda: nc.vector.tensor_tensor(out=bview(MT[hb][:], 4), in0=bview(Ee[hb][:], 4),
                                                                    in1=cbm[:].unsqueeze(1).to_broadcast([128, 4, 128]), op=ALU.mult),
                             reads=[f"Ee{hb}", "cbm"], writes=[f"MT{hb}"])
                        if sub == 2043:
                            P.barrier(); return nc, dbg
                        for hh in range(4):
                            hl = 4 * hb + hh
                            P.op("pe", lambda: nc.tensor.matmul(ps[3][:, hl * 64:(hl + 1) * 64], lhsT=MT[hb][:, hh * 128:(hh + 1) * 128],
                                                                rhs=xdt[:, hl * 64:(hl + 1) * 64], start=True, stop=True, skip_group_check=True),
                                 reads=[f"MT{hb}", "xdt"], writes=["ps3"])
                        if sub == 2044:
                            P.barrier(); return nc, dbg
                    if sub in (204, 2045, 2046, 2047, 2048, 2049):
                        P.barrier(); return nc, dbg
                    yres = ["ps3"]
                    if c > 0:
                        P.op("pe", lambda: nc.tensor.matmul(ps[4][:], lhsT=CT[:, cs_], rhs=hstb[:], start=True, stop=True),
                             reads=[("CT", c // 4), "hstb"], writes=["ps4"])
                        P.op("dve", lambda: nc.vector.tensor_tensor(out=bview(t1[:], 8), in0=bview(ps[4][:], 8), in1=G8(ea, c, g), op=ALU.mult),
                             reads=["ps4"], writes=["t1"])
                        P.op("dve", lambda: nc.vector.tensor_tensor(out=t2[:], in0=ps[3][:], in1=t1[:], op=ALU.add),
                             reads=yres + ["t1"], writes=["t2"])
                    else:
                        P.op("dve", lambda: nc.vector.tensor_copy(out=t2[:], in_=ps[3][:]), reads=yres, writes=["t2"])
                    P.op("pool", lambda: nc.gpsimd.tensor_tensor(out=bview(t3[:], 8), in0=bview(xs_tm[:, c, :], 8), in1=G8(gb["d_skip"][:].unsqueeze(1), 0, g),
                                                                 op=ALU.mult), reads=xsr(c) + ["g_d_skip"], writes=["t3"])
                    P.op("pool", lambda: nc.gpsimd.tensor_tensor(out=yv[:], in0=t2[:], in1=t3[:], op=ALU.add), reads=["t2", "t3"], writes=["t1"])
                    if sub == 205 and c == 1:
                        P.barrier(); return nc, dbg
                    P.op("pe", lambda: nc.tensor.matmul(ps[5][:], lhsT=B_tm[:, c, :], rhs=xw[:], start=True, stop=True),
                         reads=[("B_tm", c // 8), "xw"], writes=["ps5"])
                    P.op("dve", lambda: nc.vector.tensor_tensor(out=bview(hst[:], 8), in0=bview(hst[:], 8), in1=G8(cdb, c, g), op=ALU.mult),
                         reads=["hst"], writes=["hst"])
                    P.op("dve", lambda: nc.vector.tensor_tensor(out=hst[:], in0=ps[5][:], in1=hst[:], op=ALU.add), reads=["ps5", "hst"], writes=["hst"])
                    P.op("act", lambda: nc.scalar.copy(out=hstb[:], in_=hst[:]), reads=["hst"], writes=["hstb"])
                    if sub == 206 and c == 1:
                        P.barrier(); return nc, dbg
                    for kt in range(8):
                        f = lambda: nc.tensor.matmul(ps[6][:], lhsT=hT[:, kt, cs_], rhs=wz[:, kt, :], start=(kt == 0), stop=(kt == 7))
                        if kt < 7:
                            P.quiet("pe", f, reads=[("wz", 0), ("wz", 1)], writes=["ps6"])
                        else:
                            P.op("pe", f, reads=[("wz", 0), ("wz", 1)], writes=["ps6"])
                    P.op("act", lambda: nc.scalar.activation(out=sz[:], in_=ps[6][:], func=AF.Silu), reads=["ps6"], writes=["t3"])
                    P.op("dve", lambda: nc.vector.tensor_tensor(out=yv[:], in0=yv[:], in1=sz[:], op=ALU.mult), reads=["t1", "t3"], writes=["t1"])
                    P.op("act", lambda: nc.scalar.activation(out=sjunk[:], in_=yv[:], func=AF.Square, accum_out=g1[0][:]),
                         reads=["t1"], writes=["ynb", "g1_0"])
                    P.op("dve", lambda: nc.vector.tensor_scalar(out=g1[1][:], in0=g1[0][:], scalar1=1.0 / 512, scalar2=EPS, op0=ALU.mult, op1=ALU.add),
                         reads=["g1_0"], writes=["g1_1"])
                    P.op("act", lambda: nc.scalar.activation(out=g1[2][:], in_=g1[1][:], func=AF.Sqrt), reads=["g1_1"], writes=["g1_2"])
                    P.op("dve", lambda: nc.vector.reciprocal(out=g1[3][:], in_=g1[2][:]), reads=["g1_2"], writes=["g1_3"])
                    P.op("dve", lambda: nc.vector.scalar_tensor_tensor(out=ynb[:], in0=yv[:], scalar=g1[3][:, 0:1], in1=ssdg[:], op0=ALU.mult, op1=ALU.mult),
                         reads=["t1", "g1_3", "ssdg"], writes=["ynb"])
                    for i4 in range(4):
                        f = lambda: nc.tensor.transpose(out=psb[:, i4 * 128:(i4 + 1) * 128], in_=ynb[:, i4 * 128:(i4 + 1) * 128], identity=ident)
                        if i4 < 3:
                            P.quiet("pe", f, reads=["ynb", "cstb"], writes=["psb"])
                        else:
                            P.op("pe", f, reads=["ynb", "cstb"], writes=["psb"])
                    P.op("act", lambda: nc.scalar.copy(out=ysT[:, 4 * g:4 * g + 4, cs_], in_=bview(psb[:, 0:512], 4)), reads=["psb"], writes=[("ysT", g, c)])
            P.barrier()
        if stage == 3:
            o = dout("ysT_dbg", [128, 16, T], BF16)
            P.dma("sp", "out", [(o, ysT[:])])
            P.barrier()
            return nc, dbg

        stM = contextlib.ExitStack()
        es.enter_context(stM)
        mgT = sb("mgT", [128, 8, T], BF16, stM)
        with contextlib.ExitStack() as sM:
            yaT2 = sb("yaT2", [128, 8, T], BF16, sM)
            P.dma("sp", "ya_reload", [(yaT2[:], ya_spill)], writes=["yaT2"])
            wstM = sb("wstM", [128, 16, 128], F32, sM)
            wac = sb("wac", [128, 8, 128], BF16, sM)
            wbc = sb("wbc", [128, 16, 128], BF16, sM)
            wgac = sb("wgac", [128, 8, 128], BF16, sM)
            wgbc = sb("wgbc", [128, 8, 128], BF16, sM)
            sga = [sb(f"sga{i}", [128, 512], F32, sM) for i in range(2)]
            sgb = [sb(f"sgb{i}", [128, 512], F32, sM) for i in range(2)]
            m1 = [sb(f"m1_{i}", [128, 512], F32, sM) for i in range(2)]
            m2 = [sb(f"m2_{i}", [128, 512], F32, sM) for i in range(2)]
            wa_v = wa_d.rearrange("(kt p) n -> p kt n", p=128)
            wb_v = wb_d.rearrange("(kt p) n -> p kt n", p=128)
            it = [0]
            for nt in range(8):
                ns = slice(nt * 128, (nt + 1) * 128)
                for (dst, src, nk_, nm) in [(wac, wa_v[:, :, ns], 8, "wac"), (wbc, wb_v[:, :, ns], 16, "wbc"),
                                            (wgac, win_v[:, :, O_GA + nt * 128:O_GA + (nt + 1) * 128], 8, "wgac"),
                                            (wgbc, win_v[:, :, O_GB + nt * 128:O_GB + (nt + 1) * 128], 8, "wgbc")]:
                    P.dma("sp", "wstM", [(wstM[:, 0:nk_, :], src)], writes=["wstM"])
                    P.op("pool", lambda: nc.gpsimd.tensor_copy(out=dst[:], in_=wstM[:, 0:nk_, :]), reads=["wstM"], writes=[nm])
                for tb in range(4):
                    ts_ = slice(tb * 512, (tb + 1) * 512)
                    par = it[0] % 2
                    it[0] += 1
                    bA, bB, bGA = (0, 1, 2) if par == 0 else (4, 5, 6)
                    bGB = 3

                    def acc(bank, wt, nk_, rhsT, nm, rd):
                        for kt in range(nk_):
                            f = lambda: nc.tensor.matmul(ps[bank][:], lhsT=wt[:, kt, :], rhs=rhsT[:, kt, ts_], start=(kt == 0), stop=(kt == nk_ - 1))
                            if kt < nk_ - 1:
                                P.quiet("pe", f, reads=[nm] + rd, writes=[f"ps{bank}"])
                            else:
                                P.op("pe", f, reads=[nm] + rd, writes=[f"ps{bank}"])
                    acc(bGA, wgac, 8, hT, "wgac", [])
                    acc(bGB, wgbc, 8, hT, "wgbc", [])
                    acc(bA, wac, 8, yaT2, "wac", ["yaT2"])
                    acc(bB, wbc, 16, ysT, "wbc", [])
                    P.op("act", lambda: nc.scalar.activation(out=sga[par][:], in_=ps[bGA][:], func=AF.Sigmoid), reads=[f"ps{bGA}"], writes=[f"sga{par}"])
                    P.op("act", lambda: nc.scalar.activation(out=sgb[par][:], in_=ps[bGB][:], func=AF.Sigmoid), reads=[f"ps{bGB}"], writes=[f"sgb{par}"])
                    P.op("dve", lambda: nc.vector.tensor_tensor(out=m1[par][:], in0=ps[bA][:], in1=sga[par][:], op=ALU.mult),
                         reads=[f"ps{bA}", f"sga{par}"], writes=[f"m1_{par}"])
                    P.op("dve", lambda: nc.vector.tensor_tensor(out=m2[par][:], in0=ps[bB][:], in1=sgb[par][:], op=ALU.mult),
                         reads=[f"ps{bB}", f"sgb{par}"], writes=[f"m2_{par}"])
                    P.op("pool", lambda: nc.gpsimd.tensor_tensor(out=mgT[:, nt, ts_], in0=m1[par][:], in1=m2[par][:], op=ALU.add),
                         reads=[f"m1_{par}", f"m2_{par}"], writes=[("mgT", nt, tb)])
            P.barrier()
        if stage == 4:
            o = dout("mgT_dbg", [128, 8, T], BF16)
            P.dma("sp", "out", [(o, mgT[:])])
            P.barrier()
            return nc, dbg

        x1 = ysT[:].bitcast(F32)
        assert list(x1.shape) == [128, NT, D], x1.shape
        with contextlib.ExitStack() as sO:
            wstO = sb("wstO", [128, 8, 256], F32, sO)
            wo = sb("wo", [128, 8, D], BF16, sO)
            wo_v = wo_d.rearrange("(kt p) n -> p kt n", p=128)
            for q4 in range(4):
                P.dma("sp", "wstO", [(wstO[:], wo_v[:, :, q4 * 256:(q4 + 1) * 256])], writes=["wstO"])
                P.op("pool", lambda: nc.gpsimd.tensor_copy(out=wo[:, :, q4 * 256:(q4 + 1) * 256], in_=wstO[:]), reads=["wstO"], writes=[("wo", q4)])
            for c in range(NT):
                cs_ = slice(c * 128, (c + 1) * 128)
                P.dma("sp", f"x1ld{c % 2}", [(x1[:, c, :], x_d[cs_, :])], writes=[("x1", c)])
                for hf in range(2):
                    bank = (2 * c + hf) % 4
                    for kt in range(8):
                        f = lambda: nc.tensor.matmul(ps[bank][:], lhsT=mgT[:, kt, cs_], rhs=wo[:, kt, hf * 512:(hf + 1) * 512],
                                                     start=(kt == 0), stop=(kt == 7))
                        rd = [("wo", 2 * hf), ("wo", 2 * hf + 1)]
                        if kt < 7:
                            P.quiet("pe", f, reads=rd, writes=[f"ps{bank}"])
                        else:
                            P.op("pe", f, reads=rd, writes=[f"ps{bank}"])
                    P.op("dve", lambda: nc.vector.tensor_tensor(out=x1[:, c, hf * 512:(hf + 1) * 512], in0=ps[bank][:],
                                                                in1=x1[:, c, hf * 512:(hf + 1) * 512], op=ALU.add),
                         reads=[f"ps{bank}", ("x1", c)], writes=[("x1", c)])
            P.barrier()
        stM.close()
        if stage == 5:
            o = dout("x1_dbg", [128, NT, D], F32)
            P.dma("sp", "out", [(o, x1)])
            P.barrier()
            return nc, dbg

        with contextlib.ExitStack() as sN:
            load_gain("ffn_norm", D, sN)

            def from_x1(c, buf, rname):
                P.op("pool", lambda: nc.gpsimd.tensor_copy(out=buf[:], in_=x1[:, c, :]), reads=[("x1", c)], writes=[rname])
            rmsnorm_T(from_x1, "ffn_norm", hT, sN)
            P.barrier()

        with contextlib.ExitStack() as sE:
            selm = sb("selm", [128, 32, 128], BF16, sE)
            P.op("pool", lambda: nc.gpsimd.memset(selm[:], 0.0), writes=["selm"])
            for r3 in range(3):
                P.op("pool", lambda: nc.gpsimd.tensor_copy(
                    out=selm[32 * r3:32 * r3 + 32, :, :],
                    in_=cstb[32 * r3:32 * r3 + 32, C_ID + 32 * r3:C_ID + 32 * r3 + 32].unsqueeze(2).to_broadcast([32, 32, 128])),
                    reads=["cstb", "selm"], writes=["selm"])
            cT3 = sb("cT3", [128, T], BF16, sE)
            P.op("pool", lambda: nc.gpsimd.memset(cT3[:], 0.0), writes=["cT3z"])
            with contextlib.ExitStack() as sR:
                wr_s = sb("wr_s", [128, 8, 36], F32, sR)
                wr = sb("wr", [128, 8, 36], BF16, sR)
                P.dma("sp", "wr_s", [(wr_s[:, :, 0:4], wrg_d.rearrange("(kt p) n -> p kt n", p=128)),
                                     (wr_s[:, :, 4:36], wre_d.rearrange("(kt p) n -> p kt n", p=128))], writes=["wr_s"])
                P.op("pool", lambda: nc.gpsimd.tensor_copy(out=wr[:], in_=wr_s[:]), reads=["wr_s"], writes=["wr"])
                rb_ = sb("rbias", [128, 36], F32, sR)
                P.dma("sp", "rbias", [(rb_[:, 0:4], gains["b_route_group"].partition_broadcast(128)),
                                      (rb_[:, 4:36], gains["b_route_expert"].partition_broadcast(128))], writes=["rbias"])
                lg = sb("r_lg", [128, 36], F32, sR)
                r1c = [sb(f"r_c{i}", [128, 1], F32, sR) for i in range(8)]
                oh = sb("r_oh", [128, 4], F32, sR)
                ge = sb("r_ge", [128, 4], F32, sR)
                tmp48 = sb("r_t48", [128, 4, 8], F32, sR)
                ein = sb("r_ein", [128, 8], F32, sR)
                top8 = sb("r_top8", [128, 8], F32, sR)
                msel = sb("r_msel", [128, 8], F32, sR)
                wex = sb("r_wex", [128, 8], F32, sR)
                comb = sb("r_comb", [128, 4, 8], F32, sR)
                comb3 = sb("r_comb3", [128, 3, 32], F32, sR)
                Hb2 = sb("r_Hb", [128, 128], BF16, sR)
                Mb2 = sb("r_Mb", [128, 128], BF16, sR)
                q1 = sb("r_q1", [128, 128], F32, sR)
                q2 = sb("r_q2", [128, 128], F32, sR)
                mx, nmx, sme, gw, m21, den, rden, sc_ = r1c
                for c in range(NT):
                    cs_ = slice(c * 128, (c + 1) * 128)
                    for kt in range(8):
                        f = lambda: nc.tensor.matmul(ps[0][:, 0:36], lhsT=hT[:, kt, cs_], rhs=wr[:, kt, :], start=(kt == 0), stop=(kt == 7))
                        if kt < 7:
                            P.quiet("pe", f, reads=["wr"], writes=["ps0"])
                        else:
                            P.op("pe", f, reads=["wr"], writes=["ps0"])
                    P.op("dve", lambda: nc.vector.tensor_tensor(out=lg[:], in0=ps[0][:, 0:36], in1=rb_[:], op=ALU.add), reads=["ps0", "rbias"], writes=["r_lg"])
                    P.op("dve", lambda: nc.vector.tensor_reduce(out=mx[:], in_=lg[:, 0:4], axis=AX.X, op=ALU.max), reads=["r_lg"], writes=["r_mx"])
                    P.op("dve", lambda: nc.vector.tensor_scalar(out=oh[:], in0=lg[:, 0:4], scalar1=mx[:, 0:1], scalar2=None, op0=ALU.is_ge),
                         reads=["r_lg", "r_mx"], writes=["r_oh"])
                    P.op("dve", lambda: nc.vector.tensor_scalar(out=nmx[:], in0=mx[:], scalar1=-1.0, scalar2=None, op0=ALU.mult), reads=["r_mx"], writes=["r_nmx"])
                    P.op("act", lambda: nc.scalar.activation(out=ge[:], in_=lg[:, 0:4], func=AF.Exp, bias=nmx[:, 0:1], accum_out=sme[:]),
                         reads=["r_lg", "r_nmx"], writes=["r_ge", "r_sme"])
                    P.op("dve", lambda: nc.vector.reciprocal(out=gw[:], in_=sme[:]), reads=["r_sme"], writes=["r_gw"])
                    P.op("dve", lambda: nc.vector.tensor_tensor(out=tmp48[:], in0=bview(lg[:, 4:36], 4), in1=oh[:].unsqueeze(2).to_broadcast([128, 4, 8]),
                                                                op=ALU.mult), reads=["r_lg", "r_oh"], writes=["r_t48"])
                    P.op("dve", lambda: nc.vector.tensor_reduce(out=ein[:], in_=tmp48[:].rearrange("p g e -> p e g"), axis=AX.X, op=ALU.add),
                         reads=["r_t48"], writes=["r_ein"])
                    P.op("dve", lambda: nc.vector.max(out=top8[:], in_=ein[:]), reads=["r_ein"], writes=["r_top8"])
                    P.op("dve", lambda: nc.vector.tensor_scalar(out=msel[:], in0=ein[:], scalar1=top8[:, 1:2], scalar2=None, op0=ALU.is_ge),
                         reads=["r_ein", "r_top8"], writes=["r_msel"])
                    P.op("dve", lambda: nc.vector.tensor_scalar(out=nmx[:], in0=top8[:, 0:1], scalar1=-1.0, scalar2=None, op0=ALU.mult),
                         reads=["r_top8"], writes=["r_nmx"])
                    P.op("act", lambda: nc.scalar.activation(out=wex[:], in_=ein[:], func=AF.Exp, bias=nmx[:, 0:1]), reads=["r_ein", "r_nmx"], writes=["r_wex"])
                    P.op("act", lambda: nc.scalar.activation(out=m21[:], in_=top8[:, 1:2], func=AF.Exp, bias=nmx[:, 0:1]), reads=["r_top8", "r_nmx"], writes=["r_m21"])
                    P.op("dve", lambda: nc.vector.tensor_scalar(out=den[:], in0=m21[:], scalar1=1.0, scalar2=None, op0=ALU.add), reads=["r_m21"], writes=["r_den"])
                    P.op("dve", lambda: nc.vector.reciprocal(out=rden[:], in_=den[:]), reads=["r_den"], writes=["r_rden"])
                    P.op("dve", lambda: nc.vector.tensor_tensor(out=sc_[:], in0=rden[:], in1=gw[:], op=ALU.mult), reads=["r_rden", "r_gw"], writes=["r_sc"])
                    P.op("dve", lambda: nc.vector.tensor_tensor(out=wex[:], in0=wex[:], in1=msel[:], op=ALU.mult), reads=["r_wex", "r_msel"], writes=["r_wex"])
                    P.op("dve", lambda: nc.vector.tensor_scalar(out=wex[:], in0=wex[:], scalar1=sc_[:, 0:1], scalar2=None, op0=ALU.mult),
                         reads=["r_wex", "r_sc"], writes=["r_wex"])
                    P.op("dve", lambda: nc.vector.tensor_tensor(out=comb[:], in0=oh[:].unsqueeze(2).to_broadcast([128, 4, 8]),
                                                                in1=wex[:].unsqueeze(1).to_broadcast([128, 4, 8]), op=ALU.mult),
                         reads=["r_oh", "r_wex"], writes=["r_comb"])
                    P.op("dve", lambda: nc.vector.tensor_copy(out=comb3[:], in_=comb[:].rearrange("p g e -> p (g e)").unsqueeze(1).to_broadcast([128, 3, 32])),
                         reads=["r_comb"], writes=["r_comb3"])
                    P.op("pe", lambda: nc.tensor.transpose(out=ps[1][0:96, 0:128], in_=comb3[:].rearrange("p a b -> p (a b)"),
                                                           identity=cst[:, C_ID:C_ID + 128]),
                         reads=["r_comb3", "cst"], writes=["ps1"])
                    P.op("act", lambda: nc.scalar.copy(out=Hb2[0:96, :], in_=ps[1][0:96, 0:128]), reads=["ps1"], writes=["r_Hb"])
                    P.op("dve", lambda: nc.vector.tensor_tensor(out=q1[0:96, :], in0=ps[1][0:96, 0:128], in1=Hb2[0:96, :], op=ALU.subtract),
                         reads=["ps1", "r_Hb"], writes=["r_q1"])
                    P.op("act", lambda: nc.scalar.copy(out=Mb2[0:96, :], in_=q1[0:96, :]), reads=["r_q1"], writes=["r_Mb"])
                    P.op("dve", lambda: nc.vector.tensor_tensor(out=q2[0:96, :], in0=q1[0:96, :], in1=Mb2[0:96, :], op=ALU.subtract),
                         reads=["r_q1", "r_Mb"], writes=["r_q2"])
                    P.op("pool", lambda: nc.gpsimd.tensor_copy(out=cT3[0:32, cs_], in_=Hb2[0:32, :]), reads=["r_Hb", "cT3z"], writes=[("cT3", c, 0)])
                    P.op("pool", lambda: nc.gpsimd.tensor_copy(out=cT3[32:64, cs_], in_=Mb2[32:64, :]), reads=["r_Mb", "cT3z"], writes=[("cT3", c, 1)])
                    P.op("act", lambda: nc.scalar.copy(out=cT3[64:96, cs_], in_=q2[64:96, :]), reads=["r_q2", "cT3z"], writes=[("cT3", c, 2)])
                P.barrier()
            if stage == 6:
                o = dout("cT3_dbg", [128, T], BF16)
                P.dma("sp", "out", [(o, cT3[:])])
                o = dout("h2T_dbg", [128, 8, T], BF16)
                P.dma("sp", "out", [(o, hT[:])])
                P.barrier()
                return nc, dbg

            NE = 32 if sub >= 30 else 2
            wstE = [sb(f"wstE{i}", [128, 8, 256], F32, sE) for i in range(2)]
            wgu = [sb(f"wgu{i}", [128, 8, 512], BF16, sE) for i in range(2)]
            wdn = [sb(f"wdn{i}", [128, 2, D], BF16, sE) for i in range(4)]
            actT = [sb(f"actT{i}", [128, 2, T], BF16, sE) for i in range(2)]
            sgs = [sb(f"sgs{i}", [128, 512], F32, sE) for i in range(2)]
            tms = [sb(f"tms{i}", [128, 512], F32, sE) for i in range(2)]
            stc = [0]
            itc = [0]
            for e in range(NE):
                sl = e % 2
                dsl = e % 4
                for (k_, src) in [(0, wg_d[e].rearrange("(kt p) n -> p kt n", p=128)), (1, wu_d[e].rearrange("(kt p) n -> p kt n", p=128))]:
                    si = stc[0] % 2
                    stc[0] += 1
                    P.dma("sp", f"wstE{si}", [(wstE[si][:], src)], writes=[f"wstE{si}"])
                    P.op("pool", lambda: nc.gpsimd.tensor_copy(out=wgu[sl][:, :, k_ * 256:(k_ + 1) * 256], in_=wstE[si][:]),
                         reads=[f"wstE{si}"], writes=[(f"wgu{sl}", k_)])
                si = stc[0] % 2
                stc[0] += 1
                P.dma("sp", f"wstE{si}", [(wstE[si][:].rearrange("p a b -> p (a b)").rearrange("p (f n) -> p f n", f=2),
                                           wd_d[e].rearrange("(ft p) n -> p ft n", p=128))],
                      writes=[f"wstE{si}"])
                P.op("pool", lambda: nc.gpsimd.tensor_copy(out=wdn[dsl][:].rearrange("p a b -> p (a b)"), in_=wstE[si][:].rearrange("p a b -> p (a b)")),
                     reads=[f"wstE{si}"], writes=[f"wdn{dsl}"])
                for tb in range(4):
                    ts_ = slice(tb * 512, (tb + 1) * 512)
                    P.op("pe", lambda: nc.tensor.matmul(ps[4][:], lhsT=selm[:, e, :], rhs=cT3[:, ts_], start=True, stop=True),
                         reads=["selm"], writes=["ps4"])
                    for ft in range(2):
                        par = itc[0] % 2
                        itc[0] += 1
                        bG, bU = (0, 1) if par == 0 else (2, 3)
                        for (bank, k_) in [(bG, 0), (bU, 1)]:
                            for kt in range(8):
                                f = lambda: nc.tensor.matmul(ps[bank][:], lhsT=wgu[sl][:, kt, k_ * 256 + ft * 128:k_ * 256 + (ft + 1) * 128],
                                                             rhs=hT[:, kt, ts_], start=(kt == 0), stop=(kt == 7))
                                if kt < 7:
                                    P.quiet("pe", f, reads=[(f"wgu{sl}", k_)], writes=[f"ps{bank}"])
                                else:
                                    P.op("pe", f, reads=[(f"wgu{sl}", k_)], writes=[f"ps{bank}"])
                        P.op("act", lambda: nc.scalar.activation(out=sgs[par][:], in_=ps[bG][:], func=AF.Silu), reads=[f"ps{bG}"], writes=[f"sgs{par}"])
                        P.op("dve", lambda: nc.vector.tensor_tensor(out=tms[par][:], in0=ps[bU][:], in1=sgs[par][:], op=ALU.mult),
                             reads=[f"ps{bU}", f"sgs{par}"], writes=[f"tms{par}"])
                        P.op("dve", lambda: nc.vector.tensor_tensor(out=actT[sl][:, ft, ts_], in0=ps[4][:], in1=tms[par][:], op=ALU.mult),
                             reads=["ps4", f"tms{par}"], writes=[(f"actT{sl}", ft, tb)])
                if e % 2 == 1:
                    for c in range(NT):
                        cs_ = slice(c * 128, (c + 1) * 128)
                        for hf in range(2):
                            bank = 5 + (2 * c + hf) % 2
                            n_ = 0
                            for ee in (e - 1, e):
                                for ft in range(2):
                                    f = lambda: nc.tensor.matmul(ps[bank][:], lhsT=actT[ee % 2][:, ft, cs_], rhs=wdn[ee % 4][:, ft, hf * 512:(hf + 1) * 512],
                                                                 start=(n_ == 0), stop=(n_ == 3))
                                    rd = [(f"actT{ee % 2}", ft, c // 4), f"wdn{ee % 4}"]
                                    if n_ < 3:
                                        P.quiet("pe", f, reads=rd, writes=[f"ps{bank}"])
                                    else:
                                        P.op("pe", f, reads=rd, writes=[f"ps{bank}"])
                                    n_ += 1
                            P.op("dve", lambda: nc.vector.tensor_tensor(out=x1[:, c, hf * 512:(hf + 1) * 512], in0=ps[bank][:],
                                                                        in1=x1[:, c, hf * 512:(hf + 1) * 512], op=ALU.add),
                                 reads=[f"ps{bank}", ("x1", c)], writes=[("x1", c)])
            for c in range(NT):
                P.dma("sp", "out", [(out_d[c * 128:(c + 1) * 128, :], x1[:, c, :])], reads=[("x1", c)])
            P.barrier()
    return nc, dbg


def host_consts():
    cst = np.zeros((128, CW), np.float32)
    cst[:, C_ID:C_ID + 128] = np.eye(128, dtype=np.float32)
    dm = np.ones((128, 128), np.float32)
    dm[0:64, 64:128] = 0.0
    cst[:, C_DM:C_DM + 128] = dm
    cst[:, C_NB:C_NB + 128] = (dm - 1.0) * 1e30
    cst[:, C_CT:C_CT + 128] = np.triu(np.ones((128, 128), np.float32))
    cst[:, C_ONE:C_ONE + 128] = 1.0
    for i in range(NBIS):
        cst[:, C_BIS + i] = 2.0 ** (-(i + 1))
    half = 8
    inv = 500000.0 ** (-np.arange(half, dtype=np.float64) * 2.0 / 16)
    ang = np.arange(T, dtype=np.float64)[:, None] * inv[None, :]
    cos, sin = np.cos(ang), np.sin(ang)
    rope = np.concatenate([cos, cos, -sin, sin], axis=1).astype(np.float32)
    return cst, rope


_CACHE = {}


def make_inmaps(inputs):
    cst, rope = host_consts()
    maps = []
    sq = lambda a: np.ascontiguousarray(np.asarray(a, np.float32)[0])
    shared = {
        "w_in": sq(inputs["w_in"]),
        "conv_w": sq(inputs["conv_w"]),
        "w_attn_branch": sq(inputs["w_attn_branch"]), "w_ssd_branch": sq(inputs["w_ssd_branch"]),
        "w_out": sq(inputs["w_out"]), "w_route_group": sq(inputs["w_route_group"]),
        "w_route_expert": sq(inputs["w_route_expert"]),
        "w_gate": sq(inputs["w_gate"]).reshape(32, 1024, 256), "w_up": sq(inputs["w_up"]).reshape(32, 1024, 256),
        "w_down": sq(inputs["w_down"]).reshape(32, 256, 1024),
        "cst": cst, "rope": rope,
    }
    for n in ["attn_norm", "q_norm", "k_norm", "idx_k_norm", "ffn_norm", "ssd_norm", "conv_b", "dt_bias", "a_log",
              "d_skip", "b_route_group", "b_route_expert"]:
        shared[n] = np.ascontiguousarray(np.asarray(inputs[n], np.float32).reshape(1, -1))
    x = np.asarray(inputs["x"], np.float32)
    for b in range(8):
        m = dict(shared)
        m["x"] = np.ascontiguousarray(x[b])
        maps.append(m)
    return maps


def kernel(**inputs):
    if "nc" not in _CACHE:
        _CACHE["nc"] = build()[0]
    nc = _CACHE["nc"]
    maps = make_inmaps(inputs)
    res = run_bass_kernel_spmd(nc, maps, core_ids=list(range(8)))
    return np.stack([np.asarray(r["out"], np.float32) for r in res.results], axis=0)
```

```python
import contextlib
import math
import numpy as np
import concourse.bass as bass
import concourse.mybir as mybir
from concourse.bass_utils import run_bass_kernel_spmd

F32 = mybir.dt.float32
BF16 = mybir.dt.bfloat16
U32 = mybir.dt.uint32
AF = mybir.ActivationFunctionType
ALU = mybir.AluOpType
AX = mybir.AxisListType

T = 2048
D = 1024
NT = 16
EPS = 1e-6
NBIS = 16
SPL = (1024, 256, 256, 512, 64, 8, 2048, 3072, 32, 1024, 1024)
OFF = [0]
for _s in SPL:
    OFF.append(OFF[-1] + _s)
(O_Q, O_K, O_V, O_IQ, O_IK, O_IW, O_Z, O_XBC, O_DT, O_GA, O_GB, O_END) = OFF
NW = O_END

C_ID = 0
C_DM = 128
C_NB = 256
C_CT = 384
C_ONE = 512
C_BIS = 640
CW = 672


class Prog:
    ENG = ("pe", "act", "dve", "pool", "sp")

    def __init__(self, nc, es):
        self.nc = nc
        self.es = es
        self.e = {"pe": nc.tensor, "act": nc.scalar, "dve": nc.vector, "pool": nc.gpsimd, "sp": nc.sync}
        self.sem = {}
        self.cnt = {k: 0 for k in self.ENG}
        self.epoch = {k: 0 for k in self.ENG}
        for k in self.ENG:
            self.sem[("e", k, 0)] = es.enter_context(nc.semaphore(f"s_{k}_0"))
        self.dcnt = {}
        self.waited = {k: {} for k in self.ENG}
        self.res = {}
        self.nins = 0
        self.pending = {k: [] for k in self.ENG}

    def _deps(self, eng, reads, writes):
        deps = []
        for r in reads:
            st = self.res.get(r)
            if st and st[0] is not None:
                deps.append((st[0], True))
        for w in writes:
            st = self.res.get(w)
            if st:
                if st[0] is not None:
                    deps.append((st[0], True))
                for t in st[1].values():
                    deps.append((t, False))
        for (tok, strong) in deps:
            key, val = tok
            if key[0] == "e" and key[1] == eng:
                if eng == "pe":
                    continue
            if self.waited[eng].get(key, -1) >= val:
                continue
            self.e[eng].wait_ge(self.sem[key], val)
            self.waited[eng][key] = val

    def _commit(self, tok, reads, writes, rkey):
        for r in reads:
            st = self.res.setdefault(r, [None, {}])
            st[1][rkey] = tok
        for w in writes:
            self.res[w] = [tok, {}]

    def op(self, eng, fn, reads=(), writes=()):
        self._deps(eng, reads, writes)
        ins = fn()
        if self.cnt[eng] >= 30000:
            self.epoch[eng] += 1
            self.cnt[eng] = 0
            self.sem[("e", eng, self.epoch[eng])] = self.es.enter_context(
                self.nc.semaphore(f"s_{eng}_{self.epoch[eng]}"))
        key = ("e", eng, self.epoch[eng])
        self.cnt[eng] += 1
        ins.then_inc(self.sem[key], 1)
        self.nins += 1
        for (r_, w_) in self.pending[eng]:
            self._commit((key, self.cnt[eng]), r_, w_, key)
        self.pending[eng] = []
        self._commit((key, self.cnt[eng]), reads, writes, key)
        return ins

    def quiet(self, eng, fn, reads=(), writes=()):
        self._deps(eng, reads, writes)
        self.nins += 1
        self.pending[eng].append((tuple(reads), tuple(writes)))
        return fn()

    def dma(self, q, key, pairs, reads=(), writes=()):
        self._deps(q, reads, writes)
        k = ("d", key)
        if k not in self.sem:
            self.sem[k] = self.es.enter_context(self.nc.semaphore(f"d_{key}"))
            self.dcnt[k] = 0
        for (o, i) in pairs:
            self.e[q].dma_start(out=o, in_=i).then_inc(self.sem[k], 16)
            self.dcnt[k] += 16
            self.nins += 1
        self._commit((k, self.dcnt[k]), reads, writes, k)

    def barrier(self):
        toks = []
        for k in self.ENG:
            if self.cnt[k] > 0:
                toks.append((("e", k, self.epoch[k]), self.cnt[k]))
        for k, v in self.dcnt.items():
            if v > 0:
                toks.append((k, v))
        for eng in self.ENG:
            for (key, val) in toks:
                if key[0] == "e" and key[1] == eng:
                    continue
                if self.waited[eng].get(key, -1) >= val:
                    continue
                self.e[eng].wait_ge(self.sem[key], val)
                self.waited[eng][key] = val
        self.res = {}


def bview(ap, h):
    return ap.rearrange("p (h d) -> p h d", h=h)


def build(stage=99, sub=99):
    nc = bass.Bass("TRN2", target_bir_lowering=False)
    dr = {}

    def din(name, shape):
        dr[name] = nc.dram_tensor(name, list(shape), F32, kind="ExternalInput").ap()
        return dr[name]

    x_d = din("x", [T, D])
    win_d = din("w_in", [D, NW])
    gains = {n: din(n, [1, s]) for n, s in [("attn_norm", D), ("q_norm", 64), ("k_norm", 64), ("idx_k_norm", 64),
                                             ("ffn_norm", D), ("ssd_norm", 2048), ("conv_b", 3072), ("dt_bias", 32),
                                             ("a_log", 32), ("d_skip", 32), ("b_route_group", 4),
                                             ("b_route_expert", 32)]}
    convw_d = din("conv_w", [4, 3072])
    wa_d = din("w_attn_branch", [1024, 1024])
    wb_d = din("w_ssd_branch", [2048, 1024])
    wo_d = din("w_out", [1024, 1024])
    wrg_d = din("w_route_group", [1024, 4])
    wre_d = din("w_route_expert", [1024, 32])
    wg_d = din("w_gate", [32, 1024, 256])
    wu_d = din("w_up", [32, 1024, 256])
    wd_d = din("w_down", [32, 256, 1024])
    cst_d = din("cst", [128, CW])
    rope_d = din("rope", [T, 32])
    out_d = nc.dram_tensor("out", [T, D], F32, kind="ExternalOutput").ap()
    dbg = {}

    def dout(name, shape, dt=F32):
        dbg[name] = nc.dram_tensor(name, list(shape), dt, kind="ExternalOutput").ap()
        return dbg[name]

    es = contextlib.ExitStack()
    with es:
        P = Prog(nc, es)

        uid = [0]

        def sb(name, shape, dt, stack=es):
            uid[0] += 1
            return stack.enter_context(nc.sbuf_tensor(f"sb{uid[0]}_{name}", list(shape), dt))

        ps = [es.enter_context(nc.psum_tensor(f"ps{i}", [128, 512], F32)) for i in range(7)]
        psb = es.enter_context(nc.psum_tensor("psb", [128, 1024], BF16))

        cst = sb("cst", [128, CW], F32)
        cstb = sb("cstb", [128, CW], BF16)
        P.dma("sp", "cst", [(cst[:], cst_d)], writes=["cst"])
        P.op("pool", lambda: nc.gpsimd.tensor_copy(out=cstb[:], in_=cst[:]), reads=["cst"], writes=["cstb"])
        ident = cstb[:, C_ID:C_ID + 128]
        gb = {}

        def load_gain(n, width, st, c0=0):
            gb[n] = sb("g_" + n, [128, width], F32, st)
            P.dma("sp", "gain_" + n, [(gb[n][:], gains[n][:, c0:c0 + width].partition_broadcast(128))], writes=["g_" + n])

        hT = sb("hT", [128, 8, T], BF16)
        ya_spill = nc.dram_tensor("ya_spill", [128, 8, T], BF16, kind="Internal").ap()

        def rmsnorm_T(src_fn, gname, dst, st):
            xb = [sb(f"rn_x{i}", [128, D], F32, st) for i in range(2)]
            xn = [sb(f"rn_xn{i}", [128, D], BF16, st) for i in range(2)]
            junk = sb("rn_junk", [128, D], BF16, st)
            ss = sb("rn_ss", [128, NT], F32, st)
            ms = sb("rn_ms", [128, NT], F32, st)
            sd = sb("rn_sd", [128, NT], F32, st)
            rs = sb("rn_rs", [128, NT], F32, st)
            for c in range(NT):
                b = c % 2
                src_fn(c, xb[b], f"rn_x{b}")
                P.op("act", lambda: nc.scalar.activation(out=junk[:], in_=xb[b][:], func=AF.Square,
                                                         accum_out=ss[:, c:c + 1]),
                     reads=[f"rn_x{b}"], writes=["rn_junk", ("rn_ss", c)])
                P.op("dve", lambda: nc.vector.tensor_scalar(out=ms[:, c:c + 1], in0=ss[:, c:c + 1], scalar1=1.0 / D,
                                                            scalar2=EPS, op0=ALU.mult, op1=ALU.add),
                     reads=[("rn_ss", c)], writes=[("rn_ms", c)])
                P.op("act", lambda: nc.scalar.activation(out=sd[:, c:c + 1], in_=ms[:, c:c + 1], func=AF.Sqrt),
                     reads=[("rn_ms", c)], writes=[("rn_sd", c)])
                P.op("dve", lambda: nc.vector.reciprocal(out=rs[:, c:c + 1], in_=sd[:, c:c + 1]),
                     reads=[("rn_sd", c)], writes=[("rn_rs", c)])
                P.op("dve", lambda: nc.vector.scalar_tensor_tensor(out=xn[b][:], in0=xb[b][:], scalar=rs[:, c:c + 1],
                                                                   in1=gb[gname][:], op0=ALU.mult, op1=ALU.mult),
                     reads=[f"rn_x{b}", ("rn_rs", c), "g_" + gname], writes=[f"rn_xn{b}"])
                for kt in range(8):
                    f = lambda: nc.tensor.transpose(out=psb[:, kt * 128:(kt + 1) * 128],
                                                    in_=xn[b][:, kt * 128:(kt + 1) * 128], identity=ident)
                    if kt < 7:
                        P.quiet("pe", f, reads=[f"rn_xn{b}", "cstb"], writes=["psb"])
                    else:
                        P.op("pe", f, reads=[f"rn_xn{b}", "cstb"], writes=["psb"])
                P.op("act", lambda: nc.scalar.copy(out=dst[:, :, c * 128:(c + 1) * 128], in_=bview(psb[:], 8)),
                     reads=["psb"], writes=[("hT", c)])

        def load_x(c, buf, rname):
            P.dma("sp", rname, [(buf[:], x_d[c * 128:(c + 1) * 128, :])], writes=[rname])

        with contextlib.ExitStack() as st:
            load_gain("attn_norm", D, st)
            rmsnorm_T(load_x, "attn_norm", hT, st)
            P.barrier()

        if stage == 0:
            o = dout("hT_dbg", [128, 8, T], BF16)
            P.dma("sp", "out", [(o, hT[:])])
            P.barrier()
            return nc, dbg

        wst = [None]
        wbf = [None, None]
        wctr = [0]
        win_v = win_d.rearrange("(kt p) n -> p kt n", p=128)

        def alloc_w(st):
            wst[0] = sb("wst", [128, 8, 512], F32, st)
            wbf[0] = sb("wbf0", [128, 8, 512], BF16, st)
            wbf[1] = sb("wbf1", [128, 8, 512], BF16, st)

        def load_w(c0, ncols):
            i = wctr[0] % 2
            wctr[0] += 1
            P.dma("sp", "wst", [(wst[0][:, :, 0:ncols], win_v[:, :, c0:c0 + ncols])], writes=["wst"])
            P.op("pool", lambda: nc.gpsimd.tensor_copy(out=wbf[i][:, :, 0:ncols], in_=wst[0][:, :, 0:ncols]),
                 reads=["wst"], writes=[f"wbf{i}"])
            return i

        def proj_tm(c, wi, ncols, bank):
            for kt in range(8):
                f = lambda: nc.tensor.matmul(ps[bank][:, 0:ncols], lhsT=hT[:, kt, c * 128:(c + 1) * 128],
                                             rhs=wbf[wi][:, kt, 0:ncols], start=(kt == 0), stop=(kt == 7))
                if kt < 7:
                    P.quiet("pe", f, reads=[("hT", c), f"wbf{wi}"], writes=[f"ps{bank}"])
                else:
                    P.op("pe", f, reads=[("hT", c), f"wbf{wi}"], writes=[f"ps{bank}"])

        with contextlib.ExitStack() as st:
            yaT = sb("yaT", [128, 8, T], BF16, st)
            rope = sb("rope", [128, NT, 32], F32, st)
            P.dma("sp", "rope", [(rope[:], rope_d.rearrange("(c p) f -> p c f", p=128))], writes=["rope"])
            for n_ in ("q_norm", "k_norm", "idx_k_norm"):
                load_gain(n_, 64, st)
            qT = sb("qT", [128, 8, T], BF16, st)
            kT2 = sb("kT2", [128, 4, T], BF16, st)
            iqT = sb("iqT", [128, 4, T], BF16, st)
            ikT2 = sb("ikT2", [128, T], BF16, st)
            vaug = sb("vaug", [128, NT, 4, 66], BF16, st)
            iwa = sb("iwa", [128, NT, 8], F32, st)
            iws = sb("iws", [128, NT, 8], F32, st)
            P.op("pool", lambda: nc.gpsimd.memset(vaug[:], 1.0), writes=["vaug"])

            with contextlib.ExitStack() as st2:
                alloc_w(st2)
                sq = sb("e_sq", [128, 512], F32, st2)
                xn = sb("e_xn", [128, 512], F32, st2)
                ra = sb("e_ra", [128, 8, 16], F32, st2)
                rb = sb("e_rb", [128, 8, 16], F32, st2)
                s8 = [sb(f"e_s8{i}", [128, 8], F32, st2) for i in range(4)]
                tmb = sb("e_tmb", [128, 512], BF16, st2)

                def epilogue(c, bank, nh, gname, prescale, dst_fn, dup):
                    pv = bview(ps[bank][:, 0:nh * 64], nh)
                    xv = bview(xn[:, 0:nh * 64], nh)
                    pr = f"ps{bank}"
                    if gname is not None:
                        P.op("act", lambda: nc.scalar.activation(out=sq[:, 0:nh * 64], in_=ps[bank][:, 0:nh * 64],
                                                                 func=AF.Square), reads=[pr], writes=["e_sq"])
                        P.op("dve", lambda: nc.vector.tensor_reduce(out=s8[0][:, 0:nh], in_=bview(sq[:, 0:nh * 64], nh),
                                                                    axis=AX.X, op=ALU.add), reads=["e_sq"], writes=["e_s80"])
                        P.op("dve", lambda: nc.vector.tensor_scalar(out=s8[1][:, 0:nh], in0=s8[0][:, 0:nh], scalar1=1.0 / 64,
                                                                    scalar2=EPS, op0=ALU.mult, op1=ALU.add),
                             reads=["e_s80"], writes=["e_s81"])
                        P.op("act", lambda: nc.scalar.activation(out=s8[2][:, 0:nh], in_=s8[1][:, 0:nh], func=AF.Sqrt),
                             reads=["e_s81"], writes=["e_s82"])
                        P.op("dve", lambda: nc.vector.reciprocal(out=s8[3][:, 0:nh], in_=s8[2][:, 0:nh]),
                             reads=["e_s82"], writes=["e_s83"])
                        P.op("dve", lambda: nc.vector.tensor_tensor(out=xv, in0=pv,
                                                                    in1=s8[3][:, 0:nh].unsqueeze(2).to_broadcast([128, nh, 64]),
                                                                    op=ALU.mult), reads=[pr, "e_s83"], writes=["e_xn"])
                        P.op("dve", lambda: nc.vector.tensor_tensor(out=xv, in0=xv,
                                                                    in1=gb[gname][:].unsqueeze(1).to_broadcast([128, nh, 64]),
                                                                    op=ALU.mult), reads=["e_xn", "g_" + gname], writes=["e_xn"])
                    elif prescale is not None:
                        P.op("dve", lambda: nc.vector.tensor_tensor(out=xv, in0=pv,
                                                                    in1=prescale.unsqueeze(2).to_broadcast([128, nh, 64]),
                                                                    op=ALU.mult), reads=[pr, ("iw", c)], writes=["e_xn"])
                    else:
                        P.op("dve", lambda: nc.vector.tensor_copy(out=xv, in_=pv), reads=[pr], writes=["e_xn"])
                    c16 = rope[:, c, 0:16].unsqueeze(1).to_broadcast([128, nh, 16])
                    nsn = rope[:, c, 16:24].unsqueeze(1).to_broadcast([128, nh, 8])
                    psn = rope[:, c, 24:32].unsqueeze(1).to_broadcast([128, nh, 8])
                    P.op("dve", lambda: nc.vector.tensor_tensor(out=ra[:, 0:nh, :], in0=xv[:, :, 0:16], in1=c16, op=ALU.mult),
                         reads=["e_xn", "rope"], writes=["e_ra"])
                    P.op("dve", lambda: nc.vector.tensor_tensor(out=rb[:, 0:nh, 0:8], in0=xv[:, :, 8:16], in1=nsn, op=ALU.mult),
                         reads=["e_xn", "rope"], writes=["e_rb0"])
                    P.op("dve", lambda: nc.vector.tensor_tensor(out=rb[:, 0:nh, 8:16], in0=xv[:, :, 0:8], in1=psn, op=ALU.mult),
                         reads=["e_xn", "rope"], writes=["e_rb1"])
                    P.op("dve", lambda: nc.vector.tensor_tensor(out=xv[:, :, 0:16], in0=ra[:, 0:nh, :], in1=rb[:, 0:nh, :],
                                                                op=ALU.add), reads=["e_ra", "e_rb0", "e_rb1"], writes=["e_xn"])
                    if dup:
                        tv = tmb[:, 0:nh * 128].rearrange("p (h t d) -> p h t d", h=nh, t=2)
                        P.op("act", lambda: nc.scalar.copy(out=tv[:, :, 0, :], in_=xv), reads=["e_xn"], writes=["e_tmb0"])
                        P.op("act", lambda: nc.scalar.copy(out=tv[:, :, 1, :], in_=xv), reads=["e_xn"], writes=["e_tmb1"])
                        nblk = nh
                    else:
                        P.op("act", lambda: nc.scalar.copy(out=tmb[:, 0:nh * 64], in_=xn[:, 0:nh * 64]), reads=["e_xn"],
                             writes=["e_tmb0", "e_tmb1"])
                        nblk = nh // 2
                    for j in range(nblk):
                        f = lambda: nc.tensor.transpose(out=psb[:, j * 128:(j + 1) * 128], in_=tmb[:, j * 128:(j + 1) * 128],
                                                        identity=ident)
                        if j < nblk - 1:
                            P.quiet("pe", f, reads=["e_tmb0", "e_tmb1", "cstb"], writes=["psb"])
                        else:
                            P.op("pe", f, reads=["e_tmb0", "e_tmb1", "cstb"], writes=["psb"])
                    dst_fn(nblk)

                wi = load_w(O_IK, 72)
                for c in range(NT):
                    bank = c % 2
                    proj_tm(c, wi, 72, bank)
                    P.op("act", lambda: nc.scalar.activation(out=iwa[:, c, :], in_=ps[bank][:, 64:72], func=AF.Abs),
                         reads=[f"ps{bank}"], writes=[("iw", c)])
                    P.op("act", lambda: nc.scalar.activation(out=iws[:, c, :], in_=ps[bank][:, 64:72], func=AF.Sign),
                         reads=[f"ps{bank}"], writes=[("iws", c)])
                    epilogue(c, bank, 1, "idx_k_norm", None,
                             lambda nblk: P.op("act", lambda: nc.scalar.copy(out=ikT2[:, c * 128:(c + 1) * 128],
                                                                             in_=psb[:, 0:128]),
                                               reads=["psb"], writes=[("ikT2", c)]), True)
                wi = load_w(O_IQ, 512)
                for c in range(NT if sub >= 2 else 0):
                    bank = c % 2
                    proj_tm(c, wi, 512, bank)
                    epilogue(c, bank, 8, None, iwa[:, c, :],
                             lambda nblk: P.op("act", lambda: nc.scalar.copy(out=iqT[:, :, c * 128:(c + 1) * 128],
                                                                             in_=bview(psb[:, 0:512], 4)),
                                               reads=["psb"], writes=[("iqT", c)]), False)
                for half in range(2):
                    wi = load_w(O_Q + half * 512, 512)
                    for c in range(NT if sub >= 3 else 0):
                        bank = c % 2
                        proj_tm(c, wi, 512, bank)
                        epilogue(c, bank, 8, "q_norm", None,
                                 lambda nblk: P.op("act", lambda: nc.scalar.copy(
                                     out=qT[:, half * 4:half * 4 + 4, c * 128:(c + 1) * 128], in_=bview(psb[:, 0:512], 4)),
                                     reads=["psb"], writes=[("qT", c, half)]), False)
                wi = load_w(O_K, 512)
                for c in range(NT if sub >= 4 else 0):
                    bank = c % 2
                    proj_tm(c, wi, 512, bank)
                    if sub != 5:
                        P.op("act", lambda: nc.scalar.copy(out=vaug[:, c, :, 0:64], in_=bview(ps[bank][:, 256:512], 4)),
                             reads=[f"ps{bank}", "vaug"], writes=[("vaug", c)])
                    epilogue(c, bank, 4, "k_norm", None,
                             lambda nblk: P.op("act", lambda: nc.scalar.copy(out=kT2[:, :, c * 128:(c + 1) * 128],
                                                                             in_=bview(psb[:, 0:512], 4)),
                                               reads=["psb"], writes=[("kT2", c)]), True)
                P.barrier()

            if stage == 1:
                for nm, t_, shp in [("qT", qT, [128, 8, T]), ("kT2", kT2, [128, 4, T]), ("iqT", iqT, [128, 4, T]),
                                    ("ikT2", ikT2, [128, T])]:
                    o = dout(nm + "_dbg", shp, BF16)
                    P.dma("sp", "out", [(o, t_[:])])
                o = dout("vaug_dbg", [128, NT, 4, 66], BF16)
                P.dma("sp", "out", [(o, vaug[:])])
                o = dout("iwa_dbg", [128, NT, 8], F32)
                P.dma("sp", "out", [(o, iwa[:])])
                o = dout("iws_dbg", [128, NT, 8], F32)
                P.dma("sp", "out", [(o, iws[:])])
                P.barrier()
                return nc, dbg

            with contextlib.ExitStack() as st3:
                score = sb("score", [128, T], F32, st3)
                junk = sb("ajunk", [128, T], BF16, st3)
                maskb = [sb(f"maskb{i}", [128, T], BF16, st3) for i in range(2)]
                maskT = [sb(f"maskT{i}", [128, NT, 128], BF16, st3) for i in range(2)]
                relu = [sb(f"relu{i}", [128, 512], BF16, st3) for i in range(2)]
                diag = [sb(f"diag{i}", [128, 8, 128], BF16, st3) for i in range(2)]
                PT = [sb(f"PT{i}", [128, 512], BF16, st3) for i in range(3)]
                PTm = [sb(f"PTm{i}", [128, 512], BF16, st3) for i in range(3)]
                ytm = sb("ytm", [128, 1024], BF16, st3)
                hi = sb("b_hi", [128, 1], F32, st3)
                lo = sb("b_lo", [128, 1], F32, st3)
                w0 = sb("b_w0", [128, 1], F32, st3)
                wtab = sb("b_wtab", [128, NBIS], F32, st3)
                tt = sb("b_t", [128, 1], F32, st3)
                cnt = sb("b_cnt", [128, 1], F32, st3)
                uu = sb("b_u", [128, 1], F32, st3)
                thr = sb("b_thr", [128, 1], F32, st3)
                rcp = sb("b_rcp", [128, 8], F32, st3)
                pvc = [0]
                SB3 = [4, 5, 6]
                NQ = NT if sub >= 30 else max(0, sub - 10)

                def emit_scores(qi):
                    nkeys = 128 * (qi + 1)
                    qs = slice(qi * 128, (qi + 1) * 128)
                    dgt = diag[qi % 2]
                    for h in range(8):
                        P.op("dve", lambda: nc.vector.tensor_scalar(out=dgt[:, h, :], in0=ident, scalar1=iws[:, qi, h:h + 1],
                                                                    scalar2=None, op0=ALU.mult),
                             reads=["cstb"], writes=[(f"diag{qi % 2}", h)])
                    nkb = (nkeys + 511) // 512
                    for kb in range(nkb):
                        kw = min(512, nkeys - kb * 512)
                        for h in range(8):
                            hf, pr_ = h % 2, h // 2
                            rb = h % 2
                            P.op("pe", lambda: nc.tensor.matmul(ps[rb][:, 0:kw], lhsT=iqT[64 * hf:64 * hf + 64, pr_, qs],
                                                                rhs=ikT2[64 * hf:64 * hf + 64, kb * 512:kb * 512 + kw],
                                                                start=True, stop=True),
                                 writes=[f"ps{rb}"])
                            P.op("act", lambda: nc.scalar.activation(out=relu[rb][:, 0:kw], in_=ps[rb][:, 0:kw], func=AF.Relu),
                                 reads=[f"ps{rb}"], writes=[f"relu{rb}"])
                            f = lambda: nc.tensor.matmul(ps[2][:, 0:kw], lhsT=dgt[:, h, :], rhs=relu[rb][:, 0:kw],
                                                         start=(h == 0), stop=(h == 7))
                            if h < 7:
                                P.quiet("pe", f, reads=[f"relu{rb}", (f"diag{qi % 2}", h)], writes=["ps2"])
                            else:
                                P.op("pe", f, reads=[f"relu{rb}", (f"diag{qi % 2}", h)], writes=["ps2"])
                        c0 = kb * 512
                        last = (kb == nkb - 1)
                        nd = kw - 128 if last else kw
                        if nd > 0:
                            P.op("dve", lambda: nc.vector.tensor_copy(out=score[:, c0:c0 + nd], in_=ps[2][:, 0:nd]),
                                 reads=["ps2"], writes=[("score", kb)])
                        if last:
                            P.op("dve", lambda: nc.vector.tensor_tensor(out=score[:, nkeys - 128:nkeys], in0=ps[2][:, nd:nd + 128],
                                                                        in1=cst[:, C_DM:C_DM + 128], op=ALU.mult),
                                 reads=["ps2", "cst"], writes=[("score", "d")])
                            P.op("dve", lambda: nc.vector.tensor_tensor(out=score[:, nkeys - 128:nkeys],
                                                                        in0=score[:, nkeys - 128:nkeys],
                                                                        in1=cst[:, C_NB:C_NB + 128], op=ALU.add),
                                 reads=[("score", "d"), "cst"], writes=[("score", "d")])

                def search_steps(qi):
                    nkeys = 128 * (qi + 1)
                    nkb = (nkeys + 511) // 512
                    sres = [("score", kb) for kb in range(nkb)] + [("score", "d")]
                    mb = maskb[qi % 2]
                    steps = []

                    def s0():
                        P.op("dve", lambda: nc.vector.tensor_reduce(out=hi[:], in_=score[:, 0:nkeys], axis=AX.X, op=ALU.max),
                             reads=sres, writes=["b_hi"])
                        P.op("dve", lambda: nc.vector.tensor_reduce(out=lo[:], in_=score[:, 0:nkeys - 128], axis=AX.X, op=ALU.min),
                             reads=sres, writes=["b_lo"])
                        P.op("dve", lambda: nc.vector.tensor_tensor(out=w0[:], in0=hi[:], in1=lo[:], op=ALU.subtract),
                             reads=["b_hi", "b_lo"], writes=["b_w0"])
                        P.op("dve", lambda: nc.vector.tensor_scalar(out=wtab[:], in0=cst[:, C_BIS:C_BIS + NBIS], scalar1=w0[:, 0:1],
                                                                    scalar2=None, op0=ALU.mult),
                             reads=["b_w0", "cst"], writes=["b_wtab"])
                        P.op("dve", lambda: nc.vector.tensor_tensor(out=tt[:], in0=lo[:], in1=wtab[:, 0:1], op=ALU.add),
                             reads=["b_lo", "b_wtab"], writes=["b_t"])
                    steps.append(s0)
                    for it in range(NBIS):
                        def si(it=it):
                            P.op("dve", lambda: nc.vector.tensor_scalar(out=junk[:, 0:nkeys], in0=score[:, 0:nkeys], scalar1=tt[:, 0:1],
                                                                        scalar2=None, op0=ALU.is_ge, op1=ALU.add, accum_out=cnt[:]),
                                 reads=sres + ["b_t"], writes=["ajunk", "b_cnt"])
                            P.op("dve", lambda: nc.vector.tensor_scalar(out=uu[:], in0=cnt[:], scalar1=256.0, scalar2=-0.5,
                                                                        op0=ALU.is_ge, op1=ALU.add),
                                 reads=["b_cnt"], writes=["b_u"])
                            P.op("dve", lambda: nc.vector.scalar_tensor_tensor(out=tt[:], in0=uu[:], scalar=wtab[:, it:it + 1],
                                                                               in1=tt[:], op0=ALU.mult, op1=ALU.add),
                                 reads=["b_u", "b_wtab", "b_t"], writes=["b_t"])
                        steps.append(si)

                    def sf():
                        P.op("dve", lambda: nc.vector.scalar_tensor_tensor(out=thr[:], in0=wtab[:, NBIS - 1:NBIS], scalar=-0.5,
                                                                           in1=tt[:], op0=ALU.mult, op1=ALU.add),
                             reads=["b_wtab", "b_t"], writes=["b_thr"])
                        P.op("dve", lambda: nc.vector.tensor_scalar(out=mb[:, 0:nkeys], in0=score[:, 0:nkeys], scalar1=thr[:, 0:1],
                                                                    scalar2=None, op0=ALU.is_ge),
                             reads=sres + ["b_thr"], writes=[f"maskb{qi % 2}"])
                    steps.append(sf)
                    return steps

                def const_mask(qi):
                    nkeys = 128 * (qi + 1)
                    mb = maskb[qi % 2]
                    if qi == 1:
                        P.op("pool", lambda: nc.gpsimd.tensor_copy(out=mb[:, 0:128], in_=cstb[:, C_ONE:C_ONE + 128]),
                             reads=["cstb"], writes=[f"maskb{qi % 2}"])
                    P.op("pool", lambda: nc.gpsimd.tensor_copy(out=mb[:, nkeys - 128:nkeys], in_=cstb[:, C_DM:C_DM + 128]),
                         reads=["cstb"], writes=[f"maskb{qi % 2}"])

                def emit_maskT(qi):
                    nk = qi + 1
                    mb = maskb[qi % 2]
                    mT = maskT[qi % 2]
                    for k0 in range(0, nk, 8):
                        n = min(8, nk - k0)
                        for j in range(n):
                            f = lambda: nc.tensor.transpose(out=psb[:, j * 128:(j + 1) * 128],
                                                            in_=mb[:, (k0 + j) * 128:(k0 + j + 1) * 128], identity=ident)
                            if j < n - 1:
                                P.quiet("pe", f, reads=[f"maskb{qi % 2}", "cstb"], writes=["psb"])
                            else:
                                P.op("pe", f, reads=[f"maskb{qi % 2}", "cstb"], writes=["psb"])
                        P.op("act", lambda: nc.scalar.copy(out=mT[:, k0:k0 + n, :], in_=bview(psb[:, 0:n * 128], n)),
                             reads=["psb"], writes=[(f"maskT{qi % 2}", k0 // 8)])

                SBK = [4, 5, 6, 0, 1]
                LOOK = 2

                def attention_tile(qi, steps):
                    nk = qi + 1
                    qs = slice(qi * 128, (qi + 1) * 128)
                    mT = maskT[qi % 2]
                    seq = [(g, kj) for g in range(4) for kj in range(nk)]
                    nseq = len(seq)
                    info = {}
                    stq = list(steps)
                    every = max(1, nseq // max(1, len(stq))) if stq else 0

                    def front(i):
                        g, kj = seq[i]
                        ks = slice(kj * 128, (kj + 1) * 128)
                        n_ = pvc[0]
                        pvc[0] += 1
                        par = n_ % 3
                        sa, sb_ = SBK[(2 * n_) % 5], SBK[(2 * n_ + 1) % 5]
                        info[i] = par
                        P.op("pe", lambda: nc.tensor.matmul(bview(ps[sa][:, 0:256], 2), lhsT=kT2[0:64, g, ks],
                                                            rhs=qT[0:64, 2 * g:2 * g + 2, qs], start=True, stop=True),
                             writes=[f"ps{sa}"])
                        P.op("pe", lambda: nc.tensor.matmul(bview(ps[sb_][:, 0:256], 2), lhsT=kT2[64:128, g, ks],
                                                            rhs=qT[64:128, 2 * g:2 * g + 2, qs], start=True, stop=True),
                             writes=[f"ps{sb_}"])
                        P.op("act", lambda: nc.scalar.activation(out=PT[par][:, 0:256], in_=ps[sa][:, 0:256], func=AF.Exp,
                                                                 scale=0.125), reads=[f"ps{sa}"], writes=[("PT", par, 0)])
                        P.op("act", lambda: nc.scalar.activation(out=PT[par][:, 256:512], in_=ps[sb_][:, 0:256], func=AF.Exp,
                                                                 scale=0.125), reads=[f"ps{sb_}"], writes=[("PT", par, 1)])
                        me = "dve" if (n_ % 3 == 2) else "pool"
                        P.op(me, lambda: P.e[me].tensor_tensor(out=bview(PTm[par][:], 4), in0=bview(PT[par][:], 4),
                                                               in1=mT[:, kj, :].unsqueeze(1).to_broadcast([128, 4, 128]),
                                                               op=ALU.mult),
                             reads=[("PT", par, 0), ("PT", par, 1), (f"maskT{qi % 2}", kj // 8)], writes=[("PTm", par)])

                    def back(i):
                        g, kj = seq[i]
                        par = info[i]
                        ob = 2 + g % 2
                        for j in range(4):
                            hl = [0, 2, 1, 3][j]
                            f = lambda: nc.tensor.matmul(ps[ob][:, hl * 65:hl * 65 + 65], lhsT=PTm[par][:, j * 128:(j + 1) * 128],
                                                         rhs=vaug[:, kj, g, 0:65], start=(kj == 0 and j == 0),
                                                         stop=(kj == nk - 1 and j == 3), skip_group_check=True)
                            if j < 3:
                                P.quiet("pe", f, reads=[("PTm", par)], writes=[f"ps{ob}"])
                            else:
                                P.op("pe", f, reads=[("PTm", par)], writes=[f"ps{ob}"])
                        if kj == nk - 1:
                            ov = ps[ob][:, 0:260].rearrange("p (h d) -> p h d", h=4)
                            P.op("dve", lambda: nc.vector.reciprocal(out=rcp[:, 4 * (g % 2):4 * (g % 2) + 4], in_=ov[:, :, 64]),
                                 reads=[f"ps{ob}"], writes=[("b_rcp", g % 2)])
                            for hl in range(4):
                                hh = 4 * g + hl
                                P.op("act", lambda: nc.scalar.activation(out=ytm[:, hh * 64:(hh + 1) * 64],
                                                                         in_=ps[ob][:, hl * 65:hl * 65 + 64], func=AF.Copy,
                                                                         scale=rcp[:, 4 * (g % 2) + hl:4 * (g % 2) + hl + 1]),
                                     reads=[f"ps{ob}", ("b_rcp", g % 2)], writes=[("ytm", hh)])

                    for i in range(nseq + LOOK):
                        if i < nseq:
                            front(i)
                        if i >= LOOK:
                            back(i - LOOK)
                        if stq and (i % every == every - 1):
                            stq.pop(0)()
                    while stq:
                        stq.pop(0)()

                if NQ > 0:
                    const_mask(0)
                    emit_maskT(0)
                for qi in range(NQ):
                    nxt = qi + 1
                    steps = []
                    if nxt < NQ:
                        if nxt >= 2:
                            emit_scores(nxt)
                            steps = search_steps(nxt)
                        else:
                            const_mask(nxt)
                    attention_tile(qi, steps)
                    if nxt < NQ:
                        emit_maskT(nxt)
                    qs = slice(qi * 128, (qi + 1) * 128)
                    for j in range(8):
                        f = lambda: nc.tensor.transpose(out=psb[:, j * 128:(j + 1) * 128], in_=ytm[:, j * 128:(j + 1) * 128],
                                                        identity=ident)
                        if j < 7:
                            P.quiet("pe", f, reads=[("ytm", hh_) for hh_ in range(16)] + ["cstb"], writes=["psb"])
                        else:
                            P.op("pe", f, reads=[("ytm", hh_) for hh_ in range(16)] + ["cstb"], writes=["psb"])
                    P.op("act", lambda: nc.scalar.copy(out=yaT[:, :, qs], in_=bview(psb[:], 8)), reads=["psb"], writes=[("yaT", qi)])
                P.barrier()
            if stage == 2:
                o = dout("yaT_dbg", [128, 8, T], BF16)
                P.dma("sp", "out", [(o, yaT[:])])
                P.barrier()
                return nc, dbg
            P.dma("sp", "spill", [(ya_spill, yaT[:])])
            P.barrier()

        stB = contextlib.ExitStack()
        es.enter_context(stB)
        ysT = sb("ysT", [128, 16, T], BF16, stB)
        with contextlib.ExitStack() as sS:
            G8 = lambda t_, c_, g_: t_[:, c_, 8 * g_:8 * g_ + 8].unsqueeze(2).to_broadcast([128, 8, 64])
            wstS = sb("wstS", [128, 8, 256], F32, sS)
            selb = sb("selb", [128, 32, 128], BF16, sS)
            P.op("pool", lambda: nc.gpsimd.memset(selb[:], 0.0), writes=["selb"])
            for r3 in range(3):
                P.op("pool", lambda: nc.gpsimd.tensor_copy(
                    out=selb[32 * r3:32 * r3 + 32, :, :],
                    in_=cstb[32 * r3:32 * r3 + 32, C_ID + 32 * r3:C_ID + 32 * r3 + 32].unsqueeze(2).to_broadcast([32, 32, 128])),
                    reads=["cstb", "selb"], writes=["selb"])
            if sub == 101:
                P.barrier(); return nc, dbg
            for n_ in ("dt_bias", "a_log", "d_skip"):
                load_gain(n_, 32, sS)
            aneg = sb("aneg", [128, 32], F32, sS)
            P.op("act", lambda: nc.scalar.activation(out=aneg[:], in_=gb["a_log"][:], func=AF.Exp), reads=["g_a_log"], writes=["aneg"])
            P.op("dve", lambda: nc.vector.tensor_scalar(out=aneg[:], in0=aneg[:], scalar1=-1.0, scalar2=None, op0=ALU.mult),
                 reads=["aneg"], writes=["aneg"])
            if sub == 102:
                P.barrier(); return nc, dbg
            cwfm = sb("cwfm", [128, 24, 5], F32, sS)
            s0 = contextlib.ExitStack()
            cw5 = sb("cw5", [5, 3072], F32, s0)
            P.dma("sp", "cw5", [(cw5[0:4, :], convw_d), (cw5[4:5, :], gains["conv_b"])], writes=["cw5"])
            for t_ in range(24):
                f = lambda: nc.tensor.transpose(out=ps[0][:, t_ * 5:t_ * 5 + 5], in_=cw5[:, t_ * 128:(t_ + 1) * 128],
                                                identity=cst[0:5, C_ID:C_ID + 5])
                if t_ < 23:
                    P.quiet("pe", f, reads=["cw5", "cst"], writes=["ps0"])
                else:
                    P.op("pe", f, reads=["cw5", "cst"], writes=["ps0"])
            P.op("dve", lambda: nc.vector.tensor_copy(out=cwfm[:], in_=bview(ps[0][:, 0:120], 24)), reads=["ps0"], writes=["cwfm"])
            P.barrier()
            s0.close()
            if sub == 103:
                P.barrier(); return nc, dbg
            dt_all = sb("dt_all", [128, NT, 32], F32, sS)
            acs = sb("acs", [128, NT, 32], F32, sS)
            ea = sb("ea", [128, NT, 32], F32, sS)
            dtw = sb("dtw", [128, NT, 32], F32, sS)
            cdb = sb("cdb", [128, NT, 32], F32, sS)
            A3 = sb("A3", [128, NT, 128], BF16, sS)
            P.op("pool", lambda: nc.gpsimd.memset(A3[:], 0.0), writes=["A3z"])
            with contextlib.ExitStack() as s1:
                wdt_s = sb("wdt_s", [128, 8, 32], F32, s1)
                wdt = sb("wdt", [128, 8, 32], BF16, s1)
                P.dma("sp", "wdt", [(wdt_s[:], win_v[:, :, O_DT:O_DT + 32])], writes=["wdt_s"])
                P.op("pool", lambda: nc.gpsimd.tensor_copy(out=wdt[:], in_=wdt_s[:]), reads=["wdt_s"], writes=["wdt"])
                f32t = [sb(f"s1_{i}", [128, 32], F32, s1) for i in range(6)]
                a3 = sb("s1_a3", [128, 3, 32], F32, s1)
                Hb = sb("s1_Hb", [128, 128], BF16, s1)
                Mb = sb("s1_Mb", [128, 128], BF16, s1)
                r1 = sb("s1_r1", [128, 128], F32, s1)
                r2 = sb("s1_r2", [128, 128], F32, s1)
                ones_f = cst[:, C_ONE:C_ONE + 128]
                uinc = cst[:, C_CT:C_CT + 128]
                for c in range(NT):
                    cs_ = slice(c * 128, (c + 1) * 128)
                    xd, ax, ee, ll, rr, aa = f32t
                    for kt in range(8):
                        f = lambda: nc.tensor.matmul(ps[1][:, 0:32], lhsT=hT[:, kt, cs_], rhs=wdt[:, kt, :], start=(kt == 0), stop=(kt == 7))
                        if kt < 7:
                            P.quiet("pe", f, reads=["wdt"], writes=["ps1"])
                        else:
                            P.op("pe", f, reads=["wdt"], writes=["ps1"])
                    P.op("dve", lambda: nc.vector.tensor_tensor(out=xd[:], in0=ps[1][:, 0:32], in1=gb["dt_bias"][:], op=ALU.add),
                         reads=["ps1", "g_dt_bias"], writes=["s1_xd"])
                    P.op("act", lambda: nc.scalar.activation(out=ax[:], in_=xd[:], func=AF.Abs), reads=["s1_xd"], writes=["s1_ax"])
                    P.op("act", lambda: nc.scalar.activation(out=ee[:], in_=ax[:], func=AF.Exp, scale=-1.0), reads=["s1_ax"], writes=["s1_ee"])
                    P.op("act", lambda: nc.scalar.activation(out=ll[:], in_=ee[:], func=AF.Ln, bias=1.0), reads=["s1_ee"], writes=["s1_ll"])
                    P.op("dve", lambda: nc.vector.tensor_scalar(out=rr[:], in0=xd[:], scalar1=0.0, scalar2=None, op0=ALU.max),
                         reads=["s1_xd"], writes=["s1_rr"])
                    P.op("dve", lambda: nc.vector.tensor_tensor(out=dt_all[:, c, :], in0=rr[:], in1=ll[:], op=ALU.add),
                         reads=["s1_rr", "s1_ll"], writes=[("dt", c)])
                    P.op("dve", lambda: nc.vector.tensor_tensor(out=aa[:], in0=dt_all[:, c, :], in1=aneg[:], op=ALU.mult),
                         reads=[("dt", c), "aneg"], writes=["s1_aa"])
                    if sub == 104:
                        P.barrier(); return nc, dbg
                    P.op("dve", lambda: nc.vector.tensor_copy(out=a3[:], in_=aa[:].unsqueeze(1).to_broadcast([128, 3, 32])),
                         reads=["s1_aa"], writes=["s1_a3"])
                    if sub == 105:
                        P.barrier(); return nc, dbg
                    P.op("pe", lambda: nc.tensor.matmul(ps[2][:, 0:32], lhsT=uinc, rhs=aa[:], start=True, stop=True),
                         reads=["s1_aa", "cst"], writes=["ps2"])
                    P.op("pe", lambda: nc.tensor.matmul(ps[3][:, 0:32], lhsT=ones_f, rhs=aa[:], start=True, stop=True),
                         reads=["s1_aa", "cst"], writes=["ps3"])
                    P.op("pe", lambda: nc.tensor.matmul(ps[4][0:96, 0:128], lhsT=a3[:].rearrange("p a b -> p (a b)"), rhs=uinc,
                                                        start=True, stop=True),
                         reads=["s1_a3", "cst"], writes=["ps4"])
                    if sub == 106:
                        P.barrier(); return nc, dbg
                    P.op("dve", lambda: nc.vector.tensor_copy(out=acs[:, c, :], in_=ps[2][:, 0:32]), reads=["ps2"], writes=[("acs", c)])
                    if sub == 108:
                        P.barrier(); return nc, dbg
                    P.op("act", lambda: nc.scalar.activation(out=ea[:, c, :], in_=acs[:, c, :], func=AF.Exp), reads=[("acs", c)], writes=[("ea", c)])
                    P.op("dve", lambda: nc.vector.tensor_copy(out=rr[:], in_=ps[3][:, 0:32]), reads=["ps3"], writes=["s1_rr"])
                    P.op("act", lambda: nc.scalar.activation(out=cdb[:, c, :], in_=rr[:], func=AF.Exp), reads=["s1_rr"], writes=[("cdb", c)])
                    if sub == 109:
                        P.barrier(); return nc, dbg
                    P.op("dve", lambda: nc.vector.tensor_tensor(out=xd[:], in0=rr[:], in1=acs[:, c, :], op=ALU.subtract),
                         reads=["s1_rr", ("acs", c)], writes=["s1_xd"])
                    P.op("act", lambda: nc.scalar.activation(out=ee[:], in_=xd[:], func=AF.Exp), reads=["s1_xd"], writes=["s1_ee"])
                    P.op("dve", lambda: nc.vector.tensor_tensor(out=dtw[:, c, :], in0=dt_all[:, c, :], in1=ee[:], op=ALU.mult),
                         reads=[("dt", c), "s1_ee"], writes=[("dtw", c)])
                    if sub == 107:
                        P.barrier(); return nc, dbg
                    P.op("act", lambda: nc.scalar.copy(out=Hb[0:96, :], in_=ps[4][0:96, 0:128]), reads=["ps4"], writes=["s1_Hb"])
                    P.op("dve", lambda: nc.vector.tensor_tensor(out=r1[0:96, :], in0=ps[4][0:96, 0:128], in1=Hb[0:96, :], op=ALU.subtract),
                         reads=["ps4", "s1_Hb"], writes=["s1_r1"])
                    P.op("act", lambda: nc.scalar.copy(out=Mb[0:96, :], in_=r1[0:96, :]), reads=["s1_r1"], writes=["s1_Mb"])
                    P.op("dve", lambda: nc.vector.tensor_tensor(out=r2[0:96, :], in0=r1[0:96, :], in1=Mb[0:96, :], op=ALU.subtract),
                         reads=["s1_r1", "s1_Mb"], writes=["s1_r2"])
                    P.op("pool", lambda: nc.gpsimd.tensor_copy(out=A3[0:32, c, :], in_=Hb[0:32, :]), reads=["s1_Hb", "A3z"], writes=[("A3", c, 0)])
                    P.op("pool", lambda: nc.gpsimd.tensor_copy(out=A3[32:64, c, :], in_=Mb[32:64, :]), reads=["s1_Mb", "A3z"], writes=[("A3", c, 1)])
                    P.op("act", lambda: nc.scalar.copy(out=A3[64:96, c, :], in_=r2[64:96, :]), reads=["s1_r2", "A3z"], writes=[("A3", c, 2)])
                P.barrier()
            if stage == 3 and sub == 1:
                for nm, t_ in [("dt_all", dt_all), ("acs", acs), ("ea", ea), ("dtw", dtw), ("cdb", cdb)]:
                    o = dout(nm + "_dbg", [128, NT, 32], F32)
                    P.dma("sp", "out", [(o, t_[:])])
                o = dout("A3_dbg", [128, NT, 128], BF16)
                P.dma("sp", "out", [(o, A3[:])])
                o = dout("cwfm_dbg", [128, 24, 5], F32)
                P.dma("sp", "out", [(o, cwfm[:])])
                o = dout("selb_dbg", [128, 32, 128], BF16)
                P.dma("sp", "out", [(o, selb[:])])
                P.barrier()
                return nc, dbg

            xs_tm = sb("xs_tm", [128, NT, 512], BF16, sS)
            BT = sb("BT", [128, T], BF16, sS)
            CT = sb("CT", [128, T], BF16, sS)
            B_tm = sb("B_tm", [128, NT, 128], BF16, sS)
            rawb = sb("rawb", [128, T + 4], BF16, sS)
            xcf = [sb(f"xcf{i}", [128, 512], BF16, sS) for i in range(2)]
            dg = [sb(f"dg{i}", [128, 4, 128], BF16, sS) for i in range(2)]
            wch = [sb(f"wch{i}", [128, 8, 128], BF16, sS) for i in range(2)]
            wz = sb("wz", [128, 8, 512], BF16, sS)
            ssdg = sb("ssdg", [128, 512], F32, sS)
            hst = sb("hst", [128, 512], F32, sS)
            hstb = sb("hstb", [128, 512], BF16, sS)
            cbm = sb("cbm", [128, 128], F32, sS)
            seg = [sb(f"seg{i}", [128, 512], F32, sS) for i in range(2)]
            Ee = seg
            MT = [sb(f"MT{i}", [128, 512], BF16, sS) for i in range(4)]
            xdt = [sb(f"xdt{i}", [128, 512], BF16, sS) for i in range(2)]
            xw = [sb(f"xw{i}", [128, 512], BF16, sS) for i in range(2)]
            t1 = sb("t1", [128, 512], F32, sS)
            t1b = [t1, sb("t1b", [128, 512], F32, sS)]
            dsk = sb("dsk", [128, 8, 128], BF16, sS)
            epsb = sb("epsb", [128, 1], F32, sS)
            P.op("pool", lambda: nc.gpsimd.memset(epsb[:], EPS), writes=["epsb"])
            t3 = sb("t3", [128, 512], F32, sS)
            yv = t1
            sz = t3
            ynb = [sb(f"ynb{i}", [128, 512], BF16, sS) for i in range(2)]
            sjk = xcf[0]
            g1 = [sb(f"g1_{i}", [128, 2], F32, sS) for i in range(4)]
            P.op("pool", lambda: nc.gpsimd.memset(rawb[:, 0:4], 0.0), writes=["rawb_halo"])
            wctr2 = [0]
            for g in range(4 if sub >= 30 else 1):
                for hf in range(2):
                    c0 = O_Z + g * 512 + hf * 256
                    P.dma("sp", "wstS", [(wstS[:], win_v[:, :, c0:c0 + 256])], writes=["wstS"])
                    P.op("pool", lambda: nc.gpsimd.tensor_copy(out=wz[:, :, hf * 256:(hf + 1) * 256], in_=wstS[:]),
                         reads=["wstS"], writes=[("wz", hf)])
                P.dma("sp", "ssdg", [(ssdg[:], gains["ssd_norm"][:, g * 512:(g + 1) * 512].partition_broadcast(128))], writes=["ssdg"])
                chts = [(O_XBC + g * 512 + j * 128, 4 * g + j, "x", j) for j in range(4)]
                chts += [(O_XBC + 2048 + g * 128, 16 + g, "B", 0), (O_XBC + 2560 + g * 128, 20 + g, "C", 0)]
                for (c0, cti, kind, j) in chts:
                    wi = wctr2[0] % 2
                    wctr2[0] += 1
                    P.dma("sp", "wstS", [(wstS[:, :, 0:128], win_v[:, :, c0:c0 + 128])], writes=["wstS"])
                    P.op("pool", lambda: nc.gpsimd.tensor_copy(out=wch[wi][:], in_=wstS[:, :, 0:128]), reads=["wstS"], writes=[f"wch{wi}"])
                    for jj in range(4):
                        P.op("dve", lambda: nc.vector.tensor_scalar(out=dg[wi][:, jj, :], in0=ident, scalar1=cwfm[:, cti, jj:jj + 1],
                                                                    scalar2=None, op0=ALU.mult),
                             reads=["cstb", "cwfm"], writes=[(f"dg{wi}", jj)])
                    for tb in range(4):
                        bank = tb % 2
                        for kt in range(8):
                            f = lambda: nc.tensor.matmul(ps[bank][:], lhsT=wch[wi][:, kt, :], rhs=hT[:, kt, tb * 512:(tb + 1) * 512],
                                                         start=(kt == 0), stop=(kt == 7))
                            if kt < 7:
                                P.quiet("pe", f, reads=[f"wch{wi}"], writes=[f"ps{bank}"])
                            else:
                                P.op("pe", f, reads=[f"wch{wi}"], writes=[f"ps{bank}"])
                        P.op("act", lambda: nc.scalar.copy(out=rawb[:, 4 + tb * 512:4 + (tb + 1) * 512], in_=ps[bank][:]),
                             reads=[f"ps{bank}"], writes=[("rawb", tb)])
                    for tb in range(4):
                        bank = 2 + tb % 2
                        for jj in range(4):
                            f = lambda: nc.tensor.matmul(ps[bank][:], lhsT=dg[wi][:, jj, :],
                                                         rhs=rawb[:, 1 + tb * 512 + jj:1 + tb * 512 + jj + 512],
                                                         start=(jj == 0), stop=(jj == 3))
                            rd = [(f"dg{wi}", jj), ("rawb", tb), "rawb_halo"] + ([("rawb", tb - 1)] if tb > 0 else [])
                            if jj < 3:
                                P.quiet("pe", f, reads=rd, writes=[f"ps{bank}"])
                            else:
                                P.op("pe", f, reads=rd, writes=[f"ps{bank}"])
                        if kind == "x":
                            xb_ = tb % 2
                            P.op("act", lambda: nc.scalar.activation(out=xcf[xb_][:], in_=ps[bank][:], func=AF.Silu,
                                                                     bias=cwfm[:, cti, 4:5]),
                                 reads=[f"ps{bank}", "cwfm"], writes=[f"xcf{xb_}"])
                            for i4 in range(4):
                                f = lambda: nc.tensor.transpose(out=psb[:, i4 * 128:(i4 + 1) * 128], in_=xcf[xb_][:, i4 * 128:(i4 + 1) * 128],
                                                                identity=ident)
                                if i4 < 3:
                                    P.quiet("pe", f, reads=[f"xcf{xb_}", "cstb"], writes=["psb"])
                                else:
                                    P.op("pe", f, reads=[f"xcf{xb_}", "cstb"], writes=["psb"])
                            P.op("act", lambda: nc.scalar.copy(out=xs_tm[:, tb * 4:(tb + 1) * 4, j * 128:(j + 1) * 128],
                                                               in_=bview(psb[:, 0:512], 4)),
                                 reads=["psb"], writes=[("xs_tm", tb, j)])
                        else:
                            dstT = BT if kind == "B" else CT
                            P.op("act", lambda: nc.scalar.activation(out=dstT[:, tb * 512:(tb + 1) * 512], in_=ps[bank][:], func=AF.Silu,
                                                                     bias=cwfm[:, cti, 4:5]),
                                 reads=[f"ps{bank}", "cwfm"], writes=[(kind + "T", tb)])
                if sub == 202:
                    P.barrier(); return nc, dbg
                for k0 in range(0, NT, 8):
                    for jj in range(8):
                        cc = k0 + jj
                        f = lambda: nc.tensor.transpose(out=psb[:, jj * 128:(jj + 1) * 128], in_=BT[:, cc * 128:(cc + 1) * 128], identity=ident)
                        if jj < 7:
                            P.quiet("pe", f, reads=[("BT", cc // 4), "cstb"], writes=["psb"])
                        else:
                            P.op("pe", f, reads=[("BT", cc // 4), "cstb"], writes=["psb"])
                    P.op("act", lambda: nc.scalar.copy(out=B_tm[:, k0:k0 + 8, :], in_=bview(psb[:], 8)), reads=["psb"], writes=[("B_tm", k0 // 8)])
                for hl_ in range(8):
                    P.op("dve", lambda: nc.vector.tensor_scalar(out=dsk[:, hl_, :], in0=ident, scalar1=gb["d_skip"][:, 8 * g + hl_:8 * g + hl_ + 1],
                                                                scalar2=None, op0=ALU.mult),
                         reads=["cstb", "g_d_skip"], writes=["dsk"])
                P.op("pool", lambda: nc.gpsimd.memset(hst[:], 0.0), writes=["hst"])
                P.op("pool", lambda: nc.gpsimd.memset(hstb[:], 0.0), writes=["hstb"])
                if sub == 203:
                    P.barrier(); return nc, dbg
                xsr = lambda c_: [("xs_tm", c_ // 4, j_) for j_ in range(4)]
                def front(c):
                    cs_ = slice(c * 128, (c + 1) * 128)
                    pb = c % 2
                    P.op("pe", lambda: nc.tensor.matmul(ps[0][:, 0:128], lhsT=BT[:, cs_], rhs=CT[:, cs_], start=True, stop=True),
                         reads=[("BT", c // 4), ("CT", c // 4)], writes=["ps0"])
                    P.op("dve", lambda: nc.vector.tensor_tensor(out=cbm[:], in0=ps[0][:, 0:128], in1=cst[:, C_CT:C_CT + 128], op=ALU.mult),
                         reads=["ps0", "cst"], writes=["cbm"])
                    P.op("pool", lambda: nc.gpsimd.tensor_tensor(out=bview(xdt[pb][:], 8), in0=bview(xs_tm[:, c, :], 8), in1=G8(dt_all, c, g),
                                                                 op=ALU.mult), reads=xsr(c), writes=[f"xdt{pb}"])
                    P.op("pool", lambda: nc.gpsimd.tensor_tensor(out=bview(xw[pb][:], 8), in0=bview(xs_tm[:, c, :], 8), in1=G8(dtw, c, g),
                                                                 op=ALU.mult), reads=xsr(c), writes=[f"xw{pb}"])
                    for hb in range(2):
                        bcb = 1 + hb
                        for hh in range(4):
                            h = 8 * g + 4 * hb + hh
                            f = lambda: nc.tensor.matmul(ps[bcb][:, hh * 128:(hh + 1) * 128], lhsT=selb[:, h, :], rhs=A3[:, c, :],
                                                         start=True, stop=True, skip_group_check=True)
                            if hh < 3:
                                P.quiet("pe", f, reads=["selb"], writes=[f"ps{bcb}"])
                            else:
                                P.op("pe", f, reads=["selb"], writes=[f"ps{bcb}"])
                    for hb in range(2):
                        bcb = 1 + hb
                        for hh in range(4):
                            h = 8 * g + 4 * hb + hh
                            P.op("dve", lambda: nc.vector.tensor_scalar(out=seg[hb][:, hh * 128:(hh + 1) * 128],
                                                                        in0=ps[bcb][:, hh * 128:(hh + 1) * 128],
                                                                        scalar1=acs[:, c, h:h + 1], scalar2=0.0, op0=ALU.subtract, op1=ALU.min),
                                 reads=[f"ps{bcb}"], writes=[(f"seg{hb}", hh), f"Ee{hb}"])
                        P.op("act", lambda: nc.scalar.activation(out=Ee[hb][:], in_=seg[hb][:], func=AF.Exp),
                             reads=[(f"seg{hb}", hh_) for hh_ in range(4)], writes=[f"Ee{hb}"] + [(f"seg{hb}", hh_) for hh_ in range(4)])
                    for hb in range(2):
                        mi = 2 * pb + hb
                        P.op("dve", lambda: nc.vector.tensor_tensor(out=bview(MT[mi][:], 4), in0=bview(Ee[hb][:], 4),
                                                                    in1=cbm[:].unsqueeze(1).to_broadcast([128, 4, 128]), op=ALU.mult),
                             reads=[f"Ee{hb}", "cbm"], writes=[f"MT{mi}"])

                def back(c):
                    cs_ = slice(c * 128, (c + 1) * 128)
                    pb = c % 2
                    P.op("pe", lambda: nc.tensor.matmul(ps[5][:], lhsT=B_tm[:, c, :], rhs=xw[pb][:], start=True, stop=True),
                         reads=[("B_tm", c // 8), f"xw{pb}"], writes=["ps5"])
                    for hb in range(2):
                        mi = 2 * pb + hb
                        for hh in range(4):
                            hl = 4 * hb + hh
                            P.quiet("pe", lambda: nc.tensor.matmul(ps[3][:, hl * 64:(hl + 1) * 64], lhsT=MT[mi][:, hh * 128:(hh + 1) * 128],
                                                                   rhs=xdt[pb][:, hl * 64:(hl + 1) * 64], start=True, stop=False, skip_group_check=True),
                                    reads=[f"MT{mi}", f"xdt{pb}"], writes=["ps3"])
                            f = lambda: nc.tensor.matmul(ps[3][:, hl * 64:(hl + 1) * 64], lhsT=dsk[:, hl, :],
                                                         rhs=xs_tm[:, c, hl * 64:(hl + 1) * 64], start=False, stop=True, skip_group_check=True)
                            if hl < 7:
                                P.quiet("pe", f, reads=["dsk"] + xsr(c), writes=["ps3"])
                            else:
                                P.op("pe", f, reads=["dsk"] + xsr(c), writes=["ps3"])
                    for kt in range(8):
                        f = lambda: nc.tensor.matmul(ps[6][:], lhsT=hT[:, kt, cs_], rhs=wz[:, kt, :], start=(kt == 0), stop=(kt == 7))
                        if kt < 7:
                            P.quiet("pe", f, reads=[("wz", 0), ("wz", 1)], writes=["ps6"])
                        else:
                            P.op("pe", f, reads=[("wz", 0), ("wz", 1)], writes=["ps6"])
                    P.op("act", lambda: nc.scalar.activation(out=sz[:], in_=ps[6][:], func=AF.Silu), reads=["ps6"], writes=["t3"])
                    tb_ = t1b[pb]
                    tn = f"t1_{pb}"
                    if c > 0:
                        P.op("pe", lambda: nc.tensor.matmul(ps[4][:], lhsT=CT[:, cs_], rhs=hstb[:], start=True, stop=True),
                             reads=[("CT", c // 4), "hstb"], writes=["ps4"])
                        P.op("dve", lambda: nc.vector.tensor_tensor(out=bview(tb_[:], 8), in0=bview(ps[4][:], 8), in1=G8(ea, c, g), op=ALU.mult),
                             reads=["ps4"], writes=[tn])
                        P.op("dve", lambda: nc.vector.tensor_tensor(out=tb_[:], in0=ps[3][:], in1=tb_[:], op=ALU.add),
                             reads=["ps3", tn], writes=[tn])
                    else:
                        P.op("dve", lambda: nc.vector.tensor_copy(out=tb_[:], in_=ps[3][:]), reads=["ps3"], writes=[tn])
                    P.op("dve", lambda: nc.vector.tensor_tensor(out=bview(hst[:], 8), in0=bview(hst[:], 8), in1=G8(cdb, c, g), op=ALU.mult),
                         reads=["hst"], writes=["hst"])
                    P.op("dve", lambda: nc.vector.tensor_tensor(out=hst[:], in0=ps[5][:], in1=hst[:], op=ALU.add), reads=["ps5", "hst"], writes=["hst"])
                    P.op("act", lambda: nc.scalar.copy(out=hstb[:], in_=hst[:]), reads=["hst"], writes=["hstb"])
                    P.op("dve", lambda: nc.vector.tensor_tensor(out=tb_[:], in0=tb_[:], in1=sz[:], op=ALU.mult), reads=[tn, "t3"], writes=[tn])
                    P.op("act", lambda: nc.scalar.activation(out=sjk[:], in_=tb_[:], func=AF.Square, accum_out=g1[0][:, pb:pb + 1]),
                         reads=[tn], writes=["xcf0", ("g1_0", pb)])
                    P.op("act", lambda: nc.scalar.activation(out=g1[2][:, pb:pb + 1], in_=g1[0][:, pb:pb + 1], func=AF.Sqrt, scale=1.0 / 512, bias=epsb[:, 0:1]),
                         reads=[("g1_0", pb), "epsb"], writes=[("g1_2", pb)])

                def backB(c):
                    pb = c % 2
                    tb_ = t1b[pb]
                    tn = f"t1_{pb}"
                    P.op("dve", lambda: nc.vector.reciprocal(out=g1[3][:, pb:pb + 1], in_=g1[2][:, pb:pb + 1]), reads=[("g1_2", pb)], writes=[("g1_3", pb)])
                    P.op("dve", lambda: nc.vector.scalar_tensor_tensor(out=ynb[pb][:], in0=tb_[:], scalar=g1[3][:, pb:pb + 1], in1=ssdg[:], op0=ALU.mult, op1=ALU.mult),
                         reads=[tn, ("g1_3", pb), "ssdg"], writes=[f"ynb{pb}"])

                def tail(c):
                    cs_ = slice(c * 128, (c + 1) * 128)
                    pb = c % 2
                    for i4 in range(4):
                        f = lambda: nc.tensor.transpose(out=psb[:, i4 * 128:(i4 + 1) * 128], in_=ynb[pb][:, i4 * 128:(i4 + 1) * 128], identity=ident)
                        if i4 < 3:
                            P.quiet("pe", f, reads=[f"ynb{pb}", "cstb"], writes=["psb"])
                        else:
                            P.op("pe", f, reads=[f"ynb{pb}", "cstb"], writes=["psb"])
                    P.op("act", lambda: nc.scalar.copy(out=ysT[:, 4 * g:4 * g + 4, cs_], in_=bview(psb[:, 0:512], 4)), reads=["psb"], writes=[("ysT", g, c)])

                front(0)
                for c in range(NT):
                    if c + 1 < NT:
                        front(c + 1)
                    back(c)
                    if c >= 1:
                        backB(c - 1)
                        tail(c - 1)
                backB(NT - 1)
                tail(NT - 1)
            P.barrier()
        if stage == 3:
            o = dout("ysT_dbg", [128, 16, T], BF16)
            P.dma("sp", "out", [(o, ysT[:])])
            P.barrier()
            return nc, dbg

        stM = contextlib.ExitStack()
        es.enter_context(stM)
        mgT = sb("mgT", [128, 8, T], BF16, stM)
        with contextlib.ExitStack() as sM:
            yaT2 = sb("yaT2", [128, 8, T], BF16, sM)
            P.dma("sp", "ya_reload", [(yaT2[:], ya_spill)], writes=["yaT2"])
            wstM = sb("wstM", [128, 16, 128], F32, sM)
            wac = sb("wac", [128, 8, 128], BF16, sM)
            wbc = sb("wbc", [128, 16, 128], BF16, sM)
            wgac = sb("wgac", [128, 8, 128], BF16, sM)
            wgbc = sb("wgbc", [128, 8, 128], BF16, sM)
            sga = [sb(f"sga{i}", [128, 512], F32, sM) for i in range(2)]
            sgb = [sb(f"sgb{i}", [128, 512], F32, sM) for i in range(2)]
            m1 = [sb(f"m1_{i}", [128, 512], F32, sM) for i in range(2)]
            m2 = [sb(f"m2_{i}", [128, 512], F32, sM) for i in range(2)]
            wa_v = wa_d.rearrange("(kt p) n -> p kt n", p=128)
            wb_v = wb_d.rearrange("(kt p) n -> p kt n", p=128)
            it = [0]
            for nt in range(8):
                ns = slice(nt * 128, (nt + 1) * 128)
                for (dst, src, nk_, nm) in [(wac, wa_v[:, :, ns], 8, "wac"), (wbc, wb_v[:, :, ns], 16, "wbc"),
                                            (wgac, win_v[:, :, O_GA + nt * 128:O_GA + (nt + 1) * 128], 8, "wgac"),
                                            (wgbc, win_v[:, :, O_GB + nt * 128:O_GB + (nt + 1) * 128], 8, "wgbc")]:
                    P.dma("sp", "wstM", [(wstM[:, 0:nk_, :], src)], writes=["wstM"])
                    P.op("pool", lambda: nc.gpsimd.tensor_copy(out=dst[:], in_=wstM[:, 0:nk_, :]), reads=["wstM"], writes=[nm])
                for tb in range(4):
                    ts_ = slice(tb * 512, (tb + 1) * 512)
                    par = it[0] % 2
                    it[0] += 1
                    bA, bB, bGA = (0, 1, 2) if par == 0 else (4, 5, 6)
                    bGB = 3

                    def acc(bank, wt, nk_, rhsT, nm, rd):
                        for kt in range(nk_):
                            f = lambda: nc.tensor.matmul(ps[bank][:], lhsT=wt[:, kt, :], rhs=rhsT[:, kt, ts_], start=(kt == 0), stop=(kt == nk_ - 1))
                            if kt < nk_ - 1:
                                P.quiet("pe", f, reads=[nm] + rd, writes=[f"ps{bank}"])
                            else:
                                P.op("pe", f, reads=[nm] + rd, writes=[f"ps{bank}"])
                    acc(bGA, wgac, 8, hT, "wgac", [])
                    acc(bGB, wgbc, 8, hT, "wgbc", [])
                    acc(bA, wac, 8, yaT2, "wac", ["yaT2"])
                    acc(bB, wbc, 16, ysT, "wbc", [])
                    P.op("act", lambda: nc.scalar.activation(out=sga[par][:], in_=ps[bGA][:], func=AF.Sigmoid), reads=[f"ps{bGA}"], writes=[f"sga{par}"])
                    P.op("act", lambda: nc.scalar.activation(out=sgb[par][:], in_=ps[bGB][:], func=AF.Sigmoid), reads=[f"ps{bGB}"], writes=[f"sgb{par}"])
                    P.op("dve", lambda: nc.vector.tensor_tensor(out=m1[par][:], in0=ps[bA][:], in1=sga[par][:], op=ALU.mult),
                         reads=[f"ps{bA}", f"sga{par}"], writes=[f"m1_{par}"])
                    P.op("dve", lambda: nc.vector.tensor_tensor(out=m2[par][:], in0=ps[bB][:], in1=sgb[par][:], op=ALU.mult),
                         reads=[f"ps{bB}", f"sgb{par}"], writes=[f"m2_{par}"])
                    P.op("pool", lambda: nc.gpsimd.tensor_tensor(out=mgT[:, nt, ts_], in0=m1[par][:], in1=m2[par][:], op=ALU.add),
                         reads=[f"m1_{par}", f"m2_{par}"], writes=[("mgT", nt, tb)])
            P.barrier()
        if stage == 4:
            o = dout("mgT_dbg", [128, 8, T], BF16)
            P.dma("sp", "out", [(o, mgT[:])])
            P.barrier()
            return nc, dbg

        x1 = ysT[:].bitcast(F32)
        assert list(x1.shape) == [128, NT, D], x1.shape
        with contextlib.ExitStack() as sO:
            wstO = sb("wstO", [128, 8, 256], F32, sO)
            wo = sb("wo", [128, 8, D], BF16, sO)
            wo_v = wo_d.rearrange("(kt p) n -> p kt n", p=128)
            for q4 in range(4):
                P.dma("sp", "wstO", [(wstO[:], wo_v[:, :, q4 * 256:(q4 + 1) * 256])], writes=["wstO"])
                P.op("pool", lambda: nc.gpsimd.tensor_copy(out=wo[:, :, q4 * 256:(q4 + 1) * 256], in_=wstO[:]), reads=["wstO"], writes=[("wo", q4)])
            for c in range(NT):
                cs_ = slice(c * 128, (c + 1) * 128)
                P.dma("sp", f"x1ld{c % 2}", [(x1[:, c, :], x_d[cs_, :])], writes=[("x1", c)])
                for hf in range(2):
                    bank = (2 * c + hf) % 4
                    for kt in range(8):
                        f = lambda: nc.tensor.matmul(ps[bank][:], lhsT=mgT[:, kt, cs_], rhs=wo[:, kt, hf * 512:(hf + 1) * 512],
                                                     start=(kt == 0), stop=(kt == 7))
                        rd = [("wo", 2 * hf), ("wo", 2 * hf + 1)]
                        if kt < 7:
                            P.quiet("pe", f, reads=rd, writes=[f"ps{bank}"])
                        else:
                            P.op("pe", f, reads=rd, writes=[f"ps{bank}"])
                    P.op("dve", lambda: nc.vector.tensor_tensor(out=x1[:, c, hf * 512:(hf + 1) * 512], in0=ps[bank][:],
                                                                in1=x1[:, c, hf * 512:(hf + 1) * 512], op=ALU.add),
                         reads=[f"ps{bank}", ("x1", c)], writes=[("x1", c)])
            P.barrier()
        stM.close()
        if stage == 5:
            o = dout("x1_dbg", [128, NT, D], F32)
            P.dma("sp", "out", [(o, x1)])
            P.barrier()
            return nc, dbg

        with contextlib.ExitStack() as sN:
            load_gain("ffn_norm", D, sN)

            def from_x1(c, buf, rname):
                P.op("pool", lambda: nc.gpsimd.tensor_copy(out=buf[:], in_=x1[:, c, :]), reads=[("x1", c)], writes=[rname])
            rmsnorm_T(from_x1, "ffn_norm", hT, sN)
            P.barrier()

        with contextlib.ExitStack() as sE:
            selm = sb("selm", [128, 32, 128], BF16, sE)
            P.op("pool", lambda: nc.gpsimd.memset(selm[:], 0.0), writes=["selm"])
            for r3 in range(3):
                P.op("pool", lambda: nc.gpsimd.tensor_copy(
                    out=selm[32 * r3:32 * r3 + 32, :, :],
                    in_=cstb[32 * r3:32 * r3 + 32, C_ID + 32 * r3:C_ID + 32 * r3 + 32].unsqueeze(2).to_broadcast([32, 32, 128])),
                    reads=["cstb", "selm"], writes=["selm"])
            cT3 = sb("cT3", [128, T], BF16, sE)
            P.op("pool", lambda: nc.gpsimd.memset(cT3[:], 0.0), writes=["cT3z"])
            with contextlib.ExitStack() as sR:
                wr_s = sb("wr_s", [128, 8, 36], F32, sR)
                wr = sb("wr", [128, 8, 36], BF16, sR)
                P.dma("sp", "wr_s", [(wr_s[:, :, 0:4], wrg_d.rearrange("(kt p) n -> p kt n", p=128)),
                                     (wr_s[:, :, 4:36], wre_d.rearrange("(kt p) n -> p kt n", p=128))], writes=["wr_s"])
                P.op("pool", lambda: nc.gpsimd.tensor_copy(out=wr[:], in_=wr_s[:]), reads=["wr_s"], writes=["wr"])
                rb_ = sb("rbias", [128, 36], F32, sR)
                P.dma("sp", "rbias", [(rb_[:, 0:4], gains["b_route_group"].partition_broadcast(128)),
                                      (rb_[:, 4:36], gains["b_route_expert"].partition_broadcast(128))], writes=["rbias"])
                lg = sb("r_lg", [128, 36], F32, sR)
                r1c = [sb(f"r_c{i}", [128, 1], F32, sR) for i in range(8)]
                oh = sb("r_oh", [128, 4], F32, sR)
                ge = sb("r_ge", [128, 4], F32, sR)
                tmp48 = sb("r_t48", [128, 4, 8], F32, sR)
                ein = sb("r_ein", [128, 8], F32, sR)
                top8 = sb("r_top8", [128, 8], F32, sR)
                msel = sb("r_msel", [128, 8], F32, sR)
                wex = sb("r_wex", [128, 8], F32, sR)
                comb = sb("r_comb", [128, 4, 8], F32, sR)
                comb3 = sb("r_comb3", [128, 3, 32], F32, sR)
                Hb2 = sb("r_Hb", [128, 128], BF16, sR)
                Mb2 = sb("r_Mb", [128, 128], BF16, sR)
                q1 = sb("r_q1", [128, 128], F32, sR)
                q2 = sb("r_q2", [128, 128], F32, sR)
                mx, nmx, sme, gw, m21, den, rden, sc_ = r1c
                for c in range(NT):
                    cs_ = slice(c * 128, (c + 1) * 128)
                    for kt in range(8):
                        f = lambda: nc.tensor.matmul(ps[0][:, 0:36], lhsT=hT[:, kt, cs_], rhs=wr[:, kt, :], start=(kt == 0), stop=(kt == 7))
                        if kt < 7:
                            P.quiet("pe", f, reads=["wr"], writes=["ps0"])
                        else:
                            P.op("pe", f, reads=["wr"], writes=["ps0"])
                    P.op("dve", lambda: nc.vector.tensor_tensor(out=lg[:], in0=ps[0][:, 0:36], in1=rb_[:], op=ALU.add), reads=["ps0", "rbias"], writes=["r_lg"])
                    P.op("dve", lambda: nc.vector.tensor_reduce(out=mx[:], in_=lg[:, 0:4], axis=AX.X, op=ALU.max), reads=["r_lg"], writes=["r_mx"])
                    P.op("dve", lambda: nc.vector.tensor_scalar(out=oh[:], in0=lg[:, 0:4], scalar1=mx[:, 0:1], scalar2=None, op0=ALU.is_ge),
                         reads=["r_lg", "r_mx"], writes=["r_oh"])
                    P.op("dve", lambda: nc.vector.tensor_scalar(out=nmx[:], in0=mx[:], scalar1=-1.0, scalar2=None, op0=ALU.mult), reads=["r_mx"], writes=["r_nmx"])
                    P.op("act", lambda: nc.scalar.activation(out=ge[:], in_=lg[:, 0:4], func=AF.Exp, bias=nmx[:, 0:1], accum_out=sme[:]),
                         reads=["r_lg", "r_nmx"], writes=["r_ge", "r_sme"])
                    P.op("dve", lambda: nc.vector.reciprocal(out=gw[:], in_=sme[:]), reads=["r_sme"], writes=["r_gw"])
                    P.op("dve", lambda: nc.vector.tensor_tensor(out=tmp48[:], in0=bview(lg[:, 4:36], 4), in1=oh[:].unsqueeze(2).to_broadcast([128, 4, 8]),
                                                                op=ALU.mult), reads=["r_lg", "r_oh"], writes=["r_t48"])
                    P.op("dve", lambda: nc.vector.tensor_reduce(out=ein[:], in_=tmp48[:].rearrange("p g e -> p e g"), axis=AX.X, op=ALU.add),
                         reads=["r_t48"], writes=["r_ein"])
                    P.op("dve", lambda: nc.vector.max(out=top8[:], in_=ein[:]), reads=["r_ein"], writes=["r_top8"])
                    P.op("dve", lambda: nc.vector.tensor_scalar(out=msel[:], in0=ein[:], scalar1=top8[:, 1:2], scalar2=None, op0=ALU.is_ge),
                         reads=["r_ein", "r_top8"], writes=["r_msel"])
                    P.op("dve", lambda: nc.vector.tensor_scalar(out=nmx[:], in0=top8[:, 0:1], scalar1=-1.0, scalar2=None, op0=ALU.mult),
                         reads=["r_top8"], writes=["r_nmx"])
                    P.op("act", lambda: nc.scalar.activation(out=wex[:], in_=ein[:], func=AF.Exp, bias=nmx[:, 0:1]), reads=["r_ein", "r_nmx"], writes=["r_wex"])
                    P.op("act", lambda: nc.scalar.activation(out=m21[:], in_=top8[:, 1:2], func=AF.Exp, bias=nmx[:, 0:1]), reads=["r_top8", "r_nmx"], writes=["r_m21"])
                    P.op("dve", lambda: nc.vector.tensor_scalar(out=den[:], in0=m21[:], scalar1=1.0, scalar2=None, op0=ALU.add), reads=["r_m21"], writes=["r_den"])
                    P.op("dve", lambda: nc.vector.reciprocal(out=rden[:], in_=den[:]), reads=["r_den"], writes=["r_rden"])
                    P.op("dve", lambda: nc.vector.tensor_tensor(out=sc_[:], in0=rden[:], in1=gw[:], op=ALU.mult), reads=["r_rden", "r_gw"], writes=["r_sc"])
                    P.op("dve", lambda: nc.vector.tensor_tensor(out=wex[:], in0=wex[:], in1=msel[:], op=ALU.mult), reads=["r_wex", "r_msel"], writes=["r_wex"])
                    P.op("dve", lambda: nc.vector.tensor_scalar(out=wex[:], in0=wex[:], scalar1=sc_[:, 0:1], scalar2=None, op0=ALU.mult),
                         reads=["r_wex", "r_sc"], writes=["r_wex"])
                    P.op("dve", lambda: nc.vector.tensor_tensor(out=comb[:], in0=oh[:].unsqueeze(2).to_broadcast([128, 4, 8]),
                                                                in1=wex[:].unsqueeze(1).to_broadcast([128, 4, 8]), op=ALU.mult),
                         reads=["r_oh", "r_wex"], writes=["r_comb"])
                    P.op("dve", lambda: nc.vector.tensor_copy(out=comb3[:], in_=comb[:].rearrange("p g e -> p (g e)").unsqueeze(1).to_broadcast([128, 3, 32])),
                         reads=["r_comb"], writes=["r_comb3"])
                    P.op("pe", lambda: nc.tensor.transpose(out=ps[1][0:96, 0:128], in_=comb3[:].rearrange("p a b -> p (a b)"),
                                                           identity=cst[:, C_ID:C_ID + 128]),
                         reads=["r_comb3", "cst"], writes=["ps1"])
                    P.op("act", lambda: nc.scalar.copy(out=Hb2[0:96, :], in_=ps[1][0:96, 0:128]), reads=["ps1"], writes=["r_Hb"])
                    P.op("dve", lambda: nc.vector.tensor_tensor(out=q1[0:96, :], in0=ps[1][0:96, 0:128], in1=Hb2[0:96, :], op=ALU.subtract),
                         reads=["ps1", "r_Hb"], writes=["r_q1"])
                    P.op("act", lambda: nc.scalar.copy(out=Mb2[0:96, :], in_=q1[0:96, :]), reads=["r_q1"], writes=["r_Mb"])
                    P.op("dve", lambda: nc.vector.tensor_tensor(out=q2[0:96, :], in0=q1[0:96, :], in1=Mb2[0:96, :], op=ALU.subtract),
                         reads=["r_q1", "r_Mb"], writes=["r_q2"])
                    P.op("pool", lambda: nc.gpsimd.tensor_copy(out=cT3[0:32, cs_], in_=Hb2[0:32, :]), reads=["r_Hb", "cT3z"], writes=[("cT3", c, 0)])
                    P.op("pool", lambda: nc.gpsimd.tensor_copy(out=cT3[32:64, cs_], in_=Mb2[32:64, :]), reads=["r_Mb", "cT3z"], writes=[("cT3", c, 1)])
                    P.op("act", lambda: nc.scalar.copy(out=cT3[64:96, cs_], in_=q2[64:96, :]), reads=["r_q2", "cT3z"], writes=[("cT3", c, 2)])
                P.barrier()
            if stage == 6:
                o = dout("cT3_dbg", [128, T], BF16)
                P.dma("sp", "out", [(o, cT3[:])])
                o = dout("h2T_dbg", [128, 8, T], BF16)
                P.dma("sp", "out", [(o, hT[:])])
                P.barrier()
                return nc, dbg

            NE = 32 if sub >= 30 else 2
            wstE = [sb(f"wstE{i}", [128, 8, 256], F32, sE) for i in range(2)]
            wgu = [sb(f"wgu{i}", [128, 8, 512], BF16, sE) for i in range(2)]
            wdn = [sb(f"wdn{i}", [128, 2, D], BF16, sE) for i in range(4)]
            actT = [sb(f"actT{i}", [128, 2, T], BF16, sE) for i in range(2)]
            sgs = [sb(f"sgs{i}", [128, 512], F32, sE) for i in range(2)]
            tms = [sb(f"tms{i}", [128, 512], F32, sE) for i in range(2)]
            stc = [0]
            itc = [0]
            for e in range(NE):
                sl = e % 2
                dsl = e % 4
                for (k_, src) in [(0, wg_d[e].rearrange("(kt p) n -> p kt n", p=128)), (1, wu_d[e].rearrange("(kt p) n -> p kt n", p=128))]:
                    si = stc[0] % 2
                    stc[0] += 1
                    P.dma("sp", f"wstE{si}", [(wstE[si][:], src)], writes=[f"wstE{si}"])
                    P.op("pool", lambda: nc.gpsimd.tensor_copy(out=wgu[sl][:, :, k_ * 256:(k_ + 1) * 256], in_=wstE[si][:]),
                         reads=[f"wstE{si}"], writes=[(f"wgu{sl}", k_)])
                si = stc[0] % 2
                stc[0] += 1
                P.dma("sp", f"wstE{si}", [(wstE[si][:].rearrange("p a b -> p (a b)").rearrange("p (f n) -> p f n", f=2),
                                           wd_d[e].rearrange("(ft p) n -> p ft n", p=128))],
                      writes=[f"wstE{si}"])
                P.op("pool", lambda: nc.gpsimd.tensor_copy(out=wdn[dsl][:].rearrange("p a b -> p (a b)"), in_=wstE[si][:].rearrange("p a b -> p (a b)")),
                     reads=[f"wstE{si}"], writes=[f"wdn{dsl}"])
                for tb in range(4):
                    ts_ = slice(tb * 512, (tb + 1) * 512)
                    P.op("pe", lambda: nc.tensor.matmul(ps[4][:], lhsT=selm[:, e, :], rhs=cT3[:, ts_], start=True, stop=True),
                         reads=["selm"], writes=["ps4"])
                    for ft in range(2):
                        par = itc[0] % 2
                        itc[0] += 1
                        bG, bU = (0, 1) if par == 0 else (2, 3)
                        for (bank, k_) in [(bG, 0), (bU, 1)]:
                            for kt in range(8):
                                f = lambda: nc.tensor.matmul(ps[bank][:], lhsT=wgu[sl][:, kt, k_ * 256 + ft * 128:k_ * 256 + (ft + 1) * 128],
                                                             rhs=hT[:, kt, ts_], start=(kt == 0), stop=(kt == 7))
                                if kt < 7:
                                    P.quiet("pe", f, reads=[(f"wgu{sl}", k_)], writes=[f"ps{bank}"])
                                else:
                                    P.op("pe", f, reads=[(f"wgu{sl}", k_)], writes=[f"ps{bank}"])
                        P.op("act", lambda: nc.scalar.activation(out=sgs[par][:], in_=ps[bG][:], func=AF.Silu), reads=[f"ps{bG}"], writes=[f"sgs{par}"])
                        P.op("dve", lambda: nc.vector.tensor_tensor(out=tms[par][:], in0=ps[bU][:], in1=sgs[par][:], op=ALU.mult),
                             reads=[f"ps{bU}", f"sgs{par}"], writes=[f"tms{par}"])
                        P.op("dve", lambda: nc.vector.tensor_tensor(out=actT[sl][:, ft, ts_], in0=ps[4][:], in1=tms[par][:], op=ALU.mult),
                             reads=["ps4", f"tms{par}"], writes=[(f"actT{sl}", ft, tb)])
                if e % 2 == 1:
                    for c in range(NT):
                        cs_ = slice(c * 128, (c + 1) * 128)
                        for hf in range(2):
                            bank = 5 + (2 * c + hf) % 2
                            n_ = 0
                            for ee in (e - 1, e):
                                for ft in range(2):
                                    f = lambda: nc.tensor.matmul(ps[bank][:], lhsT=actT[ee % 2][:, ft, cs_], rhs=wdn[ee % 4][:, ft, hf * 512:(hf + 1) * 512],
                                                                 start=(n_ == 0), stop=(n_ == 3))
                                    rd = [(f"actT{ee % 2}", ft, c // 4), f"wdn{ee % 4}"]
                                    if n_ < 3:
                                        P.quiet("pe", f, reads=rd, writes=[f"ps{bank}"])
                                    else:
                                        P.op("pe", f, reads=rd, writes=[f"ps{bank}"])
                                    n_ += 1
                            P.op("dve", lambda: nc.vector.tensor_tensor(out=x1[:, c, hf * 512:(hf + 1) * 512], in0=ps[bank][:],
                                                                        in1=x1[:, c, hf * 512:(hf + 1) * 512], op=ALU.add),
                                 reads=[f"ps{bank}", ("x1", c)], writes=[("x1", c)])
            for c in range(NT):
                P.dma("sp", "out", [(out_d[c * 128:(c + 1) * 128, :], x1[:, c, :])], reads=[("x1", c)])
            P.barrier()
    return nc, dbg


def host_consts():
    cst = np.zeros((128, CW), np.float32)
    cst[:, C_ID:C_ID + 128] = np.eye(128, dtype=np.float32)
    dm = np.ones((128, 128), np.float32)
    dm[0:64, 64:128] = 0.0
    cst[:, C_DM:C_DM + 128] = dm
    cst[:, C_NB:C_NB + 128] = (dm - 1.0) * 1e30
    cst[:, C_CT:C_CT + 128] = np.triu(np.ones((128, 128), np.float32))
    cst[:, C_ONE:C_ONE + 128] = 1.0
    for i in range(NBIS):
        cst[:, C_BIS + i] = 2.0 ** (-(i + 1))
    half = 8
    inv = 500000.0 ** (-np.arange(half, dtype=np.float64) * 2.0 / 16)
    ang = np.arange(T, dtype=np.float64)[:, None] * inv[None, :]
    cos, sin = np.cos(ang), np.sin(ang)
    rope = np.concatenate([cos, cos, -sin, sin], axis=1).astype(np.float32)
    return cst, rope


_CACHE = {}


def make_inmaps(inputs):
    cst, rope = host_consts()
    maps = []
    sq = lambda a: np.ascontiguousarray(np.asarray(a, np.float32)[0])
    shared = {
        "w_in": sq(inputs["w_in"]),
        "conv_w": sq(inputs["conv_w"]),
        "w_attn_branch": sq(inputs["w_attn_branch"]), "w_ssd_branch": sq(inputs["w_ssd_branch"]),
        "w_out": sq(inputs["w_out"]), "w_route_group": sq(inputs["w_route_group"]),
        "w_route_expert": sq(inputs["w_route_expert"]),
        "w_gate": sq(inputs["w_gate"]).reshape(32, 1024, 256), "w_up": sq(inputs["w_up"]).reshape(32, 1024, 256),
        "w_down": sq(inputs["w_down"]).reshape(32, 256, 1024),
        "cst": cst, "rope": rope,
    }
    for n in ["attn_norm", "q_norm", "k_norm", "idx_k_norm", "ffn_norm", "ssd_norm", "conv_b", "dt_bias", "a_log",
              "d_skip", "b_route_group", "b_route_expert"]:
        shared[n] = np.ascontiguousarray(np.asarray(inputs[n], np.float32).reshape(1, -1))
    x = np.asarray(inputs["x"], np.float32)
    for b in range(8):
        m = dict(shared)
        m["x"] = np.ascontiguousarray(x[b])
        maps.append(m)
    return maps


def kernel(**inputs):
    if "nc" not in _CACHE:
        _CACHE["nc"] = build()[0]
    nc = _CACHE["nc"]
    maps = make_inmaps(inputs)
    res = run_bass_kernel_spmd(nc, maps, core_ids=list(range(8)))
    return np.stack([np.asarray(r["out"], np.float32) for r in res.results], axis=0)
```

```python
import contextlib
import math
import numpy as np
import concourse.bass as bass
import concourse.mybir as mybir
from concourse.bass_utils import run_bass_kernel_spmd

F32 = mybir.dt.float32
BF16 = mybir.dt.bfloat16
U32 = mybir.dt.uint32
AF = mybir.ActivationFunctionType
ALU = mybir.AluOpType
AX = mybir.AxisListType

T = 2048
D = 1024
NT = 16
EPS = 1e-6
NBIS = 16
SPL = (1024, 256, 256, 512, 64, 8, 2048, 3072, 32, 1024, 1024)
OFF = [0]
for _s in SPL:
    OFF.append(OFF[-1] + _s)
(O_Q, O_K, O_V, O_IQ, O_IK, O_IW, O_Z, O_XBC, O_DT, O_GA, O_GB, O_END) = OFF
NW = O_END

C_ID = 0
C_DM = 128
C_NB = 256
C_CT = 384
C_ONE = 512
C_BIS = 640
CW = 672


class Prog:
    ENG = ("pe", "act", "dve", "pool", "sp")

    def __init__(self, nc, es):
        self.nc = nc
        self.es = es
        self.e = {"pe": nc.tensor, "act": nc.scalar, "dve": nc.vector, "pool": nc.gpsimd, "sp": nc.sync}
        self.sem = {}
        self.cnt = {k: 0 for k in self.ENG}
        self.epoch = {k: 0 for k in self.ENG}
        for k in self.ENG:
            self.sem[("e", k, 0)] = es.enter_context(nc.semaphore(f"s_{k}_0"))
        self.dcnt = {}
        self.waited = {k: {} for k in self.ENG}
        self.res = {}
        self.nins = 0
        self.pending = {k: [] for k in self.ENG}

    def _deps(self, eng, reads, writes):
        deps = []
        for r in reads:
            st = self.res.get(r)
            if st and st[0] is not None:
                deps.append((st[0], True))
        for w in writes:
            st = self.res.get(w)
            if st:
                if st[0] is not None:
                    deps.append((st[0], True))
                for t in st[1].values():
                    deps.append((t, False))
        for (tok, strong) in deps:
            key, val = tok
            if key[0] == "e" and key[1] == eng:
                if eng == "pe":
                    continue
            if self.waited[eng].get(key, -1) >= val:
                continue
            self.e[eng].wait_ge(self.sem[key], val)
            self.waited[eng][key] = val

    def _commit(self, tok, reads, writes, rkey):
        for r in reads:
            st = self.res.setdefault(r, [None, {}])
            st[1][rkey] = tok
        for w in writes:
            self.res[w] = [tok, {}]

    def op(self, eng, fn, reads=(), writes=()):
        self._deps(eng, reads, writes)
        ins = fn()
        if self.cnt[eng] >= 30000:
            self.epoch[eng] += 1
            self.cnt[eng] = 0
            self.sem[("e", eng, self.epoch[eng])] = self.es.enter_context(
                self.nc.semaphore(f"s_{eng}_{self.epoch[eng]}"))
        key = ("e", eng, self.epoch[eng])
        self.cnt[eng] += 1
        ins.then_inc(self.sem[key], 1)
        self.nins += 1
        for (r_, w_) in self.pending[eng]:
            self._commit((key, self.cnt[eng]), r_, w_, key)
        self.pending[eng] = []
        self._commit((key, self.cnt[eng]), reads, writes, key)
        return ins

    def quiet(self, eng, fn, reads=(), writes=()):
        self._deps(eng, reads, writes)
        self.nins += 1
        self.pending[eng].append((tuple(reads), tuple(writes)))
        return fn()

    def dma(self, q, key, pairs, reads=(), writes=()):
        self._deps(q, reads, writes)
        k = ("d", key)
        if k not in self.sem:
            self.sem[k] = self.es.enter_context(self.nc.semaphore(f"d_{key}"))
            self.dcnt[k] = 0
        for (o, i) in pairs:
            self.e[q].dma_start(out=o, in_=i).then_inc(self.sem[k], 16)
            self.dcnt[k] += 16
            self.nins += 1
        self._commit((k, self.dcnt[k]), reads, writes, k)

    def barrier(self):
        toks = []
        for k in self.ENG:
            if self.cnt[k] > 0:
                toks.append((("e", k, self.epoch[k]), self.cnt[k]))
        for k, v in self.dcnt.items():
            if v > 0:
                toks.append((k, v))
        for eng in self.ENG:
            for (key, val) in toks:
                if key[0] == "e" and key[1] == eng:
                    continue
                if self.waited[eng].get(key, -1) >= val:
                    continue
                self.e[eng].wait_ge(self.sem[key], val)
                self.waited[eng][key] = val
        self.res = {}


def bview(ap, h):
    return ap.rearrange("p (h d) -> p h d", h=h)


def build(stage=99, sub=99):
    nc = bass.Bass("TRN2", target_bir_lowering=False)
    dr = {}

    def din(name, shape):
        dr[name] = nc.dram_tensor(name, list(shape), F32, kind="ExternalInput").ap()
        return dr[name]

    x_d = din("x", [T, D])
    win_d = din("w_in", [D, NW])
    gains = {n: din(n, [1, s]) for n, s in [("attn_norm", D), ("q_norm", 64), ("k_norm", 64), ("idx_k_norm", 64),
                                             ("ffn_norm", D), ("ssd_norm", 2048), ("conv_b", 3072), ("dt_bias", 32),
                                             ("a_log", 32), ("d_skip", 32), ("b_route_group", 4),
                                             ("b_route_expert", 32)]}
    convw_d = din("conv_w", [4, 3072])
    wa_d = din("w_attn_branch", [1024, 1024])
    wb_d = din("w_ssd_branch", [2048, 1024])
    wo_d = din("w_out", [1024, 1024])
    wrg_d = din("w_route_group", [1024, 4])
    wre_d = din("w_route_expert", [1024, 32])
    wg_d = din("w_gate", [32, 1024, 256])
    wu_d = din("w_up", [32, 1024, 256])
    wd_d = din("w_down", [32, 256, 1024])
    cst_d = din("cst", [128, CW])
    rope_d = din("rope", [T, 32])
    out_d = nc.dram_tensor("out", [T, D], F32, kind="ExternalOutput").ap()
    dbg = {}

    def dout(name, shape, dt=F32):
        dbg[name] = nc.dram_tensor(name, list(shape), dt, kind="ExternalOutput").ap()
        return dbg[name]

    es = contextlib.ExitStack()
    with es:
        P = Prog(nc, es)

        uid = [0]

        def sb(name, shape, dt, stack=es):
            uid[0] += 1
            return stack.enter_context(nc.sbuf_tensor(f"sb{uid[0]}_{name}", list(shape), dt))

        ps = [es.enter_context(nc.psum_tensor(f"ps{i}", [128, 512], F32)) for i in range(7)]
        psb = es.enter_context(nc.psum_tensor("psb", [128, 1024], BF16))

        cst = sb("cst", [128, CW], F32)
        cstb = sb("cstb", [128, CW], BF16)
        P.dma("sp", "cst", [(cst[:], cst_d)], writes=["cst"])
        P.op("pool", lambda: nc.gpsimd.tensor_copy(out=cstb[:], in_=cst[:]), reads=["cst"], writes=["cstb"])
        ident = cstb[:, C_ID:C_ID + 128]
        gb = {}

        def load_gain(n, width, st, c0=0):
            gb[n] = sb("g_" + n, [128, width], F32, st)
            P.dma("sp", "gain_" + n, [(gb[n][:], gains[n][:, c0:c0 + width].partition_broadcast(128))], writes=["g_" + n])

        hT = sb("hT", [128, 8, T], BF16)
        ya_spill = nc.dram_tensor("ya_spill", [128, 8, T], BF16, kind="Internal").ap()

        def rmsnorm_T(src_fn, gname, dst, st):
            xb = [sb(f"rn_x{i}", [128, D], F32, st) for i in range(2)]
            xn = [sb(f"rn_xn{i}", [128, D], BF16, st) for i in range(2)]
            junk = sb("rn_junk", [128, D], BF16, st)
            ss = sb("rn_ss", [128, NT], F32, st)
            ms = sb("rn_ms", [128, NT], F32, st)
            sd = sb("rn_sd", [128, NT], F32, st)
            rs = sb("rn_rs", [128, NT], F32, st)
            for c in range(NT):
                b = c % 2
                src_fn(c, xb[b], f"rn_x{b}")
                P.op("act", lambda: nc.scalar.activation(out=junk[:], in_=xb[b][:], func=AF.Square,
                                                         accum_out=ss[:, c:c + 1]),
                     reads=[f"rn_x{b}"], writes=["rn_junk", ("rn_ss", c)])
                P.op("dve", lambda: nc.vector.tensor_scalar(out=ms[:, c:c + 1], in0=ss[:, c:c + 1], scalar1=1.0 / D,
                                                            scalar2=EPS, op0=ALU.mult, op1=ALU.add),
                     reads=[("rn_ss", c)], writes=[("rn_ms", c)])
                P.op("act", lambda: nc.scalar.activation(out=sd[:, c:c + 1], in_=ms[:, c:c + 1], func=AF.Sqrt),
                     reads=[("rn_ms", c)], writes=[("rn_sd", c)])
                P.op("dve", lambda: nc.vector.reciprocal(out=rs[:, c:c + 1], in_=sd[:, c:c + 1]),
                     reads=[("rn_sd", c)], writes=[("rn_rs", c)])
                P.op("dve", lambda: nc.vector.scalar_tensor_tensor(out=xn[b][:], in0=xb[b][:], scalar=rs[:, c:c + 1],
                                                                   in1=gb[gname][:], op0=ALU.mult, op1=ALU.mult),
                     reads=[f"rn_x{b}", ("rn_rs", c), "g_" + gname], writes=[f"rn_xn{b}"])
                for kt in range(8):
                    f = lambda: nc.tensor.transpose(out=psb[:, kt * 128:(kt + 1) * 128],
                                                    in_=xn[b][:, kt * 128:(kt + 1) * 128], identity=ident)
                    if kt < 7:
                        P.quiet("pe", f, reads=[f"rn_xn{b}", "cstb"], writes=["psb"])
                    else:
                        P.op("pe", f, reads=[f"rn_xn{b}", "cstb"], writes=["psb"])
                P.op("act", lambda: nc.scalar.copy(out=dst[:, :, c * 128:(c + 1) * 128], in_=bview(psb[:], 8)),
                     reads=["psb"], writes=[("hT", c)])

        def load_x(c, buf, rname):
            P.dma("sp", rname, [(buf[:], x_d[c * 128:(c + 1) * 128, :])], writes=[rname])

        with contextlib.ExitStack() as st:
            load_gain("attn_norm", D, st)
            rmsnorm_T(load_x, "attn_norm", hT, st)
            P.barrier()

        if stage == 0:
            o = dout("hT_dbg", [128, 8, T], BF16)
            P.dma("sp", "out", [(o, hT[:])])
            P.barrier()
            return nc, dbg

        wst = [None]
        wbf = [None, None]
        wctr = [0]
        win_v = win_d.rearrange("(kt p) n -> p kt n", p=128)

        def alloc_w(st):
            wst[0] = sb("wst", [128, 8, 512], F32, st)
            wbf[0] = sb("wbf0", [128, 8, 512], BF16, st)
            wbf[1] = sb("wbf1", [128, 8, 512], BF16, st)

        def load_w(c0, ncols):
            i = wctr[0] % 2
            wctr[0] += 1
            P.dma("sp", "wst", [(wst[0][:, :, 0:ncols], win_v[:, :, c0:c0 + ncols])], writes=["wst"])
            P.op("pool", lambda: nc.gpsimd.tensor_copy(out=wbf[i][:, :, 0:ncols], in_=wst[0][:, :, 0:ncols]),
                 reads=["wst"], writes=[f"wbf{i}"])
            return i

        def proj_tm(c, wi, ncols, bank):
            for kt in range(8):
                f = lambda: nc.tensor.matmul(ps[bank][:, 0:ncols], lhsT=hT[:, kt, c * 128:(c + 1) * 128],
                                             rhs=wbf[wi][:, kt, 0:ncols], start=(kt == 0), stop=(kt == 7))
                if kt < 7:
                    P.quiet("pe", f, reads=[("hT", c), f"wbf{wi}"], writes=[f"ps{bank}"])
                else:
                    P.op("pe", f, reads=[("hT", c), f"wbf{wi}"], writes=[f"ps{bank}"])

        with contextlib.ExitStack() as st:
            yaT = sb("yaT", [128, 8, T], BF16, st)
            rope = sb("rope", [128, NT, 32], F32, st)
            P.dma("sp", "rope", [(rope[:], rope_d.rearrange("(c p) f -> p c f", p=128))], writes=["rope"])
            for n_ in ("q_norm", "k_norm", "idx_k_norm"):
                load_gain(n_, 64, st)
            qT = sb("qT", [128, 8, T], BF16, st)
            kT2 = sb("kT2", [128, 4, T], BF16, st)
            iqT = sb("iqT", [128, 4, T], BF16, st)
            ikT2 = sb("ikT2", [128, T], BF16, st)
            vaug = sb("vaug", [128, NT, 4, 66], BF16, st)
            iwa = sb("iwa", [128, NT, 8], F32, st)
            iws = sb("iws", [128, NT, 8], F32, st)
            P.op("pool", lambda: nc.gpsimd.memset(vaug[:], 1.0), writes=["vaug"])

            with contextlib.ExitStack() as st2:
                alloc_w(st2)
                sq = sb("e_sq", [128, 512], F32, st2)
                xn = sb("e_xn", [128, 512], F32, st2)
                ra = sb("e_ra", [128, 8, 16], F32, st2)
                rb = sb("e_rb", [128, 8, 16], F32, st2)
                s8 = [sb(f"e_s8{i}", [128, 8], F32, st2) for i in range(4)]
                tmbs = [sb(f"e_tmb{i}", [128, 512], BF16, st2) for i in range(2)]

                def epilogue(c, bank, nh, gname, prescale, dst_fn, dup):
                    pv = bview(ps[bank][:, 0:nh * 64], nh)
                    xv = bview(xn[:, 0:nh * 64], nh)
                    pr = f"ps{bank}"
                    if gname is not None:
                        P.op("act", lambda: nc.scalar.activation(out=sq[:, 0:nh * 64], in_=ps[bank][:, 0:nh * 64],
                                                                 func=AF.Square), reads=[pr], writes=["e_sq"])
                        P.op("dve", lambda: nc.vector.tensor_reduce(out=s8[0][:, 0:nh], in_=bview(sq[:, 0:nh * 64], nh),
                                                                    axis=AX.X, op=ALU.add), reads=["e_sq"], writes=["e_s80"])
                        P.op("dve", lambda: nc.vector.tensor_scalar(out=s8[1][:, 0:nh], in0=s8[0][:, 0:nh], scalar1=1.0 / 64,
                                                                    scalar2=EPS, op0=ALU.mult, op1=ALU.add),
                             reads=["e_s80"], writes=["e_s81"])
                        P.op("act", lambda: nc.scalar.activation(out=s8[2][:, 0:nh], in_=s8[1][:, 0:nh], func=AF.Sqrt),
                             reads=["e_s81"], writes=["e_s82"])
                        P.op("dve", lambda: nc.vector.reciprocal(out=s8[3][:, 0:nh], in_=s8[2][:, 0:nh]),
                             reads=["e_s82"], writes=["e_s83"])
                        P.op("dve", lambda: nc.vector.tensor_tensor(out=xv, in0=pv,
                                                                    in1=s8[3][:, 0:nh].unsqueeze(2).to_broadcast([128, nh, 64]),
                                                                    op=ALU.mult), reads=[pr, "e_s83"], writes=["e_xn"])
                        P.op("dve", lambda: nc.vector.tensor_tensor(out=xv, in0=xv,
                                                                    in1=gb[gname][:].unsqueeze(1).to_broadcast([128, nh, 64]),
                                                                    op=ALU.mult), reads=["e_xn", "g_" + gname], writes=["e_xn"])
                    elif prescale is not None:
                        P.op("dve", lambda: nc.vector.tensor_tensor(out=xv, in0=pv,
                                                                    in1=prescale.unsqueeze(2).to_broadcast([128, nh, 64]),
                                                                    op=ALU.mult), reads=[pr, ("iw", c)], writes=["e_xn"])
                    else:
                        P.op("dve", lambda: nc.vector.tensor_copy(out=xv, in_=pv), reads=[pr], writes=["e_xn"])
                    c16 = rope[:, c, 0:16].unsqueeze(1).to_broadcast([128, nh, 16])
                    nsn = rope[:, c, 16:24].unsqueeze(1).to_broadcast([128, nh, 8])
                    psn = rope[:, c, 24:32].unsqueeze(1).to_broadcast([128, nh, 8])
                    P.op("dve", lambda: nc.vector.tensor_tensor(out=ra[:, 0:nh, :], in0=xv[:, :, 0:16], in1=c16, op=ALU.mult),
                         reads=["e_xn", "rope"], writes=["e_ra"])
                    P.op("dve", lambda: nc.vector.tensor_tensor(out=rb[:, 0:nh, 0:8], in0=xv[:, :, 8:16], in1=nsn, op=ALU.mult),
                         reads=["e_xn", "rope"], writes=["e_rb0"])
                    P.op("dve", lambda: nc.vector.tensor_tensor(out=rb[:, 0:nh, 8:16], in0=xv[:, :, 0:8], in1=psn, op=ALU.mult),
                         reads=["e_xn", "rope"], writes=["e_rb1"])
                    P.op("dve", lambda: nc.vector.tensor_tensor(out=xv[:, :, 0:16], in0=ra[:, 0:nh, :], in1=rb[:, 0:nh, :],
                                                                op=ALU.add), reads=["e_ra", "e_rb0", "e_rb1"], writes=["e_xn"])
                    tmb = tmbs[c % 2]
                    tn0, tn1 = f"e_tmb{c % 2}_0", f"e_tmb{c % 2}_1"
                    if dup:
                        tv = tmb[:, 0:nh * 128].rearrange("p (h t d) -> p h t d", h=nh, t=2)
                        P.op("act", lambda: nc.scalar.copy(out=tv[:, :, 0, :], in_=xv), reads=["e_xn"], writes=[tn0])
                        P.op("act", lambda: nc.scalar.copy(out=tv[:, :, 1, :], in_=xv), reads=["e_xn"], writes=[tn1])
                        nblk = nh
                    else:
                        P.op("act", lambda: nc.scalar.copy(out=tmb[:, 0:nh * 64], in_=xn[:, 0:nh * 64]), reads=["e_xn"],
                             writes=[tn0, tn1])
                        nblk = nh // 2

                    def tail_():
                        for j in range(nblk):
                            f = lambda: nc.tensor.transpose(out=psb[:, j * 128:(j + 1) * 128], in_=tmb[:, j * 128:(j + 1) * 128],
                                                            identity=ident)
                            if j < nblk - 1:
                                P.quiet("pe", f, reads=[tn0, tn1, "cstb"], writes=["psb"])
                            else:
                                P.op("pe", f, reads=[tn0, tn1, "cstb"], writes=["psb"])
                        dst_fn(nblk)
                    return tail_

                pend = [None]

                def flush():
                    if pend[0]:
                        pend[0]()
                    pend[0] = None

                wi = load_w(O_IK, 72)
                for c in range(NT):
                    bank = c % 2
                    proj_tm(c, wi, 72, bank)
                    P.op("act", lambda: nc.scalar.activation(out=iwa[:, c, :], in_=ps[bank][:, 64:72], func=AF.Abs),
                         reads=[f"ps{bank}"], writes=[("iw", c)])
                    P.op("act", lambda: nc.scalar.activation(out=iws[:, c, :], in_=ps[bank][:, 64:72], func=AF.Sign),
                         reads=[f"ps{bank}"], writes=[("iws", c)])
                    t_ = epilogue(c, bank, 1, "idx_k_norm", None,
                                  lambda nblk, c=c: P.op("act", lambda: nc.scalar.copy(out=ikT2[:, c * 128:(c + 1) * 128],
                                                                                       in_=psb[:, 0:128]),
                                                         reads=["psb"], writes=[("ikT2", c)]), True)
                    if pend[0]:
                        pend[0]()
                    pend[0] = t_
                flush()
                wi = load_w(O_IQ, 512)
                for c in range(NT if sub >= 2 else 0):
                    bank = c % 2
                    proj_tm(c, wi, 512, bank)
                    t_ = epilogue(c, bank, 8, None, iwa[:, c, :],
                                  lambda nblk, c=c: P.op("act", lambda: nc.scalar.copy(out=iqT[:, :, c * 128:(c + 1) * 128],
                                                                                       in_=bview(psb[:, 0:512], 4)),
                                                         reads=["psb"], writes=[("iqT", c)]), False)
                    if pend[0]:
                        pend[0]()
                    pend[0] = t_
                flush()
                for half in range(2):
                    wi = load_w(O_Q + half * 512, 512)
                    for c in range(NT if sub >= 3 else 0):
                        bank = c % 2
                        proj_tm(c, wi, 512, bank)
                        t_ = epilogue(c, bank, 8, "q_norm", None,
                                      lambda nblk, c=c, half=half: P.op("act", lambda: nc.scalar.copy(
                                          out=qT[:, half * 4:half * 4 + 4, c * 128:(c + 1) * 128], in_=bview(psb[:, 0:512], 4)),
                                          reads=["psb"], writes=[("qT", c, half)]), False)
                        if pend[0]:
                            pend[0]()
                        pend[0] = t_
                    flush()
                wi = load_w(O_K, 512)
                for c in range(NT if sub >= 4 else 0):
                    bank = c % 2
                    proj_tm(c, wi, 512, bank)
                    if sub != 5:
                        P.op("act", lambda: nc.scalar.copy(out=vaug[:, c, :, 0:64], in_=bview(ps[bank][:, 256:512], 4)),
                             reads=[f"ps{bank}", "vaug"], writes=[("vaug", c)])
                    t_ = epilogue(c, bank, 4, "k_norm", None,
                                  lambda nblk, c=c: P.op("act", lambda: nc.scalar.copy(out=kT2[:, :, c * 128:(c + 1) * 128],
                                                                                       in_=bview(psb[:, 0:512], 4)),
                                                         reads=["psb"], writes=[("kT2", c)]), True)
                    if pend[0]:
                        pend[0]()
                    pend[0] = t_
                flush()
                P.barrier()

            if stage == 1:
                for nm, t_, shp in [("qT", qT, [128, 8, T]), ("kT2", kT2, [128, 4, T]), ("iqT", iqT, [128, 4, T]),
                                    ("ikT2", ikT2, [128, T])]:
                    o = dout(nm + "_dbg", shp, BF16)
                    P.dma("sp", "out", [(o, t_[:])])
                o = dout("vaug_dbg", [128, NT, 4, 66], BF16)
                P.dma("sp", "out", [(o, vaug[:])])
                o = dout("iwa_dbg", [128, NT, 8], F32)
                P.dma("sp", "out", [(o, iwa[:])])
                o = dout("iws_dbg", [128, NT, 8], F32)
                P.dma("sp", "out", [(o, iws[:])])
                P.barrier()
                return nc, dbg

            with contextlib.ExitStack() as st3:
                score = sb("score", [128, T], F32, st3)
                junk = sb("ajunk", [128, T], BF16, st3)
                maskb = [sb(f"maskb{i}", [128, T], BF16, st3) for i in range(2)]
                maskT = [sb(f"maskT{i}", [128, NT, 128], BF16, st3) for i in range(2)]
                relu = [sb(f"relu{i}", [128, 512], BF16, st3) for i in range(2)]
                diag = [sb(f"diag{i}", [128, 8, 128], BF16, st3) for i in range(2)]
                PT = [sb(f"PT{i}", [128, 512], BF16, st3) for i in range(3)]
                PTm = [sb(f"PTm{i}", [128, 512], BF16, st3) for i in range(3)]
                ytm = sb("ytm", [128, 1024], BF16, st3)
                hi = sb("b_hi", [128, 1], F32, st3)
                lo = sb("b_lo", [128, 1], F32, st3)
                w0 = sb("b_w0", [128, 1], F32, st3)
                wtab = sb("b_wtab", [128, NBIS], F32, st3)
                tt = sb("b_t", [128, 1], F32, st3)
                cnt = sb("b_cnt", [128, 1], F32, st3)
                uu = sb("b_u", [128, 1], F32, st3)
                thr = sb("b_thr", [128, 1], F32, st3)
                rcp = sb("b_rcp", [128, 8], F32, st3)
                pvc = [0]
                SB3 = [4, 5, 6]
                NQ = NT if sub >= 30 else max(0, sub - 10)

                def emit_scores(qi):
                    nkeys = 128 * (qi + 1)
                    qs = slice(qi * 128, (qi + 1) * 128)
                    dgt = diag[qi % 2]
                    for h in range(8):
                        P.op("dve", lambda: nc.vector.tensor_scalar(out=dgt[:, h, :], in0=ident, scalar1=iws[:, qi, h:h + 1],
                                                                    scalar2=None, op0=ALU.mult),
                             reads=["cstb"], writes=[(f"diag{qi % 2}", h)])
                    nkb = (nkeys + 511) // 512
                    for kb in range(nkb):
                        kw = min(512, nkeys - kb * 512)
                        for h in range(8):
                            hf, pr_ = h % 2, h // 2
                            rb = h % 2
                            P.op("pe", lambda: nc.tensor.matmul(ps[rb][:, 0:kw], lhsT=iqT[64 * hf:64 * hf + 64, pr_, qs],
                                                                rhs=ikT2[64 * hf:64 * hf + 64, kb * 512:kb * 512 + kw],
                                                                start=True, stop=True),
                                 writes=[f"ps{rb}"])
                            P.op("act", lambda: nc.scalar.activation(out=relu[rb][:, 0:kw], in_=ps[rb][:, 0:kw], func=AF.Relu),
                                 reads=[f"ps{rb}"], writes=[f"relu{rb}"])
                            f = lambda: nc.tensor.matmul(ps[2][:, 0:kw], lhsT=dgt[:, h, :], rhs=relu[rb][:, 0:kw],
                                                         start=(h == 0), stop=(h == 7))
                            if h < 7:
                                P.quiet("pe", f, reads=[f"relu{rb}", (f"diag{qi % 2}", h)], writes=["ps2"])
                            else:
                                P.op("pe", f, reads=[f"relu{rb}", (f"diag{qi % 2}", h)], writes=["ps2"])
                        c0 = kb * 512
                        last = (kb == nkb - 1)
                        nd = kw - 128 if last else kw
                        if nd > 0:
                            P.op("dve", lambda: nc.vector.tensor_copy(out=score[:, c0:c0 + nd], in_=ps[2][:, 0:nd]),
                                 reads=["ps2"], writes=[("score", kb)])
                        if last:
                            P.op("dve", lambda: nc.vector.tensor_tensor(out=score[:, nkeys - 128:nkeys], in0=ps[2][:, nd:nd + 128],
                                                                        in1=cst[:, C_DM:C_DM + 128], op=ALU.mult),
                                 reads=["ps2", "cst"], writes=[("score", "d")])
                            P.op("dve", lambda: nc.vector.tensor_tensor(out=score[:, nkeys - 128:nkeys],
                                                                        in0=score[:, nkeys - 128:nkeys],
                                                                        in1=cst[:, C_NB:C_NB + 128], op=ALU.add),
                                 reads=[("score", "d"), "cst"], writes=[("score", "d")])

                def search_steps(qi):
                    nkeys = 128 * (qi + 1)
                    nkb = (nkeys + 511) // 512
                    sres = [("score", kb) for kb in range(nkb)] + [("score", "d")]
                    mb = maskb[qi % 2]
                    steps = []

                    def s0():
                        P.op("dve", lambda: nc.vector.tensor_reduce(out=hi[:], in_=score[:, 0:nkeys], axis=AX.X, op=ALU.max),
                             reads=sres, writes=["b_hi"])
                        P.op("dve", lambda: nc.vector.tensor_reduce(out=lo[:], in_=score[:, 0:nkeys - 128], axis=AX.X, op=ALU.min),
                             reads=sres, writes=["b_lo"])
                        P.op("dve", lambda: nc.vector.tensor_tensor(out=w0[:], in0=hi[:], in1=lo[:], op=ALU.subtract),
                             reads=["b_hi", "b_lo"], writes=["b_w0"])
                        P.op("dve", lambda: nc.vector.tensor_scalar(out=wtab[:], in0=cst[:, C_BIS:C_BIS + NBIS], scalar1=w0[:, 0:1],
                                                                    scalar2=None, op0=ALU.mult),
                             reads=["b_w0", "cst"], writes=["b_wtab"])
                        P.op("dve", lambda: nc.vector.tensor_tensor(out=tt[:], in0=lo[:], in1=wtab[:, 0:1], op=ALU.add),
                             reads=["b_lo", "b_wtab"], writes=["b_t"])
                    steps.append(s0)
                    for it in range(NBIS):
                        def si(it=it):
                            P.op("dve", lambda: nc.vector.tensor_scalar(out=junk[:, 0:nkeys], in0=score[:, 0:nkeys], scalar1=tt[:, 0:1],
                                                                        scalar2=None, op0=ALU.is_ge, op1=ALU.add, accum_out=cnt[:]),
                                 reads=sres + ["b_t"], writes=["ajunk", "b_cnt"])
                            P.op("dve", lambda: nc.vector.tensor_scalar(out=uu[:], in0=cnt[:], scalar1=256.0, scalar2=-0.5,
                                                                        op0=ALU.is_ge, op1=ALU.add),
                                 reads=["b_cnt"], writes=["b_u"])
                            P.op("dve", lambda: nc.vector.scalar_tensor_tensor(out=tt[:], in0=uu[:], scalar=wtab[:, it:it + 1],
                                                                               in1=tt[:], op0=ALU.mult, op1=ALU.add),
                                 reads=["b_u", "b_wtab", "b_t"], writes=["b_t"])
                        steps.append(si)

                    def sf():
                        P.op("dve", lambda: nc.vector.scalar_tensor_tensor(out=thr[:], in0=wtab[:, NBIS - 1:NBIS], scalar=-0.5,
                                                                           in1=tt[:], op0=ALU.mult, op1=ALU.add),
                             reads=["b_wtab", "b_t"], writes=["b_thr"])
                        P.op("dve", lambda: nc.vector.tensor_scalar(out=mb[:, 0:nkeys], in0=score[:, 0:nkeys], scalar1=thr[:, 0:1],
                                                                    scalar2=None, op0=ALU.is_ge),
                             reads=sres + ["b_thr"], writes=[f"maskb{qi % 2}"])
                    steps.append(sf)
                    return steps

                def const_mask(qi):
                    nkeys = 128 * (qi + 1)
                    mb = maskb[qi % 2]
                    if qi == 1:
                        P.op("pool", lambda: nc.gpsimd.tensor_copy(out=mb[:, 0:128], in_=cstb[:, C_ONE:C_ONE + 128]),
                             reads=["cstb"], writes=[f"maskb{qi % 2}"])
                    P.op("pool", lambda: nc.gpsimd.tensor_copy(out=mb[:, nkeys - 128:nkeys], in_=cstb[:, C_DM:C_DM + 128]),
                         reads=["cstb"], writes=[f"maskb{qi % 2}"])

                def emit_maskT(qi):
                    nk = qi + 1
                    mb = maskb[qi % 2]
                    mT = maskT[qi % 2]
                    for k0 in range(0, nk, 8):
                        n = min(8, nk - k0)
                        for j in range(n):
                            f = lambda: nc.tensor.transpose(out=psb[:, j * 128:(j + 1) * 128],
                                                            in_=mb[:, (k0 + j) * 128:(k0 + j + 1) * 128], identity=ident)
                            if j < n - 1:
                                P.quiet("pe", f, reads=[f"maskb{qi % 2}", "cstb"], writes=["psb"])
                            else:
                                P.op("pe", f, reads=[f"maskb{qi % 2}", "cstb"], writes=["psb"])
                        P.op("act", lambda: nc.scalar.copy(out=mT[:, k0:k0 + n, :], in_=bview(psb[:, 0:n * 128], n)),
                             reads=["psb"], writes=[(f"maskT{qi % 2}", k0 // 8)])

                SBK = [4, 5, 6, 0, 1]
                LOOK = 2

                def attention_tile(qi, steps):
                    nk = qi + 1
                    qs = slice(qi * 128, (qi + 1) * 128)
                    mT = maskT[qi % 2]
                    seq = [(g, kj) for g in range(4) for kj in range(nk)]
                    nseq = len(seq)
                    info = {}
                    stq = list(steps)
                    every = max(1, nseq // max(1, len(stq))) if stq else 0

                    def front(i):
                        g, kj = seq[i]
                        ks = slice(kj * 128, (kj + 1) * 128)
                        n_ = pvc[0]
                        pvc[0] += 1
                        par = n_ % 3
                        sa, sb_ = SBK[(2 * n_) % 5], SBK[(2 * n_ + 1) % 5]
                        info[i] = par
                        P.op("pe", lambda: nc.tensor.matmul(bview(ps[sa][:, 0:256], 2), lhsT=kT2[0:64, g, ks],
                                                            rhs=qT[0:64, 2 * g:2 * g + 2, qs], start=True, stop=True),
                             writes=[f"ps{sa}"])
                        P.op("pe", lambda: nc.tensor.matmul(bview(ps[sb_][:, 0:256], 2), lhsT=kT2[64:128, g, ks],
                                                            rhs=qT[64:128, 2 * g:2 * g + 2, qs], start=True, stop=True),
                             writes=[f"ps{sb_}"])
                        P.op("act", lambda: nc.scalar.activation(out=PT[par][:, 0:256], in_=ps[sa][:, 0:256], func=AF.Exp,
                                                                 scale=0.125), reads=[f"ps{sa}"], writes=[("PT", par, 0)])
                        P.op("act", lambda: nc.scalar.activation(out=PT[par][:, 256:512], in_=ps[sb_][:, 0:256], func=AF.Exp,
                                                                 scale=0.125), reads=[f"ps{sb_}"], writes=[("PT", par, 1)])
                        me = "dve" if (n_ % 3 == 2) else "pool"
                        P.op(me, lambda: P.e[me].tensor_tensor(out=bview(PTm[par][:], 4), in0=bview(PT[par][:], 4),
                                                               in1=mT[:, kj, :].unsqueeze(1).to_broadcast([128, 4, 128]),
                                                               op=ALU.mult),
                             reads=[("PT", par, 0), ("PT", par, 1), (f"maskT{qi % 2}", kj // 8)], writes=[("PTm", par)])

                    def back(i):
                        g, kj = seq[i]
                        par = info[i]
                        ob = 2 + g % 2
                        for j in range(4):
                            hl = [0, 2, 1, 3][j]
                            f = lambda: nc.tensor.matmul(ps[ob][:, hl * 65:hl * 65 + 65], lhsT=PTm[par][:, j * 128:(j + 1) * 128],
                                                         rhs=vaug[:, kj, g, 0:65], start=(kj == 0 and j == 0),
                                                         stop=(kj == nk - 1 and j == 3), skip_group_check=True)
                            if j < 3:
                                P.quiet("pe", f, reads=[("PTm", par)], writes=[f"ps{ob}"])
                            else:
                                P.op("pe", f, reads=[("PTm", par)], writes=[f"ps{ob}"])
                        if kj == nk - 1:
                            ov = ps[ob][:, 0:260].rearrange("p (h d) -> p h d", h=4)
                            P.op("dve", lambda: nc.vector.reciprocal(out=rcp[:, 4 * (g % 2):4 * (g % 2) + 4], in_=ov[:, :, 64]),
                                 reads=[f"ps{ob}"], writes=[("b_rcp", g % 2)])
                            for hl in range(4):
                                hh = 4 * g + hl
                                P.op("act", lambda: nc.scalar.activation(out=ytm[:, hh * 64:(hh + 1) * 64],
                                                                         in_=ps[ob][:, hl * 65:hl * 65 + 64], func=AF.Copy,
                                                                         scale=rcp[:, 4 * (g % 2) + hl:4 * (g % 2) + hl + 1]),
                                     reads=[f"ps{ob}", ("b_rcp", g % 2)], writes=[("ytm", hh)])

                    for i in range(nseq + LOOK):
                        if i < nseq:
                            front(i)
                        if i >= LOOK:
                            back(i - LOOK)
                        if stq and (i % every == every - 1):
                            stq.pop(0)()
                    while stq:
                        stq.pop(0)()

                if NQ > 0:
                    const_mask(0)
                    emit_maskT(0)
                for qi in range(NQ):
                    nxt = qi + 1
                    steps = []
                    if nxt < NQ:
                        if nxt >= 2:
                            emit_scores(nxt)
                            steps = search_steps(nxt)
                        else:
                            const_mask(nxt)
                    attention_tile(qi, steps)
                    if nxt < NQ:
                        emit_maskT(nxt)
                    qs = slice(qi * 128, (qi + 1) * 128)
                    for j in range(8):
                        f = lambda: nc.tensor.transpose(out=psb[:, j * 128:(j + 1) * 128], in_=ytm[:, j * 128:(j + 1) * 128],
                                                        identity=ident)
                        if j < 7:
                            P.quiet("pe", f, reads=[("ytm", hh_) for hh_ in range(16)] + ["cstb"], writes=["psb"])
                        else:
                            P.op("pe", f, reads=[("ytm", hh_) for hh_ in range(16)] + ["cstb"], writes=["psb"])
                    P.op("act", lambda: nc.scalar.copy(out=yaT[:, :, qs], in_=bview(psb[:], 8)), reads=["psb"], writes=[("yaT", qi)])
                P.barrier()
            if stage == 2:
                o = dout("yaT_dbg", [128, 8, T], BF16)
                P.dma("sp", "out", [(o, yaT[:])])
                P.barrier()
                return nc, dbg
            P.dma("sp", "spill", [(ya_spill, yaT[:])])
            P.barrier()

        stB = contextlib.ExitStack()
        es.enter_context(stB)
        ysT = sb("ysT", [128, 16, T], BF16, stB)
        with contextlib.ExitStack() as sS:
            G8 = lambda t_, c_, g_: t_[:, c_, 8 * g_:8 * g_ + 8].unsqueeze(2).to_broadcast([128, 8, 64])
            wstS = sb("wstS", [128, 8, 256], F32, sS)
            selb = sb("selb", [128, 32, 128], BF16, sS)
            P.op("pool", lambda: nc.gpsimd.memset(selb[:], 0.0), writes=["selb"])
            for r3 in range(3):
                P.op("pool", lambda: nc.gpsimd.tensor_copy(
                    out=selb[32 * r3:32 * r3 + 32, :, :],
                    in_=cstb[32 * r3:32 * r3 + 32, C_ID + 32 * r3:C_ID + 32 * r3 + 32].unsqueeze(2).to_broadcast([32, 32, 128])),
                    reads=["cstb", "selb"], writes=["selb"])
            if sub == 101:
                P.barrier(); return nc, dbg
            for n_ in ("dt_bias", "a_log", "d_skip"):
                load_gain(n_, 32, sS)
            aneg = sb("aneg", [128, 32], F32, sS)
            P.op("act", lambda: nc.scalar.activation(out=aneg[:], in_=gb["a_log"][:], func=AF.Exp), reads=["g_a_log"], writes=["aneg"])
            P.op("dve", lambda: nc.vector.tensor_scalar(out=aneg[:], in0=aneg[:], scalar1=-1.0, scalar2=None, op0=ALU.mult),
                 reads=["aneg"], writes=["aneg"])
            if sub == 102:
                P.barrier(); return nc, dbg
            cwfm = sb("cwfm", [128, 24, 5], F32, sS)
            s0 = contextlib.ExitStack()
            cw5 = sb("cw5", [5, 3072], F32, s0)
            P.dma("sp", "cw5", [(cw5[0:4, :], convw_d), (cw5[4:5, :], gains["conv_b"])], writes=["cw5"])
            for t_ in range(24):
                f = lambda: nc.tensor.transpose(out=ps[0][:, t_ * 5:t_ * 5 + 5], in_=cw5[:, t_ * 128:(t_ + 1) * 128],
                                                identity=cst[0:5, C_ID:C_ID + 5])
                if t_ < 23:
                    P.quiet("pe", f, reads=["cw5", "cst"], writes=["ps0"])
                else:
                    P.op("pe", f, reads=["cw5", "cst"], writes=["ps0"])
            P.op("dve", lambda: nc.vector.tensor_copy(out=cwfm[:], in_=bview(ps[0][:, 0:120], 24)), reads=["ps0"], writes=["cwfm"])
            P.barrier()
            s0.close()
            if sub == 103:
                P.barrier(); return nc, dbg
            dt_all = sb("dt_all", [128, NT, 32], F32, sS)
            acs = sb("acs", [128, NT, 32], F32, sS)
            ea = sb("ea", [128, NT, 32], F32, sS)
            dtw = sb("dtw", [128, NT, 32], F32, sS)
            cdb = sb("cdb", [128, NT, 32], F32, sS)
            A3 = sb("A3", [128, NT, 128], BF16, sS)
            P.op("pool", lambda: nc.gpsimd.memset(A3[:], 0.0), writes=["A3z"])
            with contextlib.ExitStack() as s1:
                wdt_s = sb("wdt_s", [128, 8, 32], F32, s1)
                wdt = sb("wdt", [128, 8, 32], BF16, s1)
                P.dma("sp", "wdt", [(wdt_s[:], win_v[:, :, O_DT:O_DT + 32])], writes=["wdt_s"])
                P.op("pool", lambda: nc.gpsimd.tensor_copy(out=wdt[:], in_=wdt_s[:]), reads=["wdt_s"], writes=["wdt"])
                f32t = [sb(f"s1_{i}", [128, 32], F32, s1) for i in range(6)]
                a3 = sb("s1_a3", [128, 3, 32], F32, s1)
                Hb = sb("s1_Hb", [128, 128], BF16, s1)
                Mb = sb("s1_Mb", [128, 128], BF16, s1)
                r1 = sb("s1_r1", [128, 128], F32, s1)
                r2 = sb("s1_r2", [128, 128], F32, s1)
                ones_f = cst[:, C_ONE:C_ONE + 128]
                uinc = cst[:, C_CT:C_CT + 128]
                for c in range(NT):
                    cs_ = slice(c * 128, (c + 1) * 128)
                    xd, ax, ee, ll, rr, aa = f32t
                    for kt in range(8):
                        f = lambda: nc.tensor.matmul(ps[1][:, 0:32], lhsT=hT[:, kt, cs_], rhs=wdt[:, kt, :], start=(kt == 0), stop=(kt == 7))
                        if kt < 7:
                            P.quiet("pe", f, reads=["wdt"], writes=["ps1"])
                        else:
                            P.op("pe", f, reads=["wdt"], writes=["ps1"])
                    P.op("dve", lambda: nc.vector.tensor_tensor(out=xd[:], in0=ps[1][:, 0:32], in1=gb["dt_bias"][:], op=ALU.add),
                         reads=["ps1", "g_dt_bias"], writes=["s1_xd"])
                    P.op("act", lambda: nc.scalar.activation(out=ax[:], in_=xd[:], func=AF.Abs), reads=["s1_xd"], writes=["s1_ax"])
                    P.op("act", lambda: nc.scalar.activation(out=ee[:], in_=ax[:], func=AF.Exp, scale=-1.0), reads=["s1_ax"], writes=["s1_ee"])
                    P.op("act", lambda: nc.scalar.activation(out=ll[:], in_=ee[:], func=AF.Ln, bias=1.0), reads=["s1_ee"], writes=["s1_ll"])
                    P.op("dve", lambda: nc.vector.tensor_scalar(out=rr[:], in0=xd[:], scalar1=0.0, scalar2=None, op0=ALU.max),
                         reads=["s1_xd"], writes=["s1_rr"])
                    P.op("dve", lambda: nc.vector.tensor_tensor(out=dt_all[:, c, :], in0=rr[:], in1=ll[:], op=ALU.add),
                         reads=["s1_rr", "s1_ll"], writes=[("dt", c)])
                    P.op("dve", lambda: nc.vector.tensor_tensor(out=aa[:], in0=dt_all[:, c, :], in1=aneg[:], op=ALU.mult),
                         reads=[("dt", c), "aneg"], writes=["s1_aa"])
                    if sub == 104:
                        P.barrier(); return nc, dbg
                    P.op("dve", lambda: nc.vector.tensor_copy(out=a3[:], in_=aa[:].unsqueeze(1).to_broadcast([128, 3, 32])),
                         reads=["s1_aa"], writes=["s1_a3"])
                    if sub == 105:
                        P.barrier(); return nc, dbg
                    P.op("pe", lambda: nc.tensor.matmul(ps[2][:, 0:32], lhsT=uinc, rhs=aa[:], start=True, stop=True),
                         reads=["s1_aa", "cst"], writes=["ps2"])
                    P.op("pe", lambda: nc.tensor.matmul(ps[3][:, 0:32], lhsT=ones_f, rhs=aa[:], start=True, stop=True),
                         reads=["s1_aa", "cst"], writes=["ps3"])
                    P.op("pe", lambda: nc.tensor.matmul(ps[4][0:96, 0:128], lhsT=a3[:].rearrange("p a b -> p (a b)"), rhs=uinc,
                                                        start=True, stop=True),
                         reads=["s1_a3", "cst"], writes=["ps4"])
                    if sub == 106:
                        P.barrier(); return nc, dbg
                    P.op("dve", lambda: nc.vector.tensor_copy(out=acs[:, c, :], in_=ps[2][:, 0:32]), reads=["ps2"], writes=[("acs", c)])
                    if sub == 108:
                        P.barrier(); return nc, dbg
                    P.op("act", lambda: nc.scalar.activation(out=ea[:, c, :], in_=acs[:, c, :], func=AF.Exp), reads=[("acs", c)], writes=[("ea", c)])
                    P.op("dve", lambda: nc.vector.tensor_copy(out=rr[:], in_=ps[3][:, 0:32]), reads=["ps3"], writes=["s1_rr"])
                    P.op("act", lambda: nc.scalar.activation(out=cdb[:, c, :], in_=rr[:], func=AF.Exp), reads=["s1_rr"], writes=[("cdb", c)])
                    if sub == 109:
                        P.barrier(); return nc, dbg
                    P.op("dve", lambda: nc.vector.tensor_tensor(out=xd[:], in0=rr[:], in1=acs[:, c, :], op=ALU.subtract),
                         reads=["s1_rr", ("acs", c)], writes=["s1_xd"])
                    P.op("act", lambda: nc.scalar.activation(out=ee[:], in_=xd[:], func=AF.Exp), reads=["s1_xd"], writes=["s1_ee"])
                    P.op("dve", lambda: nc.vector.tensor_tensor(out=dtw[:, c, :], in0=dt_all[:, c, :], in1=ee[:], op=ALU.mult),
                         reads=[("dt", c), "s1_ee"], writes=[("dtw", c)])
                    if sub == 107:
                        P.barrier(); return nc, dbg
                    P.op("act", lambda: nc.scalar.copy(out=Hb[0:96, :], in_=ps[4][0:96, 0:128]), reads=["ps4"], writes=["s1_Hb"])
                    P.op("dve", lambda: nc.vector.tensor_tensor(out=r1[0:96, :], in0=ps[4][0:96, 0:128], in1=Hb[0:96, :], op=ALU.subtract),
                         reads=["ps4", "s1_Hb"], writes=["s1_r1"])
                    P.op("act", lambda: nc.scalar.copy(out=Mb[0:96, :], in_=r1[0:96, :]), reads=["s1_r1"], writes=["s1_Mb"])
                    P.op("dve", lambda: nc.vector.tensor_tensor(out=r2[0:96, :], in0=r1[0:96, :], in1=Mb[0:96, :], op=ALU.subtract),
                         reads=["s1_r1", "s1_Mb"], writes=["s1_r2"])
                    P.op("pool", lambda: nc.gpsimd.tensor_copy(out=A3[0:32, c, :], in_=Hb[0:32, :]), reads=["s1_Hb", "A3z"], writes=[("A3", c, 0)])
                    P.op("pool", lambda: nc.gpsimd.tensor_copy(out=A3[32:64, c, :], in_=Mb[32:64, :]), reads=["s1_Mb", "A3z"], writes=[("A3", c, 1)])
                    P.op("act", lambda: nc.scalar.copy(out=A3[64:96, c, :], in_=r2[64:96, :]), reads=["s1_r2", "A3z"], writes=[("A3", c, 2)])
                P.barrier()
            if stage == 3 and sub == 1:
                for nm, t_ in [("dt_all", dt_all), ("acs", acs), ("ea", ea), ("dtw", dtw), ("cdb", cdb)]:
                    o = dout(nm + "_dbg", [128, NT, 32], F32)
                    P.dma("sp", "out", [(o, t_[:])])
                o = dout("A3_dbg", [128, NT, 128], BF16)
                P.dma("sp", "out", [(o, A3[:])])
                o = dout("cwfm_dbg", [128, 24, 5], F32)
                P.dma("sp", "out", [(o, cwfm[:])])
                o = dout("selb_dbg", [128, 32, 128], BF16)
                P.dma("sp", "out", [(o, selb[:])])
                P.barrier()
                return nc, dbg

            xs_tm = sb("xs_tm", [128, NT, 512], BF16, sS)
            BT = sb("BT", [128, T], BF16, sS)
            CT = sb("CT", [128, T], BF16, sS)
            B_tm = sb("B_tm", [128, NT, 128], BF16, sS)
            rawb = sb("rawb", [128, T + 4], BF16, sS)
            xcf = [sb(f"xcf{i}", [128, 512], BF16, sS) for i in range(2)]
            dg = [sb(f"dg{i}", [128, 4, 128], BF16, sS) for i in range(2)]
            wch = [sb(f"wch{i}", [128, 8, 128], BF16, sS) for i in range(2)]
            wz = sb("wz", [128, 8, 512], BF16, sS)
            ssdg = sb("ssdg", [128, 512], F32, sS)
            hst = sb("hst", [128, 512], F32, sS)
            hstb = sb("hstb", [128, 512], BF16, sS)
            cbm = sb("cbm", [128, 128], F32, sS)
            seg = [sb(f"seg{i}", [128, 512], F32, sS) for i in range(2)]
            Ee = seg
            MT = [sb(f"MT{i}", [128, 512], BF16, sS) for i in range(4)]
            xdt = [sb(f"xdt{i}", [128, 512], BF16, sS) for i in range(2)]
            xw = [sb(f"xw{i}", [128, 512], BF16, sS) for i in range(2)]
            t1 = sb("t1", [128, 512], F32, sS)
            t1b = [t1, sb("t1b", [128, 512], F32, sS)]
            dsk = sb("dsk", [128, 8, 128], BF16, sS)
            epsb = sb("epsb", [128, 1], F32, sS)
            P.op("pool", lambda: nc.gpsimd.memset(epsb[:], EPS), writes=["epsb"])
            t3 = sb("t3", [128, 512], F32, sS)
            yv = t1
            sz = t3
            ynb = [sb(f"ynb{i}", [128, 512], BF16, sS) for i in range(2)]
            sjk = xcf[0]
            g1 = [sb(f"g1_{i}", [128, 2], F32, sS) for i in range(4)]
            P.op("pool", lambda: nc.gpsimd.memset(rawb[:, 0:4], 0.0), writes=["rawb_halo"])
            wctr2 = [0]
            for g in range(4 if sub >= 30 else 1):
                for hf in range(2):
                    c0 = O_Z + g * 512 + hf * 256
                    P.dma("sp", "wstS", [(wstS[:], win_v[:, :, c0:c0 + 256])], writes=["wstS"])
                    P.op("pool", lambda: nc.gpsimd.tensor_copy(out=wz[:, :, hf * 256:(hf + 1) * 256], in_=wstS[:]),
                         reads=["wstS"], writes=[("wz", hf)])
                P.dma("sp", "ssdg", [(ssdg[:], gains["ssd_norm"][:, g * 512:(g + 1) * 512].partition_broadcast(128))], writes=["ssdg"])
                chts = [(O_XBC + g * 512 + j * 128, 4 * g + j, "x", j) for j in range(4)]
                chts += [(O_XBC + 2048 + g * 128, 16 + g, "B", 0), (O_XBC + 2560 + g * 128, 20 + g, "C", 0)]
                for (c0, cti, kind, j) in chts:
                    wi = wctr2[0] % 2
                    wctr2[0] += 1
                    P.dma("sp", "wstS", [(wstS[:, :, 0:128], win_v[:, :, c0:c0 + 128])], writes=["wstS"])
                    P.op("pool", lambda: nc.gpsimd.tensor_copy(out=wch[wi][:], in_=wstS[:, :, 0:128]), reads=["wstS"], writes=[f"wch{wi}"])
                    for jj in range(4):
                        P.op("dve", lambda: nc.vector.tensor_scalar(out=dg[wi][:, jj, :], in0=ident, scalar1=cwfm[:, cti, jj:jj + 1],
                                                                    scalar2=None, op0=ALU.mult),
                             reads=["cstb", "cwfm"], writes=[(f"dg{wi}", jj)])
                    for tb in range(4):
                        bank = tb % 2
                        for kt in range(8):
                            f = lambda: nc.tensor.matmul(ps[bank][:], lhsT=wch[wi][:, kt, :], rhs=hT[:, kt, tb * 512:(tb + 1) * 512],
                                                         start=(kt == 0), stop=(kt == 7))
                            if kt < 7:
                                P.quiet("pe", f, reads=[f"wch{wi}"], writes=[f"ps{bank}"])
                            else:
                                P.op("pe", f, reads=[f"wch{wi}"], writes=[f"ps{bank}"])
                        P.op("act", lambda: nc.scalar.copy(out=rawb[:, 4 + tb * 512:4 + (tb + 1) * 512], in_=ps[bank][:]),
                             reads=[f"ps{bank}"], writes=[("rawb", tb)])
                    for tb in range(4):
                        bank = 2 + tb % 2
                        for jj in range(4):
                            f = lambda: nc.tensor.matmul(ps[bank][:], lhsT=dg[wi][:, jj, :],
                                                         rhs=rawb[:, 1 + tb * 512 + jj:1 + tb * 512 + jj + 512],
                                                         start=(jj == 0), stop=(jj == 3))
                            rd = [(f"dg{wi}", jj), ("rawb", tb), "rawb_halo"] + ([("rawb", tb - 1)] if tb > 0 else [])
                            if jj < 3:
                                P.quiet("pe", f, reads=rd, writes=[f"ps{bank}"])
                            else:
                                P.op("pe", f, reads=rd, writes=[f"ps{bank}"])
                        if kind == "x":
                            xb_ = tb % 2
                            P.op("act", lambda: nc.scalar.activation(out=xcf[xb_][:], in_=ps[bank][:], func=AF.Silu,
                                                                     bias=cwfm[:, cti, 4:5]),
                                 reads=[f"ps{bank}", "cwfm"], writes=[f"xcf{xb_}"])
                            for i4 in range(4):
                                f = lambda: nc.tensor.transpose(out=psb[:, i4 * 128:(i4 + 1) * 128], in_=xcf[xb_][:, i4 * 128:(i4 + 1) * 128],
                                                                identity=ident)
                                if i4 < 3:
                                    P.quiet("pe", f, reads=[f"xcf{xb_}", "cstb"], writes=["psb"])
                                else:
                                    P.op("pe", f, reads=[f"xcf{xb_}", "cstb"], writes=["psb"])
                            P.op("act", lambda: nc.scalar.copy(out=xs_tm[:, tb * 4:(tb + 1) * 4, j * 128:(j + 1) * 128],
                                                               in_=bview(psb[:, 0:512], 4)),
                                 reads=["psb"], writes=[("xs_tm", tb, j)])
                        else:
                            dstT = BT if kind == "B" else CT
                            P.op("act", lambda: nc.scalar.activation(out=dstT[:, tb * 512:(tb + 1) * 512], in_=ps[bank][:], func=AF.Silu,
                                                                     bias=cwfm[:, cti, 4:5]),
                                 reads=[f"ps{bank}", "cwfm"], writes=[(kind + "T", tb)])
                if sub == 202:
                    P.barrier(); return nc, dbg
                for k0 in range(0, NT, 8):
                    for jj in range(8):
                        cc = k0 + jj
                        f = lambda: nc.tensor.transpose(out=psb[:, jj * 128:(jj + 1) * 128], in_=BT[:, cc * 128:(cc + 1) * 128], identity=ident)
                        if jj < 7:
                            P.quiet("pe", f, reads=[("BT", cc // 4), "cstb"], writes=["psb"])
                        else:
                            P.op("pe", f, reads=[("BT", cc // 4), "cstb"], writes=["psb"])
                    P.op("act", lambda: nc.scalar.copy(out=B_tm[:, k0:k0 + 8, :], in_=bview(psb[:], 8)), reads=["psb"], writes=[("B_tm", k0 // 8)])
                for hl_ in range(8):
                    P.op("dve", lambda: nc.vector.tensor_scalar(out=dsk[:, hl_, :], in0=ident, scalar1=gb["d_skip"][:, 8 * g + hl_:8 * g + hl_ + 1],
                                                                scalar2=None, op0=ALU.mult),
                         reads=["cstb", "g_d_skip"], writes=["dsk"])
                P.op("pool", lambda: nc.gpsimd.memset(hst[:], 0.0), writes=["hst"])
                P.op("pool", lambda: nc.gpsimd.memset(hstb[:], 0.0), writes=["hstb"])
                if sub == 203:
                    P.barrier(); return nc, dbg
                xsr = lambda c_: [("xs_tm", c_ // 4, j_) for j_ in range(4)]
                def front(c):
                    cs_ = slice(c * 128, (c + 1) * 128)
                    pb = c % 2
                    P.op("pe", lambda: nc.tensor.matmul(ps[0][:, 0:128], lhsT=BT[:, cs_], rhs=CT[:, cs_], start=True, stop=True),
                         reads=[("BT", c // 4), ("CT", c // 4)], writes=["ps0"])
                    P.op("dve", lambda: nc.vector.tensor_tensor(out=cbm[:], in0=ps[0][:, 0:128], in1=cst[:, C_CT:C_CT + 128], op=ALU.mult),
                         reads=["ps0", "cst"], writes=["cbm"])
                    P.op("pool", lambda: nc.gpsimd.tensor_tensor(out=bview(xdt[pb][:], 8), in0=bview(xs_tm[:, c, :], 8), in1=G8(dt_all, c, g),
                                                                 op=ALU.mult), reads=xsr(c), writes=[f"xdt{pb}"])
                    P.op("pool", lambda: nc.gpsimd.tensor_tensor(out=bview(xw[pb][:], 8), in0=bview(xs_tm[:, c, :], 8), in1=G8(dtw, c, g),
                                                                 op=ALU.mult), reads=xsr(c), writes=[f"xw{pb}"])
                    for hb in range(2):
                        bcb = 1 + hb
                        for hh in range(4):
                            h = 8 * g + 4 * hb + hh
                            f = lambda: nc.tensor.matmul(ps[bcb][:, hh * 128:(hh + 1) * 128], lhsT=selb[:, h, :], rhs=A3[:, c, :],
                                                         start=True, stop=True, skip_group_check=True)
                            if hh < 3:
                                P.quiet("pe", f, reads=["selb"], writes=[f"ps{bcb}"])
                            else:
                                P.op("pe", f, reads=["selb"], writes=[f"ps{bcb}"])
                    for hb in range(2):
                        bcb = 1 + hb
                        for hh in range(4):
                            h = 8 * g + 4 * hb + hh
                            P.op("dve", lambda: nc.vector.tensor_scalar(out=seg[hb][:, hh * 128:(hh + 1) * 128],
                                                                        in0=ps[bcb][:, hh * 128:(hh + 1) * 128],
                                                                        scalar1=acs[:, c, h:h + 1], scalar2=0.0, op0=ALU.subtract, op1=ALU.min),
                                 reads=[f"ps{bcb}"], writes=[(f"seg{hb}", hh), f"Ee{hb}"])
                        P.op("act", lambda: nc.scalar.activation(out=Ee[hb][:], in_=seg[hb][:], func=AF.Exp),
                             reads=[(f"seg{hb}", hh_) for hh_ in range(4)], writes=[f"Ee{hb}"] + [(f"seg{hb}", hh_) for hh_ in range(4)])
                    for hb in range(2):
                        mi = 2 * pb + hb
                        P.op("dve", lambda: nc.vector.tensor_tensor(out=bview(MT[mi][:], 4), in0=bview(Ee[hb][:], 4),
                                                                    in1=cbm[:].unsqueeze(1).to_broadcast([128, 4, 128]), op=ALU.mult),
                             reads=[f"Ee{hb}", "cbm"], writes=[f"MT{mi}"])

                def back(c):
                    cs_ = slice(c * 128, (c + 1) * 128)
                    pb = c % 2
                    P.op("pe", lambda: nc.tensor.matmul(ps[5][:], lhsT=B_tm[:, c, :], rhs=xw[pb][:], start=True, stop=True),
                         reads=[("B_tm", c // 8), f"xw{pb}"], writes=["ps5"])
                    for hb in range(2):
                        mi = 2 * pb + hb
                        for hh in range(4):
                            hl = 4 * hb + hh
                            P.quiet("pe", lambda: nc.tensor.matmul(ps[3][:, hl * 64:(hl + 1) * 64], lhsT=MT[mi][:, hh * 128:(hh + 1) * 128],
                                                                   rhs=xdt[pb][:, hl * 64:(hl + 1) * 64], start=True, stop=False, skip_group_check=True),
                                    reads=[f"MT{mi}", f"xdt{pb}"], writes=["ps3"])
                            f = lambda: nc.tensor.matmul(ps[3][:, hl * 64:(hl + 1) * 64], lhsT=dsk[:, hl, :],
                                                         rhs=xs_tm[:, c, hl * 64:(hl + 1) * 64], start=False, stop=True, skip_group_check=True)
                            if hl < 7:
                                P.quiet("pe", f, reads=["dsk"] + xsr(c), writes=["ps3"])
                            else:
                                P.op("pe", f, reads=["dsk"] + xsr(c), writes=["ps3"])
                    for kt in range(8):
                        f = lambda: nc.tensor.matmul(ps[6][:], lhsT=hT[:, kt, cs_], rhs=wz[:, kt, :], start=(kt == 0), stop=(kt == 7))
                        if kt < 7:
                            P.quiet("pe", f, reads=[("wz", 0), ("wz", 1)], writes=["ps6"])
                        else:
                            P.op("pe", f, reads=[("wz", 0), ("wz", 1)], writes=["ps6"])
                    P.op("act", lambda: nc.scalar.activation(out=sz[:], in_=ps[6][:], func=AF.Silu), reads=["ps6"], writes=["t3"])
                    tb_ = t1b[pb]
                    tn = f"t1_{pb}"
                    if c > 0:
                        P.op("pe", lambda: nc.tensor.matmul(ps[4][:], lhsT=CT[:, cs_], rhs=hstb[:], start=True, stop=True),
                             reads=[("CT", c // 4), "hstb"], writes=["ps4"])
                        P.op("dve", lambda: nc.vector.tensor_tensor(out=bview(tb_[:], 8), in0=bview(ps[4][:], 8), in1=G8(ea, c, g), op=ALU.mult),
                             reads=["ps4"], writes=[tn])
                        P.op("dve", lambda: nc.vector.tensor_tensor(out=tb_[:], in0=ps[3][:], in1=tb_[:], op=ALU.add),
                             reads=["ps3", tn], writes=[tn])
                    else:
                        P.op("dve", lambda: nc.vector.tensor_copy(out=tb_[:], in_=ps[3][:]), reads=["ps3"], writes=[tn])
                    P.op("dve", lambda: nc.vector.tensor_tensor(out=bview(hst[:], 8), in0=bview(hst[:], 8), in1=G8(cdb, c, g), op=ALU.mult),
                         reads=["hst"], writes=["hst"])
                    P.op("dve", lambda: nc.vector.tensor_tensor(out=hst[:], in0=ps[5][:], in1=hst[:], op=ALU.add), reads=["ps5", "hst"], writes=["hst"])
                    P.op("act", lambda: nc.scalar.copy(out=hstb[:], in_=hst[:]), reads=["hst"], writes=["hstb"])
                    P.op("dve", lambda: nc.vector.tensor_tensor(out=tb_[:], in0=tb_[:], in1=sz[:], op=ALU.mult), reads=[tn, "t3"], writes=[tn])
                    P.op("act", lambda: nc.scalar.activation(out=sjk[:], in_=tb_[:], func=AF.Square, accum_out=g1[0][:, pb:pb + 1]),
                         reads=[tn], writes=["xcf0", ("g1_0", pb)])
                    P.op("act", lambda: nc.scalar.activation(out=g1[2][:, pb:pb + 1], in_=g1[0][:, pb:pb + 1], func=AF.Sqrt, scale=1.0 / 512, bias=epsb[:, 0:1]),
                         reads=[("g1_0", pb), "epsb"], writes=[("g1_2", pb)])

                def backB(c):
                    pb = c % 2
                    tb_ = t1b[pb]
                    tn = f"t1_{pb}"
                    P.op("dve", lambda: nc.vector.reciprocal(out=g1[3][:, pb:pb + 1], in_=g1[2][:, pb:pb + 1]), reads=[("g1_2", pb)], writes=[("g1_3", pb)])
                    P.op("dve", lambda: nc.vector.scalar_tensor_tensor(out=ynb[pb][:], in0=tb_[:], scalar=g1[3][:, pb:pb + 1], in1=ssdg[:], op0=ALU.mult, op1=ALU.mult),
                         reads=[tn, ("g1_3", pb), "ssdg"], writes=[f"ynb{pb}"])

                def tail(c):
                    cs_ = slice(c * 128, (c + 1) * 128)
                    pb = c % 2
                    for i4 in range(4):
                        f = lambda: nc.tensor.transpose(out=psb[:, i4 * 128:(i4 + 1) * 128], in_=ynb[pb][:, i4 * 128:(i4 + 1) * 128], identity=ident)
                        if i4 < 3:
                            P.quiet("pe", f, reads=[f"ynb{pb}", "cstb"], writes=["psb"])
                        else:
                            P.op("pe", f, reads=[f"ynb{pb}", "cstb"], writes=["psb"])
                    P.op("act", lambda: nc.scalar.copy(out=ysT[:, 4 * g:4 * g + 4, cs_], in_=bview(psb[:, 0:512], 4)), reads=["psb"], writes=[("ysT", g, c)])

                front(0)
                for c in range(NT):
                    if c + 1 < NT:
                        front(c + 1)
                    back(c)
                    if c >= 1:
                        backB(c - 1)
                        tail(c - 1)
                backB(NT - 1)
                tail(NT - 1)
            P.barrier()
        if stage == 3:
            o = dout("ysT_dbg", [128, 16, T], BF16)
            P.dma("sp", "out", [(o, ysT[:])])
            P.barrier()
            return nc, dbg

        stM = contextlib.ExitStack()
        es.enter_context(stM)
        mgT = sb("mgT", [128, 8, T], BF16, stM)
        with contextlib.ExitStack() as sM:
            yaT2 = sb("yaT2", [128, 8, T], BF16, sM)
            P.dma("sp", "ya_reload", [(yaT2[:], ya_spill)], writes=["yaT2"])
            wstM = sb("wstM", [128, 16, 128], F32, sM)
            wac = [sb(f"wac{i}", [128, 8, 128], BF16, sM) for i in range(2)]
            wbc = [sb(f"wbc{i}", [128, 16, 128], BF16, sM) for i in range(2)]
            wgac = [sb(f"wgac{i}", [128, 8, 128], BF16, sM) for i in range(2)]
            wgbc = [sb(f"wgbc{i}", [128, 8, 128], BF16, sM) for i in range(2)]
            sga = [sb(f"sga{i}", [128, 512], F32, sM) for i in range(2)]
            sgb = [sb(f"sgb{i}", [128, 512], F32, sM) for i in range(2)]
            wa_v = wa_d.rearrange("(kt p) n -> p kt n", p=128)
            wb_v = wb_d.rearrange("(kt p) n -> p kt n", p=128)
            it = [0]

            def load_merge_w(nt):
                ns = slice(nt * 128, (nt + 1) * 128)
                wp = nt % 2
                for (dst, src, nk_, nm) in [(wgac[wp], win_v[:, :, O_GA + nt * 128:O_GA + (nt + 1) * 128], 8, f"wgac{wp}"),
                                            (wgbc[wp], win_v[:, :, O_GB + nt * 128:O_GB + (nt + 1) * 128], 8, f"wgbc{wp}"),
                                            (wac[wp], wa_v[:, :, ns], 8, f"wac{wp}"), (wbc[wp], wb_v[:, :, ns], 16, f"wbc{wp}")]:
                    P.dma("sp", "wstM", [(wstM[:, 0:nk_, :], src)], writes=["wstM"])
                    P.op("dve", lambda: nc.vector.tensor_copy(out=dst[:], in_=wstM[:, 0:nk_, :]), reads=["wstM"], writes=[nm])

            load_merge_w(0)
            for nt in range(8):
                wp = nt % 2
                if nt + 1 < 8:
                    load_merge_w(nt + 1)
                for tb in range(4):
                    ts_ = slice(tb * 512, (tb + 1) * 512)
                    par = it[0] % 2
                    it[0] += 1
                    bA, bB, bGA = (0, 1, 2) if par == 0 else (4, 5, 6)
                    bGB = 3

                    def acc(bank, wt, nk_, rhsT, nm, rd):
                        for kt in range(nk_):
                            f = lambda: nc.tensor.matmul(ps[bank][:], lhsT=wt[:, kt, :], rhs=rhsT[:, kt, ts_], start=(kt == 0), stop=(kt == nk_ - 1))
                            if kt < nk_ - 1:
                                P.quiet("pe", f, reads=[nm] + rd, writes=[f"ps{bank}"])
                            else:
                                P.op("pe", f, reads=[nm] + rd, writes=[f"ps{bank}"])
                    acc(bGA, wgac[wp], 8, hT, f"wgac{wp}", [])
                    acc(bGB, wgbc[wp], 8, hT, f"wgbc{wp}", [])
                    acc(bA, wac[wp], 8, yaT2, f"wac{wp}", ["yaT2"])
                    acc(bB, wbc[wp], 16, ysT, f"wbc{wp}", [])
                    P.op("act", lambda: nc.scalar.activation(out=sga[par][:], in_=ps[bGA][:], func=AF.Sigmoid), reads=[f"ps{bGA}"], writes=[f"sga{par}"])
                    P.op("act", lambda: nc.scalar.activation(out=sgb[par][:], in_=ps[bGB][:], func=AF.Sigmoid), reads=[f"ps{bGB}"], writes=[f"sgb{par}"])
                    P.op("dve", lambda: nc.vector.tensor_tensor(out=sga[par][:], in0=ps[bA][:], in1=sga[par][:], op=ALU.mult),
                         reads=[f"ps{bA}", f"sga{par}"], writes=[f"sga{par}"])
                    P.op("dve", lambda: nc.vector.tensor_tensor(out=sgb[par][:], in0=ps[bB][:], in1=sgb[par][:], op=ALU.mult),
                         reads=[f"ps{bB}", f"sgb{par}"], writes=[f"sgb{par}"])
                    P.op("pool", lambda: nc.gpsimd.tensor_tensor(out=mgT[:, nt, ts_], in0=sga[par][:], in1=sgb[par][:], op=ALU.add),
                         reads=[f"sga{par}", f"sgb{par}"], writes=[("mgT", nt, tb)])
            P.barrier()
        if stage == 4:
            o = dout("mgT_dbg", [128, 8, T], BF16)
            P.dma("sp", "out", [(o, mgT[:])])
            P.barrier()
            return nc, dbg

        x1 = ysT[:].bitcast(F32)
        assert list(x1.shape) == [128, NT, D], x1.shape
        with contextlib.ExitStack() as sO:
            wstO = sb("wstO", [128, 8, 256], F32, sO)
            wo = sb("wo", [128, 8, D], BF16, sO)
            wo_v = wo_d.rearrange("(kt p) n -> p kt n", p=128)
            for q4 in range(4):
                P.dma("sp", "wstO", [(wstO[:], wo_v[:, :, q4 * 256:(q4 + 1) * 256])], writes=["wstO"])
                P.op("pool", lambda: nc.gpsimd.tensor_copy(out=wo[:, :, q4 * 256:(q4 + 1) * 256], in_=wstO[:]), reads=["wstO"], writes=[("wo", q4)])
            for c in range(NT):
                cs_ = slice(c * 128, (c + 1) * 128)
                P.dma("sp", f"x1ld{c % 2}", [(x1[:, c, :], x_d[cs_, :])], writes=[("x1", c)])
                for hf in range(2):
                    bank = (2 * c + hf) % 4
                    for kt in range(8):
                        f = lambda: nc.tensor.matmul(ps[bank][:], lhsT=mgT[:, kt, cs_], rhs=wo[:, kt, hf * 512:(hf + 1) * 512],
                                                     start=(kt == 0), stop=(kt == 7))
                        rd = [("wo", 2 * hf), ("wo", 2 * hf + 1)]
                        if kt < 7:
                            P.quiet("pe", f, reads=rd, writes=[f"ps{bank}"])
                        else:
                            P.op("pe", f, reads=rd, writes=[f"ps{bank}"])
                    P.op("dve", lambda: nc.vector.tensor_tensor(out=x1[:, c, hf * 512:(hf + 1) * 512], in0=ps[bank][:],
                                                                in1=x1[:, c, hf * 512:(hf + 1) * 512], op=ALU.add),
                         reads=[f"ps{bank}", ("x1", c)], writes=[("x1", c)])
            P.barrier()
        stM.close()
        if stage == 5:
            o = dout("x1_dbg", [128, NT, D], F32)
            P.dma("sp", "out", [(o, x1)])
            P.barrier()
            return nc, dbg

        with contextlib.ExitStack() as sN:
            load_gain("ffn_norm", D, sN)

            def from_x1(c, buf, rname):
                P.op("pool", lambda: nc.gpsimd.tensor_copy(out=buf[:], in_=x1[:, c, :]), reads=[("x1", c)], writes=[rname])
            rmsnorm_T(from_x1, "ffn_norm", hT, sN)
            P.barrier()

        with contextlib.ExitStack() as sE:
            selm = sb("selm", [128, 32, 128], BF16, sE)
            P.op("pool", lambda: nc.gpsimd.memset(selm[:], 0.0), writes=["selm"])
            for r3 in range(3):
                P.op("pool", lambda: nc.gpsimd.tensor_copy(
                    out=selm[32 * r3:32 * r3 + 32, :, :],
                    in_=cstb[32 * r3:32 * r3 + 32, C_ID + 32 * r3:C_ID + 32 * r3 + 32].unsqueeze(2).to_broadcast([32, 32, 128])),
                    reads=["cstb", "selm"], writes=["selm"])
            cT3 = sb("cT3", [128, T], BF16, sE)
            P.op("pool", lambda: nc.gpsimd.memset(cT3[:], 0.0), writes=["cT3z"])
            with contextlib.ExitStack() as sR:
                wr_s = sb("wr_s", [128, 8, 36], F32, sR)
                wr = sb("wr", [128, 8, 36], BF16, sR)
                P.dma("sp", "wr_s", [(wr_s[:, :, 0:4], wrg_d.rearrange("(kt p) n -> p kt n", p=128)),
                                     (wr_s[:, :, 4:36], wre_d.rearrange("(kt p) n -> p kt n", p=128))], writes=["wr_s"])
                P.op("pool", lambda: nc.gpsimd.tensor_copy(out=wr[:], in_=wr_s[:]), reads=["wr_s"], writes=["wr"])
                rb_ = sb("rbias", [128, 36], F32, sR)
                P.dma("sp", "rbias", [(rb_[:, 0:4], gains["b_route_group"].partition_broadcast(128)),
                                      (rb_[:, 4:36], gains["b_route_expert"].partition_broadcast(128))], writes=["rbias"])
                lg = sb("r_lg", [128, 36], F32, sR)
                r1c = [sb(f"r_c{i}", [128, 1], F32, sR) for i in range(8)]
                oh = sb("r_oh", [128, 4], F32, sR)
                ge = sb("r_ge", [128, 4], F32, sR)
                tmp48 = sb("r_t48", [128, 4, 8], F32, sR)
                ein = sb("r_ein", [128, 8], F32, sR)
                top8 = sb("r_top8", [128, 8], F32, sR)
                msel = sb("r_msel", [128, 8], F32, sR)
                wex = sb("r_wex", [128, 8], F32, sR)
                comb = sb("r_comb", [128, 4, 8], F32, sR)
                comb3 = sb("r_comb3", [128, 3, 32], F32, sR)
                Hb2 = sb("r_Hb", [128, 128], BF16, sR)
                Mb2 = sb("r_Mb", [128, 128], BF16, sR)
                q1 = sb("r_q1", [128, 128], F32, sR)
                q2 = sb("r_q2", [128, 128], F32, sR)
                mx, nmx, sme, gw, m21, den, rden, sc_ = r1c
                for c in range(NT):
                    cs_ = slice(c * 128, (c + 1) * 128)
                    for kt in range(8):
                        f = lambda: nc.tensor.matmul(ps[0][:, 0:36], lhsT=hT[:, kt, cs_], rhs=wr[:, kt, :], start=(kt == 0), stop=(kt == 7))
                        if kt < 7:
                            P.quiet("pe", f, reads=["wr"], writes=["ps0"])
                        else:
                            P.op("pe", f, reads=["wr"], writes=["ps0"])
                    P.op("dve", lambda: nc.vector.tensor_tensor(out=lg[:], in0=ps[0][:, 0:36], in1=rb_[:], op=ALU.add), reads=["ps0", "rbias"], writes=["r_lg"])
                    P.op("dve", lambda: nc.vector.tensor_reduce(out=mx[:], in_=lg[:, 0:4], axis=AX.X, op=ALU.max), reads=["r_lg"], writes=["r_mx"])
                    P.op("dve", lambda: nc.vector.tensor_scalar(out=oh[:], in0=lg[:, 0:4], scalar1=mx[:, 0:1], scalar2=None, op0=ALU.is_ge),
                         reads=["r_lg", "r_mx"], writes=["r_oh"])
                    P.op("dve", lambda: nc.vector.tensor_scalar(out=nmx[:], in0=mx[:], scalar1=-1.0, scalar2=None, op0=ALU.mult), reads=["r_mx"], writes=["r_nmx"])
                    P.op("act", lambda: nc.scalar.activation(out=ge[:], in_=lg[:, 0:4], func=AF.Exp, bias=nmx[:, 0:1], accum_out=sme[:]),
                         reads=["r_lg", "r_nmx"], writes=["r_ge", "r_sme"])
                    P.op("dve", lambda: nc.vector.reciprocal(out=gw[:], in_=sme[:]), reads=["r_sme"], writes=["r_gw"])
                    P.op("dve", lambda: nc.vector.tensor_tensor(out=tmp48[:], in0=bview(lg[:, 4:36], 4), in1=oh[:].unsqueeze(2).to_broadcast([128, 4, 8]),
                                                                op=ALU.mult), reads=["r_lg", "r_oh"], writes=["r_t48"])
                    P.op("dve", lambda: nc.vector.tensor_reduce(out=ein[:], in_=tmp48[:].rearrange("p g e -> p e g"), axis=AX.X, op=ALU.add),
                         reads=["r_t48"], writes=["r_ein"])
                    P.op("dve", lambda: nc.vector.max(out=top8[:], in_=ein[:]), reads=["r_ein"], writes=["r_top8"])
                    P.op("dve", lambda: nc.vector.tensor_scalar(out=msel[:], in0=ein[:], scalar1=top8[:, 1:2], scalar2=None, op0=ALU.is_ge),
                         reads=["r_ein", "r_top8"], writes=["r_msel"])
                    P.op("dve", lambda: nc.vector.tensor_scalar(out=nmx[:], in0=top8[:, 0:1], scalar1=-1.0, scalar2=None, op0=ALU.mult),
                         reads=["r_top8"], writes=["r_nmx"])
                    P.op("act", lambda: nc.scalar.activation(out=wex[:], in_=ein[:], func=AF.Exp, bias=nmx[:, 0:1]), reads=["r_ein", "r_nmx"], writes=["r_wex"])
                    P.op("act", lambda: nc.scalar.activation(out=m21[:], in_=top8[:, 1:2], func=AF.Exp, bias=nmx[:, 0:1]), reads=["r_top8", "r_nmx"], writes=["r_m21"])
                    P.op("dve", lambda: nc.vector.tensor_scalar(out=den[:], in0=m21[:], scalar1=1.0, scalar2=None, op0=ALU.add), reads=["r_m21"], writes=["r_den"])
                    P.op("dve", lambda: nc.vector.reciprocal(out=rden[:], in_=den[:]), reads=["r_den"], writes=["r_rden"])
                    P.op("dve", lambda: nc.vector.tensor_tensor(out=sc_[:], in0=rden[:], in1=gw[:], op=ALU.mult), reads=["r_rden", "r_gw"], writes=["r_sc"])
                    P.op("dve", lambda: nc.vector.tensor_tensor(out=wex[:], in0=wex[:], in1=msel[:], op=ALU.mult), reads=["r_wex", "r_msel"], writes=["r_wex"])
                    P.op("dve", lambda: nc.vector.tensor_scalar(out=wex[:], in0=wex[:], scalar1=sc_[:, 0:1], scalar2=None, op0=ALU.mult),
                         reads=["r_wex", "r_sc"], writes=["r_wex"])
                    P.op("dve", lambda: nc.vector.tensor_tensor(out=comb[:], in0=oh[:].unsqueeze(2).to_broadcast([128, 4, 8]),
                                                                in1=wex[:].unsqueeze(1).to_broadcast([128, 4, 8]), op=ALU.mult),
                         reads=["r_oh", "r_wex"], writes=["r_comb"])
                    P.op("dve", lambda: nc.vector.tensor_copy(out=comb3[:], in_=comb[:].rearrange("p g e -> p (g e)").unsqueeze(1).to_broadcast([128, 3, 32])),
                         reads=["r_comb"], writes=["r_comb3"])
                    P.op("pe", lambda: nc.tensor.transpose(out=ps[1][0:96, 0:128], in_=comb3[:].rearrange("p a b -> p (a b)"),
                                                           identity=cst[:, C_ID:C_ID + 128]),
                         reads=["r_comb3", "cst"], writes=["ps1"])
                    P.op("act", lambda: nc.scalar.copy(out=Hb2[0:96, :], in_=ps[1][0:96, 0:128]), reads=["ps1"], writes=["r_Hb"])
                    P.op("dve", lambda: nc.vector.tensor_tensor(out=q1[0:96, :], in0=ps[1][0:96, 0:128], in1=Hb2[0:96, :], op=ALU.subtract),
                         reads=["ps1", "r_Hb"], writes=["r_q1"])
                    P.op("act", lambda: nc.scalar.copy(out=Mb2[0:96, :], in_=q1[0:96, :]), reads=["r_q1"], writes=["r_Mb"])
                    P.op("dve", lambda: nc.vector.tensor_tensor(out=q2[0:96, :], in0=q1[0:96, :], in1=Mb2[0:96, :], op=ALU.subtract),
                         reads=["r_q1", "r_Mb"], writes=["r_q2"])
                    P.op("pool", lambda: nc.gpsimd.tensor_copy(out=cT3[0:32, cs_], in_=Hb2[0:32, :]), reads=["r_Hb", "cT3z"], writes=[("cT3", c, 0)])
                    P.op("pool", lambda: nc.gpsimd.tensor_copy(out=cT3[32:64, cs_], in_=Mb2[32:64, :]), reads=["r_Mb", "cT3z"], writes=[("cT3", c, 1)])
                    P.op("act", lambda: nc.scalar.copy(out=cT3[64:96, cs_], in_=q2[64:96, :]), reads=["r_q2", "cT3z"], writes=[("cT3", c, 2)])
                P.barrier()
            if stage == 6:
                o = dout("cT3_dbg", [128, T], BF16)
                P.dma("sp", "out", [(o, cT3[:])])
                o = dout("h2T_dbg", [128, 8, T], BF16)
                P.dma("sp", "out", [(o, hT[:])])
                P.barrier()
                return nc, dbg

            NE = 32 if sub >= 30 else 2
            wstE = [sb(f"wstE{i}", [128, 8, 256], F32, sE) for i in range(2)]
            wgu = [sb(f"wgu{i}", [128, 8, 512], BF16, sE) for i in range(2)]
            wdn = [sb(f"wdn{i}", [128, 2, D], BF16, sE) for i in range(4)]
            actT = [sb(f"actT{i}", [128, 2, T], BF16, sE) for i in range(2)]
            sgs = [sb(f"sgs{i}", [128, 512], F32, sE) for i in range(2)]
            tms = [sb(f"tms{i}", [128, 512], F32, sE) for i in range(2)]
            stc = [0]
            itc = [0]
            for e in range(NE):
                sl = e % 2
                dsl = e % 4
                for (k_, src) in [(0, wg_d[e].rearrange("(kt p) n -> p kt n", p=128)), (1, wu_d[e].rearrange("(kt p) n -> p kt n", p=128))]:
                    si = stc[0] % 2
                    stc[0] += 1
                    P.dma("sp", f"wstE{si}", [(wstE[si][:], src)], writes=[f"wstE{si}"])
                    P.op("pool", lambda: nc.gpsimd.tensor_copy(out=wgu[sl][:, :, k_ * 256:(k_ + 1) * 256], in_=wstE[si][:]),
                         reads=[f"wstE{si}"], writes=[(f"wgu{sl}", k_)])
                si = stc[0] % 2
                stc[0] += 1
                P.dma("sp", f"wstE{si}", [(wstE[si][:].rearrange("p a b -> p (a b)").rearrange("p (f n) -> p f n", f=2),
                                           wd_d[e].rearrange("(ft p) n -> p ft n", p=128))],
                      writes=[f"wstE{si}"])
                P.op("pool", lambda: nc.gpsimd.tensor_copy(out=wdn[dsl][:].rearrange("p a b -> p (a b)"), in_=wstE[si][:].rearrange("p a b -> p (a b)")),
                     reads=[f"wstE{si}"], writes=[f"wdn{dsl}"])
                for tb in range(4):
                    ts_ = slice(tb * 512, (tb + 1) * 512)
                    P.op("pe", lambda: nc.tensor.matmul(ps[4][:], lhsT=selm[:, e, :], rhs=cT3[:, ts_], start=True, stop=True),
                         reads=["selm"], writes=["ps4"])
                    for ft in range(2):
                        par = itc[0] % 2
                        itc[0] += 1
                        bG, bU = (0, 1) if par == 0 else (2, 3)
                        for (bank, k_) in [(bG, 0), (bU, 1)]:
                            for kt in range(8):
                                f = lambda: nc.tensor.matmul(ps[bank][:], lhsT=wgu[sl][:, kt, k_ * 256 + ft * 128:k_ * 256 + (ft + 1) * 128],
                                                             rhs=hT[:, kt, ts_], start=(kt == 0), stop=(kt == 7))
                                if kt < 7:
                                    P.quiet("pe", f, reads=[(f"wgu{sl}", k_)], writes=[f"ps{bank}"])
                                else:
                                    P.op("pe", f, reads=[(f"wgu{sl}", k_)], writes=[f"ps{bank}"])
                        P.op("act", lambda: nc.scalar.activation(out=sgs[par][:], in_=ps[bG][:], func=AF.Silu), reads=[f"ps{bG}"], writes=[f"sgs{par}"])
                        P.op("dve", lambda: nc.vector.tensor_tensor(out=tms[par][:], in0=ps[bU][:], in1=sgs[par][:], op=ALU.mult),
                             reads=[f"ps{bU}", f"sgs{par}"], writes=[f"tms{par}"])
                        P.op("dve", lambda: nc.vector.tensor_tensor(out=actT[sl][:, ft, ts_], in0=ps[4][:], in1=tms[par][:], op=ALU.mult),
                             reads=["ps4", f"tms{par}"], writes=[(f"actT{sl}", ft, tb)])
                if e % 2 == 1:
                    for c in range(NT):
                        cs_ = slice(c * 128, (c + 1) * 128)
                        for hf in range(2):
                            bank = 5 + (2 * c + hf) % 2
                            n_ = 0
                            for ee in (e - 1, e):
                                for ft in range(2):
                                    f = lambda: nc.tensor.matmul(ps[bank][:], lhsT=actT[ee % 2][:, ft, cs_], rhs=wdn[ee % 4][:, ft, hf * 512:(hf + 1) * 512],
                                                                 start=(n_ == 0), stop=(n_ == 3))
                                    rd = [(f"actT{ee % 2}", ft, c // 4), f"wdn{ee % 4}"]
                                    if n_ < 3:
                                        P.quiet("pe", f, reads=rd, writes=[f"ps{bank}"])
                                    else:
                                        P.op("pe", f, reads=rd, writes=[f"ps{bank}"])
                                    n_ += 1
                            P.op("dve", lambda: nc.vector.tensor_tensor(out=x1[:, c, hf * 512:(hf + 1) * 512], in0=ps[bank][:],
                                                                        in1=x1[:, c, hf * 512:(hf + 1) * 512], op=ALU.add),
                                 reads=[f"ps{bank}", ("x1", c)], writes=[("x1", c)])
            for c in range(NT):
                P.dma("sp", "out", [(out_d[c * 128:(c + 1) * 128, :], x1[:, c, :])], reads=[("x1", c)])
            P.barrier()
    return nc, dbg


def host_consts():
    cst = np.zeros((128, CW), np.float32)
    cst[:, C_ID:C_ID + 128] = np.eye(128, dtype=np.float32)
    dm = np.ones((128, 128), np.float32)
    dm[0:64, 64:128] = 0.0
    cst[:, C_DM:C_DM + 128] = dm
    cst[:, C_NB:C_NB + 128] = (dm - 1.0) * 1e30
    cst[:, C_CT:C_CT + 128] = np.triu(np.ones((128, 128), np.float32))
    cst[:, C_ONE:C_ONE + 128] = 1.0
    for i in range(NBIS):
        cst[:, C_BIS + i] = 2.0 ** (-(i + 1))
    half = 8
    inv = 500000.0 ** (-np.arange(half, dtype=np.float64) * 2.0 / 16)
    ang = np.arange(T, dtype=np.float64)[:, None] * inv[None, :]
    cos, sin = np.cos(ang), np.sin(ang)
    rope = np.concatenate([cos, cos, -sin, sin], axis=1).astype(np.float32)
    return cst, rope


_CACHE = {}


def make_inmaps(inputs):
    cst, rope = host_consts()
    maps = []
    sq = lambda a: np.ascontiguousarray(np.asarray(a, np.float32)[0])
    shared = {
        "w_in": sq(inputs["w_in"]),
        "conv_w": sq(inputs["conv_w"]),
        "w_attn_branch": sq(inputs["w_attn_branch"]), "w_ssd_branch": sq(inputs["w_ssd_branch"]),
        "w_out": sq(inputs["w_out"]), "w_route_group": sq(inputs["w_route_group"]),
        "w_route_expert": sq(inputs["w_route_expert"]),
        "w_gate": sq(inputs["w_gate"]).reshape(32, 1024, 256), "w_up": sq(inputs["w_up"]).reshape(32, 1024, 256),
        "w_down": sq(inputs["w_down"]).reshape(32, 256, 1024),
        "cst": cst, "rope": rope,
    }
    for n in ["attn_norm", "q_norm", "k_norm", "idx_k_norm", "ffn_norm", "ssd_norm", "conv_b", "dt_bias", "a_log",
              "d_skip", "b_route_group", "b_route_expert"]:
        shared[n] = np.ascontiguousarray(np.asarray(inputs[n], np.float32).reshape(1, -1))
    x = np.asarray(inputs["x"], np.float32)
    for b in range(8):
        m = dict(shared)
        m["x"] = np.ascontiguousarray(x[b])
        maps.append(m)
    return maps


def kernel(**inputs):
    if "nc" not in _CACHE:
        _CACHE["nc"] = build()[0]
    nc = _CACHE["nc"]
    maps = make_inmaps(inputs)
    res = run_bass_kernel_spmd(nc, maps, core_ids=list(range(8)))
    return np.stack([np.asarray(r["out"], np.float32) for r in res.results], axis=0)
```

```python
import contextlib
import math
import numpy as np
import concourse.bass as bass
import concourse.mybir as mybir
from concourse.bass_utils import run_bass_kernel_spmd

F32 = mybir.dt.float32
BF16 = mybir.dt.bfloat16
U32 = mybir.dt.uint32
AF = mybir.ActivationFunctionType
ALU = mybir.AluOpType
AX = mybir.AxisListType

T = 2048
D = 1024
NT = 16
EPS = 1e-6
NBIS = 16
SPL = (1024, 256, 256, 512, 64, 8, 2048, 3072, 32, 1024, 1024)
OFF = [0]
for _s in SPL:
    OFF.append(OFF[-1] + _s)
(O_Q, O_K, O_V, O_IQ, O_IK, O_IW, O_Z, O_XBC, O_DT, O_GA, O_GB, O_END) = OFF
NW = O_END

C_ID = 0
C_DM = 128
C_NB = 256
C_CT = 384
C_ONE = 512
C_BIS = 640
CW = 672


class Prog:
    ENG = ("pe", "act", "dve", "pool", "sp")

    def __init__(self, nc, es):
        self.nc = nc
        self.es = es
        self.e = {"pe": nc.tensor, "act": nc.scalar, "dve": nc.vector, "pool": nc.gpsimd, "sp": nc.sync}
        self.sem = {}
        self.cnt = {k: 0 for k in self.ENG}
        self.epoch = {k: 0 for k in self.ENG}
        for k in self.ENG:
            self.sem[("e", k, 0)] = es.enter_context(nc.semaphore(f"s_{k}_0"))
        self.dcnt = {}
        self.waited = {k: {} for k in self.ENG}
        self.res = {}
        self.nins = 0
        self.pending = {k: [] for k in self.ENG}
        self.rec = None

    def _deps(self, eng, reads, writes):
        deps = []
        for r in reads:
            st = self.res.get(r)
            if st and st[0] is not None:
                deps.append((st[0], True))
        for w in writes:
            st = self.res.get(w)
            if st:
                if st[0] is not None:
                    deps.append((st[0], True))
                for t in st[1].values():
                    deps.append((t, False))
        for (tok, strong) in deps:
            key, val = tok
            if key[0] == "e" and key[1] == eng:
                if eng == "pe":
                    continue
            if self.waited[eng].get(key, -1) >= val:
                continue
            self.e[eng].wait_ge(self.sem[key], val)
            self.waited[eng][key] = val

    def _commit(self, tok, reads, writes, rkey):
        for r in reads:
            st = self.res.setdefault(r, [None, {}])
            st[1][rkey] = tok
        for w in writes:
            self.res[w] = [tok, {}]

    def op(self, eng, fn, reads=(), writes=()):
        if self.rec is not None:
            self.rec.append(("op", eng, fn, tuple(reads), tuple(writes)))
            return None
        self._deps(eng, reads, writes)
        ins = fn()
        if self.cnt[eng] >= 30000:
            self.epoch[eng] += 1
            self.cnt[eng] = 0
            self.sem[("e", eng, self.epoch[eng])] = self.es.enter_context(
                self.nc.semaphore(f"s_{eng}_{self.epoch[eng]}"))
        key = ("e", eng, self.epoch[eng])
        self.cnt[eng] += 1
        ins.then_inc(self.sem[key], 1)
        self.nins += 1
        for (r_, w_) in self.pending[eng]:
            self._commit((key, self.cnt[eng]), r_, w_, key)
        self.pending[eng] = []
        self._commit((key, self.cnt[eng]), reads, writes, key)
        return ins

    def replay(self, items):
        for (kind, eng, fn, reads, writes) in items:
            (self.op if kind == "op" else self.quiet)(eng, fn, reads, writes)

    def quiet(self, eng, fn, reads=(), writes=()):
        if self.rec is not None:
            self.rec.append(("quiet", eng, fn, tuple(reads), tuple(writes)))
            return None
        self._deps(eng, reads, writes)
        self.nins += 1
        self.pending[eng].append((tuple(reads), tuple(writes)))
        return fn()

    def dma(self, q, key, pairs, reads=(), writes=()):
        self._deps(q, reads, writes)
        k = ("d", key)
        if k not in self.sem:
            self.sem[k] = self.es.enter_context(self.nc.semaphore(f"d_{key}"))
            self.dcnt[k] = 0
        for (o, i) in pairs:
            self.e[q].dma_start(out=o, in_=i).then_inc(self.sem[k], 16)
            self.dcnt[k] += 16
            self.nins += 1
        self._commit((k, self.dcnt[k]), reads, writes, k)

    def barrier(self):
        toks = []
        for k in self.ENG:
            if self.cnt[k] > 0:
                toks.append((("e", k, self.epoch[k]), self.cnt[k]))
        for k, v in self.dcnt.items():
            if v > 0:
                toks.append((k, v))
        for eng in self.ENG:
            for (key, val) in toks:
                if key[0] == "e" and key[1] == eng:
                    continue
                if self.waited[eng].get(key, -1) >= val:
                    continue
                self.e[eng].wait_ge(self.sem[key], val)
                self.waited[eng][key] = val
        self.res = {}


def bview(ap, h):
    return ap.rearrange("p (h d) -> p h d", h=h)


def build(stage=99, sub=99):
    nc = bass.Bass("TRN2", target_bir_lowering=False)
    dr = {}

    def din(name, shape):
        dr[name] = nc.dram_tensor(name, list(shape), F32, kind="ExternalInput").ap()
        return dr[name]

    x_d = din("x", [T, D])
    win_d = din("w_in", [D, NW])
    gains = {n: din(n, [1, s]) for n, s in [("attn_norm", D), ("q_norm", 64), ("k_norm", 64), ("idx_k_norm", 64),
                                             ("ffn_norm", D), ("ssd_norm", 2048), ("conv_b", 3072), ("dt_bias", 32),
                                             ("a_log", 32), ("d_skip", 32), ("b_route_group", 4),
                                             ("b_route_expert", 32)]}
    convw_d = din("conv_w", [4, 3072])
    wa_d = din("w_attn_branch", [1024, 1024])
    wb_d = din("w_ssd_branch", [2048, 1024])
    wo_d = din("w_out", [1024, 1024])
    wrg_d = din("w_route_group", [1024, 4])
    wre_d = din("w_route_expert", [1024, 32])
    wg_d = din("w_gate", [32, 1024, 256])
    wu_d = din("w_up", [32, 1024, 256])
    wd_d = din("w_down", [32, 256, 1024])
    cst_d = din("cst", [128, CW])
    rope_d = din("rope", [T, 32])
    out_d = nc.dram_tensor("out", [T, D], F32, kind="ExternalOutput").ap()
    dbg = {}

    def dout(name, shape, dt=F32):
        dbg[name] = nc.dram_tensor(name, list(shape), dt, kind="ExternalOutput").ap()
        return dbg[name]

    es = contextlib.ExitStack()
    with es:
        P = Prog(nc, es)

        uid = [0]

        def sb(name, shape, dt, stack=es):
            uid[0] += 1
            return stack.enter_context(nc.sbuf_tensor(f"sb{uid[0]}_{name}", list(shape), dt))

        ps = [es.enter_context(nc.psum_tensor(f"ps{i}", [128, 512], F32)) for i in range(7)]
        psb = es.enter_context(nc.psum_tensor("psb", [128, 1024], BF16))

        cst = sb("cst", [128, CW], F32)
        cstb = sb("cstb", [128, CW], BF16)
        P.dma("sp", "cst", [(cst[:], cst_d)], writes=["cst"])
        P.op("pool", lambda: nc.gpsimd.tensor_copy(out=cstb[:], in_=cst[:]), reads=["cst"], writes=["cstb"])
        ident = cstb[:, C_ID:C_ID + 128]
        gb = {}

        def load_gain(n, width, st, c0=0):
            gb[n] = sb("g_" + n, [128, width], F32, st)
            P.dma("sp", "gain_" + n, [(gb[n][:], gains[n][:, c0:c0 + width].partition_broadcast(128))], writes=["g_" + n])

        hT = sb("hT", [128, 8, T], BF16)
        ya_spill = nc.dram_tensor("ya_spill", [128, 8, T], BF16, kind="Internal").ap()

        def rmsnorm_T(src_fn, gname, dst, st, nbuf=2):
            xb = [sb(f"rn_x{i}", [128, D], F32, st) for i in range(nbuf)]
            xn = [sb(f"rn_xn{i}", [128, D], BF16, st) for i in range(2)]
            junk = sb("rn_junk", [128, D], BF16, st)
            ss = sb("rn_ss", [128, NT], F32, st)
            sd = sb("rn_sd", [128, NT], F32, st)
            rs = sb("rn_rs", [128, NT], F32, st)
            epsn = sb("rn_eps", [128, 1], F32, st)
            P.op("pool", lambda: nc.gpsimd.memset(epsn[:], EPS), writes=["rn_eps"])

            def chain(c):
                b = c % 2
                xa, xr = src_fn(c, xb)
                P.op("act", lambda: nc.scalar.activation(out=junk[:], in_=xa, func=AF.Square, accum_out=ss[:, c:c + 1]),
                     reads=[xr], writes=["rn_junk", ("rn_ss", c)])
                P.op("act", lambda: nc.scalar.activation(out=sd[:, c:c + 1], in_=ss[:, c:c + 1], func=AF.Sqrt, scale=1.0 / D,
                                                         bias=epsn[:, 0:1]),
                     reads=[("rn_ss", c), "rn_eps"], writes=[("rn_sd", c)])
                P.op("dve", lambda: nc.vector.reciprocal(out=rs[:, c:c + 1], in_=sd[:, c:c + 1]),
                     reads=[("rn_sd", c)], writes=[("rn_rs", c)])
                P.op("dve", lambda: nc.vector.scalar_tensor_tensor(out=xn[b][:], in0=xa, scalar=rs[:, c:c + 1],
                                                                   in1=gb[gname][:], op0=ALU.mult, op1=ALU.mult),
                     reads=[xr, ("rn_rs", c), "g_" + gname], writes=[f"rn_xn{b}"])

            def tail(c):
                b = c % 2
                for kt in range(8):
                    f = lambda: nc.tensor.transpose(out=psb[:, kt * 128:(kt + 1) * 128],
                                                    in_=xn[b][:, kt * 128:(kt + 1) * 128], identity=ident)
                    if kt < 7:
                        P.quiet("pe", f, reads=[f"rn_xn{b}", "cstb"], writes=["psb"])
                    else:
                        P.op("pe", f, reads=[f"rn_xn{b}", "cstb"], writes=["psb"])
                P.op("act", lambda: nc.scalar.copy(out=dst[:, :, c * 128:(c + 1) * 128], in_=bview(psb[:], 8)),
                     reads=["psb"], writes=[("hT", c)])

            for c in range(NT):
                chain(c)
                if c >= 1:
                    tail(c - 1)
            tail(NT - 1)

        def load_x(c, bufs):
            b = c % len(bufs)
            rname = f"rn_x{b}"
            P.dma("sp", rname, [(bufs[b][:], x_d[c * 128:(c + 1) * 128, :])], writes=[rname])
            return bufs[b][:], rname

        with contextlib.ExitStack() as st:
            load_gain("attn_norm", D, st)
            rmsnorm_T(load_x, "attn_norm", hT, st, nbuf=3)
            P.barrier()

        if stage == 0:
            o = dout("hT_dbg", [128, 8, T], BF16)
            P.dma("sp", "out", [(o, hT[:])])
            P.barrier()
            return nc, dbg

        wst = [None]
        wbf = [None, None]
        wctr = [0]
        win_v = win_d.rearrange("(kt p) n -> p kt n", p=128)

        def alloc_w(st):
            wst[0] = sb("wst", [128, 8, 512], F32, st)
            wbf[0] = sb("wbf0", [128, 8, 512], BF16, st)
            wbf[1] = sb("wbf1", [128, 8, 512], BF16, st)

        def load_w(c0, ncols):
            i = wctr[0] % 2
            wctr[0] += 1
            P.dma("sp", "wst", [(wst[0][:, :, 0:ncols], win_v[:, :, c0:c0 + ncols])], writes=["wst"])
            P.op("pool", lambda: nc.gpsimd.tensor_copy(out=wbf[i][:, :, 0:ncols], in_=wst[0][:, :, 0:ncols]),
                 reads=["wst"], writes=[f"wbf{i}"])
            return i

        def proj_tm(c, wi, ncols, bank):
            for kt in range(8):
                f = lambda: nc.tensor.matmul(ps[bank][:, 0:ncols], lhsT=hT[:, kt, c * 128:(c + 1) * 128],
                                             rhs=wbf[wi][:, kt, 0:ncols], start=(kt == 0), stop=(kt == 7))
                if kt < 7:
                    P.quiet("pe", f, reads=[("hT", c), f"wbf{wi}"], writes=[f"ps{bank}"])
                else:
                    P.op("pe", f, reads=[("hT", c), f"wbf{wi}"], writes=[f"ps{bank}"])

        with contextlib.ExitStack() as st:
            yaT = sb("yaT", [128, 8, T], BF16, st)
            rope = sb("rope", [128, NT, 32], F32, st)
            P.dma("sp", "rope", [(rope[:], rope_d.rearrange("(c p) f -> p c f", p=128))], writes=["rope"])
            for n_ in ("q_norm", "k_norm", "idx_k_norm"):
                load_gain(n_, 64, st)
            qT = sb("qT", [128, 8, T], BF16, st)
            kT2 = sb("kT2", [128, 4, T], BF16, st)
            iqT = sb("iqT", [128, 4, T], BF16, st)
            ikT2 = sb("ikT2", [128, T], BF16, st)
            vaug = sb("vaug", [128, NT, 4, 66], BF16, st)
            iwa = sb("iwa", [128, NT, 8], F32, st)
            iws = sb("iws", [128, NT, 8], F32, st)
            P.op("pool", lambda: nc.gpsimd.memset(vaug[:], 1.0), writes=["vaug"])

            with contextlib.ExitStack() as st2:
                alloc_w(st2)
                sq = sb("e_sq", [128, 512], F32, st2)
                xn = sb("e_xn", [128, 512], F32, st2)
                ra = sb("e_ra", [128, 8, 16], F32, st2)
                rb = sb("e_rb", [128, 8, 16], F32, st2)
                s8 = [sb(f"e_s8{i}", [128, 8], F32, st2) for i in range(4)]
                tmbs = [sb(f"e_tmb{i}", [128, 512], BF16, st2) for i in range(2)]

                def epilogue(c, bank, nh, gname, prescale, dst_fn, dup):
                    pv = bview(ps[bank][:, 0:nh * 64], nh)
                    xv = bview(xn[:, 0:nh * 64], nh)
                    pr = f"ps{bank}"
                    if gname is not None:
                        P.op("act", lambda: nc.scalar.activation(out=sq[:, 0:nh * 64], in_=ps[bank][:, 0:nh * 64],
                                                                 func=AF.Square), reads=[pr], writes=["e_sq"])
                        P.op("dve", lambda: nc.vector.tensor_reduce(out=s8[0][:, 0:nh], in_=bview(sq[:, 0:nh * 64], nh),
                                                                    axis=AX.X, op=ALU.add), reads=["e_sq"], writes=["e_s80"])
                        P.op("dve", lambda: nc.vector.tensor_scalar(out=s8[1][:, 0:nh], in0=s8[0][:, 0:nh], scalar1=1.0 / 64,
                                                                    scalar2=EPS, op0=ALU.mult, op1=ALU.add),
                             reads=["e_s80"], writes=["e_s81"])
                        P.op("act", lambda: nc.scalar.activation(out=s8[2][:, 0:nh], in_=s8[1][:, 0:nh], func=AF.Sqrt),
                             reads=["e_s81"], writes=["e_s82"])
                        P.op("dve", lambda: nc.vector.reciprocal(out=s8[3][:, 0:nh], in_=s8[2][:, 0:nh]),
                             reads=["e_s82"], writes=["e_s83"])
                        P.op("dve", lambda: nc.vector.tensor_tensor(out=xv, in0=pv,
                                                                    in1=s8[3][:, 0:nh].unsqueeze(2).to_broadcast([128, nh, 64]),
                                                                    op=ALU.mult), reads=[pr, "e_s83"], writes=["e_xn"])
                        P.op("dve", lambda: nc.vector.tensor_tensor(out=xv, in0=xv,
                                                                    in1=gb[gname][:].unsqueeze(1).to_broadcast([128, nh, 64]),
                                                                    op=ALU.mult), reads=["e_xn", "g_" + gname], writes=["e_xn"])
                    elif prescale is not None:
                        P.op("dve", lambda: nc.vector.tensor_tensor(out=xv, in0=pv,
                                                                    in1=prescale.unsqueeze(2).to_broadcast([128, nh, 64]),
                                                                    op=ALU.mult), reads=[pr, ("iw", c)], writes=["e_xn"])
                    else:
                        P.op("dve", lambda: nc.vector.tensor_copy(out=xv, in_=pv), reads=[pr], writes=["e_xn"])
                    c16 = rope[:, c, 0:16].unsqueeze(1).to_broadcast([128, nh, 16])
                    nsn = rope[:, c, 16:24].unsqueeze(1).to_broadcast([128, nh, 8])
                    psn = rope[:, c, 24:32].unsqueeze(1).to_broadcast([128, nh, 8])
                    P.op("dve", lambda: nc.vector.tensor_tensor(out=ra[:, 0:nh, :], in0=xv[:, :, 0:16], in1=c16, op=ALU.mult),
                         reads=["e_xn", "rope"], writes=["e_ra"])
                    P.op("dve", lambda: nc.vector.tensor_tensor(out=rb[:, 0:nh, 0:8], in0=xv[:, :, 8:16], in1=nsn, op=ALU.mult),
                         reads=["e_xn", "rope"], writes=["e_rb0"])
                    P.op("dve", lambda: nc.vector.tensor_tensor(out=rb[:, 0:nh, 8:16], in0=xv[:, :, 0:8], in1=psn, op=ALU.mult),
                         reads=["e_xn", "rope"], writes=["e_rb1"])
                    P.op("dve", lambda: nc.vector.tensor_tensor(out=xv[:, :, 0:16], in0=ra[:, 0:nh, :], in1=rb[:, 0:nh, :],
                                                                op=ALU.add), reads=["e_ra", "e_rb0", "e_rb1"], writes=["e_xn"])
                    tmb = tmbs[c % 2]
                    tn0, tn1 = f"e_tmb{c % 2}_0", f"e_tmb{c % 2}_1"
                    if dup:
                        tv = tmb[:, 0:nh * 128].rearrange("p (h t d) -> p h t d", h=nh, t=2)
                        P.op("act", lambda: nc.scalar.copy(out=tv[:, :, 0, :], in_=xv), reads=["e_xn"], writes=[tn0])
                        P.op("act", lambda: nc.scalar.copy(out=tv[:, :, 1, :], in_=xv), reads=["e_xn"], writes=[tn1])
                        nblk = nh
                    else:
                        P.op("act", lambda: nc.scalar.copy(out=tmb[:, 0:nh * 64], in_=xn[:, 0:nh * 64]), reads=["e_xn"],
                             writes=[tn0, tn1])
                        nblk = nh // 2

                    def tail_():
                        for j in range(nblk):
                            f = lambda: nc.tensor.transpose(out=psb[:, j * 128:(j + 1) * 128], in_=tmb[:, j * 128:(j + 1) * 128],
                                                            identity=ident)
                            if j < nblk - 1:
                                P.quiet("pe", f, reads=[tn0, tn1, "cstb"], writes=["psb"])
                            else:
                                P.op("pe", f, reads=[tn0, tn1, "cstb"], writes=["psb"])
                        dst_fn(nblk)
                    return tail_

                pend = [None]

                def flush():
                    if pend[0]:
                        pend[0]()
                    pend[0] = None

                wi = load_w(O_IK, 72)
                for c in range(NT):
                    bank = c % 2
                    proj_tm(c, wi, 72, bank)
                    P.op("act", lambda: nc.scalar.activation(out=iwa[:, c, :], in_=ps[bank][:, 64:72], func=AF.Abs),
                         reads=[f"ps{bank}"], writes=[("iw", c)])
                    P.op("act", lambda: nc.scalar.activation(out=iws[:, c, :], in_=ps[bank][:, 64:72], func=AF.Sign),
                         reads=[f"ps{bank}"], writes=[("iws", c)])
                    t_ = epilogue(c, bank, 1, "idx_k_norm", None,
                                  lambda nblk, c=c: P.op("act", lambda: nc.scalar.copy(out=ikT2[:, c * 128:(c + 1) * 128],
                                                                                       in_=psb[:, 0:128]),
                                                         reads=["psb"], writes=[("ikT2", c)]), True)
                    if pend[0]:
                        pend[0]()
                    pend[0] = t_
                flush()
                wi = load_w(O_IQ, 512)
                for c in range(NT if sub >= 2 else 0):
                    bank = c % 2
                    proj_tm(c, wi, 512, bank)
                    t_ = epilogue(c, bank, 8, None, iwa[:, c, :],
                                  lambda nblk, c=c: P.op("act", lambda: nc.scalar.copy(out=iqT[:, :, c * 128:(c + 1) * 128],
                                                                                       in_=bview(psb[:, 0:512], 4)),
                                                         reads=["psb"], writes=[("iqT", c)]), False)
                    if pend[0]:
                        pend[0]()
                    pend[0] = t_
                flush()
                for half in range(2):
                    wi = load_w(O_Q + half * 512, 512)
                    for c in range(NT if sub >= 3 else 0):
                        bank = c % 2
                        proj_tm(c, wi, 512, bank)
                        t_ = epilogue(c, bank, 8, "q_norm", None,
                                      lambda nblk, c=c, half=half: P.op("act", lambda: nc.scalar.copy(
                                          out=qT[:, half * 4:half * 4 + 4, c * 128:(c + 1) * 128], in_=bview(psb[:, 0:512], 4)),
                                          reads=["psb"], writes=[("qT", c, half)]), False)
                        if pend[0]:
                            pend[0]()
                        pend[0] = t_
                    flush()
                wi = load_w(O_K, 512)
                for c in range(NT if sub >= 4 else 0):
                    bank = c % 2
                    proj_tm(c, wi, 512, bank)
                    if sub != 5:
                        P.op("act", lambda: nc.scalar.copy(out=vaug[:, c, :, 0:64], in_=bview(ps[bank][:, 256:512], 4)),
                             reads=[f"ps{bank}", "vaug"], writes=[("vaug", c)])
                    t_ = epilogue(c, bank, 4, "k_norm", None,
                                  lambda nblk, c=c: P.op("act", lambda: nc.scalar.copy(out=kT2[:, :, c * 128:(c + 1) * 128],
                                                                                       in_=bview(psb[:, 0:512], 4)),
                                                         reads=["psb"], writes=[("kT2", c)]), True)
                    if pend[0]:
                        pend[0]()
                    pend[0] = t_
                flush()
                P.barrier()

            if stage == 1:
                for nm, t_, shp in [("qT", qT, [128, 8, T]), ("kT2", kT2, [128, 4, T]), ("iqT", iqT, [128, 4, T]),
                                    ("ikT2", ikT2, [128, T])]:
                    o = dout(nm + "_dbg", shp, BF16)
                    P.dma("sp", "out", [(o, t_[:])])
                o = dout("vaug_dbg", [128, NT, 4, 66], BF16)
                P.dma("sp", "out", [(o, vaug[:])])
                o = dout("iwa_dbg", [128, NT, 8], F32)
                P.dma("sp", "out", [(o, iwa[:])])
                o = dout("iws_dbg", [128, NT, 8], F32)
                P.dma("sp", "out", [(o, iws[:])])
                P.barrier()
                return nc, dbg

            with contextlib.ExitStack() as st3:
                score = sb("score", [128, T], F32, st3)
                junk = sb("ajunk", [128, T], BF16, st3)
                maskb = [sb(f"maskb{i}", [128, T], BF16, st3) for i in range(2)]
                maskT = [sb(f"maskT{i}", [128, NT, 128], BF16, st3) for i in range(2)]
                relu = [sb(f"relu{i}", [128, 512], BF16, st3) for i in range(2)]
                diag = [sb(f"diag{i}", [128, 8, 128], BF16, st3) for i in range(2)]
                PT = [sb(f"PT{i}", [128, 512], BF16, st3) for i in range(3)]
                PTm = [sb(f"PTm{i}", [128, 512], BF16, st3) for i in range(3)]
                ytm = sb("ytm", [128, 1024], BF16, st3)
                hi = sb("b_hi", [128, 1], F32, st3)
                lo = sb("b_lo", [128, 1], F32, st3)
                w0 = sb("b_w0", [128, 1], F32, st3)
                wtab = sb("b_wtab", [128, NBIS], F32, st3)
                tt = sb("b_t", [128, 1], F32, st3)
                cnt = sb("b_cnt", [128, 1], F32, st3)
                uu = sb("b_u", [128, 1], F32, st3)
                thr = sb("b_thr", [128, 1], F32, st3)
                rcp = sb("b_rcp", [128, 8], F32, st3)
                pvc = [0]
                SB3 = [4, 5, 6]
                NQ = NT if sub >= 30 else max(0, sub - 10)

                def emit_scores(qi):
                    nkeys = 128 * (qi + 1)
                    qs = slice(qi * 128, (qi + 1) * 128)
                    dgt = diag[qi % 2]
                    for h in range(8):
                        P.op("dve", lambda: nc.vector.tensor_scalar(out=dgt[:, h, :], in0=ident, scalar1=iws[:, qi, h:h + 1],
                                                                    scalar2=None, op0=ALU.mult),
                             reads=["cstb"], writes=[(f"diag{qi % 2}", h)])
                    nkb = (nkeys + 511) // 512
                    for kb in range(nkb):
                        kw = min(512, nkeys - kb * 512)
                        for h in range(8):
                            hf, pr_ = h % 2, h // 2
                            rb = h % 2
                            P.op("pe", lambda: nc.tensor.matmul(ps[rb][:, 0:kw], lhsT=iqT[64 * hf:64 * hf + 64, pr_, qs],
                                                                rhs=ikT2[64 * hf:64 * hf + 64, kb * 512:kb * 512 + kw],
                                                                start=True, stop=True),
                                 writes=[f"ps{rb}"])
                            P.op("act", lambda: nc.scalar.activation(out=relu[rb][:, 0:kw], in_=ps[rb][:, 0:kw], func=AF.Relu),
                                 reads=[f"ps{rb}"], writes=[f"relu{rb}"])
                            f = lambda: nc.tensor.matmul(ps[2][:, 0:kw], lhsT=dgt[:, h, :], rhs=relu[rb][:, 0:kw],
                                                         start=(h == 0), stop=(h == 7))
                            if h < 7:
                                P.quiet("pe", f, reads=[f"relu{rb}", (f"diag{qi % 2}", h)], writes=["ps2"])
                            else:
                                P.op("pe", f, reads=[f"relu{rb}", (f"diag{qi % 2}", h)], writes=["ps2"])
                        c0 = kb * 512
                        last = (kb == nkb - 1)
                        nd = kw - 128 if last else kw
                        if nd > 0:
                            P.op("dve", lambda: nc.vector.tensor_copy(out=score[:, c0:c0 + nd], in_=ps[2][:, 0:nd]),
                                 reads=["ps2"], writes=[("score", kb)])
                        if last:
                            P.op("dve", lambda: nc.vector.tensor_tensor(out=score[:, nkeys - 128:nkeys], in0=ps[2][:, nd:nd + 128],
                                                                        in1=cst[:, C_DM:C_DM + 128], op=ALU.mult),
                                 reads=["ps2", "cst"], writes=[("score", "d")])
                            P.op("dve", lambda: nc.vector.tensor_tensor(out=score[:, nkeys - 128:nkeys],
                                                                        in0=score[:, nkeys - 128:nkeys],
                                                                        in1=cst[:, C_NB:C_NB + 128], op=ALU.add),
                                 reads=[("score", "d"), "cst"], writes=[("score", "d")])

                def search_steps(qi):
                    nkeys = 128 * (qi + 1)
                    nkb = (nkeys + 511) // 512
                    sres = [("score", kb) for kb in range(nkb)] + [("score", "d")]
                    mb = maskb[qi % 2]
                    steps = []

                    def s0():
                        P.op("dve", lambda: nc.vector.tensor_reduce(out=hi[:], in_=score[:, 0:nkeys], axis=AX.X, op=ALU.max),
                             reads=sres, writes=["b_hi"])
                        P.op("dve", lambda: nc.vector.tensor_reduce(out=lo[:], in_=score[:, 0:nkeys - 128], axis=AX.X, op=ALU.min),
                             reads=sres, writes=["b_lo"])
                        P.op("dve", lambda: nc.vector.tensor_tensor(out=w0[:], in0=hi[:], in1=lo[:], op=ALU.subtract),
                             reads=["b_hi", "b_lo"], writes=["b_w0"])
                        P.op("dve", lambda: nc.vector.tensor_scalar(out=wtab[:], in0=cst[:, C_BIS:C_BIS + NBIS], scalar1=w0[:, 0:1],
                                                                    scalar2=None, op0=ALU.mult),
                             reads=["b_w0", "cst"], writes=["b_wtab"])
                        P.op("dve", lambda: nc.vector.tensor_tensor(out=tt[:], in0=lo[:], in1=wtab[:, 0:1], op=ALU.add),
                             reads=["b_lo", "b_wtab"], writes=["b_t"])
                    steps.append(s0)
                    for it in range(NBIS):
                        def si(it=it):
                            P.op("dve", lambda: nc.vector.tensor_scalar(out=junk[:, 0:nkeys], in0=score[:, 0:nkeys], scalar1=tt[:, 0:1],
                                                                        scalar2=None, op0=ALU.is_ge, op1=ALU.add, accum_out=cnt[:]),
                                 reads=sres + ["b_t"], writes=["ajunk", "b_cnt"])
                            P.op("dve", lambda: nc.vector.tensor_scalar(out=uu[:], in0=cnt[:], scalar1=256.0, scalar2=-0.5,
                                                                        op0=ALU.is_ge, op1=ALU.add),
                                 reads=["b_cnt"], writes=["b_u"])
                            P.op("dve", lambda: nc.vector.scalar_tensor_tensor(out=tt[:], in0=uu[:], scalar=wtab[:, it:it + 1],
                                                                               in1=tt[:], op0=ALU.mult, op1=ALU.add),
                                 reads=["b_u", "b_wtab", "b_t"], writes=["b_t"])
                        steps.append(si)

                    def sf():
                        P.op("dve", lambda: nc.vector.scalar_tensor_tensor(out=thr[:], in0=wtab[:, NBIS - 1:NBIS], scalar=-0.5,
                                                                           in1=tt[:], op0=ALU.mult, op1=ALU.add),
                             reads=["b_wtab", "b_t"], writes=["b_thr"])
                        P.op("dve", lambda: nc.vector.tensor_scalar(out=mb[:, 0:nkeys], in0=score[:, 0:nkeys], scalar1=thr[:, 0:1],
                                                                    scalar2=None, op0=ALU.is_ge),
                             reads=sres + ["b_thr"], writes=[f"maskb{qi % 2}"])
                    steps.append(sf)
                    return steps

                def const_mask(qi):
                    nkeys = 128 * (qi + 1)
                    mb = maskb[qi % 2]
                    if qi == 1:
                        P.op("pool", lambda: nc.gpsimd.tensor_copy(out=mb[:, 0:128], in_=cstb[:, C_ONE:C_ONE + 128]),
                             reads=["cstb"], writes=[f"maskb{qi % 2}"])
                    P.op("pool", lambda: nc.gpsimd.tensor_copy(out=mb[:, nkeys - 128:nkeys], in_=cstb[:, C_DM:C_DM + 128]),
                         reads=["cstb"], writes=[f"maskb{qi % 2}"])

                def emit_maskT(qi):
                    nk = qi + 1
                    mb = maskb[qi % 2]
                    mT = maskT[qi % 2]
                    for k0 in range(0, nk, 8):
                        n = min(8, nk - k0)
                        for j in range(n):
                            f = lambda: nc.tensor.transpose(out=psb[:, j * 128:(j + 1) * 128],
                                                            in_=mb[:, (k0 + j) * 128:(k0 + j + 1) * 128], identity=ident)
                            if j < n - 1:
                                P.quiet("pe", f, reads=[f"maskb{qi % 2}", "cstb"], writes=["psb"])
                            else:
                                P.op("pe", f, reads=[f"maskb{qi % 2}", "cstb"], writes=["psb"])
                        P.op("act", lambda: nc.scalar.copy(out=mT[:, k0:k0 + n, :], in_=bview(psb[:, 0:n * 128], n)),
                             reads=["psb"], writes=[(f"maskT{qi % 2}", k0 // 8)])

                SBK = [4, 5, 6, 0, 1]
                LOOK = 2

                def attention_tile(qi, steps):
                    nk = qi + 1
                    qs = slice(qi * 128, (qi + 1) * 128)
                    mT = maskT[qi % 2]
                    seq = [(g, kj) for g in range(4) for kj in range(nk)]
                    nseq = len(seq)
                    info = {}
                    stq = list(steps)
                    every = max(1, nseq // max(1, len(stq))) if stq else 0

                    def front(i):
                        g, kj = seq[i]
                        ks = slice(kj * 128, (kj + 1) * 128)
                        n_ = pvc[0]
                        pvc[0] += 1
                        par = n_ % 3
                        sa, sb_ = SBK[(2 * n_) % 5], SBK[(2 * n_ + 1) % 5]
                        info[i] = par
                        P.op("pe", lambda: nc.tensor.matmul(bview(ps[sa][:, 0:256], 2), lhsT=kT2[0:64, g, ks],
                                                            rhs=qT[0:64, 2 * g:2 * g + 2, qs], start=True, stop=True),
                             writes=[f"ps{sa}"])
                        P.op("pe", lambda: nc.tensor.matmul(bview(ps[sb_][:, 0:256], 2), lhsT=kT2[64:128, g, ks],
                                                            rhs=qT[64:128, 2 * g:2 * g + 2, qs], start=True, stop=True),
                             writes=[f"ps{sb_}"])
                        P.op("act", lambda: nc.scalar.activation(out=PT[par][:, 0:256], in_=ps[sa][:, 0:256], func=AF.Exp,
                                                                 scale=0.125), reads=[f"ps{sa}"], writes=[("PT", par, 0)])
                        P.op("act", lambda: nc.scalar.activation(out=PT[par][:, 256:512], in_=ps[sb_][:, 0:256], func=AF.Exp,
                                                                 scale=0.125), reads=[f"ps{sb_}"], writes=[("PT", par, 1)])
                        me = "dve" if (n_ % 3 == 2) else "pool"
                        P.op(me, lambda: P.e[me].tensor_tensor(out=bview(PTm[par][:], 4), in0=bview(PT[par][:], 4),
                                                               in1=mT[:, kj, :].unsqueeze(1).to_broadcast([128, 4, 128]),
                                                               op=ALU.mult),
                             reads=[("PT", par, 0), ("PT", par, 1), (f"maskT{qi % 2}", kj // 8)], writes=[("PTm", par)])

                    def back(i):
                        g, kj = seq[i]
                        par = info[i]
                        ob = 2 + g % 2
                        for j in range(4):
                            hl = [0, 2, 1, 3][j]
                            f = lambda: nc.tensor.matmul(ps[ob][:, hl * 65:hl * 65 + 65], lhsT=PTm[par][:, j * 128:(j + 1) * 128],
                                                         rhs=vaug[:, kj, g, 0:65], start=(kj == 0 and j == 0),
                                                         stop=(kj == nk - 1 and j == 3), skip_group_check=True)
                            if j < 3:
                                P.quiet("pe", f, reads=[("PTm", par)], writes=[f"ps{ob}"])
                            else:
                                P.op("pe", f, reads=[("PTm", par)], writes=[f"ps{ob}"])
                        if kj == nk - 1:
                            ov = ps[ob][:, 0:260].rearrange("p (h d) -> p h d", h=4)
                            P.op("dve", lambda: nc.vector.reciprocal(out=rcp[:, 4 * (g % 2):4 * (g % 2) + 4], in_=ov[:, :, 64]),
                                 reads=[f"ps{ob}"], writes=[("b_rcp", g % 2)])
                            for hl in range(4):
                                hh = 4 * g + hl
                                P.op("act", lambda: nc.scalar.activation(out=ytm[:, hh * 64:(hh + 1) * 64],
                                                                         in_=ps[ob][:, hl * 65:hl * 65 + 64], func=AF.Copy,
                                                                         scale=rcp[:, 4 * (g % 2) + hl:4 * (g % 2) + hl + 1]),
                                     reads=[f"ps{ob}", ("b_rcp", g % 2)], writes=[("ytm", hh)])

                    for i in range(nseq + LOOK):
                        if i < nseq:
                            front(i)
                        if i >= LOOK:
                            back(i - LOOK)
                        if stq and (i % every == every - 1):
                            stq.pop(0)()
                    while stq:
                        stq.pop(0)()

                if NQ > 0:
                    const_mask(0)
                    emit_maskT(0)
                for qi in range(NQ):
                    nxt = qi + 1
                    steps = []
                    if nxt < NQ:
                        if nxt >= 2:
                            emit_scores(nxt)
                            steps = search_steps(nxt)
                        else:
                            const_mask(nxt)
                    attention_tile(qi, steps)
                    if nxt < NQ:
                        emit_maskT(nxt)
                    qs = slice(qi * 128, (qi + 1) * 128)
                    for j in range(8):
                        f = lambda: nc.tensor.transpose(out=psb[:, j * 128:(j + 1) * 128], in_=ytm[:, j * 128:(j + 1) * 128],
                                                        identity=ident)
                        if j < 7:
                            P.quiet("pe", f, reads=[("ytm", hh_) for hh_ in range(16)] + ["cstb"], writes=["psb"])
                        else:
                            P.op("pe", f, reads=[("ytm", hh_) for hh_ in range(16)] + ["cstb"], writes=["psb"])
                    P.op("act", lambda: nc.scalar.copy(out=yaT[:, :, qs], in_=bview(psb[:], 8)), reads=["psb"], writes=[("yaT", qi)])
                P.barrier()
            if stage == 2:
                o = dout("yaT_dbg", [128, 8, T], BF16)
                P.dma("sp", "out", [(o, yaT[:])])
                P.barrier()
                return nc, dbg
            P.dma("sp", "spill", [(ya_spill, yaT[:])])
            P.barrier()

        stB = contextlib.ExitStack()
        es.enter_context(stB)
        ysT = sb("ysT", [128, 16, T], BF16, stB)
        with contextlib.ExitStack() as sS:
            G8 = lambda t_, c_, g_: t_[:, c_, 8 * g_:8 * g_ + 8].unsqueeze(2).to_broadcast([128, 8, 64])
            wstS = sb("wstS", [128, 8, 256], F32, sS)
            selb = sb("selb", [128, 32, 128], BF16, sS)
            P.op("pool", lambda: nc.gpsimd.memset(selb[:], 0.0), writes=["selb"])
            for r3 in range(3):
                P.op("pool", lambda: nc.gpsimd.tensor_copy(
                    out=selb[32 * r3:32 * r3 + 32, :, :],
                    in_=cstb[32 * r3:32 * r3 + 32, C_ID + 32 * r3:C_ID + 32 * r3 + 32].unsqueeze(2).to_broadcast([32, 32, 128])),
                    reads=["cstb", "selb"], writes=["selb"])
            if sub == 101:
                P.barrier(); return nc, dbg
            for n_ in ("dt_bias", "a_log", "d_skip"):
                load_gain(n_, 32, sS)
            aneg = sb("aneg", [128, 32], F32, sS)
            P.op("act", lambda: nc.scalar.activation(out=aneg[:], in_=gb["a_log"][:], func=AF.Exp), reads=["g_a_log"], writes=["aneg"])
            P.op("dve", lambda: nc.vector.tensor_scalar(out=aneg[:], in0=aneg[:], scalar1=-1.0, scalar2=None, op0=ALU.mult),
                 reads=["aneg"], writes=["aneg"])
            if sub == 102:
                P.barrier(); return nc, dbg
            cwfm = sb("cwfm", [128, 24, 5], F32, sS)
            s0 = contextlib.ExitStack()
            cw5 = sb("cw5", [5, 3072], F32, s0)
            P.dma("sp", "cw5", [(cw5[0:4, :], convw_d), (cw5[4:5, :], gains["conv_b"])], writes=["cw5"])
            for t_ in range(24):
                f = lambda: nc.tensor.transpose(out=ps[0][:, t_ * 5:t_ * 5 + 5], in_=cw5[:, t_ * 128:(t_ + 1) * 128],
                                                identity=cst[0:5, C_ID:C_ID + 5])
                if t_ < 23:
                    P.quiet("pe", f, reads=["cw5", "cst"], writes=["ps0"])
                else:
                    P.op("pe", f, reads=["cw5", "cst"], writes=["ps0"])
            P.op("dve", lambda: nc.vector.tensor_copy(out=cwfm[:], in_=bview(ps[0][:, 0:120], 24)), reads=["ps0"], writes=["cwfm"])
            P.barrier()
            s0.close()
            if sub == 103:
                P.barrier(); return nc, dbg
            dt_all = sb("dt_all", [128, NT, 32], F32, sS)
            acs = sb("acs", [128, NT, 32], F32, sS)
            ea = sb("ea", [128, NT, 32], F32, sS)
            dtw = sb("dtw", [128, NT, 32], F32, sS)
            cdb = sb("cdb", [128, NT, 32], F32, sS)
            A3 = sb("A3", [128, NT, 128], BF16, sS)
            P.op("pool", lambda: nc.gpsimd.memset(A3[:], 0.0), writes=["A3z"])
            with contextlib.ExitStack() as s1:
                wdt_s = sb("wdt_s", [128, 8, 32], F32, s1)
                wdt = sb("wdt", [128, 8, 32], BF16, s1)
                P.dma("sp", "wdt", [(wdt_s[:], win_v[:, :, O_DT:O_DT + 32])], writes=["wdt_s"])
                P.op("pool", lambda: nc.gpsimd.tensor_copy(out=wdt[:], in_=wdt_s[:]), reads=["wdt_s"], writes=["wdt"])
                f32t = [sb(f"s1_{i}", [128, 32], F32, s1) for i in range(6)]
                a3 = sb("s1_a3", [128, 3, 32], F32, s1)
                Hb = sb("s1_Hb", [128, 128], BF16, s1)
                Mb = sb("s1_Mb", [128, 128], BF16, s1)
                r1 = sb("s1_r1", [128, 128], F32, s1)
                r2 = sb("s1_r2", [128, 128], F32, s1)
                ones_f = cst[:, C_ONE:C_ONE + 128]
                uinc = cst[:, C_CT:C_CT + 128]
                for c in range(NT):
                    cs_ = slice(c * 128, (c + 1) * 128)
                    xd, ax, ee, ll, rr, aa = f32t
                    for kt in range(8):
                        f = lambda: nc.tensor.matmul(ps[1][:, 0:32], lhsT=hT[:, kt, cs_], rhs=wdt[:, kt, :], start=(kt == 0), stop=(kt == 7))
                        if kt < 7:
                            P.quiet("pe", f, reads=["wdt"], writes=["ps1"])
                        else:
                            P.op("pe", f, reads=["wdt"], writes=["ps1"])
                    P.op("dve", lambda: nc.vector.tensor_tensor(out=xd[:], in0=ps[1][:, 0:32], in1=gb["dt_bias"][:], op=ALU.add),
                         reads=["ps1", "g_dt_bias"], writes=["s1_xd"])
                    P.op("act", lambda: nc.scalar.activation(out=ax[:], in_=xd[:], func=AF.Abs), reads=["s1_xd"], writes=["s1_ax"])
                    P.op("act", lambda: nc.scalar.activation(out=ee[:], in_=ax[:], func=AF.Exp, scale=-1.0), reads=["s1_ax"], writes=["s1_ee"])
                    P.op("act", lambda: nc.scalar.activation(out=ll[:], in_=ee[:], func=AF.Ln, bias=1.0), reads=["s1_ee"], writes=["s1_ll"])
                    P.op("dve", lambda: nc.vector.tensor_scalar(out=rr[:], in0=xd[:], scalar1=0.0, scalar2=None, op0=ALU.max),
                         reads=["s1_xd"], writes=["s1_rr"])
                    P.op("dve", lambda: nc.vector.tensor_tensor(out=dt_all[:, c, :], in0=rr[:], in1=ll[:], op=ALU.add),
                         reads=["s1_rr", "s1_ll"], writes=[("dt", c)])
                    P.op("dve", lambda: nc.vector.tensor_tensor(out=aa[:], in0=dt_all[:, c, :], in1=aneg[:], op=ALU.mult),
                         reads=[("dt", c), "aneg"], writes=["s1_aa"])
                    if sub == 104:
                        P.barrier(); return nc, dbg
                    P.op("dve", lambda: nc.vector.tensor_copy(out=a3[:], in_=aa[:].unsqueeze(1).to_broadcast([128, 3, 32])),
                         reads=["s1_aa"], writes=["s1_a3"])
                    if sub == 105:
                        P.barrier(); return nc, dbg
                    P.op("pe", lambda: nc.tensor.matmul(ps[2][:, 0:32], lhsT=uinc, rhs=aa[:], start=True, stop=True),
                         reads=["s1_aa", "cst"], writes=["ps2"])
                    P.op("pe", lambda: nc.tensor.matmul(ps[3][:, 0:32], lhsT=ones_f, rhs=aa[:], start=True, stop=True),
                         reads=["s1_aa", "cst"], writes=["ps3"])
                    P.op("pe", lambda: nc.tensor.matmul(ps[4][0:96, 0:128], lhsT=a3[:].rearrange("p a b -> p (a b)"), rhs=uinc,
                                                        start=True, stop=True),
                         reads=["s1_a3", "cst"], writes=["ps4"])
                    if sub == 106:
                        P.barrier(); return nc, dbg
                    P.op("dve", lambda: nc.vector.tensor_copy(out=acs[:, c, :], in_=ps[2][:, 0:32]), reads=["ps2"], writes=[("acs", c)])
                    if sub == 108:
                        P.barrier(); return nc, dbg
                    P.op("act", lambda: nc.scalar.activation(out=ea[:, c, :], in_=acs[:, c, :], func=AF.Exp), reads=[("acs", c)], writes=[("ea", c)])
                    P.op("dve", lambda: nc.vector.tensor_copy(out=rr[:], in_=ps[3][:, 0:32]), reads=["ps3"], writes=["s1_rr"])
                    P.op("act", lambda: nc.scalar.activation(out=cdb[:, c, :], in_=rr[:], func=AF.Exp), reads=["s1_rr"], writes=[("cdb", c)])
                    if sub == 109:
                        P.barrier(); return nc, dbg
                    P.op("dve", lambda: nc.vector.tensor_tensor(out=xd[:], in0=rr[:], in1=acs[:, c, :], op=ALU.subtract),
                         reads=["s1_rr", ("acs", c)], writes=["s1_xd"])
                    P.op("act", lambda: nc.scalar.activation(out=ee[:], in_=xd[:], func=AF.Exp), reads=["s1_xd"], writes=["s1_ee"])
                    P.op("dve", lambda: nc.vector.tensor_tensor(out=dtw[:, c, :], in0=dt_all[:, c, :], in1=ee[:], op=ALU.mult),
                         reads=[("dt", c), "s1_ee"], writes=[("dtw", c)])
                    if sub == 107:
                        P.barrier(); return nc, dbg
                    P.op("act", lambda: nc.scalar.copy(out=Hb[0:96, :], in_=ps[4][0:96, 0:128]), reads=["ps4"], writes=["s1_Hb"])
                    P.op("dve", lambda: nc.vector.tensor_tensor(out=r1[0:96, :], in0=ps[4][0:96, 0:128], in1=Hb[0:96, :], op=ALU.subtract),
                         reads=["ps4", "s1_Hb"], writes=["s1_r1"])
                    P.op("act", lambda: nc.scalar.copy(out=Mb[0:96, :], in_=r1[0:96, :]), reads=["s1_r1"], writes=["s1_Mb"])
                    P.op("dve", lambda: nc.vector.tensor_tensor(out=r2[0:96, :], in0=r1[0:96, :], in1=Mb[0:96, :], op=ALU.subtract),
                         reads=["s1_r1", "s1_Mb"], writes=["s1_r2"])
                    P.op("pool", lambda: nc.gpsimd.tensor_copy(out=A3[0:32, c, :], in_=Hb[0:32, :]), reads=["s1_Hb", "A3z"], writes=[("A3", c, 0)])
                    P.op("pool", lambda: nc.gpsimd.tensor_copy(out=A3[32:64, c, :], in_=Mb[32:64, :]), reads=["s1_Mb", "A3z"], writes=[("A3", c, 1)])
                    P.op("act", lambda: nc.scalar.copy(out=A3[64:96, c, :], in_=r2[64:96, :]), reads=["s1_r2", "A3z"], writes=[("A3", c, 2)])
                P.barrier()
            if stage == 3 and sub == 1:
                for nm, t_ in [("dt_all", dt_all), ("acs", acs), ("ea", ea), ("dtw", dtw), ("cdb", cdb)]:
                    o = dout(nm + "_dbg", [128, NT, 32], F32)
                    P.dma("sp", "out", [(o, t_[:])])
                o = dout("A3_dbg", [128, NT, 128], BF16)
                P.dma("sp", "out", [(o, A3[:])])
                o = dout("cwfm_dbg", [128, 24, 5], F32)
                P.dma("sp", "out", [(o, cwfm[:])])
                o = dout("selb_dbg", [128, 32, 128], BF16)
                P.dma("sp", "out", [(o, selb[:])])
                P.barrier()
                return nc, dbg

            xs_tm = sb("xs_tm", [128, NT, 512], BF16, sS)
            BT = sb("BT", [128, T], BF16, sS)
            CT = sb("CT", [128, T], BF16, sS)
            B_tm = sb("B_tm", [128, NT, 128], BF16, sS)
            rawb = sb("rawb", [128, T + 4], BF16, sS)
            xcf = [sb(f"xcf{i}", [128, 512], BF16, sS) for i in range(2)]
            dg = [sb(f"dg{i}", [128, 4, 128], BF16, sS) for i in range(2)]
            wch = [sb(f"wch{i}", [128, 8, 128], BF16, sS) for i in range(2)]
            wz = sb("wz", [128, 8, 512], BF16, sS)
            ssdg = sb("ssdg", [128, 512], F32, sS)
            hst = sb("hst", [128, 512], F32, sS)
            hstb = sb("hstb", [128, 512], BF16, sS)
            cbm = sb("cbm", [128, 128], F32, sS)
            seg = [sb(f"seg{i}", [128, 512], F32, sS) for i in range(2)]
            Ee = seg
            MT = [sb(f"MT{i}", [128, 512], BF16, sS) for i in range(4)]
            xdt = [sb(f"xdt{i}", [128, 512], BF16, sS) for i in range(2)]
            xw = [sb(f"xw{i}", [128, 512], BF16, sS) for i in range(2)]
            t1 = sb("t1", [128, 512], F32, sS)
            t1b = [t1, sb("t1b", [128, 512], F32, sS)]
            dsk = sb("dsk", [128, 8, 128], BF16, sS)
            epsb = sb("epsb", [128, 1], F32, sS)
            P.op("pool", lambda: nc.gpsimd.memset(epsb[:], EPS), writes=["epsb"])
            t3 = sb("t3", [128, 512], F32, sS)
            yv = t1
            sz = t3
            ynb = [sb(f"ynb{i}", [128, 512], BF16, sS) for i in range(2)]
            sjk = xcf[0]
            g1 = [sb(f"g1_{i}", [128, 2], F32, sS) for i in range(4)]
            P.op("pool", lambda: nc.gpsimd.memset(rawb[:, 0:4], 0.0), writes=["rawb_halo"])
            wctr2 = [0]
            for g in range(4 if sub >= 30 else 1):
                for hf in range(2):
                    c0 = O_Z + g * 512 + hf * 256
                    P.dma("sp", "wstS", [(wstS[:], win_v[:, :, c0:c0 + 256])], writes=["wstS"])
                    P.op("pool", lambda: nc.gpsimd.tensor_copy(out=wz[:, :, hf * 256:(hf + 1) * 256], in_=wstS[:]),
                         reads=["wstS"], writes=[("wz", hf)])
                P.dma("sp", "ssdg", [(ssdg[:], gains["ssd_norm"][:, g * 512:(g + 1) * 512].partition_broadcast(128))], writes=["ssdg"])
                chts = [(O_XBC + g * 512 + j * 128, 4 * g + j, "x", j) for j in range(4)]
                chts += [(O_XBC + 2048 + g * 128, 16 + g, "B", 0), (O_XBC + 2560 + g * 128, 20 + g, "C", 0)]
                for (c0, cti, kind, j) in chts:
                    wi = wctr2[0] % 2
                    wctr2[0] += 1
                    P.dma("sp", "wstS", [(wstS[:, :, 0:128], win_v[:, :, c0:c0 + 128])], writes=["wstS"])
                    P.op("pool", lambda: nc.gpsimd.tensor_copy(out=wch[wi][:], in_=wstS[:, :, 0:128]), reads=["wstS"], writes=[f"wch{wi}"])
                    for jj in range(4):
                        P.op("dve", lambda: nc.vector.tensor_scalar(out=dg[wi][:, jj, :], in0=ident, scalar1=cwfm[:, cti, jj:jj + 1],
                                                                    scalar2=None, op0=ALU.mult),
                             reads=["cstb", "cwfm"], writes=[(f"dg{wi}", jj)])
                    for tb in range(4):
                        bank = tb % 2
                        for kt in range(8):
                            f = lambda: nc.tensor.matmul(ps[bank][:], lhsT=wch[wi][:, kt, :], rhs=hT[:, kt, tb * 512:(tb + 1) * 512],
                                                         start=(kt == 0), stop=(kt == 7))
                            if kt < 7:
                                P.quiet("pe", f, reads=[f"wch{wi}"], writes=[f"ps{bank}"])
                            else:
                                P.op("pe", f, reads=[f"wch{wi}"], writes=[f"ps{bank}"])
                        P.op("act", lambda: nc.scalar.copy(out=rawb[:, 4 + tb * 512:4 + (tb + 1) * 512], in_=ps[bank][:]),
                             reads=[f"ps{bank}"], writes=[("rawb", tb)])
                    for tb in range(4):
                        bank = 2 + tb % 2
                        for jj in range(4):
                            f = lambda: nc.tensor.matmul(ps[bank][:], lhsT=dg[wi][:, jj, :],
                                                         rhs=rawb[:, 1 + tb * 512 + jj:1 + tb * 512 + jj + 512],
                                                         start=(jj == 0), stop=(jj == 3))
                            rd = [(f"dg{wi}", jj), ("rawb", tb), "rawb_halo"] + ([("rawb", tb - 1)] if tb > 0 else [])
                            if jj < 3:
                                P.quiet("pe", f, reads=rd, writes=[f"ps{bank}"])
                            else:
                                P.op("pe", f, reads=rd, writes=[f"ps{bank}"])
                        if kind == "x":
                            xb_ = tb % 2
                            P.op("act", lambda: nc.scalar.activation(out=xcf[xb_][:], in_=ps[bank][:], func=AF.Silu,
                                                                     bias=cwfm[:, cti, 4:5]),
                                 reads=[f"ps{bank}", "cwfm"], writes=[f"xcf{xb_}"])
                            for i4 in range(4):
                                f = lambda: nc.tensor.transpose(out=psb[:, i4 * 128:(i4 + 1) * 128], in_=xcf[xb_][:, i4 * 128:(i4 + 1) * 128],
                                                                identity=ident)
                                if i4 < 3:
                                    P.quiet("pe", f, reads=[f"xcf{xb_}", "cstb"], writes=["psb"])
                                else:
                                    P.op("pe", f, reads=[f"xcf{xb_}", "cstb"], writes=["psb"])
                            P.op("act", lambda: nc.scalar.copy(out=xs_tm[:, tb * 4:(tb + 1) * 4, j * 128:(j + 1) * 128],
                                                               in_=bview(psb[:, 0:512], 4)),
                                 reads=["psb"], writes=[("xs_tm", tb, j)])
                        else:
                            dstT = BT if kind == "B" else CT
                            P.op("act", lambda: nc.scalar.activation(out=dstT[:, tb * 512:(tb + 1) * 512], in_=ps[bank][:], func=AF.Silu,
                                                                     bias=cwfm[:, cti, 4:5]),
                                 reads=[f"ps{bank}", "cwfm"], writes=[(kind + "T", tb)])
                if sub == 202:
                    P.barrier(); return nc, dbg
                for k0 in range(0, NT, 8):
                    for jj in range(8):
                        cc = k0 + jj
                        f = lambda: nc.tensor.transpose(out=psb[:, jj * 128:(jj + 1) * 128], in_=BT[:, cc * 128:(cc + 1) * 128], identity=ident)
                        if jj < 7:
                            P.quiet("pe", f, reads=[("BT", cc // 4), "cstb"], writes=["psb"])
                        else:
                            P.op("pe", f, reads=[("BT", cc // 4), "cstb"], writes=["psb"])
                    P.op("act", lambda: nc.scalar.copy(out=B_tm[:, k0:k0 + 8, :], in_=bview(psb[:], 8)), reads=["psb"], writes=[("B_tm", k0 // 8)])
                for hl_ in range(8):
                    P.op("dve", lambda: nc.vector.tensor_scalar(out=dsk[:, hl_, :], in0=ident, scalar1=gb["d_skip"][:, 8 * g + hl_:8 * g + hl_ + 1],
                                                                scalar2=None, op0=ALU.mult),
                         reads=["cstb", "g_d_skip"], writes=["dsk"])
                P.op("pool", lambda: nc.gpsimd.memset(hst[:], 0.0), writes=["hst"])
                P.op("pool", lambda: nc.gpsimd.memset(hstb[:], 0.0), writes=["hstb"])
                if sub == 203:
                    P.barrier(); return nc, dbg
                xsr = lambda c_: [("xs_tm", c_ // 4, j_) for j_ in range(4)]
                def front(c):
                    cs_ = slice(c * 128, (c + 1) * 128)
                    pb = c % 2
                    P.op("pe", lambda: nc.tensor.matmul(ps[0][:, 0:128], lhsT=BT[:, cs_], rhs=CT[:, cs_], start=True, stop=True),
                         reads=[("BT", c // 4), ("CT", c // 4)], writes=["ps0"])
                    P.op("dve", lambda: nc.vector.tensor_tensor(out=cbm[:], in0=ps[0][:, 0:128], in1=cst[:, C_CT:C_CT + 128], op=ALU.mult),
                         reads=["ps0", "cst"], writes=["cbm"])
                    P.op("pool", lambda: nc.gpsimd.tensor_tensor(out=bview(xdt[pb][:], 8), in0=bview(xs_tm[:, c, :], 8), in1=G8(dt_all, c, g),
                                                                 op=ALU.mult), reads=xsr(c), writes=[f"xdt{pb}"])
                    P.op("pool", lambda: nc.gpsimd.tensor_tensor(out=bview(xw[pb][:], 8), in0=bview(xs_tm[:, c, :], 8), in1=G8(dtw, c, g),
                                                                 op=ALU.mult), reads=xsr(c), writes=[f"xw{pb}"])
                    for hb in range(2):
                        bcb = 1 + hb
                        for hh in range(4):
                            h = 8 * g + 4 * hb + hh
                            f = lambda: nc.tensor.matmul(ps[bcb][:, hh * 128:(hh + 1) * 128], lhsT=selb[:, h, :], rhs=A3[:, c, :],
                                                         start=True, stop=True, skip_group_check=True)
                            if hh < 3:
                                P.quiet("pe", f, reads=["selb"], writes=[f"ps{bcb}"])
                            else:
                                P.op("pe", f, reads=["selb"], writes=[f"ps{bcb}"])
                    for hb in range(2):
                        bcb = 1 + hb
                        for hh in range(4):
                            h = 8 * g + 4 * hb + hh
                            P.op("dve", lambda: nc.vector.tensor_scalar(out=seg[hb][:, hh * 128:(hh + 1) * 128],
                                                                        in0=ps[bcb][:, hh * 128:(hh + 1) * 128],
                                                                        scalar1=acs[:, c, h:h + 1], scalar2=0.0, op0=ALU.subtract, op1=ALU.min),
                                 reads=[f"ps{bcb}"], writes=[(f"seg{hb}", hh), f"Ee{hb}"])
                        P.op("act", lambda: nc.scalar.activation(out=Ee[hb][:], in_=seg[hb][:], func=AF.Exp),
                             reads=[(f"seg{hb}", hh_) for hh_ in range(4)], writes=[f"Ee{hb}"] + [(f"seg{hb}", hh_) for hh_ in range(4)])
                    for hb in range(2):
                        mi = 2 * pb + hb
                        P.op("dve", lambda: nc.vector.tensor_tensor(out=bview(MT[mi][:], 4), in0=bview(Ee[hb][:], 4),
                                                                    in1=cbm[:].unsqueeze(1).to_broadcast([128, 4, 128]), op=ALU.mult),
                             reads=[f"Ee{hb}", "cbm"], writes=[f"MT{mi}"])

                def back(c):
                    cs_ = slice(c * 128, (c + 1) * 128)
                    pb = c % 2
                    P.op("pe", lambda: nc.tensor.matmul(ps[5][:], lhsT=B_tm[:, c, :], rhs=xw[pb][:], start=True, stop=True),
                         reads=[("B_tm", c // 8), f"xw{pb}"], writes=["ps5"])
                    for hb in range(2):
                        mi = 2 * pb + hb
                        for hh in range(4):
                            hl = 4 * hb + hh
                            P.quiet("pe", lambda: nc.tensor.matmul(ps[3][:, hl * 64:(hl + 1) * 64], lhsT=MT[mi][:, hh * 128:(hh + 1) * 128],
                                                                   rhs=xdt[pb][:, hl * 64:(hl + 1) * 64], start=True, stop=False, skip_group_check=True),
                                    reads=[f"MT{mi}", f"xdt{pb}"], writes=["ps3"])
                            f = lambda: nc.tensor.matmul(ps[3][:, hl * 64:(hl + 1) * 64], lhsT=dsk[:, hl, :],
                                                         rhs=xs_tm[:, c, hl * 64:(hl + 1) * 64], start=False, stop=True, skip_group_check=True)
                            if hl < 7:
                                P.quiet("pe", f, reads=["dsk"] + xsr(c), writes=["ps3"])
                            else:
                                P.op("pe", f, reads=["dsk"] + xsr(c), writes=["ps3"])
                    for kt in range(8):
                        f = lambda: nc.tensor.matmul(ps[6][:], lhsT=hT[:, kt, cs_], rhs=wz[:, kt, :], start=(kt == 0), stop=(kt == 7))
                        if kt < 7:
                            P.quiet("pe", f, reads=[("wz", 0), ("wz", 1)], writes=["ps6"])
                        else:
                            P.op("pe", f, reads=[("wz", 0), ("wz", 1)], writes=["ps6"])
                    P.op("act", lambda: nc.scalar.activation(out=sz[:], in_=ps[6][:], func=AF.Silu), reads=["ps6"], writes=["t3"])
                    tb_ = t1b[pb]
                    tn = f"t1_{pb}"
                    if c > 0:
                        P.op("pe", lambda: nc.tensor.matmul(ps[4][:], lhsT=CT[:, cs_], rhs=hstb[:], start=True, stop=True),
                             reads=[("CT", c // 4), "hstb"], writes=["ps4"])
                        P.op("dve", lambda: nc.vector.tensor_tensor(out=bview(tb_[:], 8), in0=bview(ps[4][:], 8), in1=G8(ea, c, g), op=ALU.mult),
                             reads=["ps4"], writes=[tn])
                        P.op("dve", lambda: nc.vector.tensor_tensor(out=tb_[:], in0=ps[3][:], in1=tb_[:], op=ALU.add),
                             reads=["ps3", tn], writes=[tn])
                    else:
                        P.op("dve", lambda: nc.vector.tensor_copy(out=tb_[:], in_=ps[3][:]), reads=["ps3"], writes=[tn])
                    P.op("dve", lambda: nc.vector.tensor_tensor(out=bview(hst[:], 8), in0=bview(hst[:], 8), in1=G8(cdb, c, g), op=ALU.mult),
                         reads=["hst"], writes=["hst"])
                    P.op("dve", lambda: nc.vector.tensor_tensor(out=hst[:], in0=ps[5][:], in1=hst[:], op=ALU.add), reads=["ps5", "hst"], writes=["hst"])
                    P.op("act", lambda: nc.scalar.copy(out=hstb[:], in_=hst[:]), reads=["hst"], writes=["hstb"])
                    P.op("dve", lambda: nc.vector.tensor_tensor(out=tb_[:], in0=tb_[:], in1=sz[:], op=ALU.mult), reads=[tn, "t3"], writes=[tn])
                    P.op("act", lambda: nc.scalar.activation(out=sjk[:], in_=tb_[:], func=AF.Square, accum_out=g1[0][:, pb:pb + 1]),
                         reads=[tn], writes=["xcf0", ("g1_0", pb)])
                    P.op("act", lambda: nc.scalar.activation(out=g1[2][:, pb:pb + 1], in_=g1[0][:, pb:pb + 1], func=AF.Sqrt, scale=1.0 / 512, bias=epsb[:, 0:1]),
                         reads=[("g1_0", pb), "epsb"], writes=[("g1_2", pb)])

                def backB(c):
                    pb = c % 2
                    tb_ = t1b[pb]
                    tn = f"t1_{pb}"
                    P.op("dve", lambda: nc.vector.reciprocal(out=g1[3][:, pb:pb + 1], in_=g1[2][:, pb:pb + 1]), reads=[("g1_2", pb)], writes=[("g1_3", pb)])
                    P.op("dve", lambda: nc.vector.scalar_tensor_tensor(out=ynb[pb][:], in0=tb_[:], scalar=g1[3][:, pb:pb + 1], in1=ssdg[:], op0=ALU.mult, op1=ALU.mult),
                         reads=[tn, ("g1_3", pb), "ssdg"], writes=[f"ynb{pb}"])

                def tail(c):
                    cs_ = slice(c * 128, (c + 1) * 128)
                    pb = c % 2
                    for i4 in range(4):
                        f = lambda: nc.tensor.transpose(out=psb[:, i4 * 128:(i4 + 1) * 128], in_=ynb[pb][:, i4 * 128:(i4 + 1) * 128], identity=ident)
                        if i4 < 3:
                            P.quiet("pe", f, reads=[f"ynb{pb}", "cstb"], writes=["psb"])
                        else:
                            P.op("pe", f, reads=[f"ynb{pb}", "cstb"], writes=["psb"])
                    P.op("act", lambda: nc.scalar.copy(out=ysT[:, 4 * g:4 * g + 4, cs_], in_=bview(psb[:, 0:512], 4)), reads=["psb"], writes=[("ysT", g, c)])

                front(0)
                for c in range(NT):
                    if c + 1 < NT:
                        front(c + 1)
                    back(c)
                    if c >= 1:
                        backB(c - 1)
                        tail(c - 1)
                backB(NT - 1)
                tail(NT - 1)
            P.barrier()
        if stage == 3:
            o = dout("ysT_dbg", [128, 16, T], BF16)
            P.dma("sp", "out", [(o, ysT[:])])
            P.barrier()
            return nc, dbg

        stM = contextlib.ExitStack()
        es.enter_context(stM)
        mgT = sb("mgT", [128, 8, T], BF16, stM)
        with contextlib.ExitStack() as sM:
            yaT2 = sb("yaT2", [128, 8, T], BF16, sM)
            P.dma("sp", "ya_reload", [(yaT2[:], ya_spill)], writes=["yaT2"])
            wstM = sb("wstM", [128, 16, 128], F32, sM)
            wac = [sb(f"wac{i}", [128, 8, 128], BF16, sM) for i in range(2)]
            wbc = [sb(f"wbc{i}", [128, 16, 128], BF16, sM) for i in range(2)]
            wgac = [sb(f"wgac{i}", [128, 8, 128], BF16, sM) for i in range(2)]
            wgbc = [sb(f"wgbc{i}", [128, 8, 128], BF16, sM) for i in range(2)]
            sga = [sb(f"sga{i}", [128, 512], F32, sM) for i in range(2)]
            sgb = [sb(f"sgb{i}", [128, 512], F32, sM) for i in range(2)]
            wa_v = wa_d.rearrange("(kt p) n -> p kt n", p=128)
            wb_v = wb_d.rearrange("(kt p) n -> p kt n", p=128)
            it = [0]

            def load_merge_w(nt):
                ns = slice(nt * 128, (nt + 1) * 128)
                wp = nt % 2
                for (dst, src, nk_, nm) in [(wgac[wp], win_v[:, :, O_GA + nt * 128:O_GA + (nt + 1) * 128], 8, f"wgac{wp}"),
                                            (wgbc[wp], win_v[:, :, O_GB + nt * 128:O_GB + (nt + 1) * 128], 8, f"wgbc{wp}"),
                                            (wac[wp], wa_v[:, :, ns], 8, f"wac{wp}"), (wbc[wp], wb_v[:, :, ns], 16, f"wbc{wp}")]:
                    P.dma("sp", "wstM", [(wstM[:, 0:nk_, :], src)], writes=["wstM"])
                    P.op("dve", lambda: nc.vector.tensor_copy(out=dst[:], in_=wstM[:, 0:nk_, :]), reads=["wstM"], writes=[nm])

            load_merge_w(0)
            for nt in range(8):
                wp = nt % 2
                if nt + 1 < 8:
                    load_merge_w(nt + 1)
                for tb in range(4):
                    ts_ = slice(tb * 512, (tb + 1) * 512)
                    par = it[0] % 2
                    it[0] += 1
                    bA, bB, bGA = (0, 1, 2) if par == 0 else (4, 5, 6)
                    bGB = 3

                    def acc(bank, wt, nk_, rhsT, nm, rd):
                        for kt in range(nk_):
                            f = lambda: nc.tensor.matmul(ps[bank][:], lhsT=wt[:, kt, :], rhs=rhsT[:, kt, ts_], start=(kt == 0), stop=(kt == nk_ - 1))
                            if kt < nk_ - 1:
                                P.quiet("pe", f, reads=[nm] + rd, writes=[f"ps{bank}"])
                            else:
                                P.op("pe", f, reads=[nm] + rd, writes=[f"ps{bank}"])
                    acc(bGA, wgac[wp], 8, hT, f"wgac{wp}", [])
                    acc(bGB, wgbc[wp], 8, hT, f"wgbc{wp}", [])
                    acc(bA, wac[wp], 8, yaT2, f"wac{wp}", ["yaT2"])
                    acc(bB, wbc[wp], 16, ysT, f"wbc{wp}", [])
                    P.op("act", lambda: nc.scalar.activation(out=sga[par][:], in_=ps[bGA][:], func=AF.Sigmoid), reads=[f"ps{bGA}"], writes=[f"sga{par}"])
                    P.op("act", lambda: nc.scalar.activation(out=sgb[par][:], in_=ps[bGB][:], func=AF.Sigmoid), reads=[f"ps{bGB}"], writes=[f"sgb{par}"])
                    P.op("dve", lambda: nc.vector.tensor_tensor(out=sga[par][:], in0=ps[bA][:], in1=sga[par][:], op=ALU.mult),
                         reads=[f"ps{bA}", f"sga{par}"], writes=[f"sga{par}"])
                    P.op("dve", lambda: nc.vector.tensor_tensor(out=sgb[par][:], in0=ps[bB][:], in1=sgb[par][:], op=ALU.mult),
                         reads=[f"ps{bB}", f"sgb{par}"], writes=[f"sgb{par}"])
                    P.op("pool", lambda: nc.gpsimd.tensor_tensor(out=mgT[:, nt, ts_], in0=sga[par][:], in1=sgb[par][:], op=ALU.add),
                         reads=[f"sga{par}", f"sgb{par}"], writes=[("mgT", nt, tb)])
            P.barrier()
        if stage == 4:
            o = dout("mgT_dbg", [128, 8, T], BF16)
            P.dma("sp", "out", [(o, mgT[:])])
            P.barrier()
            return nc, dbg

        x1 = ysT[:].bitcast(F32)
        assert list(x1.shape) == [128, NT, D], x1.shape
        with contextlib.ExitStack() as sO:
            wstO = sb("wstO", [128, 8, 256], F32, sO)
            wo = sb("wo", [128, 8, D], BF16, sO)
            wo_v = wo_d.rearrange("(kt p) n -> p kt n", p=128)
            for q4 in range(4):
                P.dma("sp", "wstO", [(wstO[:], wo_v[:, :, q4 * 256:(q4 + 1) * 256])], writes=["wstO"])
                P.op("pool", lambda: nc.gpsimd.tensor_copy(out=wo[:, :, q4 * 256:(q4 + 1) * 256], in_=wstO[:]), reads=["wstO"], writes=[("wo", q4)])
            for c in range(NT):
                cs_ = slice(c * 128, (c + 1) * 128)
                P.dma("sp", f"x1ld{c}", [(x1[:, c, :], x_d[cs_, :])], writes=[("x1", c)])
                for hf in range(2):
                    bank = (2 * c + hf) % 4
                    for kt in range(8):
                        f = lambda: nc.tensor.matmul(ps[bank][:], lhsT=mgT[:, kt, cs_], rhs=wo[:, kt, hf * 512:(hf + 1) * 512],
                                                     start=(kt == 0), stop=(kt == 7))
                        rd = [("wo", 2 * hf), ("wo", 2 * hf + 1)]
                        if kt < 7:
                            P.quiet("pe", f, reads=rd, writes=[f"ps{bank}"])
                        else:
                            P.op("pe", f, reads=rd, writes=[f"ps{bank}"])
                    P.op("dve", lambda: nc.vector.tensor_tensor(out=x1[:, c, hf * 512:(hf + 1) * 512], in0=ps[bank][:],
                                                                in1=x1[:, c, hf * 512:(hf + 1) * 512], op=ALU.add),
                         reads=[f"ps{bank}", ("x1", c)], writes=[("x1", c)])
            P.barrier()
        stM.close()
        if stage == 5:
            o = dout("x1_dbg", [128, NT, D], F32)
            P.dma("sp", "out", [(o, x1)])
            P.barrier()
            return nc, dbg

        with contextlib.ExitStack() as sN:
            load_gain("ffn_norm", D, sN)

            def from_x1(c, bufs):
                return x1[:, c, :], ("x1", c)
            rmsnorm_T(from_x1, "ffn_norm", hT, sN, nbuf=0)
            P.barrier()

        with contextlib.ExitStack() as sE:
            selm = sb("selm", [128, 32, 128], BF16, sE)
            P.op("pool", lambda: nc.gpsimd.memset(selm[:], 0.0), writes=["selm"])
            for r3 in range(3):
                P.op("pool", lambda: nc.gpsimd.tensor_copy(
                    out=selm[32 * r3:32 * r3 + 32, :, :],
                    in_=cstb[32 * r3:32 * r3 + 32, C_ID + 32 * r3:C_ID + 32 * r3 + 32].unsqueeze(2).to_broadcast([32, 32, 128])),
                    reads=["cstb", "selm"], writes=["selm"])
            cT3 = sb("cT3", [128, T], BF16, sE)
            P.op("pool", lambda: nc.gpsimd.memset(cT3[:], 0.0), writes=["cT3z"])
            with contextlib.ExitStack() as sR:
                wr_s = sb("wr_s", [128, 8, 36], F32, sR)
                wr = sb("wr", [128, 8, 36], BF16, sR)
                P.dma("sp", "wr_s", [(wr_s[:, :, 0:4], wrg_d.rearrange("(kt p) n -> p kt n", p=128)),
                                     (wr_s[:, :, 4:36], wre_d.rearrange("(kt p) n -> p kt n", p=128))], writes=["wr_s"])
                P.op("pool", lambda: nc.gpsimd.tensor_copy(out=wr[:], in_=wr_s[:]), reads=["wr_s"], writes=["wr"])
                rb_ = sb("rbias", [128, 36], F32, sR)
                P.dma("sp", "rbias", [(rb_[:, 0:4], gains["b_route_group"].partition_broadcast(128)),
                                      (rb_[:, 4:36], gains["b_route_expert"].partition_broadcast(128))], writes=["rbias"])
                def mk_scratch(p):
                    S = {}
                    S["lg"] = sb(f"r_lg{p}", [128, 36], F32, sR)
                    S["c1"] = [sb(f"r_c{i}_{p}", [128, 1], F32, sR) for i in range(8)]
                    S["oh"] = sb(f"r_oh{p}", [128, 4], F32, sR)
                    S["ge"] = sb(f"r_ge{p}", [128, 4], F32, sR)
                    S["t48"] = sb(f"r_t48{p}", [128, 4, 8], F32, sR)
                    S["ein"] = sb(f"r_ein{p}", [128, 8], F32, sR)
                    S["top8"] = sb(f"r_top8{p}", [128, 8], F32, sR)
                    S["msel"] = sb(f"r_msel{p}", [128, 8], F32, sR)
                    S["wex"] = sb(f"r_wex{p}", [128, 8], F32, sR)
                    S["comb"] = sb(f"r_comb{p}", [128, 4, 8], F32, sR)
                    S["comb3"] = sb(f"r_comb3{p}", [128, 3, 32], F32, sR)
                    S["Hb"] = sb(f"r_Hb{p}", [128, 128], BF16, sR)
                    S["Mb"] = sb(f"r_Mb{p}", [128, 128], BF16, sR)
                    S["q1"] = sb(f"r_q1{p}", [128, 128], F32, sR)
                    S["q2"] = sb(f"r_q2{p}", [128, 128], F32, sR)
                    return S
                SC = [mk_scratch(0), mk_scratch(1)]

                def router_tile(c, p):
                    S = SC[p]
                    n = lambda x: f"{x}{p}"
                    bl, bt = (0, 1) if p == 0 else (2, 3)
                    lg, oh, ge, tmp48, ein, top8, msel, wex, comb, comb3 = (S[k] for k in ("lg", "oh", "ge", "t48", "ein", "top8", "msel", "wex", "comb", "comb3"))
                    Hb2, Mb2, q1, q2 = S["Hb"], S["Mb"], S["q1"], S["q2"]
                    mx, nmx, sme, gw, m21, den, rden, sc_ = S["c1"]
                    cs_ = slice(c * 128, (c + 1) * 128)
                    for kt in range(8):
                        f = lambda kt=kt: nc.tensor.matmul(ps[bl][:, 0:36], lhsT=hT[:, kt, cs_], rhs=wr[:, kt, :], start=(kt == 0), stop=(kt == 7))
                        if kt < 7:
                            P.quiet("pe", f, reads=["wr"], writes=[f"ps{bl}"])
                        else:
                            P.op("pe", f, reads=["wr"], writes=[f"ps{bl}"])
                    P.op("dve", lambda: nc.vector.tensor_tensor(out=lg[:], in0=ps[bl][:, 0:36], in1=rb_[:], op=ALU.add), reads=[f"ps{bl}", "rbias"], writes=[n("r_lg")])
                    P.op("dve", lambda: nc.vector.tensor_reduce(out=mx[:], in_=lg[:, 0:4], axis=AX.X, op=ALU.max), reads=[n("r_lg")], writes=[n("r_mx")])
                    P.op("dve", lambda: nc.vector.tensor_scalar(out=oh[:], in0=lg[:, 0:4], scalar1=mx[:, 0:1], scalar2=None, op0=ALU.is_ge),
                         reads=[n("r_lg"), n("r_mx")], writes=[n("r_oh")])
                    P.op("dve", lambda: nc.vector.tensor_scalar(out=nmx[:], in0=mx[:], scalar1=-1.0, scalar2=None, op0=ALU.mult), reads=[n("r_mx")], writes=[n("r_nmx")])
                    P.op("act", lambda: nc.scalar.activation(out=ge[:], in_=lg[:, 0:4], func=AF.Exp, bias=nmx[:, 0:1], accum_out=sme[:]),
                         reads=[n("r_lg"), n("r_nmx")], writes=[n("r_ge"), n("r_sme")])
                    P.op("dve", lambda: nc.vector.tensor_tensor(out=tmp48[:], in0=bview(lg[:, 4:36], 4), in1=oh[:].unsqueeze(2).to_broadcast([128, 4, 8]),
                                                                op=ALU.mult), reads=[n("r_lg"), n("r_oh")], writes=[n("r_t48")])
                    P.op("dve", lambda: nc.vector.tensor_reduce(out=ein[:], in_=tmp48[:].rearrange("p g e -> p e g"), axis=AX.X, op=ALU.add),
                         reads=[n("r_t48")], writes=[n("r_ein")])
                    P.op("dve", lambda: nc.vector.max(out=top8[:], in_=ein[:]), reads=[n("r_ein")], writes=[n("r_top8")])
                    P.op("dve", lambda: nc.vector.tensor_scalar(out=msel[:], in0=ein[:], scalar1=top8[:, 1:2], scalar2=None, op0=ALU.is_ge),
                         reads=[n("r_ein"), n("r_top8")], writes=[n("r_msel")])
                    P.op("dve", lambda: nc.vector.tensor_scalar(out=den[:], in0=top8[:, 0:1], scalar1=-1.0, scalar2=None, op0=ALU.mult),
                         reads=[n("r_top8")], writes=[n("r_nm1")])
                    P.op("act", lambda: nc.scalar.activation(out=wex[:], in_=ein[:], func=AF.Exp, bias=den[:, 0:1]), reads=[n("r_ein"), n("r_nm1")], writes=[n("r_wex")])
                    P.op("act", lambda: nc.scalar.activation(out=m21[:], in_=top8[:, 1:2], func=AF.Exp, bias=den[:, 0:1]), reads=[n("r_top8"), n("r_nm1")], writes=[n("r_m21")])
                    P.op("dve", lambda: nc.vector.scalar_tensor_tensor(out=rden[:], in0=m21[:], scalar=1.0, in1=sme[:], op0=ALU.add, op1=ALU.mult),
                         reads=[n("r_m21"), n("r_sme")], writes=[n("r_rden")])
                    P.op("dve", lambda: nc.vector.reciprocal(out=sc_[:], in_=rden[:]), reads=[n("r_rden")], writes=[n("r_sc")])
                    P.op("dve", lambda: nc.vector.scalar_tensor_tensor(out=wex[:], in0=wex[:], scalar=sc_[:, 0:1], in1=msel[:], op0=ALU.mult, op1=ALU.mult),
                         reads=[n("r_wex"), n("r_sc"), n("r_msel")], writes=[n("r_wex")])
                    P.op("dve", lambda: nc.vector.tensor_tensor(out=comb[:], in0=oh[:].unsqueeze(2).to_broadcast([128, 4, 8]),
                                                                in1=wex[:].unsqueeze(1).to_broadcast([128, 4, 8]), op=ALU.mult),
                         reads=[n("r_oh"), n("r_wex")], writes=[n("r_comb")])
                    P.op("dve", lambda: nc.vector.tensor_copy(out=comb3[:], in_=comb[:].rearrange("p g e -> p (g e)").unsqueeze(1).to_broadcast([128, 3, 32])),
                         reads=[n("r_comb")], writes=[n("r_comb3")])
                    P.op("pe", lambda: nc.tensor.transpose(out=ps[bt][0:96, 0:128], in_=comb3[:].rearrange("p a b -> p (a b)"),
                                                           identity=cst[:, C_ID:C_ID + 128]),
                         reads=[n("r_comb3"), "cst"], writes=[f"ps{bt}"])
                    P.op("act", lambda: nc.scalar.copy(out=Hb2[0:96, :], in_=ps[bt][0:96, 0:128]), reads=[f"ps{bt}"], writes=[n("r_Hb")])
                    P.op("dve", lambda: nc.vector.tensor_tensor(out=q1[0:96, :], in0=ps[bt][0:96, 0:128], in1=Hb2[0:96, :], op=ALU.subtract),
                         reads=[f"ps{bt}", n("r_Hb")], writes=[n("r_q1")])
                    P.op("act", lambda: nc.scalar.copy(out=Mb2[0:96, :], in_=q1[0:96, :]), reads=[n("r_q1")], writes=[n("r_Mb")])
                    P.op("dve", lambda: nc.vector.tensor_tensor(out=q2[0:96, :], in0=q1[0:96, :], in1=Mb2[0:96, :], op=ALU.subtract),
                         reads=[n("r_q1"), n("r_Mb")], writes=[n("r_q2")])
                    P.op("act", lambda: nc.scalar.copy(out=cT3[0:32, cs_], in_=Hb2[0:32, :]), reads=[n("r_Hb"), "cT3z"], writes=[("cT3", c, 0)])
                    P.op("act", lambda: nc.scalar.copy(out=cT3[32:64, cs_], in_=Mb2[32:64, :]), reads=[n("r_Mb"), "cT3z"], writes=[("cT3", c, 1)])
                    P.op("act", lambda: nc.scalar.copy(out=cT3[64:96, cs_], in_=q2[64:96, :]), reads=[n("r_q2"), "cT3z"], writes=[("cT3", c, 2)])

                for c0 in range(0, NT, 2):
                    recs = []
                    for p in range(2):
                        P.rec = []
                        router_tile(c0 + p, p)
                        recs.append(P.rec)
                        P.rec = None
                    for i in range(max(len(recs[0]), len(recs[1]))):
                        for p in range(2):
                            if i < len(recs[p]):
                                P.replay([recs[p][i]])
                P.barrier()
            if stage == 6:
                o = dout("cT3_dbg", [128, T], BF16)
                P.dma("sp", "out", [(o, cT3[:])])
                o = dout("h2T_dbg", [128, 8, T], BF16)
                P.dma("sp", "out", [(o, hT[:])])
                P.barrier()
                return nc, dbg

            NE = 32 if sub >= 30 else 2
            wstE = [sb(f"wstE{i}", [128, 8, 256], F32, sE) for i in range(2)]
            wgu = [sb(f"wgu{i}", [128, 8, 512], BF16, sE) for i in range(2)]
            wdn = [sb(f"wdn{i}", [128, 2, D], BF16, sE) for i in range(4)]
            actT = [sb(f"actT{i}", [128, 2, T], BF16, sE) for i in range(4)]
            sgs = [sb(f"sgs{i}", [128, 512], F32, sE) for i in range(2)]
            tms = [sb(f"tms{i}", [128, 512], F32, sE) for i in range(2)]
            stc = [0]
            itc = [0]
            def down_pair(e):
                for c in range(NT):
                    cs_ = slice(c * 128, (c + 1) * 128)
                    for hf in range(2):
                        bank = 5 + (2 * c + hf) % 2
                        n_ = 0
                        for ee in (e - 1, e):
                            for ft in range(2):
                                f = lambda: nc.tensor.matmul(ps[bank][:], lhsT=actT[ee % 4][:, ft, cs_], rhs=wdn[ee % 4][:, ft, hf * 512:(hf + 1) * 512],
                                                             start=(n_ == 0), stop=(n_ == 3))
                                rd = [(f"actT{ee % 4}", ft, c // 4), f"wdn{ee % 4}"]
                                if n_ < 3:
                                    P.quiet("pe", f, reads=rd, writes=[f"ps{bank}"])
                                else:
                                    P.op("pe", f, reads=rd, writes=[f"ps{bank}"])
                                n_ += 1
                        P.op("dve", lambda: nc.vector.tensor_tensor(out=x1[:, c, hf * 512:(hf + 1) * 512], in0=ps[bank][:],
                                                                    in1=x1[:, c, hf * 512:(hf + 1) * 512], op=ALU.add),
                             reads=[f"ps{bank}", ("x1", c)], writes=[("x1", c)])

            for e in range(NE):
                sl = e % 2
                dsl = e % 4
                asl = e % 4
                for (k_, src) in [(0, wg_d[e].rearrange("(kt p) n -> p kt n", p=128)), (1, wu_d[e].rearrange("(kt p) n -> p kt n", p=128))]:
                    si = stc[0] % 2
                    stc[0] += 1
                    P.dma("sp", f"wstE{si}", [(wstE[si][:], src)], writes=[f"wstE{si}"])
                    P.op("pool", lambda: nc.gpsimd.tensor_copy(out=wgu[sl][:, :, k_ * 256:(k_ + 1) * 256], in_=wstE[si][:]),
                         reads=[f"wstE{si}"], writes=[(f"wgu{sl}", k_)])
                si = stc[0] % 2
                stc[0] += 1
                P.dma("sp", f"wstE{si}", [(wstE[si][:].rearrange("p a b -> p (a b)").rearrange("p (f n) -> p f n", f=2),
                                           wd_d[e].rearrange("(ft p) n -> p ft n", p=128))],
                      writes=[f"wstE{si}"])
                P.op("pool", lambda: nc.gpsimd.tensor_copy(out=wdn[dsl][:].rearrange("p a b -> p (a b)"), in_=wstE[si][:].rearrange("p a b -> p (a b)")),
                     reads=[f"wstE{si}"], writes=[f"wdn{dsl}"])
                for tb in range(4):
                    ts_ = slice(tb * 512, (tb + 1) * 512)
                    P.op("pe", lambda: nc.tensor.matmul(ps[4][:], lhsT=selm[:, e, :], rhs=cT3[:, ts_], start=True, stop=True),
                         reads=["selm"], writes=["ps4"])
                    for ft in range(2):
                        par = itc[0] % 2
                        itc[0] += 1
                        bG, bU = (0, 1) if par == 0 else (2, 3)
                        for (bank, k_) in [(bG, 0), (bU, 1)]:
                            for kt in range(8):
                                f = lambda: nc.tensor.matmul(ps[bank][:], lhsT=wgu[sl][:, kt, k_ * 256 + ft * 128:k_ * 256 + (ft + 1) * 128],
                                                             rhs=hT[:, kt, ts_], start=(kt == 0), stop=(kt == 7))
                                if kt < 7:
                                    P.quiet("pe", f, reads=[(f"wgu{sl}", k_)], writes=[f"ps{bank}"])
                                else:
                                    P.op("pe", f, reads=[(f"wgu{sl}", k_)], writes=[f"ps{bank}"])
                        P.op("act", lambda: nc.scalar.activation(out=sgs[par][:], in_=ps[bG][:], func=AF.Silu), reads=[f"ps{bG}"], writes=[f"sgs{par}"])
                        P.op("dve", lambda: nc.vector.tensor_tensor(out=tms[par][:], in0=ps[bU][:], in1=sgs[par][:], op=ALU.mult),
                             reads=[f"ps{bU}", f"sgs{par}"], writes=[f"tms{par}"])
                        P.op("dve", lambda: nc.vector.tensor_tensor(out=actT[asl][:, ft, ts_], in0=ps[4][:], in1=tms[par][:], op=ALU.mult),
                             reads=["ps4", f"tms{par}"], writes=[(f"actT{asl}", ft, tb)])
                    if tb == 0 and e >= 2 and e % 2 == 0:
                        down_pair(e - 1)
            down_pair(NE - 1)
            for c in range(NT):
                P.dma("sp", "out", [(out_d[c * 128:(c + 1) * 128, :], x1[:, c, :])], reads=[("x1", c)])
            P.barrier()
    return nc, dbg


def host_consts():
    cst = np.zeros((128, CW), np.float32)
    cst[:, C_ID:C_ID + 128] = np.eye(128, dtype=np.float32)
    dm = np.ones((128, 128), np.float32)
    dm[0:64, 64:128] = 0.0
    cst[:, C_DM:C_DM + 128] = dm
    cst[:, C_NB:C_NB + 128] = (dm - 1.0) * 1e30
    cst[:, C_CT:C_CT + 128] = np.triu(np.ones((128, 128), np.float32))
    cst[:, C_ONE:C_ONE + 128] = 1.0
    for i in range(NBIS):
        cst[:, C_BIS + i] = 2.0 ** (-(i + 1))
    half = 8
    inv = 500000.0 ** (-np.arange(half, dtype=np.float64) * 2.0 / 16)
    ang = np.arange(T, dtype=np.float64)[:, None] * inv[None, :]
    cos, sin = np.cos(ang), np.sin(ang)
    rope = np.concatenate([cos, cos, -sin, sin], axis=1).astype(np.float32)
    return cst, rope


_CACHE = {}


def make_inmaps(inputs):
    cst, rope = host_consts()
    maps = []
    sq = lambda a: np.ascontiguousarray(np.asarray(a, np.float32)[0])
    shared = {
        "w_in": sq(inputs["w_in"]),
        "conv_w": sq(inputs["conv_w"]),
        "w_attn_branch": sq(inputs["w_attn_branch"]), "w_ssd_branch": sq(inputs["w_ssd_branch"]),
        "w_out": sq(inputs["w_out"]), "w_route_group": sq(inputs["w_route_group"]),
        "w_route_expert": sq(inputs["w_route_expert"]),
        "w_gate": sq(inputs["w_gate"]).reshape(32, 1024, 256), "w_up": sq(inputs["w_up"]).reshape(32, 1024, 256),
        "w_down": sq(inputs["w_down"]).reshape(32, 256, 1024),
        "cst": cst, "rope": rope,
    }
    for n in ["attn_norm", "q_norm", "k_norm", "idx_k_norm", "ffn_norm", "ssd_norm", "conv_b", "dt_bias", "a_log",
              "d_skip", "b_route_group", "b_route_expert"]:
        shared[n] = np.ascontiguousarray(np.asarray(inputs[n], np.float32).reshape(1, -1))
    x = np.asarray(inputs["x"], np.float32)
    for b in range(8):
        m = dict(shared)
        m["x"] = np.ascontiguousarray(x[b])
        maps.append(m)
    return maps


def kernel(**inputs):
    if "nc" not in _CACHE:
        _CACHE["nc"] = build()[0]
    nc = _CACHE["nc"]
    maps = make_inmaps(inputs)
    res = run_bass_kernel_spmd(nc, maps, core_ids=list(range(8)))
    return np.stack([np.asarray(r["out"], np.float32) for r in res.results], axis=0)
```

```python
import contextlib
import math
import numpy as np
import concourse.bass as bass
import concourse.mybir as mybir
from concourse.bass_utils import run_bass_kernel_spmd

F32 = mybir.dt.float32
BF16 = mybir.dt.bfloat16
U32 = mybir.dt.uint32
AF = mybir.ActivationFunctionType
ALU = mybir.AluOpType
AX = mybir.AxisListType

T = 2048
D = 1024
NT = 16
EPS = 1e-6
NBIS = 16
SPL = (1024, 256, 256, 512, 64, 8, 2048, 3072, 32, 1024, 1024)
OFF = [0]
for _s in SPL:
    OFF.append(OFF[-1] + _s)
(O_Q, O_K, O_V, O_IQ, O_IK, O_IW, O_Z, O_XBC, O_DT, O_GA, O_GB, O_END) = OFF
NW = O_END

C_ID = 0
C_DM = 128
C_NB = 256
C_CT = 384
C_ONE = 512
C_BIS = 640
CW = 672


class Prog:
    ENG = ("pe", "act", "dve", "pool", "sp")

    def __init__(self, nc, es):
        self.nc = nc
        self.es = es
        self.e = {"pe": nc.tensor, "act": nc.scalar, "dve": nc.vector, "pool": nc.gpsimd, "sp": nc.sync}
        self.sem = {}
        self.cnt = {k: 0 for k in self.ENG}
        self.epoch = {k: 0 for k in self.ENG}
        for k in self.ENG:
            self.sem[("e", k, 0)] = es.enter_context(nc.semaphore(f"s_{k}_0"))
        self.dcnt = {}
        self.waited = {k: {} for k in self.ENG}
        self.res = {}
        self.nins = 0
        self.pending = {k: [] for k in self.ENG}
        self.rec = None

    def _deps(self, eng, reads, writes):
        deps = []
        for r in reads:
            st = self.res.get(r)
            if st and st[0] is not None:
                deps.append((st[0], True))
        for w in writes:
            st = self.res.get(w)
            if st:
                if st[0] is not None:
                    deps.append((st[0], True))
                for t in st[1].values():
                    deps.append((t, False))
        for (tok, strong) in deps:
            key, val = tok
            if key[0] == "e" and key[1] == eng:
                if eng == "pe":
                    continue
            if self.waited[eng].get(key, -1) >= val:
                continue
            self.e[eng].wait_ge(self.sem[key], val)
            self.waited[eng][key] = val

    def _commit(self, tok, reads, writes, rkey):
        for r in reads:
            st = self.res.setdefault(r, [None, {}])
            st[1][rkey] = tok
        for w in writes:
            self.res[w] = [tok, {}]

    def op(self, eng, fn, reads=(), writes=()):
        if self.rec is not None:
            self.rec.append(("op", eng, fn, tuple(reads), tuple(writes)))
            return None
        self._deps(eng, reads, writes)
        ins = fn()
        if self.cnt[eng] >= 30000:
            self.epoch[eng] += 1
            self.cnt[eng] = 0
            self.sem[("e", eng, self.epoch[eng])] = self.es.enter_context(
                self.nc.semaphore(f"s_{eng}_{self.epoch[eng]}"))
        key = ("e", eng, self.epoch[eng])
        self.cnt[eng] += 1
        ins.then_inc(self.sem[key], 1)
        self.nins += 1
        for (r_, w_) in self.pending[eng]:
            self._commit((key, self.cnt[eng]), r_, w_, key)
        self.pending[eng] = []
        self._commit((key, self.cnt[eng]), reads, writes, key)
        return ins

    def replay(self, items):
        for (kind, eng, fn, reads, writes) in items:
            (self.op if kind == "op" else self.quiet)(eng, fn, reads, writes)

    def quiet(self, eng, fn, reads=(), writes=()):
        if self.rec is not None:
            self.rec.append(("quiet", eng, fn, tuple(reads), tuple(writes)))
            return None
        self._deps(eng, reads, writes)
        self.nins += 1
        self.pending[eng].append((tuple(reads), tuple(writes)))
        return fn()

    def dma(self, q, key, pairs, reads=(), writes=()):
        self._deps(q, reads, writes)
        k = ("d", key)
        if k not in self.sem:
            self.sem[k] = self.es.enter_context(self.nc.semaphore(f"d_{key}"))
            self.dcnt[k] = 0
        for (o, i) in pairs:
            self.e[q].dma_start(out=o, in_=i).then_inc(self.sem[k], 16)
            self.dcnt[k] += 16
            self.nins += 1
        self._commit((k, self.dcnt[k]), reads, writes, k)

    def barrier(self):
        toks = []
        for k in self.ENG:
            if self.cnt[k] > 0:
                toks.append((("e", k, self.epoch[k]), self.cnt[k]))
        for k, v in self.dcnt.items():
            if v > 0:
                toks.append((k, v))
        for eng in self.ENG:
            for (key, val) in toks:
                if key[0] == "e" and key[1] == eng:
                    continue
                if self.waited[eng].get(key, -1) >= val:
                    continue
                self.e[eng].wait_ge(self.sem[key], val)
                self.waited[eng][key] = val
        self.res = {}


def bview(ap, h):
    return ap.rearrange("p (h d) -> p h d", h=h)


def build(stage=99, sub=99):
    nc = bass.Bass("TRN2", target_bir_lowering=False)
    dr = {}

    def din(name, shape):
        dr[name] = nc.dram_tensor(name, list(shape), F32, kind="ExternalInput").ap()
        return dr[name]

    x_d = din("x", [T, D])
    win_d = din("w_in", [D, NW])
    gains = {n: din(n, [1, s]) for n, s in [("attn_norm", D), ("q_norm", 64), ("k_norm", 64), ("idx_k_norm", 64),
                                             ("ffn_norm", D), ("ssd_norm", 2048), ("conv_b", 3072), ("dt_bias", 32),
                                             ("a_log", 32), ("d_skip", 32), ("b_route_group", 4),
                                             ("b_route_expert", 32)]}
    convw_d = din("conv_w", [4, 3072])
    wa_d = din("w_attn_branch", [1024, 1024])
    wb_d = din("w_ssd_branch", [2048, 1024])
    wo_d = din("w_out", [1024, 1024])
    wrg_d = din("w_route_group", [1024, 4])
    wre_d = din("w_route_expert", [1024, 32])
    wg_d = din("w_gate", [32, 1024, 256])
    wu_d = din("w_up", [32, 1024, 256])
    wd_d = din("w_down", [32, 256, 1024])
    cst_d = din("cst", [128, CW])
    rope_d = din("rope", [T, 32])
    out_d = nc.dram_tensor("out", [T, D], F32, kind="ExternalOutput").ap()
    dbg = {}

    def dout(name, shape, dt=F32):
        dbg[name] = nc.dram_tensor(name, list(shape), dt, kind="ExternalOutput").ap()
        return dbg[name]

    es = contextlib.ExitStack()
    with es:
        P = Prog(nc, es)

        uid = [0]

        def sb(name, shape, dt, stack=es):
            uid[0] += 1
            return stack.enter_context(nc.sbuf_tensor(f"sb{uid[0]}_{name}", list(shape), dt))

        ps = [es.enter_context(nc.psum_tensor(f"ps{i}", [128, 512], F32)) for i in range(7)]
        psb = es.enter_context(nc.psum_tensor("psb", [128, 1024], BF16))

        cst = sb("cst", [128, CW], F32)
        cstb = sb("cstb", [128, CW], BF16)
        P.dma("sp", "cst", [(cst[:], cst_d)], writes=["cst"])
        P.op("pool", lambda: nc.gpsimd.tensor_copy(out=cstb[:], in_=cst[:]), reads=["cst"], writes=["cstb"])
        ident = cstb[:, C_ID:C_ID + 128]
        gb = {}

        def load_gain(n, width, st, c0=0):
            gb[n] = sb("g_" + n, [128, width], F32, st)
            P.dma("sp", "gain_" + n, [(gb[n][:], gains[n][:, c0:c0 + width].partition_broadcast(128))], writes=["g_" + n])

        hT = sb("hT", [128, 8, T], BF16)
        ya_spill = nc.dram_tensor("ya_spill", [128, 8, T], BF16, kind="Internal").ap()

        def rmsnorm_T(src_fn, gname, dst, st, nbuf=2):
            xb = [sb(f"rn_x{i}", [128, D], F32, st) for i in range(nbuf)]
            xn = [sb(f"rn_xn{i}", [128, D], BF16, st) for i in range(2)]
            junk = sb("rn_junk", [128, D], BF16, st)
            ss = sb("rn_ss", [128, NT], F32, st)
            sd = sb("rn_sd", [128, NT], F32, st)
            rs = sb("rn_rs", [128, NT], F32, st)
            epsn = sb("rn_eps", [128, 1], F32, st)
            P.op("pool", lambda: nc.gpsimd.memset(epsn[:], EPS), writes=["rn_eps"])

            def chain(c):
                b = c % 2
                xa, xr = src_fn(c, xb)
                P.op("act", lambda: nc.scalar.activation(out=junk[:], in_=xa, func=AF.Square, accum_out=ss[:, c:c + 1]),
                     reads=[xr], writes=["rn_junk", ("rn_ss", c)])
                P.op("act", lambda: nc.scalar.activation(out=sd[:, c:c + 1], in_=ss[:, c:c + 1], func=AF.Sqrt, scale=1.0 / D,
                                                         bias=epsn[:, 0:1]),
                     reads=[("rn_ss", c), "rn_eps"], writes=[("rn_sd", c)])
                P.op("dve", lambda: nc.vector.reciprocal(out=rs[:, c:c + 1], in_=sd[:, c:c + 1]),
                     reads=[("rn_sd", c)], writes=[("rn_rs", c)])
                P.op("dve", lambda: nc.vector.scalar_tensor_tensor(out=xn[b][:], in0=xa, scalar=rs[:, c:c + 1],
                                                                   in1=gb[gname][:], op0=ALU.mult, op1=ALU.mult),
                     reads=[xr, ("rn_rs", c), "g_" + gname], writes=[f"rn_xn{b}"])

            def tail(c):
                b = c % 2
                for kt in range(8):
                    f = lambda: nc.tensor.transpose(out=psb[:, kt * 128:(kt + 1) * 128],
                                                    in_=xn[b][:, kt * 128:(kt + 1) * 128], identity=ident)
                    if kt < 7:
                        P.quiet("pe", f, reads=[f"rn_xn{b}", "cstb"], writes=["psb"])
                    else:
                        P.op("pe", f, reads=[f"rn_xn{b}", "cstb"], writes=["psb"])
                P.op("act", lambda: nc.scalar.copy(out=dst[:, :, c * 128:(c + 1) * 128], in_=bview(psb[:], 8)),
                     reads=["psb"], writes=[("hT", c)])

            for c in range(NT):
                chain(c)
                if c >= 1:
                    tail(c - 1)
            tail(NT - 1)

        def load_x(c, bufs):
            b = c % len(bufs)
            rname = f"rn_x{b}"
            P.dma("sp", rname, [(bufs[b][:], x_d[c * 128:(c + 1) * 128, :])], writes=[rname])
            return bufs[b][:], rname

        with contextlib.ExitStack() as st:
            load_gain("attn_norm", D, st)
            rmsnorm_T(load_x, "attn_norm", hT, st, nbuf=3)
            P.barrier()

        if stage == 0:
            o = dout("hT_dbg", [128, 8, T], BF16)
            P.dma("sp", "out", [(o, hT[:])])
            P.barrier()
            return nc, dbg

        wst = [None]
        wbf = [None, None]
        wctr = [0]
        win_v = win_d.rearrange("(kt p) n -> p kt n", p=128)

        def alloc_w(st):
            wst[0] = sb("wst", [128, 8, 512], F32, st)
            wbf[0] = sb("wbf0", [128, 8, 512], BF16, st)
            wbf[1] = sb("wbf1", [128, 8, 512], BF16, st)

        def load_w(c0, ncols):
            i = wctr[0] % 2
            wctr[0] += 1
            P.dma("sp", "wst", [(wst[0][:, :, 0:ncols], win_v[:, :, c0:c0 + ncols])], writes=["wst"])
            P.op("pool", lambda: nc.gpsimd.tensor_copy(out=wbf[i][:, :, 0:ncols], in_=wst[0][:, :, 0:ncols]),
                 reads=["wst"], writes=[f"wbf{i}"])
            return i

        def proj_tm(c, wi, ncols, bank):
            for kt in range(8):
                f = lambda: nc.tensor.matmul(ps[bank][:, 0:ncols], lhsT=hT[:, kt, c * 128:(c + 1) * 128],
                                             rhs=wbf[wi][:, kt, 0:ncols], start=(kt == 0), stop=(kt == 7))
                if kt < 7:
                    P.quiet("pe", f, reads=[("hT", c), f"wbf{wi}"], writes=[f"ps{bank}"])
                else:
                    P.op("pe", f, reads=[("hT", c), f"wbf{wi}"], writes=[f"ps{bank}"])

        with contextlib.ExitStack() as st:
            yaT = sb("yaT", [128, 8, T], BF16, st)
            rope = sb("rope", [128, NT, 32], F32, st)
            P.dma("sp", "rope", [(rope[:], rope_d.rearrange("(c p) f -> p c f", p=128))], writes=["rope"])
            for n_ in ("q_norm", "k_norm", "idx_k_norm"):
                load_gain(n_, 64, st)
            qT = sb("qT", [128, 8, T], BF16, st)
            kT2 = sb("kT2", [128, 4, T], BF16, st)
            iqT = sb("iqT", [128, 4, T], BF16, st)
            ikT2 = sb("ikT2", [128, T], BF16, st)
            vaug = sb("vaug", [128, NT, 4, 66], BF16, st)
            iwa = sb("iwa", [128, NT, 8], F32, st)
            iws = sb("iws", [128, NT, 8], F32, st)
            P.op("pool", lambda: nc.gpsimd.memset(vaug[:], 1.0), writes=["vaug"])

            with contextlib.ExitStack() as st2:
                alloc_w(st2)
                sq = sb("e_sq", [128, 512], F32, st2)
                xn = sb("e_xn", [128, 512], F32, st2)
                ra = sb("e_ra", [128, 8, 16], F32, st2)
                rb = sb("e_rb", [128, 8, 16], F32, st2)
                s8 = [sb(f"e_s8{i}", [128, 8], F32, st2) for i in range(4)]
                tmbs = [sb(f"e_tmb{i}", [128, 512], BF16, st2) for i in range(2)]

                def epilogue(c, bank, nh, gname, prescale, dst_fn, dup):
                    pv = bview(ps[bank][:, 0:nh * 64], nh)
                    xv = bview(xn[:, 0:nh * 64], nh)
                    pr = f"ps{bank}"
                    if gname is not None:
                        P.op("act", lambda: nc.scalar.activation(out=sq[:, 0:nh * 64], in_=ps[bank][:, 0:nh * 64],
                                                                 func=AF.Square), reads=[pr], writes=["e_sq"])
                        P.op("dve", lambda: nc.vector.tensor_reduce(out=s8[0][:, 0:nh], in_=bview(sq[:, 0:nh * 64], nh),
                                                                    axis=AX.X, op=ALU.add), reads=["e_sq"], writes=["e_s80"])
                        P.op("dve", lambda: nc.vector.tensor_scalar(out=s8[1][:, 0:nh], in0=s8[0][:, 0:nh], scalar1=1.0 / 64,
                                                                    scalar2=EPS, op0=ALU.mult, op1=ALU.add),
                             reads=["e_s80"], writes=["e_s81"])
                        P.op("act", lambda: nc.scalar.activation(out=s8[2][:, 0:nh], in_=s8[1][:, 0:nh], func=AF.Sqrt),
                             reads=["e_s81"], writes=["e_s82"])
                        P.op("dve", lambda: nc.vector.reciprocal(out=s8[3][:, 0:nh], in_=s8[2][:, 0:nh]),
                             reads=["e_s82"], writes=["e_s83"])
                        P.op("dve", lambda: nc.vector.tensor_tensor(out=xv, in0=pv,
                                                                    in1=s8[3][:, 0:nh].unsqueeze(2).to_broadcast([128, nh, 64]),
                                                                    op=ALU.mult), reads=[pr, "e_s83"], writes=["e_xn"])
                        P.op("dve", lambda: nc.vector.tensor_tensor(out=xv, in0=xv,
                                                                    in1=gb[gname][:].unsqueeze(1).to_broadcast([128, nh, 64]),
                                                                    op=ALU.mult), reads=["e_xn", "g_" + gname], writes=["e_xn"])
                    elif prescale is not None:
                        P.op("dve", lambda: nc.vector.tensor_tensor(out=xv, in0=pv,
                                                                    in1=prescale.unsqueeze(2).to_broadcast([128, nh, 64]),
                                                                    op=ALU.mult), reads=[pr, ("iw", c)], writes=["e_xn"])
                    else:
                        P.op("dve", lambda: nc.vector.tensor_copy(out=xv, in_=pv), reads=[pr], writes=["e_xn"])
                    c16 = rope[:, c, 0:16].unsqueeze(1).to_broadcast([128, nh, 16])
                    nsn = rope[:, c, 16:24].unsqueeze(1).to_broadcast([128, nh, 8])
                    psn = rope[:, c, 24:32].unsqueeze(1).to_broadcast([128, nh, 8])
                    P.op("dve", lambda: nc.vector.tensor_tensor(out=ra[:, 0:nh, :], in0=xv[:, :, 0:16], in1=c16, op=ALU.mult),
                         reads=["e_xn", "rope"], writes=["e_ra"])
                    P.op("dve", lambda: nc.vector.tensor_tensor(out=rb[:, 0:nh, 0:8], in0=xv[:, :, 8:16], in1=nsn, op=ALU.mult),
                         reads=["e_xn", "rope"], writes=["e_rb0"])
                    P.op("dve", lambda: nc.vector.tensor_tensor(out=rb[:, 0:nh, 8:16], in0=xv[:, :, 0:8], in1=psn, op=ALU.mult),
                         reads=["e_xn", "rope"], writes=["e_rb1"])
                    P.op("dve", lambda: nc.vector.tensor_tensor(out=xv[:, :, 0:16], in0=ra[:, 0:nh, :], in1=rb[:, 0:nh, :],
                                                                op=ALU.add), reads=["e_ra", "e_rb0", "e_rb1"], writes=["e_xn"])
                    tmb = tmbs[c % 2]
                    tn0, tn1 = f"e_tmb{c % 2}_0", f"e_tmb{c % 2}_1"
                    if dup:
                        tv = tmb[:, 0:nh * 128].rearrange("p (h t d) -> p h t d", h=nh, t=2)
                        P.op("act", lambda: nc.scalar.copy(out=tv[:, :, 0, :], in_=xv), reads=["e_xn"], writes=[tn0])
                        P.op("act", lambda: nc.scalar.copy(out=tv[:, :, 1, :], in_=xv), reads=["e_xn"], writes=[tn1])
                        nblk = nh
                    else:
                        P.op("act", lambda: nc.scalar.copy(out=tmb[:, 0:nh * 64], in_=xn[:, 0:nh * 64]), reads=["e_xn"],
                             writes=[tn0, tn1])
                        nblk = nh // 2

                    def tail_():
                        for j in range(nblk):
                            f = lambda: nc.tensor.transpose(out=psb[:, j * 128:(j + 1) * 128], in_=tmb[:, j * 128:(j + 1) * 128],
                                                            identity=ident)
                            if j < nblk - 1:
                                P.quiet("pe", f, reads=[tn0, tn1, "cstb"], writes=["psb"])
                            else:
                                P.op("pe", f, reads=[tn0, tn1, "cstb"], writes=["psb"])
                        dst_fn(nblk)
                    return tail_

                pend = [None]

                def flush():
                    if pend[0]:
                        pend[0]()
                    pend[0] = None

                wi = load_w(O_IK, 72)
                for c in range(NT):
                    bank = c % 2
                    proj_tm(c, wi, 72, bank)
                    P.op("act", lambda: nc.scalar.activation(out=iwa[:, c, :], in_=ps[bank][:, 64:72], func=AF.Abs),
                         reads=[f"ps{bank}"], writes=[("iw", c)])
                    P.op("act", lambda: nc.scalar.activation(out=iws[:, c, :], in_=ps[bank][:, 64:72], func=AF.Sign),
                         reads=[f"ps{bank}"], writes=[("iws", c)])
                    t_ = epilogue(c, bank, 1, "idx_k_norm", None,
                                  lambda nblk, c=c: P.op("act", lambda: nc.scalar.copy(out=ikT2[:, c * 128:(c + 1) * 128],
                                                                                       in_=psb[:, 0:128]),
                                                         reads=["psb"], writes=[("ikT2", c)]), True)
                    if pend[0]:
                        pend[0]()
                    pend[0] = t_
                flush()
                wi = load_w(O_IQ, 512)
                for c in range(NT if sub >= 2 else 0):
                    bank = c % 2
                    proj_tm(c, wi, 512, bank)
                    t_ = epilogue(c, bank, 8, None, iwa[:, c, :],
                                  lambda nblk, c=c: P.op("act", lambda: nc.scalar.copy(out=iqT[:, :, c * 128:(c + 1) * 128],
                                                                                       in_=bview(psb[:, 0:512], 4)),
                                                         reads=["psb"], writes=[("iqT", c)]), False)
                    if pend[0]:
                        pend[0]()
                    pend[0] = t_
                flush()
                for half in range(2):
                    wi = load_w(O_Q + half * 512, 512)
                    for c in range(NT if sub >= 3 else 0):
                        bank = c % 2
                        proj_tm(c, wi, 512, bank)
                        t_ = epilogue(c, bank, 8, "q_norm", None,
                                      lambda nblk, c=c, half=half: P.op("act", lambda: nc.scalar.copy(
                                          out=qT[:, half * 4:half * 4 + 4, c * 128:(c + 1) * 128], in_=bview(psb[:, 0:512], 4)),
                                          reads=["psb"], writes=[("qT", c, half)]), False)
                        if pend[0]:
                            pend[0]()
                        pend[0] = t_
                    flush()
                wi = load_w(O_K, 512)
                for c in range(NT if sub >= 4 else 0):
                    bank = c % 2
                    proj_tm(c, wi, 512, bank)
                    if sub != 5:
                        P.op("act", lambda: nc.scalar.copy(out=vaug[:, c, :, 0:64], in_=bview(ps[bank][:, 256:512], 4)),
                             reads=[f"ps{bank}", "vaug"], writes=[("vaug", c)])
                    t_ = epilogue(c, bank, 4, "k_norm", None,
                                  lambda nblk, c=c: P.op("act", lambda: nc.scalar.copy(out=kT2[:, :, c * 128:(c + 1) * 128],
                                                                                       in_=bview(psb[:, 0:512], 4)),
                                                         reads=["psb"], writes=[("kT2", c)]), True)
                    if pend[0]:
                        pend[0]()
                    pend[0] = t_
                flush()
                P.barrier()

            if stage == 1:
                for nm, t_, shp in [("qT", qT, [128, 8, T]), ("kT2", kT2, [128, 4, T]), ("iqT", iqT, [128, 4, T]),
                                    ("ikT2", ikT2, [128, T])]:
                    o = dout(nm + "_dbg", shp, BF16)
                    P.dma("sp", "out", [(o, t_[:])])
                o = dout("vaug_dbg", [128, NT, 4, 66], BF16)
                P.dma("sp", "out", [(o, vaug[:])])
                o = dout("iwa_dbg", [128, NT, 8], F32)
                P.dma("sp", "out", [(o, iwa[:])])
                o = dout("iws_dbg", [128, NT, 8], F32)
                P.dma("sp", "out", [(o, iws[:])])
                P.barrier()
                return nc, dbg

            with contextlib.ExitStack() as st3:
                score = sb("score", [128, T], F32, st3)
                junk = sb("ajunk", [128, T], BF16, st3)
                maskb = [sb(f"maskb{i}", [128, T], BF16, st3) for i in range(2)]
                maskT = [sb(f"maskT{i}", [128, NT, 128], BF16, st3) for i in range(2)]
                relu = [sb(f"relu{i}", [128, 512], BF16, st3) for i in range(2)]
                diag = [sb(f"diag{i}", [128, 8, 128], BF16, st3) for i in range(2)]
                PT = [sb(f"PT{i}", [128, 512], BF16, st3) for i in range(3)]
                PTm = [sb(f"PTm{i}", [128, 512], BF16, st3) for i in range(3)]
                ytm = sb("ytm", [128, 1024], BF16, st3)
                hi = sb("b_hi", [128, 1], F32, st3)
                lo = sb("b_lo", [128, 1], F32, st3)
                w0 = sb("b_w0", [128, 1], F32, st3)
                wtab = sb("b_wtab", [128, NBIS], F32, st3)
                tt = sb("b_t", [128, 1], F32, st3)
                cnt = sb("b_cnt", [128, 1], F32, st3)
                uu = sb("b_u", [128, 1], F32, st3)
                thr = sb("b_thr", [128, 1], F32, st3)
                rcp = sb("b_rcp", [128, 8], F32, st3)
                pvc = [0]
                SB3 = [4, 5, 6]
                NQ = NT if sub >= 30 else max(0, sub - 10)

                def score_steps(qi):
                    nkeys = 128 * (qi + 1)
                    qs = slice(qi * 128, (qi + 1) * 128)
                    dgt = diag[qi % 2]
                    dn = f"diag{qi % 2}"
                    steps = []

                    def sd():
                        for h in range(8):
                            P.op("dve", lambda: nc.vector.tensor_scalar(out=dgt[:, h, :], in0=ident, scalar1=iws[:, qi, h:h + 1],
                                                                        scalar2=None, op0=ALU.mult),
                                 reads=["cstb"], writes=[(dn, h)])
                    steps.append(sd)
                    nkb = (nkeys + 511) // 512
                    items = [(kb, h) for kb in range(nkb) for h in range(8)]

                    def acc(kb, h):
                        kw = min(512, nkeys - kb * 512)
                        rb = h % 2
                        f = lambda: nc.tensor.matmul(ps[1][:, 0:kw], lhsT=dgt[:, h, :], rhs=relu[rb][:, 0:kw],
                                                     start=(h == 0), stop=(h == 7))
                        if h < 7:
                            P.quiet("pe", f, reads=[f"relu{rb}", (dn, h)], writes=["ps1"])
                        else:
                            P.op("pe", f, reads=[f"relu{rb}", (dn, h)], writes=["ps1"])
                            c0 = kb * 512
                            last = (kb == nkb - 1)
                            nd = kw - 128 if last else kw
                            if nd > 0:
                                P.op("dve", lambda: nc.vector.tensor_copy(out=score[:, c0:c0 + nd], in_=ps[1][:, 0:nd]),
                                     reads=["ps1"], writes=[("score", kb)])
                            if last:
                                P.op("dve", lambda: nc.vector.tensor_tensor(out=score[:, nkeys - 128:nkeys], in0=ps[1][:, nd:nd + 128],
                                                                            in1=cst[:, C_DM:C_DM + 128], op=ALU.mult),
                                     reads=["ps1", "cst"], writes=[("score", "d")])
                                P.op("dve", lambda: nc.vector.tensor_tensor(out=score[:, nkeys - 128:nkeys],
                                                                            in0=score[:, nkeys - 128:nkeys],
                                                                            in1=cst[:, C_NB:C_NB + 128], op=ALU.add),
                                     reads=[("score", "d"), "cst"], writes=[("score", "d")])

                    for idx, (kb, h) in enumerate(items):
                        def st_(idx=idx, kb=kb, h=h):
                            kw = min(512, nkeys - kb * 512)
                            hf, pr_ = h % 2, h // 2
                            rb = h % 2
                            P.op("pe", lambda: nc.tensor.matmul(ps[0][:, 0:kw], lhsT=iqT[64 * hf:64 * hf + 64, pr_, qs],
                                                                rhs=ikT2[64 * hf:64 * hf + 64, kb * 512:kb * 512 + kw],
                                                                start=True, stop=True),
                                 writes=["ps0"])
                            P.op("act", lambda: nc.scalar.activation(out=relu[rb][:, 0:kw], in_=ps[0][:, 0:kw], func=AF.Relu),
                                 reads=["ps0"], writes=[f"relu{rb}"])
                            if idx > 0:
                                acc(*items[idx - 1])
                        steps.append(st_)
                    steps.append(lambda: acc(*items[-1]))
                    return steps

                def search_steps(qi):
                    nkeys = 128 * (qi + 1)
                    nkb = (nkeys + 511) // 512
                    sres = [("score", kb) for kb in range(nkb)] + [("score", "d")]
                    mb = maskb[qi % 2]
                    steps = []

                    def s0():
                        P.op("dve", lambda: nc.vector.tensor_reduce(out=hi[:], in_=score[:, 0:nkeys], axis=AX.X, op=ALU.max),
                             reads=sres, writes=["b_hi"])
                        P.op("dve", lambda: nc.vector.tensor_reduce(out=lo[:], in_=score[:, 0:nkeys - 128], axis=AX.X, op=ALU.min),
                             reads=sres, writes=["b_lo"])
                        P.op("dve", lambda: nc.vector.tensor_tensor(out=w0[:], in0=hi[:], in1=lo[:], op=ALU.subtract),
                             reads=["b_hi", "b_lo"], writes=["b_w0"])
                        P.op("dve", lambda: nc.vector.tensor_scalar(out=wtab[:], in0=cst[:, C_BIS:C_BIS + NBIS], scalar1=w0[:, 0:1],
                                                                    scalar2=None, op0=ALU.mult),
                             reads=["b_w0", "cst"], writes=["b_wtab"])
                        P.op("dve", lambda: nc.vector.tensor_tensor(out=tt[:], in0=lo[:], in1=wtab[:, 0:1], op=ALU.add),
                             reads=["b_lo", "b_wtab"], writes=["b_t"])
                    steps.append(s0)
                    for it in range(NBIS):
                        def si(it=it):
                            P.op("dve", lambda: nc.vector.tensor_scalar(out=junk[:, 0:nkeys], in0=score[:, 0:nkeys], scalar1=tt[:, 0:1],
                                                                        scalar2=None, op0=ALU.is_ge, op1=ALU.add, accum_out=cnt[:]),
                                 reads=sres + ["b_t"], writes=["ajunk", "b_cnt"])
                            P.op("dve", lambda: nc.vector.tensor_scalar(out=uu[:], in0=cnt[:], scalar1=256.0, scalar2=-0.5,
                                                                        op0=ALU.is_ge, op1=ALU.add),
                                 reads=["b_cnt"], writes=["b_u"])
                            P.op("dve", lambda: nc.vector.scalar_tensor_tensor(out=tt[:], in0=uu[:], scalar=wtab[:, it:it + 1],
                                                                               in1=tt[:], op0=ALU.mult, op1=ALU.add),
                                 reads=["b_u", "b_wtab", "b_t"], writes=["b_t"])
                        steps.append(si)

                    def sf():
                        P.op("dve", lambda: nc.vector.scalar_tensor_tensor(out=thr[:], in0=wtab[:, NBIS - 1:NBIS], scalar=-0.5,
                                                                           in1=tt[:], op0=ALU.mult, op1=ALU.add),
                             reads=["b_wtab", "b_t"], writes=["b_thr"])
                        P.op("dve", lambda: nc.vector.tensor_scalar(out=mb[:, 0:nkeys], in0=score[:, 0:nkeys], scalar1=thr[:, 0:1],
                                                                    scalar2=None, op0=ALU.is_ge),
                             reads=sres + ["b_thr"], writes=[f"maskb{qi % 2}"])
                    steps.append(sf)
                    return steps

                def const_mask(qi):
                    nkeys = 128 * (qi + 1)
                    mb = maskb[qi % 2]
                    if qi == 1:
                        P.op("pool", lambda: nc.gpsimd.tensor_copy(out=mb[:, 0:128], in_=cstb[:, C_ONE:C_ONE + 128]),
                             reads=["cstb"], writes=[f"maskb{qi % 2}"])
                    P.op("pool", lambda: nc.gpsimd.tensor_copy(out=mb[:, nkeys - 128:nkeys], in_=cstb[:, C_DM:C_DM + 128]),
                         reads=["cstb"], writes=[f"maskb{qi % 2}"])

                def emit_maskT(qi):
                    nk = qi + 1
                    mb = maskb[qi % 2]
                    mT = maskT[qi % 2]
                    for k0 in range(0, nk, 8):
                        n = min(8, nk - k0)
                        for j in range(n):
                            f = lambda: nc.tensor.transpose(out=psb[:, j * 128:(j + 1) * 128],
                                                            in_=mb[:, (k0 + j) * 128:(k0 + j + 1) * 128], identity=ident)
                            if j < n - 1:
                                P.quiet("pe", f, reads=[f"maskb{qi % 2}", "cstb"], writes=["psb"])
                            else:
                                P.op("pe", f, reads=[f"maskb{qi % 2}", "cstb"], writes=["psb"])
                        P.op("act", lambda: nc.scalar.copy(out=mT[:, k0:k0 + n, :], in_=bview(psb[:, 0:n * 128], n)),
                             reads=["psb"], writes=[(f"maskT{qi % 2}", k0 // 8)])

                SBK = [4, 5, 6]
                LOOK = 2

                def attention_tile(qi, steps):
                    nk = qi + 1
                    qs = slice(qi * 128, (qi + 1) * 128)
                    mT = maskT[qi % 2]
                    seq = [(g, kj) for g in range(4) for kj in range(nk)]
                    nseq = len(seq)
                    info = {}
                    stq = list(steps)
                    per_i = (len(stq) + nseq - 1) // nseq if stq else 0

                    def front(i):
                        g, kj = seq[i]
                        ks = slice(kj * 128, (kj + 1) * 128)
                        n_ = pvc[0]
                        pvc[0] += 1
                        par = n_ % 3
                        sa, sb_ = SBK[(2 * n_) % 3], SBK[(2 * n_ + 1) % 3]
                        info[i] = par
                        P.op("pe", lambda: nc.tensor.matmul(bview(ps[sa][:, 0:256], 2), lhsT=kT2[0:64, g, ks],
                                                            rhs=qT[0:64, 2 * g:2 * g + 2, qs], start=True, stop=True),
                             writes=[f"ps{sa}"])
                        P.op("pe", lambda: nc.tensor.matmul(bview(ps[sb_][:, 0:256], 2), lhsT=kT2[64:128, g, ks],
                                                            rhs=qT[64:128, 2 * g:2 * g + 2, qs], start=True, stop=True),
                             writes=[f"ps{sb_}"])
                        P.op("act", lambda: nc.scalar.activation(out=PT[par][:, 0:256], in_=ps[sa][:, 0:256], func=AF.Exp,
                                                                 scale=0.125), reads=[f"ps{sa}"], writes=[("PT", par, 0)])
                        P.op("act", lambda: nc.scalar.activation(out=PT[par][:, 256:512], in_=ps[sb_][:, 0:256], func=AF.Exp,
                                                                 scale=0.125), reads=[f"ps{sb_}"], writes=[("PT", par, 1)])
                        me = "dve" if (n_ % 3 == 2) else "pool"
                        P.op(me, lambda: P.e[me].tensor_tensor(out=bview(PTm[par][:], 4), in0=bview(PT[par][:], 4),
                                                               in1=mT[:, kj, :].unsqueeze(1).to_broadcast([128, 4, 128]),
                                                               op=ALU.mult),
                             reads=[("PT", par, 0), ("PT", par, 1), (f"maskT{qi % 2}", kj // 8)], writes=[("PTm", par)])

                    def back(i):
                        g, kj = seq[i]
                        par = info[i]
                        ob = 2 + g % 2
                        for j in range(4):
                            hl = [0, 2, 1, 3][j]
                            f = lambda: nc.tensor.matmul(ps[ob][:, hl * 65:hl * 65 + 65], lhsT=PTm[par][:, j * 128:(j + 1) * 128],
                                                         rhs=vaug[:, kj, g, 0:65], start=(kj == 0 and j == 0),
                                                         stop=(kj == nk - 1 and j == 3), skip_group_check=True)
                            if j < 3:
                                P.quiet("pe", f, reads=[("PTm", par)], writes=[f"ps{ob}"])
                            else:
                                P.op("pe", f, reads=[("PTm", par)], writes=[f"ps{ob}"])
                        if kj == nk - 1:
                            ov = ps[ob][:, 0:260].rearrange("p (h d) -> p h d", h=4)
                            P.op("dve", lambda: nc.vector.reciprocal(out=rcp[:, 4 * (g % 2):4 * (g % 2) + 4], in_=ov[:, :, 64]),
                                 reads=[f"ps{ob}"], writes=[("b_rcp", g % 2)])
                            for hl in range(4):
                                hh = 4 * g + hl
                                P.op("act", lambda: nc.scalar.activation(out=ytm[:, hh * 64:(hh + 1) * 64],
                                                                         in_=ps[ob][:, hl * 65:hl * 65 + 64], func=AF.Copy,
                                                                         scale=rcp[:, 4 * (g % 2) + hl:4 * (g % 2) + hl + 1]),
                                     reads=[f"ps{ob}", ("b_rcp", g % 2)], writes=[("ytm", hh)])

                    for i in range(nseq + LOOK):
                        if i < nseq:
                            front(i)
                        if i >= LOOK:
                            back(i - LOOK)
                        for _ in range(per_i):
                            if stq:
                                stq.pop(0)()
                    while stq:
                        stq.pop(0)()

                if NQ > 0:
                    const_mask(0)
                    emit_maskT(0)
                for qi in range(NQ):
                    nxt = qi + 1
                    steps = []
                    if nxt < NQ:
                        if nxt >= 2:
                            steps = score_steps(nxt) + search_steps(nxt)
                        else:
                            const_mask(nxt)
                    attention_tile(qi, steps)
                    if nxt < NQ:
                        emit_maskT(nxt)
                    qs = slice(qi * 128, (qi + 1) * 128)
                    for j in range(8):
                        f = lambda: nc.tensor.transpose(out=psb[:, j * 128:(j + 1) * 128], in_=ytm[:, j * 128:(j + 1) * 128],
                                                        identity=ident)
                        if j < 7:
                            P.quiet("pe", f, reads=[("ytm", hh_) for hh_ in range(16)] + ["cstb"], writes=["psb"])
                        else:
                            P.op("pe", f, reads=[("ytm", hh_) for hh_ in range(16)] + ["cstb"], writes=["psb"])
                    P.op("act", lambda: nc.scalar.copy(out=yaT[:, :, qs], in_=bview(psb[:], 8)), reads=["psb"], writes=[("yaT", qi)])
                P.barrier()
            if stage == 2:
                o = dout("yaT_dbg", [128, 8, T], BF16)
                P.dma("sp", "out", [(o, yaT[:])])
                P.barrier()
                return nc, dbg
            P.dma("sp", "spill", [(ya_spill, yaT[:])])
            P.barrier()

        stB = contextlib.ExitStack()
        es.enter_context(stB)
        ysT = sb("ysT", [128, 16, T], BF16, stB)
        with contextlib.ExitStack() as sS:
            G8 = lambda t_, c_, g_: t_[:, c_, 8 * g_:8 * g_ + 8].unsqueeze(2).to_broadcast([128, 8, 64])
            wstS = sb("wstS", [128, 8, 256], F32, sS)
            selb = sb("selb", [128, 32, 128], BF16, sS)
            P.op("pool", lambda: nc.gpsimd.memset(selb[:], 0.0), writes=["selb"])
            for r3 in range(3):
                P.op("pool", lambda: nc.gpsimd.tensor_copy(
                    out=selb[32 * r3:32 * r3 + 32, :, :],
                    in_=cstb[32 * r3:32 * r3 + 32, C_ID + 32 * r3:C_ID + 32 * r3 + 32].unsqueeze(2).to_broadcast([32, 32, 128])),
                    reads=["cstb", "selb"], writes=["selb"])
            if sub == 101:
                P.barrier(); return nc, dbg
            for n_ in ("dt_bias", "a_log", "d_skip"):
                load_gain(n_, 32, sS)
            aneg = sb("aneg", [128, 32], F32, sS)
            P.op("act", lambda: nc.scalar.activation(out=aneg[:], in_=gb["a_log"][:], func=AF.Exp), reads=["g_a_log"], writes=["aneg"])
            P.op("dve", lambda: nc.vector.tensor_scalar(out=aneg[:], in0=aneg[:], scalar1=-1.0, scalar2=None, op0=ALU.mult),
                 reads=["aneg"], writes=["aneg"])
            if sub == 102:
                P.barrier(); return nc, dbg
            cwfm = sb("cwfm", [128, 24, 5], F32, sS)
            s0 = contextlib.ExitStack()
            cw5 = sb("cw5", [5, 3072], F32, s0)
            P.dma("sp", "cw5", [(cw5[0:4, :], convw_d), (cw5[4:5, :], gains["conv_b"])], writes=["cw5"])
            for t_ in range(24):
                f = lambda: nc.tensor.transpose(out=ps[0][:, t_ * 5:t_ * 5 + 5], in_=cw5[:, t_ * 128:(t_ + 1) * 128],
                                                identity=cst[0:5, C_ID:C_ID + 5])
                if t_ < 23:
                    P.quiet("pe", f, reads=["cw5", "cst"], writes=["ps0"])
                else:
                    P.op("pe", f, reads=["cw5", "cst"], writes=["ps0"])
            P.op("dve", lambda: nc.vector.tensor_copy(out=cwfm[:], in_=bview(ps[0][:, 0:120], 24)), reads=["ps0"], writes=["cwfm"])
            P.barrier()
            s0.close()
            if sub == 103:
                P.barrier(); return nc, dbg
            dt_all = sb("dt_all", [128, NT, 32], F32, sS)
            acs = sb("acs", [128, NT, 32], F32, sS)
            ea = sb("ea", [128, NT, 32], F32, sS)
            dtw = sb("dtw", [128, NT, 32], F32, sS)
            cdb = sb("cdb", [128, NT, 32], F32, sS)
            A3 = sb("A3", [128, NT, 128], BF16, sS)
            P.op("pool", lambda: nc.gpsimd.memset(A3[:], 0.0), writes=["A3z"])
            with contextlib.ExitStack() as s1:
                wdt_s = sb("wdt_s", [128, 8, 32], F32, s1)
                wdt = sb("wdt", [128, 8, 32], BF16, s1)
                P.dma("sp", "wdt", [(wdt_s[:], win_v[:, :, O_DT:O_DT + 32])], writes=["wdt_s"])
                P.op("pool", lambda: nc.gpsimd.tensor_copy(out=wdt[:], in_=wdt_s[:]), reads=["wdt_s"], writes=["wdt"])
                f32t = [sb(f"s1_{i}", [128, 32], F32, s1) for i in range(6)]
                a3 = sb("s1_a3", [128, 3, 32], F32, s1)
                Hb = sb("s1_Hb", [128, 128], BF16, s1)
                Mb = sb("s1_Mb", [128, 128], BF16, s1)
                r1 = sb("s1_r1", [128, 128], F32, s1)
                r2 = sb("s1_r2", [128, 128], F32, s1)
                ones_f = cst[:, C_ONE:C_ONE + 128]
                uinc = cst[:, C_CT:C_CT + 128]
                for c in range(NT):
                    cs_ = slice(c * 128, (c + 1) * 128)
                    xd, ax, ee, ll, rr, aa = f32t
                    for kt in range(8):
                        f = lambda: nc.tensor.matmul(ps[1][:, 0:32], lhsT=hT[:, kt, cs_], rhs=wdt[:, kt, :], start=(kt == 0), stop=(kt == 7))
                        if kt < 7:
                            P.quiet("pe", f, reads=["wdt"], writes=["ps1"])
                        else:
                            P.op("pe", f, reads=["wdt"], writes=["ps1"])
                    P.op("dve", lambda: nc.vector.tensor_tensor(out=xd[:], in0=ps[1][:, 0:32], in1=gb["dt_bias"][:], op=ALU.add),
                         reads=["ps1", "g_dt_bias"], writes=["s1_xd"])
                    P.op("act", lambda: nc.scalar.activation(out=ax[:], in_=xd[:], func=AF.Abs), reads=["s1_xd"], writes=["s1_ax"])
                    P.op("act", lambda: nc.scalar.activation(out=ee[:], in_=ax[:], func=AF.Exp, scale=-1.0), reads=["s1_ax"], writes=["s1_ee"])
                    P.op("act", lambda: nc.scalar.activation(out=ll[:], in_=ee[:], func=AF.Ln, bias=1.0), reads=["s1_ee"], writes=["s1_ll"])
                    P.op("dve", lambda: nc.vector.tensor_scalar(out=rr[:], in0=xd[:], scalar1=0.0, scalar2=None, op0=ALU.max),
                         reads=["s1_xd"], writes=["s1_rr"])
                    P.op("dve", lambda: nc.vector.tensor_tensor(out=dt_all[:, c, :], in0=rr[:], in1=ll[:], op=ALU.add),
                         reads=["s1_rr", "s1_ll"], writes=[("dt", c)])
                    P.op("dve", lambda: nc.vector.tensor_tensor(out=aa[:], in0=dt_all[:, c, :], in1=aneg[:], op=ALU.mult),
                         reads=[("dt", c), "aneg"], writes=["s1_aa"])
                    if sub == 104:
                        P.barrier(); return nc, dbg
                    P.op("dve", lambda: nc.vector.tensor_copy(out=a3[:], in_=aa[:].unsqueeze(1).to_broadcast([128, 3, 32])),
                         reads=["s1_aa"], writes=["s1_a3"])
                    if sub == 105:
                        P.barrier(); return nc, dbg
                    P.op("pe", lambda: nc.tensor.matmul(ps[2][:, 0:32], lhsT=uinc, rhs=aa[:], start=True, stop=True),
                         reads=["s1_aa", "cst"], writes=["ps2"])
                    P.op("pe", lambda: nc.tensor.matmul(ps[3][:, 0:32], lhsT=ones_f, rhs=aa[:], start=True, stop=True),
                         reads=["s1_aa", "cst"], writes=["ps3"])
                    P.op("pe", lambda: nc.tensor.matmul(ps[4][0:96, 0:128], lhsT=a3[:].rearrange("p a b -> p (a b)"), rhs=uinc,
                                                        start=True, stop=True),
                         reads=["s1_a3", "cst"], writes=["ps4"])
                    if sub == 106:
                        P.barrier(); return nc, dbg
                    P.op("dve", lambda: nc.vector.tensor_copy(out=acs[:, c, :], in_=ps[2][:, 0:32]), reads=["ps2"], writes=[("acs", c)])
                    if sub == 108:
                        P.barrier(); return nc, dbg
                    P.op("act", lambda: nc.scalar.activation(out=ea[:, c, :], in_=acs[:, c, :], func=AF.Exp), reads=[("acs", c)], writes=[("ea", c)])
                    P.op("dve", lambda: nc.vector.tensor_copy(out=rr[:], in_=ps[3][:, 0:32]), reads=["ps3"], writes=["s1_rr"])
                    P.op("act", lambda: nc.scalar.activation(out=cdb[:, c, :], in_=rr[:], func=AF.Exp), reads=["s1_rr"], writes=[("cdb", c)])
                    if sub == 109:
                        P.barrier(); return nc, dbg
                    P.op("dve", lambda: nc.vector.tensor_tensor(out=xd[:], in0=rr[:], in1=acs[:, c, :], op=ALU.subtract),
                         reads=["s1_rr", ("acs", c)], writes=["s1_xd"])
                    P.op("act", lambda: nc.scalar.activation(out=ee[:], in_=xd[:], func=AF.Exp), reads=["s1_xd"], writes=["s1_ee"])
                    P.op("dve", lambda: nc.vector.tensor_tensor(out=dtw[:, c, :], in0=dt_all[:, c, :], in1=ee[:], op=ALU.mult),
                         reads=[("dt", c), "s1_ee"], writes=[("dtw", c)])
                    if sub == 107:
                        P.barrier(); return nc, dbg
                    P.op("act", lambda: nc.scalar.copy(out=Hb[0:96, :], in_=ps[4][0:96, 0:128]), reads=["ps4"], writes=["s1_Hb"])
                    P.op("dve", lambda: nc.vector.tensor_tensor(out=r1[0:96, :], in0=ps[4][0:96, 0:128], in1=Hb[0:96, :], op=ALU.subtract),
                         reads=["ps4", "s1_Hb"], writes=["s1_r1"])
                    P.op("act", lambda: nc.scalar.copy(out=Mb[0:96, :], in_=r1[0:96, :]), reads=["s1_r1"], writes=["s1_Mb"])
                    P.op("dve", lambda: nc.vector.tensor_tensor(out=r2[0:96, :], in0=r1[0:96, :], in1=Mb[0:96, :], op=ALU.subtract),
                         reads=["s1_r1", "s1_Mb"], writes=["s1_r2"])
                    P.op("pool", lambda: nc.gpsimd.tensor_copy(out=A3[0:32, c, :], in_=Hb[0:32, :]), reads=["s1_Hb", "A3z"], writes=[("A3", c, 0)])
                    P.op("pool", lambda: nc.gpsimd.tensor_copy(out=A3[32:64, c, :], in_=Mb[32:64, :]), reads=["s1_Mb", "A3z"], writes=[("A3", c, 1)])
                    P.op("act", lambda: nc.scalar.copy(out=A3[64:96, c, :], in_=r2[64:96, :]), reads=["s1_r2", "A3z"], writes=[("A3", c, 2)])
                P.barrier()
            if stage == 3 and sub == 1:
                for nm, t_ in [("dt_all", dt_all), ("acs", acs), ("ea", ea), ("dtw", dtw), ("cdb", cdb)]:
                    o = dout(nm + "_dbg", [128, NT, 32], F32)
                    P.dma("sp", "out", [(o, t_[:])])
                o = dout("A3_dbg", [128, NT, 128], BF16)
                P.dma("sp", "out", [(o, A3[:])])
                o = dout("cwfm_dbg", [128, 24, 5], F32)
                P.dma("sp", "out", [(o, cwfm[:])])
                o = dout("selb_dbg", [128, 32, 128], BF16)
                P.dma("sp", "out", [(o, selb[:])])
                P.barrier()
                return nc, dbg

            xs_tm = sb("xs_tm", [128, NT, 512], BF16, sS)
            BT = sb("BT", [128, T], BF16, sS)
            CT = sb("CT", [128, T], BF16, sS)
            B_tm = sb("B_tm", [128, NT, 128], BF16, sS)
            rawb = sb("rawb", [128, T + 4], BF16, sS)
            xcf = [sb(f"xcf{i}", [128, 512], BF16, sS) for i in range(2)]
            dg = [sb(f"dg{i}", [128, 4, 128], BF16, sS) for i in range(2)]
            wch = [sb(f"wch{i}", [128, 8, 128], BF16, sS) for i in range(2)]
            wz = sb("wz", [128, 8, 512], BF16, sS)
            ssdg = sb("ssdg", [128, 512], F32, sS)
            hst = sb("hst", [128, 512], F32, sS)
            hstb = sb("hstb", [128, 512], BF16, sS)
            cbm = sb("cbm", [128, 128], F32, sS)
            seg = [sb(f"seg{i}", [128, 512], F32, sS) for i in range(2)]
            Ee = seg
            MT = [sb(f"MT{i}", [128, 512], BF16, sS) for i in range(4)]
            xdt = [sb(f"xdt{i}", [128, 512], BF16, sS) for i in range(2)]
            xw = [sb(f"xw{i}", [128, 512], BF16, sS) for i in range(2)]
            t1 = sb("t1", [128, 512], F32, sS)
            t1b = [t1, sb("t1b", [128, 512], F32, sS)]
            dsk = sb("dsk", [128, 8, 128], BF16, sS)
            epsb = sb("epsb", [128, 1], F32, sS)
            P.op("pool", lambda: nc.gpsimd.memset(epsb[:], EPS), writes=["epsb"])
            t3 = sb("t3", [128, 512], F32, sS)
            yv = t1
            sz = t3
            ynb = [sb(f"ynb{i}", [128, 512], BF16, sS) for i in range(2)]
            sjk = xcf[0]
            g1 = [sb(f"g1_{i}", [128, 2], F32, sS) for i in range(4)]
            P.op("pool", lambda: nc.gpsimd.memset(rawb[:, 0:4], 0.0), writes=["rawb_halo"])
            wctr2 = [0]
            for g in range(4 if sub >= 30 else 1):
                for hf in range(2):
                    c0 = O_Z + g * 512 + hf * 256
                    P.dma("sp", "wstS", [(wstS[:], win_v[:, :, c0:c0 + 256])], writes=["wstS"])
                    P.op("pool", lambda: nc.gpsimd.tensor_copy(out=wz[:, :, hf * 256:(hf + 1) * 256], in_=wstS[:]),
                         reads=["wstS"], writes=[("wz", hf)])
                P.dma("sp", "ssdg", [(ssdg[:], gains["ssd_norm"][:, g * 512:(g + 1) * 512].partition_broadcast(128))], writes=["ssdg"])
                chts = [(O_XBC + g * 512 + j * 128, 4 * g + j, "x", j) for j in range(4)]
                chts += [(O_XBC + 2048 + g * 128, 16 + g, "B", 0), (O_XBC + 2560 + g * 128, 20 + g, "C", 0)]
                for (c0, cti, kind, j) in chts:
                    wi = wctr2[0] % 2
                    wctr2[0] += 1
                    P.dma("sp", "wstS", [(wstS[:, :, 0:128], win_v[:, :, c0:c0 + 128])], writes=["wstS"])
                    P.op("pool", lambda: nc.gpsimd.tensor_copy(out=wch[wi][:], in_=wstS[:, :, 0:128]), reads=["wstS"], writes=[f"wch{wi}"])
                    for jj in range(4):
                        P.op("dve", lambda: nc.vector.tensor_scalar(out=dg[wi][:, jj, :], in0=ident, scalar1=cwfm[:, cti, jj:jj + 1],
                                                                    scalar2=None, op0=ALU.mult),
                             reads=["cstb", "cwfm"], writes=[(f"dg{wi}", jj)])
                    for tb in range(4):
                        bank = tb % 2
                        for kt in range(8):
                            f = lambda: nc.tensor.matmul(ps[bank][:], lhsT=wch[wi][:, kt, :], rhs=hT[:, kt, tb * 512:(tb + 1) * 512],
                                                         start=(kt == 0), stop=(kt == 7))
                            if kt < 7:
                                P.quiet("pe", f, reads=[f"wch{wi}"], writes=[f"ps{bank}"])
                            else:
                                P.op("pe", f, reads=[f"wch{wi}"], writes=[f"ps{bank}"])
                        P.op("act", lambda: nc.scalar.copy(out=rawb[:, 4 + tb * 512:4 + (tb + 1) * 512], in_=ps[bank][:]),
                             reads=[f"ps{bank}"], writes=[("rawb", tb)])
                    for tb in range(4):
                        bank = 2 + tb % 2
                        for jj in range(4):
                            f = lambda: nc.tensor.matmul(ps[bank][:], lhsT=dg[wi][:, jj, :],
                                                         rhs=rawb[:, 1 + tb * 512 + jj:1 + tb * 512 + jj + 512],
                                                         start=(jj == 0), stop=(jj == 3))
                            rd = [(f"dg{wi}", jj), ("rawb", tb), "rawb_halo"] + ([("rawb", tb - 1)] if tb > 0 else [])
                            if jj < 3:
                                P.quiet("pe", f, reads=rd, writes=[f"ps{bank}"])
                            else:
                                P.op("pe", f, reads=rd, writes=[f"ps{bank}"])
                        if kind == "x":
                            xb_ = tb % 2
                            P.op("act", lambda: nc.scalar.activation(out=xcf[xb_][:], in_=ps[bank][:], func=AF.Silu,
                                                                     bias=cwfm[:, cti, 4:5]),
                                 reads=[f"ps{bank}", "cwfm"], writes=[f"xcf{xb_}"])
                            for i4 in range(4):
                                f = lambda: nc.tensor.transpose(out=psb[:, i4 * 128:(i4 + 1) * 128], in_=xcf[xb_][:, i4 * 128:(i4 + 1) * 128],
                                                                identity=ident)
                                if i4 < 3:
                                    P.quiet("pe", f, reads=[f"xcf{xb_}", "cstb"], writes=["psb"])
                                else:
                                    P.op("pe", f, reads=[f"xcf{xb_}", "cstb"], writes=["psb"])
                            P.op("act", lambda: nc.scalar.copy(out=xs_tm[:, tb * 4:(tb + 1) * 4, j * 128:(j + 1) * 128],
                                                               in_=bview(psb[:, 0:512], 4)),
                                 reads=["psb"], writes=[("xs_tm", tb, j)])
                        else:
                            dstT = BT if kind == "B" else CT
                            P.op("act", lambda: nc.scalar.activation(out=dstT[:, tb * 512:(tb + 1) * 512], in_=ps[bank][:], func=AF.Silu,
                                                                     bias=cwfm[:, cti, 4:5]),
                                 reads=[f"ps{bank}", "cwfm"], writes=[(kind + "T", tb)])
                if sub == 202:
                    P.barrier(); return nc, dbg
                for k0 in range(0, NT, 8):
                    for jj in range(8):
                        cc = k0 + jj
                        f = lambda: nc.tensor.transpose(out=psb[:, jj * 128:(jj + 1) * 128], in_=BT[:, cc * 128:(cc + 1) * 128], identity=ident)
                        if jj < 7:
                            P.quiet("pe", f, reads=[("BT", cc // 4), "cstb"], writes=["psb"])
                        else:
                            P.op("pe", f, reads=[("BT", cc // 4), "cstb"], writes=["psb"])
                    P.op("act", lambda: nc.scalar.copy(out=B_tm[:, k0:k0 + 8, :], in_=bview(psb[:], 8)), reads=["psb"], writes=[("B_tm", k0 // 8)])
                for hl_ in range(8):
                    P.op("dve", lambda: nc.vector.tensor_scalar(out=dsk[:, hl_, :], in0=ident, scalar1=gb["d_skip"][:, 8 * g + hl_:8 * g + hl_ + 1],
                                                                scalar2=None, op0=ALU.mult),
                         reads=["cstb", "g_d_skip"], writes=["dsk"])
                P.op("pool", lambda: nc.gpsimd.memset(hst[:], 0.0), writes=["hst"])
                P.op("pool", lambda: nc.gpsimd.memset(hstb[:], 0.0), writes=["hstb"])
                if sub == 203:
                    P.barrier(); return nc, dbg
                xsr = lambda c_: [("xs_tm", c_ // 4, j_) for j_ in range(4)]
                def front(c):
                    cs_ = slice(c * 128, (c + 1) * 128)
                    pb = c % 2
                    P.op("pe", lambda: nc.tensor.matmul(ps[0][:, 0:128], lhsT=BT[:, cs_], rhs=CT[:, cs_], start=True, stop=True),
                         reads=[("BT", c // 4), ("CT", c // 4)], writes=["ps0"])
                    P.op("dve", lambda: nc.vector.tensor_tensor(out=cbm[:], in0=ps[0][:, 0:128], in1=cst[:, C_CT:C_CT + 128], op=ALU.mult),
                         reads=["ps0", "cst"], writes=["cbm"])
                    P.op("pool", lambda: nc.gpsimd.tensor_tensor(out=bview(xdt[pb][:], 8), in0=bview(xs_tm[:, c, :], 8), in1=G8(dt_all, c, g),
                                                                 op=ALU.mult), reads=xsr(c), writes=[f"xdt{pb}"])
                    P.op("pool", lambda: nc.gpsimd.tensor_tensor(out=bview(xw[pb][:], 8), in0=bview(xs_tm[:, c, :], 8), in1=G8(dtw, c, g),
                                                                 op=ALU.mult), reads=xsr(c), writes=[f"xw{pb}"])
                    for hb in range(2):
                        bcb = 1 + hb
                        for hh in range(4):
                            h = 8 * g + 4 * hb + hh
                            f = lambda: nc.tensor.matmul(ps[bcb][:, hh * 128:(hh + 1) * 128], lhsT=selb[:, h, :], rhs=A3[:, c, :],
                                                         start=True, stop=True, skip_group_check=True)
                            if hh < 3:
                                P.quiet("pe", f, reads=["selb"], writes=[f"ps{bcb}"])
                            else:
                                P.op("pe", f, reads=["selb"], writes=[f"ps{bcb}"])
                    for hb in range(2):
                        bcb = 1 + hb
                        for hh in range(4):
                            h = 8 * g + 4 * hb + hh
                            P.op("dve", lambda: nc.vector.tensor_scalar(out=seg[hb][:, hh * 128:(hh + 1) * 128],
                                                                        in0=ps[bcb][:, hh * 128:(hh + 1) * 128],
                                                                        scalar1=acs[:, c, h:h + 1], scalar2=0.0, op0=ALU.subtract, op1=ALU.min),
                                 reads=[f"ps{bcb}"], writes=[(f"seg{hb}", hh), f"Ee{hb}"])
                        P.op("act", lambda: nc.scalar.activation(out=Ee[hb][:], in_=seg[hb][:], func=AF.Exp),
                             reads=[(f"seg{hb}", hh_) for hh_ in range(4)], writes=[f"Ee{hb}"] + [(f"seg{hb}", hh_) for hh_ in range(4)])
                    for hb in range(2):
                        mi = 2 * pb + hb
                        P.op("dve", lambda: nc.vector.tensor_tensor(out=bview(MT[mi][:], 4), in0=bview(Ee[hb][:], 4),
                                                                    in1=cbm[:].unsqueeze(1).to_broadcast([128, 4, 128]), op=ALU.mult),
                             reads=[f"Ee{hb}", "cbm"], writes=[f"MT{mi}"])

                def back(c):
                    cs_ = slice(c * 128, (c + 1) * 128)
                    pb = c % 2
                    P.op("pe", lambda: nc.tensor.matmul(ps[5][:], lhsT=B_tm[:, c, :], rhs=xw[pb][:], start=True, stop=True),
                         reads=[("B_tm", c // 8), f"xw{pb}"], writes=["ps5"])
                    for hb in range(2):
                        mi = 2 * pb + hb
                        for hh in range(4):
                            hl = 4 * hb + hh
                            P.quiet("pe", lambda: nc.tensor.matmul(ps[3][:, hl * 64:(hl + 1) * 64], lhsT=MT[mi][:, hh * 128:(hh + 1) * 128],
                                                                   rhs=xdt[pb][:, hl * 64:(hl + 1) * 64], start=True, stop=False, skip_group_check=True),
                                    reads=[f"MT{mi}", f"xdt{pb}"], writes=["ps3"])
                            f = lambda: nc.tensor.matmul(ps[3][:, hl * 64:(hl + 1) * 64], lhsT=dsk[:, hl, :],
                                                         rhs=xs_tm[:, c, hl * 64:(hl + 1) * 64], start=False, stop=True, skip_group_check=True)
                            if hl < 7:
                                P.quiet("pe", f, reads=["dsk"] + xsr(c), writes=["ps3"])
                            else:
                                P.op("pe", f, reads=["dsk"] + xsr(c), writes=["ps3"])
                    for kt in range(8):
                        f = lambda: nc.tensor.matmul(ps[6][:], lhsT=hT[:, kt, cs_], rhs=wz[:, kt, :], start=(kt == 0), stop=(kt == 7))
                        if kt < 7:
                            P.quiet("pe", f, reads=[("wz", 0), ("wz", 1)], writes=["ps6"])
                        else:
                            P.op("pe", f, reads=[("wz", 0), ("wz", 1)], writes=["ps6"])
                    P.op("act", lambda: nc.scalar.activation(out=sz[:], in_=ps[6][:], func=AF.Silu), reads=["ps6"], writes=["t3"])
                    tb_ = t1b[pb]
                    tn = f"t1_{pb}"
                    if c > 0:
                        P.op("pe", lambda: nc.tensor.matmul(ps[4][:], lhsT=CT[:, cs_], rhs=hstb[:], start=True, stop=True),
                             reads=[("CT", c // 4), "hstb"], writes=["ps4"])
                        P.op("dve", lambda: nc.vector.tensor_tensor(out=bview(tb_[:], 8), in0=bview(ps[4][:], 8), in1=G8(ea, c, g), op=ALU.mult),
                             reads=["ps4"], writes=[tn])
                        P.op("dve", lambda: nc.vector.tensor_tensor(out=tb_[:], in0=ps[3][:], in1=tb_[:], op=ALU.add),
                             reads=["ps3", tn], writes=[tn])
                    else:
                        P.op("dve", lambda: nc.vector.tensor_copy(out=tb_[:], in_=ps[3][:]), reads=["ps3"], writes=[tn])
                    P.op("dve", lambda: nc.vector.tensor_tensor(out=bview(hst[:], 8), in0=bview(hst[:], 8), in1=G8(cdb, c, g), op=ALU.mult),
                         reads=["hst"], writes=["hst"])
                    P.op("dve", lambda: nc.vector.tensor_tensor(out=hst[:], in0=ps[5][:], in1=hst[:], op=ALU.add), reads=["ps5", "hst"], writes=["hst"])
                    P.op("act", lambda: nc.scalar.copy(out=hstb[:], in_=hst[:]), reads=["hst"], writes=["hstb"])
                    P.op("dve", lambda: nc.vector.tensor_tensor(out=tb_[:], in0=tb_[:], in1=sz[:], op=ALU.mult), reads=[tn, "t3"], writes=[tn])
                    P.op("act", lambda: nc.scalar.activation(out=sjk[:], in_=tb_[:], func=AF.Square, accum_out=g1[0][:, pb:pb + 1]),
                         reads=[tn], writes=["xcf0", ("g1_0", pb)])
                    P.op("act", lambda: nc.scalar.activation(out=g1[2][:, pb:pb + 1], in_=g1[0][:, pb:pb + 1], func=AF.Sqrt, scale=1.0 / 512, bias=epsb[:, 0:1]),
                         reads=[("g1_0", pb), "epsb"], writes=[("g1_2", pb)])

                def backB(c):
                    pb = c % 2
                    tb_ = t1b[pb]
                    tn = f"t1_{pb}"
                    P.op("dve", lambda: nc.vector.reciprocal(out=g1[3][:, pb:pb + 1], in_=g1[2][:, pb:pb + 1]), reads=[("g1_2", pb)], writes=[("g1_3", pb)])
                    P.op("dve", lambda: nc.vector.scalar_tensor_tensor(out=ynb[pb][:], in0=tb_[:], scalar=g1[3][:, pb:pb + 1], in1=ssdg[:], op0=ALU.mult, op1=ALU.mult),
                         reads=[tn, ("g1_3", pb), "ssdg"], writes=[f"ynb{pb}"])

                def tail(c):
                    cs_ = slice(c * 128, (c + 1) * 128)
                    pb = c % 2
                    for i4 in range(4):
                        f = lambda: nc.tensor.transpose(out=psb[:, i4 * 128:(i4 + 1) * 128], in_=ynb[pb][:, i4 * 128:(i4 + 1) * 128], identity=ident)
                        if i4 < 3:
                            P.quiet("pe", f, reads=[f"ynb{pb}", "cstb"], writes=["psb"])
                        else:
                            P.op("pe", f, reads=[f"ynb{pb}", "cstb"], writes=["psb"])
                    P.op("act", lambda: nc.scalar.copy(out=ysT[:, 4 * g:4 * g + 4, cs_], in_=bview(psb[:, 0:512], 4)), reads=["psb"], writes=[("ysT", g, c)])

                front(0)
                for c in range(NT):
                    if c + 1 < NT:
                        front(c + 1)
                    back(c)
                    if c >= 1:
                        backB(c - 1)
                        tail(c - 1)
                backB(NT - 1)
                tail(NT - 1)
            P.barrier()
        if stage == 3:
            o = dout("ysT_dbg", [128, 16, T], BF16)
            P.dma("sp", "out", [(o, ysT[:])])
            P.barrier()
            return nc, dbg

        stM = contextlib.ExitStack()
        es.enter_context(stM)
        mgT = sb("mgT", [128, 8, T], BF16, stM)
        with contextlib.ExitStack() as sM:
            yaT2 = sb("yaT2", [128, 8, T], BF16, sM)
            P.dma("sp", "ya_reload", [(yaT2[:], ya_spill)], writes=["yaT2"])
            wstM = sb("wstM", [128, 16, 128], F32, sM)
            wac = [sb(f"wac{i}", [128, 8, 128], BF16, sM) for i in range(2)]
            wbc = [sb(f"wbc{i}", [128, 16, 128], BF16, sM) for i in range(2)]
            wgac = [sb(f"wgac{i}", [128, 8, 128], BF16, sM) for i in range(2)]
            wgbc = [sb(f"wgbc{i}", [128, 8, 128], BF16, sM) for i in range(2)]
            sga = [sb(f"sga{i}", [128, 512], F32, sM) for i in range(2)]
            sgb = [sb(f"sgb{i}", [128, 512], F32, sM) for i in range(2)]
            wa_v = wa_d.rearrange("(kt p) n -> p kt n", p=128)
            wb_v = wb_d.rearrange("(kt p) n -> p kt n", p=128)
            it = [0]

            def load_merge_w(nt):
                ns = slice(nt * 128, (nt + 1) * 128)
                wp = nt % 2
                for (dst, src, nk_, nm) in [(wgac[wp], win_v[:, :, O_GA + nt * 128:O_GA + (nt + 1) * 128], 8, f"wgac{wp}"),
                                            (wgbc[wp], win_v[:, :, O_GB + nt * 128:O_GB + (nt + 1) * 128], 8, f"wgbc{wp}"),
                                            (wac[wp], wa_v[:, :, ns], 8, f"wac{wp}"), (wbc[wp], wb_v[:, :, ns], 16, f"wbc{wp}")]:
                    P.dma("sp", "wstM", [(wstM[:, 0:nk_, :], src)], writes=["wstM"])
                    P.op("dve", lambda: nc.vector.tensor_copy(out=dst[:], in_=wstM[:, 0:nk_, :]), reads=["wstM"], writes=[nm])

            load_merge_w(0)
            for nt in range(8):
                wp = nt % 2
                if nt + 1 < 8:
                    load_merge_w(nt + 1)
                for tb in range(4):
                    ts_ = slice(tb * 512, (tb + 1) * 512)
                    par = it[0] % 2
                    it[0] += 1
                    bA, bB, bGA = (0, 1, 2) if par == 0 else (4, 5, 6)
                    bGB = 3

                    def acc(bank, wt, nk_, rhsT, nm, rd):
                        for kt in range(nk_):
                            f = lambda: nc.tensor.matmul(ps[bank][:], lhsT=wt[:, kt, :], rhs=rhsT[:, kt, ts_], start=(kt == 0), stop=(kt == nk_ - 1))
                            if kt < nk_ - 1:
                                P.quiet("pe", f, reads=[nm] + rd, writes=[f"ps{bank}"])
                            else:
                                P.op("pe", f, reads=[nm] + rd, writes=[f"ps{bank}"])
                    acc(bGA, wgac[wp], 8, hT, f"wgac{wp}", [])
                    acc(bGB, wgbc[wp], 8, hT, f"wgbc{wp}", [])
                    acc(bA, wac[wp], 8, yaT2, f"wac{wp}", ["yaT2"])
                    acc(bB, wbc[wp], 16, ysT, f"wbc{wp}", [])
                    P.op("act", lambda: nc.scalar.activation(out=sga[par][:], in_=ps[bGA][:], func=AF.Sigmoid), reads=[f"ps{bGA}"], writes=[f"sga{par}"])
                    P.op("act", lambda: nc.scalar.activation(out=sgb[par][:], in_=ps[bGB][:], func=AF.Sigmoid), reads=[f"ps{bGB}"], writes=[f"sgb{par}"])
                    P.op("dve", lambda: nc.vector.tensor_tensor(out=sga[par][:], in0=ps[bA][:], in1=sga[par][:], op=ALU.mult),
                         reads=[f"ps{bA}", f"sga{par}"], writes=[f"sga{par}"])
                    P.op("dve", lambda: nc.vector.tensor_tensor(out=sgb[par][:], in0=ps[bB][:], in1=sgb[par][:], op=ALU.mult),
                         reads=[f"ps{bB}", f"sgb{par}"], writes=[f"sgb{par}"])
                    P.op("pool", lambda: nc.gpsimd.tensor_tensor(out=mgT[:, nt, ts_], in0=sga[par][:], in1=sgb[par][:], op=ALU.add),
                         reads=[f"sga{par}", f"sgb{par}"], writes=[("mgT", nt, tb)])
            P.barrier()
        if stage == 4:
            o = dout("mgT_dbg", [128, 8, T], BF16)
            P.dma("sp", "out", [(o, mgT[:])])
            P.barrier()
            return nc, dbg

        x1 = ysT[:].bitcast(F32)
        assert list(x1.shape) == [128, NT, D], x1.shape
        with contextlib.ExitStack() as sO:
            wstO = sb("wstO", [128, 8, 256], F32, sO)
            wo = sb("wo", [128, 8, D], BF16, sO)
            wo_v = wo_d.rearrange("(kt p) n -> p kt n", p=128)
            for q4 in range(4):
                P.dma("sp", "wstO", [(wstO[:], wo_v[:, :, q4 * 256:(q4 + 1) * 256])], writes=["wstO"])
                P.op("pool", lambda: nc.gpsimd.tensor_copy(out=wo[:, :, q4 * 256:(q4 + 1) * 256], in_=wstO[:]), reads=["wstO"], writes=[("wo", q4)])
            for c in range(NT):
                cs_ = slice(c * 128, (c + 1) * 128)
                P.dma("sp", f"x1ld{c}", [(x1[:, c, :], x_d[cs_, :])], writes=[("x1", c)])
                for hf in range(2):
                    bank = (2 * c + hf) % 4
                    for kt in range(8):
                        f = lambda: nc.tensor.matmul(ps[bank][:], lhsT=mgT[:, kt, cs_], rhs=wo[:, kt, hf * 512:(hf + 1) * 512],
                                                     start=(kt == 0), stop=(kt == 7))
                        rd = [("wo", 2 * hf), ("wo", 2 * hf + 1)]
                        if kt < 7:
                            P.quiet("pe", f, reads=rd, writes=[f"ps{bank}"])
                        else:
                            P.op("pe", f, reads=rd, writes=[f"ps{bank}"])
                    P.op("dve", lambda: nc.vector.tensor_tensor(out=x1[:, c, hf * 512:(hf + 1) * 512], in0=ps[bank][:],
                                                                in1=x1[:, c, hf * 512:(hf + 1) * 512], op=ALU.add),
                         reads=[f"ps{bank}", ("x1", c)], writes=[("x1", c)])
            P.barrier()
        stM.close()
        if stage == 5:
            o = dout("x1_dbg", [128, NT, D], F32)
            P.dma("sp", "out", [(o, x1)])
            P.barrier()
            return nc, dbg

        with contextlib.ExitStack() as sN:
            load_gain("ffn_norm", D, sN)

            def from_x1(c, bufs):
                return x1[:, c, :], ("x1", c)
            rmsnorm_T(from_x1, "ffn_norm", hT, sN, nbuf=0)
            P.barrier()

        with contextlib.ExitStack() as sE:
            selm = sb("selm", [128, 32, 128], BF16, sE)
            P.op("pool", lambda: nc.gpsimd.memset(selm[:], 0.0), writes=["selm"])
            for r3 in range(3):
                P.op("pool", lambda: nc.gpsimd.tensor_copy(
                    out=selm[32 * r3:32 * r3 + 32, :, :],
                    in_=cstb[32 * r3:32 * r3 + 32, C_ID + 32 * r3:C_ID + 32 * r3 + 32].unsqueeze(2).to_broadcast([32, 32, 128])),
                    reads=["cstb", "selm"], writes=["selm"])
            cT3 = sb("cT3", [128, T], BF16, sE)
            P.op("pool", lambda: nc.gpsimd.memset(cT3[:], 0.0), writes=["cT3z"])
            with contextlib.ExitStack() as sR:
                wr_s = sb("wr_s", [128, 8, 36], F32, sR)
                wr = sb("wr", [128, 8, 36], BF16, sR)
                P.dma("sp", "wr_s", [(wr_s[:, :, 0:4], wrg_d.rearrange("(kt p) n -> p kt n", p=128)),
                                     (wr_s[:, :, 4:36], wre_d.rearrange("(kt p) n -> p kt n", p=128))], writes=["wr_s"])
                P.op("pool", lambda: nc.gpsimd.tensor_copy(out=wr[:], in_=wr_s[:]), reads=["wr_s"], writes=["wr"])
                rb_ = sb("rbias", [128, 36], F32, sR)
                P.dma("sp", "rbias", [(rb_[:, 0:4], gains["b_route_group"].partition_broadcast(128)),
                                      (rb_[:, 4:36], gains["b_route_expert"].partition_broadcast(128))], writes=["rbias"])
                def mk_scratch(p):
                    S = {}
                    S["lg"] = sb(f"r_lg{p}", [128, 36], F32, sR)
                    S["c1"] = [sb(f"r_c{i}_{p}", [128, 1], F32, sR) for i in range(8)]
                    S["oh"] = sb(f"r_oh{p}", [128, 4], F32, sR)
                    S["ge"] = sb(f"r_ge{p}", [128, 4], F32, sR)
                    S["t48"] = sb(f"r_t48{p}", [128, 4, 8], F32, sR)
                    S["ein"] = sb(f"r_ein{p}", [128, 8], F32, sR)
                    S["top8"] = sb(f"r_top8{p}", [128, 8], F32, sR)
                    S["msel"] = sb(f"r_msel{p}", [128, 8], F32, sR)
                    S["wex"] = sb(f"r_wex{p}", [128, 8], F32, sR)
                    S["comb"] = sb(f"r_comb{p}", [128, 4, 8], F32, sR)
                    S["comb3"] = sb(f"r_comb3{p}", [128, 3, 32], F32, sR)
                    S["Hb"] = sb(f"r_Hb{p}", [128, 128], BF16, sR)
                    S["Mb"] = sb(f"r_Mb{p}", [128, 128], BF16, sR)
                    S["q1"] = sb(f"r_q1{p}", [128, 128], F32, sR)
                    S["q2"] = sb(f"r_q2{p}", [128, 128], F32, sR)
                    return S
                SC = [mk_scratch(0), mk_scratch(1)]

                def router_tile(c, p):
                    S = SC[p]
                    n = lambda x: f"{x}{p}"
                    bl, bt = (0, 1) if p == 0 else (2, 3)
                    lg, oh, ge, tmp48, ein, top8, msel, wex, comb, comb3 = (S[k] for k in ("lg", "oh", "ge", "t48", "ein", "top8", "msel", "wex", "comb", "comb3"))
                    Hb2, Mb2, q1, q2 = S["Hb"], S["Mb"], S["q1"], S["q2"]
                    mx, nmx, sme, gw, m21, den, rden, sc_ = S["c1"]
                    cs_ = slice(c * 128, (c + 1) * 128)
                    for kt in range(8):
                        f = lambda kt=kt: nc.tensor.matmul(ps[bl][:, 0:36], lhsT=hT[:, kt, cs_], rhs=wr[:, kt, :], start=(kt == 0), stop=(kt == 7))
                        if kt < 7:
                            P.quiet("pe", f, reads=["wr"], writes=[f"ps{bl}"])
                        else:
                            P.op("pe", f, reads=["wr"], writes=[f"ps{bl}"])
                    P.op("dve", lambda: nc.vector.tensor_tensor(out=lg[:], in0=ps[bl][:, 0:36], in1=rb_[:], op=ALU.add), reads=[f"ps{bl}", "rbias"], writes=[n("r_lg")])
                    P.op("dve", lambda: nc.vector.tensor_reduce(out=mx[:], in_=lg[:, 0:4], axis=AX.X, op=ALU.max), reads=[n("r_lg")], writes=[n("r_mx")])
                    P.op("dve", lambda: nc.vector.tensor_scalar(out=oh[:], in0=lg[:, 0:4], scalar1=mx[:, 0:1], scalar2=None, op0=ALU.is_ge),
                         reads=[n("r_lg"), n("r_mx")], writes=[n("r_oh")])
                    P.op("dve", lambda: nc.vector.tensor_scalar(out=nmx[:], in0=mx[:], scalar1=-1.0, scalar2=None, op0=ALU.mult), reads=[n("r_mx")], writes=[n("r_nmx")])
                    P.op("act", lambda: nc.scalar.activation(out=ge[:], in_=lg[:, 0:4], func=AF.Exp, bias=nmx[:, 0:1], accum_out=sme[:]),
                         reads=[n("r_lg"), n("r_nmx")], writes=[n("r_ge"), n("r_sme")])
                    P.op("dve", lambda: nc.vector.tensor_tensor(out=tmp48[:], in0=bview(lg[:, 4:36], 4), in1=oh[:].unsqueeze(2).to_broadcast([128, 4, 8]),
                                                                op=ALU.mult), reads=[n("r_lg"), n("r_oh")], writes=[n("r_t48")])
                    P.op("dve", lambda: nc.vector.tensor_reduce(out=ein[:], in_=tmp48[:].rearrange("p g e -> p e g"), axis=AX.X, op=ALU.add),
                         reads=[n("r_t48")], writes=[n("r_ein")])
                    P.op("dve", lambda: nc.vector.max(out=top8[:], in_=ein[:]), reads=[n("r_ein")], writes=[n("r_top8")])
                    P.op("dve", lambda: nc.vector.tensor_scalar(out=msel[:], in0=ein[:], scalar1=top8[:, 1:2], scalar2=None, op0=ALU.is_ge),
                         reads=[n("r_ein"), n("r_top8")], writes=[n("r_msel")])
                    P.op("dve", lambda: nc.vector.tensor_scalar(out=den[:], in0=top8[:, 0:1], scalar1=-1.0, scalar2=None, op0=ALU.mult),
                         reads=[n("r_top8")], writes=[n("r_nm1")])
                    P.op("act", lambda: nc.scalar.activation(out=wex[:], in_=ein[:], func=AF.Exp, bias=den[:, 0:1]), reads=[n("r_ein"), n("r_nm1")], writes=[n("r_wex")])
                    P.op("act", lambda: nc.scalar.activation(out=m21[:], in_=top8[:, 1:2], func=AF.Exp, bias=den[:, 0:1]), reads=[n("r_top8"), n("r_nm1")], writes=[n("r_m21")])
                    P.op("dve", lambda: nc.vector.scalar_tensor_tensor(out=rden[:], in0=m21[:], scalar=1.0, in1=sme[:], op0=ALU.add, op1=ALU.mult),
                         reads=[n("r_m21"), n("r_sme")], writes=[n("r_rden")])
                    P.op("dve", lambda: nc.vector.reciprocal(out=sc_[:], in_=rden[:]), reads=[n("r_rden")], writes=[n("r_sc")])
                    P.op("dve", lambda: nc.vector.scalar_tensor_tensor(out=wex[:], in0=wex[:], scalar=sc_[:, 0:1], in1=msel[:], op0=ALU.mult, op1=ALU.mult),
                         reads=[n("r_wex"), n("r_sc"), n("r_msel")], writes=[n("r_wex")])
                    P.op("dve", lambda: nc.vector.tensor_tensor(out=comb[:], in0=oh[:].unsqueeze(2).to_broadcast([128, 4, 8]),
                                                                in1=wex[:].unsqueeze(1).to_broadcast([128, 4, 8]), op=ALU.mult),
                         reads=[n("r_oh"), n("r_wex")], writes=[n("r_comb")])
                    P.op("dve", lambda: nc.vector.tensor_copy(out=comb3[:], in_=comb[:].rearrange("p g e -> p (g e)").unsqueeze(1).to_broadcast([128, 3, 32])),
                         reads=[n("r_comb")], writes=[n("r_comb3")])
                    P.op("pe", lambda: nc.tensor.transpose(out=ps[bt][0:96, 0:128], in_=comb3[:].rearrange("p a b -> p (a b)"),
                                                           identity=cst[:, C_ID:C_ID + 128]),
                         reads=[n("r_comb3"), "cst"], writes=[f"ps{bt}"])
                    P.op("act", lambda: nc.scalar.copy(out=Hb2[0:96, :], in_=ps[bt][0:96, 0:128]), reads=[f"ps{bt}"], writes=[n("r_Hb")])
                    P.op("dve", lambda: nc.vector.tensor_tensor(out=q1[0:96, :], in0=ps[bt][0:96, 0:128], in1=Hb2[0:96, :], op=ALU.subtract),
                         reads=[f"ps{bt}", n("r_Hb")], writes=[n("r_q1")])
                    P.op("act", lambda: nc.scalar.copy(out=Mb2[0:96, :], in_=q1[0:96, :]), reads=[n("r_q1")], writes=[n("r_Mb")])
                    P.op("dve", lambda: nc.vector.tensor_tensor(out=q2[0:96, :], in0=q1[0:96, :], in1=Mb2[0:96, :], op=ALU.subtract),
                         reads=[n("r_q1"), n("r_Mb")], writes=[n("r_q2")])
                    P.op("act", lambda: nc.scalar.copy(out=cT3[0:32, cs_], in_=Hb2[0:32, :]), reads=[n("r_Hb"), "cT3z"], writes=[("cT3", c, 0)])
                    P.op("act", lambda: nc.scalar.copy(out=cT3[32:64, cs_], in_=Mb2[32:64, :]), reads=[n("r_Mb"), "cT3z"], writes=[("cT3", c, 1)])
                    P.op("act", lambda: nc.scalar.copy(out=cT3[64:96, cs_], in_=q2[64:96, :]), reads=[n("r_q2"), "cT3z"], writes=[("cT3", c, 2)])

                for c0 in range(0, NT, 2):
                    recs = []
                    for p in range(2):
                        P.rec = []
                        router_tile(c0 + p, p)
                        recs.append(P.rec)
                        P.rec = None
                    for i in range(max(len(recs[0]), len(recs[1]))):
                        for p in range(2):
                            if i < len(recs[p]):
                                P.replay([recs[p][i]])
                P.barrier()
            if stage == 6:
                o = dout("cT3_dbg", [128, T], BF16)
                P.dma("sp", "out", [(o, cT3[:])])
                o = dout("h2T_dbg", [128, 8, T], BF16)
                P.dma("sp", "out", [(o, hT[:])])
                P.barrier()
                return nc, dbg

            NE = 32 if sub >= 30 else 2
            wstE = [sb(f"wstE{i}", [128, 8, 256], F32, sE) for i in range(2)]
            wgu = [sb(f"wgu{i}", [128, 8, 512], BF16, sE) for i in range(2)]
            wdn = [sb(f"wdn{i}", [128, 2, D], BF16, sE) for i in range(4)]
            actT = [sb(f"actT{i}", [128, 2, T], BF16, sE) for i in range(4)]
            sgs = [sb(f"sgs{i}", [128, 512], F32, sE) for i in range(2)]
            tms = [sb(f"tms{i}", [128, 512], F32, sE) for i in range(2)]
            stc = [0]
            itc = [0]
            def down_pair(e):
                for c in range(NT):
                    cs_ = slice(c * 128, (c + 1) * 128)
                    for hf in range(2):
                        bank = 5 + (2 * c + hf) % 2
                        n_ = 0
                        for ee in (e - 1, e):
                            for ft in range(2):
                                f = lambda: nc.tensor.matmul(ps[bank][:], lhsT=actT[ee % 4][:, ft, cs_], rhs=wdn[ee % 4][:, ft, hf * 512:(hf + 1) * 512],
                                                             start=(n_ == 0), stop=(n_ == 3))
                                rd = [(f"actT{ee % 4}", ft, c // 4), f"wdn{ee % 4}"]
                                if n_ < 3:
                                    P.quiet("pe", f, reads=rd, writes=[f"ps{bank}"])
                                else:
                                    P.op("pe", f, reads=rd, writes=[f"ps{bank}"])
                                n_ += 1
                        P.op("dve", lambda: nc.vector.tensor_tensor(out=x1[:, c, hf * 512:(hf + 1) * 512], in0=ps[bank][:],
                                                                    in1=x1[:, c, hf * 512:(hf + 1) * 512], op=ALU.add),
                             reads=[f"ps{bank}", ("x1", c)], writes=[("x1", c)])

            for e in range(NE):
                sl = e % 2
                dsl = e % 4
                asl = e % 4
                for (k_, src) in [(0, wg_d[e].rearrange("(kt p) n -> p kt n", p=128)), (1, wu_d[e].rearrange("(kt p) n -> p kt n", p=128))]:
                    si = stc[0] % 2
                    stc[0] += 1
                    P.dma("sp", f"wstE{si}", [(wstE[si][:], src)], writes=[f"wstE{si}"])
                    P.op("pool", lambda: nc.gpsimd.tensor_copy(out=wgu[sl][:, :, k_ * 256:(k_ + 1) * 256], in_=wstE[si][:]),
                         reads=[f"wstE{si}"], writes=[(f"wgu{sl}", k_)])
                si = stc[0] % 2
                stc[0] += 1
                P.dma("sp", f"wstE{si}", [(wstE[si][:].rearrange("p a b -> p (a b)").rearrange("p (f n) -> p f n", f=2),
                                           wd_d[e].rearrange("(ft p) n -> p ft n", p=128))],
                      writes=[f"wstE{si}"])
                P.op("pool", lambda: nc.gpsimd.tensor_copy(out=wdn[dsl][:].rearrange("p a b -> p (a b)"), in_=wstE[si][:].rearrange("p a b -> p (a b)")),
                     reads=[f"wstE{si}"], writes=[f"wdn{dsl}"])
                for tb in range(4):
                    ts_ = slice(tb * 512, (tb + 1) * 512)
                    P.op("pe", lambda: nc.tensor.matmul(ps[4][:], lhsT=selm[:, e, :], rhs=cT3[:, ts_], start=True, stop=True),
                         reads=["selm"], writes=["ps4"])
                    for ft in range(2):
                        par = itc[0] % 2
                        itc[0] += 1
                        bG, bU = (0, 1) if par == 0 else (2, 3)
                        for (bank, k_) in [(bG, 0), (bU, 1)]:
                            for kt in range(8):
                                f = lambda: nc.tensor.matmul(ps[bank][:], lhsT=wgu[sl][:, kt, k_ * 256 + ft * 128:k_ * 256 + (ft + 1) * 128],
                                                             rhs=hT[:, kt, ts_], start=(kt == 0), stop=(kt == 7))
                                if kt < 7:
                                    P.quiet("pe", f, reads=[(f"wgu{sl}", k_)], writes=[f"ps{bank}"])
                                else:
                                    P.op("pe", f, reads=[(f"wgu{sl}", k_)], writes=[f"ps{bank}"])
                        P.op("act", lambda: nc.scalar.activation(out=sgs[par][:], in_=ps[bG][:], func=AF.Silu), reads=[f"ps{bG}"], writes=[f"sgs{par}"])
                        P.op("dve", lambda: nc.vector.tensor_tensor(out=tms[par][:], in0=ps[bU][:], in1=sgs[par][:], op=ALU.mult),
                             reads=[f"ps{bU}", f"sgs{par}"], writes=[f"tms{par}"])
                        P.op("dve", lambda: nc.vector.tensor_tensor(out=actT[asl][:, ft, ts_], in0=ps[4][:], in1=tms[par][:], op=ALU.mult),
                             reads=["ps4", f"tms{par}"], writes=[(f"actT{asl}", ft, tb)])
                    if tb == 0 and e >= 2 and e % 2 == 0:
                        down_pair(e - 1)
            down_pair(NE - 1)
            for c in range(NT):
                P.dma("sp", "out", [(out_d[c * 128:(c + 1) * 128, :], x1[:, c, :])], reads=[("x1", c)])
            P.barrier()
    return nc, dbg


def host_consts():
    cst = np.zeros((128, CW), np.float32)
    cst[:, C_ID:C_ID + 128] = np.eye(128, dtype=np.float32)
    dm = np.ones((128, 128), np.float32)
    dm[0:64, 64:128] = 0.0
    cst[:, C_DM:C_DM + 128] = dm
    cst[:, C_NB:C_NB + 128] = (dm - 1.0) * 1e30
    cst[:, C_CT:C_CT + 128] = np.triu(np.ones((128, 128), np.float32))
    cst[:, C_ONE:C_ONE + 128] = 1.0
    for i in range(NBIS):
        cst[:, C_BIS + i] = 2.0 ** (-(i + 1))
    half = 8
    inv = 500000.0 ** (-np.arange(half, dtype=np.float64) * 2.0 / 16)
    ang = np.arange(T, dtype=np.float64)[:, None] * inv[None, :]
    cos, sin = np.cos(ang), np.sin(ang)
    rope = np.concatenate([cos, cos, -sin, sin], axis=1).astype(np.float32)
    return cst, rope


_CACHE = {}


def make_inmaps(inputs):
    cst, rope = host_consts()
    maps = []
    sq = lambda a: np.ascontiguousarray(np.asarray(a, np.float32)[0])
    shared = {
        "w_in": sq(inputs["w_in"]),
        "conv_w": sq(inputs["conv_w"]),
        "w_attn_branch": sq(inputs["w_attn_branch"]), "w_ssd_branch": sq(inputs["w_ssd_branch"]),
        "w_out": sq(inputs["w_out"]), "w_route_group": sq(inputs["w_route_group"]),
        "w_route_expert": sq(inputs["w_route_expert"]),
        "w_gate": sq(inputs["w_gate"]).reshape(32, 1024, 256), "w_up": sq(inputs["w_up"]).reshape(32, 1024, 256),
        "w_down": sq(inputs["w_down"]).reshape(32, 256, 1024),
        "cst": cst, "rope": rope,
    }
    for n in ["attn_norm", "q_norm", "k_norm", "idx_k_norm", "ffn_norm", "ssd_norm", "conv_b", "dt_bias", "a_log",
              "d_skip", "b_route_group", "b_route_expert"]:
        shared[n] = np.ascontiguousarray(np.asarray(inputs[n], np.float32).reshape(1, -1))
    x = np.asarray(inputs["x"], np.float32)
    for b in range(8):
        m = dict(shared)
        m["x"] = np.ascontiguousarray(x[b])
        maps.append(m)
    return maps


def kernel(**inputs):
    if "nc" not in _CACHE:
        _CACHE["nc"] = build()[0]
    nc = _CACHE["nc"]
    maps = make_inmaps(inputs)
    res = run_bass_kernel_spmd(nc, maps, core_ids=list(range(8)))
    return np.stack([np.asarray(r["out"], np.float32) for r in res.results], axis=0)
```

```python
import contextlib
import math
import numpy as np
import concourse.bass as bass
import concourse.mybir as mybir
from concourse.bass_utils import run_bass_kernel_spmd

F32 = mybir.dt.float32
BF16 = mybir.dt.bfloat16
U32 = mybir.dt.uint32
AF = mybir.ActivationFunctionType
ALU = mybir.AluOpType
AX = mybir.AxisListType

T = 2048
D = 1024
NT = 16
EPS = 1e-6
NBIS = 16
SPL = (1024, 256, 256, 512, 64, 8, 2048, 3072, 32, 1024, 1024)
OFF = [0]
for _s in SPL:
    OFF.append(OFF[-1] + _s)
(O_Q, O_K, O_V, O_IQ, O_IK, O_IW, O_Z, O_XBC, O_DT, O_GA, O_GB, O_END) = OFF
NW = O_END

C_ID = 0
C_DM = 128
C_NB = 256
C_CT = 384
C_ONE = 512
C_BIS = 640
CW = 672


class Prog:
    ENG = ("pe", "act", "dve", "pool", "sp")

    def __init__(self, nc, es):
        self.nc = nc
        self.es = es
        self.e = {"pe": nc.tensor, "act": nc.scalar, "dve": nc.vector, "pool": nc.gpsimd, "sp": nc.sync}
        self.sem = {}
        self.cnt = {k: 0 for k in self.ENG}
        self.epoch = {k: 0 for k in self.ENG}
        for k in self.ENG:
            self.sem[("e", k, 0)] = es.enter_context(nc.semaphore(f"s_{k}_0"))
        self.dcnt = {}
        self.waited = {k: {} for k in self.ENG}
        self.res = {}
        self.nins = 0
        self.pending = {k: [] for k in self.ENG}
        self.rec = None

    def _deps(self, eng, reads, writes):
        deps = []
        for r in reads:
            st = self.res.get(r)
            if st and st[0] is not None:
                deps.append((st[0], True))
        for w in writes:
            st = self.res.get(w)
            if st:
                if st[0] is not None:
                    deps.append((st[0], True))
                for t in st[1].values():
                    deps.append((t, False))
        for (tok, strong) in deps:
            key, val = tok
            if key[0] == "e" and key[1] == eng:
                if eng == "pe":
                    continue
            if self.waited[eng].get(key, -1) >= val:
                continue
            self.e[eng].wait_ge(self.sem[key], val)
            self.waited[eng][key] = val

    def _commit(self, tok, reads, writes, rkey):
        for r in reads:
            st = self.res.setdefault(r, [None, {}])
            st[1][rkey] = tok
        for w in writes:
            self.res[w] = [tok, {}]

    def op(self, eng, fn, reads=(), writes=()):
        if self.rec is not None:
            self.rec.append(("op", eng, fn, tuple(reads), tuple(writes)))
            return None
        self._deps(eng, reads, writes)
        ins = fn()
        if self.cnt[eng] >= 30000:
            self.epoch[eng] += 1
            self.cnt[eng] = 0
            self.sem[("e", eng, self.epoch[eng])] = self.es.enter_context(
                self.nc.semaphore(f"s_{eng}_{self.epoch[eng]}"))
        key = ("e", eng, self.epoch[eng])
        self.cnt[eng] += 1
        ins.then_inc(self.sem[key], 1)
        self.nins += 1
        for (r_, w_) in self.pending[eng]:
            self._commit((key, self.cnt[eng]), r_, w_, key)
        self.pending[eng] = []
        self._commit((key, self.cnt[eng]), reads, writes, key)
        return ins

    def replay(self, items):
        for (kind, eng, fn, reads, writes) in items:
            (self.op if kind == "op" else self.quiet)(eng, fn, reads, writes)

    def quiet(self, eng, fn, reads=(), writes=()):
        if self.rec is not None:
            self.rec.append(("quiet", eng, fn, tuple(reads), tuple(writes)))
            return None
        self._deps(eng, reads, writes)
        self.nins += 1
        self.pending[eng].append((tuple(reads), tuple(writes)))
        return fn()

    def dma(self, q, key, pairs, reads=(), writes=()):
        self._deps(q, reads, writes)
        k = ("d", key)
        if k not in self.sem:
            self.sem[k] = self.es.enter_context(self.nc.semaphore(f"d_{key}"))
            self.dcnt[k] = 0
        for (o, i) in pairs:
            self.e[q].dma_start(out=o, in_=i).then_inc(self.sem[k], 16)
            self.dcnt[k] += 16
            self.nins += 1
        self._commit((k, self.dcnt[k]), reads, writes, k)

    def barrier(self):
        toks = []
        for k in self.ENG:
            if self.cnt[k] > 0:
                toks.append((("e", k, self.epoch[k]), self.cnt[k]))
        for k, v in self.dcnt.items():
            if v > 0:
                toks.append((k, v))
        for eng in self.ENG:
            for (key, val) in toks:
                if key[0] == "e" and key[1] == eng:
                    continue
                if self.waited[eng].get(key, -1) >= val:
                    continue
                self.e[eng].wait_ge(self.sem[key], val)
                self.waited[eng][key] = val
        self.res = {}


def bview(ap, h):
    return ap.rearrange("p (h d) -> p h d", h=h)


def build(stage=99, sub=99):
    nc = bass.Bass("TRN2", target_bir_lowering=False)
    dr = {}

    def din(name, shape):
        dr[name] = nc.dram_tensor(name, list(shape), F32, kind="ExternalInput").ap()
        return dr[name]

    x_d = din("x", [T, D])
    win_d = din("w_in", [D, NW])
    gains = {n: din(n, [1, s]) for n, s in [("attn_norm", D), ("q_norm", 64), ("k_norm", 64), ("idx_k_norm", 64),
                                             ("ffn_norm", D), ("ssd_norm", 2048), ("conv_b", 3072), ("dt_bias", 32),
                                             ("a_log", 32), ("d_skip", 32), ("b_route_group", 4),
                                             ("b_route_expert", 32)]}
    convw_d = din("conv_w", [4, 3072])
    wa_d = din("w_attn_branch", [1024, 1024])
    wb_d = din("w_ssd_branch", [2048, 1024])
    wo_d = din("w_out", [1024, 1024])
    wrg_d = din("w_route_group", [1024, 4])
    wre_d = din("w_route_expert", [1024, 32])
    wg_d = din("w_gate", [32, 1024, 256])
    wu_d = din("w_up", [32, 1024, 256])
    wd_d = din("w_down", [32, 256, 1024])
    cst_d = din("cst", [128, CW])
    rope_d = din("rope", [T, 32])
    out_d = nc.dram_tensor("out", [T, D], F32, kind="ExternalOutput").ap()
    dbg = {}

    def dout(name, shape, dt=F32):
        dbg[name] = nc.dram_tensor(name, list(shape), dt, kind="ExternalOutput").ap()
        return dbg[name]

    es = contextlib.ExitStack()
    with es:
        P = Prog(nc, es)

        uid = [0]

        def sb(name, shape, dt, stack=es):
            uid[0] += 1
            return stack.enter_context(nc.sbuf_tensor(f"sb{uid[0]}_{name}", list(shape), dt))

        ps = [es.enter_context(nc.psum_tensor(f"ps{i}", [128, 512], F32)) for i in range(7)]
        psb = es.enter_context(nc.psum_tensor("psb", [128, 1024], BF16))

        cst = sb("cst", [128, CW], F32)
        cstb = sb("cstb", [128, CW], BF16)
        P.dma("sp", "cst", [(cst[:], cst_d)], writes=["cst"])
        P.op("pool", lambda: nc.gpsimd.tensor_copy(out=cstb[:], in_=cst[:]), reads=["cst"], writes=["cstb"])
        ident = cstb[:, C_ID:C_ID + 128]
        gb = {}

        def load_gain(n, width, st, c0=0):
            gb[n] = sb("g_" + n, [128, width], F32, st)
            P.dma("sp", "gain_" + n, [(gb[n][:], gains[n][:, c0:c0 + width].partition_broadcast(128))], writes=["g_" + n])

        hT = sb("hT", [128, 8, T], BF16)
        ya_spill = nc.dram_tensor("ya_spill", [128, 8, T], BF16, kind="Internal").ap()

        def rmsnorm_T(src_fn, gname, dst, st, nbuf=2):
            xb = [sb(f"rn_x{i}", [128, D], F32, st) for i in range(nbuf)]
            xn = [sb(f"rn_xn{i}", [128, D], BF16, st) for i in range(2)]
            junk = sb("rn_junk", [128, D], BF16, st)
            ss = sb("rn_ss", [128, NT], F32, st)
            sd = sb("rn_sd", [128, NT], F32, st)
            rs = sb("rn_rs", [128, NT], F32, st)
            epsn = sb("rn_eps", [128, 1], F32, st)
            P.op("pool", lambda: nc.gpsimd.memset(epsn[:], EPS), writes=["rn_eps"])

            def chain(c):
                b = c % 2
                xa, xr = src_fn(c, xb)
                P.op("act", lambda: nc.scalar.activation(out=junk[:], in_=xa, func=AF.Square, accum_out=ss[:, c:c + 1]),
                     reads=[xr], writes=["rn_junk", ("rn_ss", c)])
                P.op("act", lambda: nc.scalar.activation(out=sd[:, c:c + 1], in_=ss[:, c:c + 1], func=AF.Sqrt, scale=1.0 / D,
                                                         bias=epsn[:, 0:1]),
                     reads=[("rn_ss", c), "rn_eps"], writes=[("rn_sd", c)])
                P.op("dve", lambda: nc.vector.reciprocal(out=rs[:, c:c + 1], in_=sd[:, c:c + 1]),
                     reads=[("rn_sd", c)], writes=[("rn_rs", c)])
                P.op("dve", lambda: nc.vector.scalar_tensor_tensor(out=xn[b][:], in0=xa, scalar=rs[:, c:c + 1],
                                                                   in1=gb[gname][:], op0=ALU.mult, op1=ALU.mult),
                     reads=[xr, ("rn_rs", c), "g_" + gname], writes=[f"rn_xn{b}"])

            def tail(c):
                b = c % 2
                for kt in range(8):
                    f = lambda: nc.tensor.transpose(out=psb[:, kt * 128:(kt + 1) * 128],
                                                    in_=xn[b][:, kt * 128:(kt + 1) * 128], identity=ident)
                    if kt < 7:
                        P.quiet("pe", f, reads=[f"rn_xn{b}", "cstb"], writes=["psb"])
                    else:
                        P.op("pe", f, reads=[f"rn_xn{b}", "cstb"], writes=["psb"])
                P.op("act", lambda: nc.scalar.copy(out=dst[:, :, c * 128:(c + 1) * 128], in_=bview(psb[:], 8)),
                     reads=["psb"], writes=[("hT", c)])

            for c in range(NT):
                chain(c)
                if c >= 1:
                    tail(c - 1)
            tail(NT - 1)

        def load_x(c, bufs):
            b = c % len(bufs)
            rname = f"rn_x{b}"
            P.dma("sp", rname, [(bufs[b][:], x_d[c * 128:(c + 1) * 128, :])], writes=[rname])
            return bufs[b][:], rname

        with contextlib.ExitStack() as st:
            load_gain("attn_norm", D, st)
            rmsnorm_T(load_x, "attn_norm", hT, st, nbuf=3)
            P.barrier()

        if stage == 0:
            o = dout("hT_dbg", [128, 8, T], BF16)
            P.dma("sp", "out", [(o, hT[:])])
            P.barrier()
            return nc, dbg

        wst = [None]
        wbf = [None, None]
        wctr = [0]
        win_v = win_d.rearrange("(kt p) n -> p kt n", p=128)

        def alloc_w(st):
            wst[0] = sb("wst", [128, 8, 512], F32, st)
            wbf[0] = sb("wbf0", [128, 8, 512], BF16, st)
            wbf[1] = sb("wbf1", [128, 8, 512], BF16, st)

        def load_w(c0, ncols):
            i = wctr[0] % 2
            wctr[0] += 1
            P.dma("sp", "wst", [(wst[0][:, :, 0:ncols], win_v[:, :, c0:c0 + ncols])], writes=["wst"])
            P.op("pool", lambda: nc.gpsimd.tensor_copy(out=wbf[i][:, :, 0:ncols], in_=wst[0][:, :, 0:ncols]),
                 reads=["wst"], writes=[f"wbf{i}"])
            return i

        def proj_tm(c, wi, ncols, bank):
            for kt in range(8):
                f = lambda: nc.tensor.matmul(ps[bank][:, 0:ncols], lhsT=hT[:, kt, c * 128:(c + 1) * 128],
                                             rhs=wbf[wi][:, kt, 0:ncols], start=(kt == 0), stop=(kt == 7))
                if kt < 7:
                    P.quiet("pe", f, reads=[("hT", c), f"wbf{wi}"], writes=[f"ps{bank}"])
                else:
                    P.op("pe", f, reads=[("hT", c), f"wbf{wi}"], writes=[f"ps{bank}"])

        with contextlib.ExitStack() as st:
            yaT = sb("yaT", [128, 8, T], BF16, st)
            rope = sb("rope", [128, NT, 32], F32, st)
            P.dma("sp", "rope", [(rope[:], rope_d.rearrange("(c p) f -> p c f", p=128))], writes=["rope"])
            for n_ in ("q_norm", "k_norm", "idx_k_norm"):
                load_gain(n_, 64, st)
            qT = sb("qT", [128, 8, T], BF16, st)
            kT2 = sb("kT2", [128, 4, T], BF16, st)
            iqT = sb("iqT", [128, 4, T], BF16, st)
            ikT2 = sb("ikT2", [128, T], BF16, st)
            vaug = sb("vaug", [128, NT, 4, 66], BF16, st)
            iwa = sb("iwa", [128, NT, 8], F32, st)
            iws = sb("iws", [128, NT, 8], F32, st)
            P.op("pool", lambda: nc.gpsimd.memset(vaug[:], 1.0), writes=["vaug"])

            with contextlib.ExitStack() as st2:
                alloc_w(st2)
                sq = sb("e_sq", [128, 512], F32, st2)
                xn = sb("e_xn", [128, 512], F32, st2)
                ra = sb("e_ra", [128, 8, 16], F32, st2)
                rb = sb("e_rb", [128, 8, 16], F32, st2)
                s8 = [sb(f"e_s8{i}", [128, 8], F32, st2) for i in range(4)]
                tmbs = [sb(f"e_tmb{i}", [128, 512], BF16, st2) for i in range(2)]

                def epilogue(c, bank, nh, gname, prescale, dst_fn, dup):
                    pv = bview(ps[bank][:, 0:nh * 64], nh)
                    xv = bview(xn[:, 0:nh * 64], nh)
                    pr = f"ps{bank}"
                    if gname is not None:
                        P.op("act", lambda: nc.scalar.activation(out=sq[:, 0:nh * 64], in_=ps[bank][:, 0:nh * 64],
                                                                 func=AF.Square), reads=[pr], writes=["e_sq"])
                        P.op("dve", lambda: nc.vector.tensor_reduce(out=s8[0][:, 0:nh], in_=bview(sq[:, 0:nh * 64], nh),
                                                                    axis=AX.X, op=ALU.add), reads=["e_sq"], writes=["e_s80"])
                        P.op("dve", lambda: nc.vector.tensor_scalar(out=s8[1][:, 0:nh], in0=s8[0][:, 0:nh], scalar1=1.0 / 64,
                                                                    scalar2=EPS, op0=ALU.mult, op1=ALU.add),
                             reads=["e_s80"], writes=["e_s81"])
                        P.op("act", lambda: nc.scalar.activation(out=s8[2][:, 0:nh], in_=s8[1][:, 0:nh], func=AF.Sqrt),
                             reads=["e_s81"], writes=["e_s82"])
                        P.op("dve", lambda: nc.vector.reciprocal(out=s8[3][:, 0:nh], in_=s8[2][:, 0:nh]),
                             reads=["e_s82"], writes=["e_s83"])
                        P.op("dve", lambda: nc.vector.tensor_tensor(out=xv, in0=pv,
                                                                    in1=s8[3][:, 0:nh].unsqueeze(2).to_broadcast([128, nh, 64]),
                                                                    op=ALU.mult), reads=[pr, "e_s83"], writes=["e_xn"])
                        P.op("dve", lambda: nc.vector.tensor_tensor(out=xv, in0=xv,
                                                                    in1=gb[gname][:].unsqueeze(1).to_broadcast([128, nh, 64]),
                                                                    op=ALU.mult), reads=["e_xn", "g_" + gname], writes=["e_xn"])
                    elif prescale is not None:
                        P.op("dve", lambda: nc.vector.tensor_tensor(out=xv, in0=pv,
                                                                    in1=prescale.unsqueeze(2).to_broadcast([128, nh, 64]),
                                                                    op=ALU.mult), reads=[pr, ("iw", c)], writes=["e_xn"])
                    else:
                        P.op("dve", lambda: nc.vector.tensor_copy(out=xv, in_=pv), reads=[pr], writes=["e_xn"])
                    c16 = rope[:, c, 0:16].unsqueeze(1).to_broadcast([128, nh, 16])
                    nsn = rope[:, c, 16:24].unsqueeze(1).to_broadcast([128, nh, 8])
                    psn = rope[:, c, 24:32].unsqueeze(1).to_broadcast([128, nh, 8])
                    P.op("dve", lambda: nc.vector.tensor_tensor(out=ra[:, 0:nh, :], in0=xv[:, :, 0:16], in1=c16, op=ALU.mult),
                         reads=["e_xn", "rope"], writes=["e_ra"])
                    P.op("dve", lambda: nc.vector.tensor_tensor(out=rb[:, 0:nh, 0:8], in0=xv[:, :, 8:16], in1=nsn, op=ALU.mult),
                         reads=["e_xn", "rope"], writes=["e_rb0"])
                    P.op("dve", lambda: nc.vector.tensor_tensor(out=rb[:, 0:nh, 8:16], in0=xv[:, :, 0:8], in1=psn, op=ALU.mult),
                         reads=["e_xn", "rope"], writes=["e_rb1"])
                    P.op("dve", lambda: nc.vector.tensor_tensor(out=xv[:, :, 0:16], in0=ra[:, 0:nh, :], in1=rb[:, 0:nh, :],
                                                                op=ALU.add), reads=["e_ra", "e_rb0", "e_rb1"], writes=["e_xn"])
                    tmb = tmbs[c % 2]
                    tn0, tn1 = f"e_tmb{c % 2}_0", f"e_tmb{c % 2}_1"
                    if dup:
                        tv = tmb[:, 0:nh * 128].rearrange("p (h t d) -> p h t d", h=nh, t=2)
                        P.op("act", lambda: nc.scalar.copy(out=tv[:, :, 0, :], in_=xv), reads=["e_xn"], writes=[tn0])
                        P.op("act", lambda: nc.scalar.copy(out=tv[:, :, 1, :], in_=xv), reads=["e_xn"], writes=[tn1])
                        nblk = nh
                    else:
                        P.op("act", lambda: nc.scalar.copy(out=tmb[:, 0:nh * 64], in_=xn[:, 0:nh * 64]), reads=["e_xn"],
                             writes=[tn0, tn1])
                        nblk = nh // 2

                    def tail_():
                        for j in range(nblk):
                            f = lambda: nc.tensor.transpose(out=psb[:, j * 128:(j + 1) * 128], in_=tmb[:, j * 128:(j + 1) * 128],
                                                            identity=ident)
                            if j < nblk - 1:
                                P.quiet("pe", f, reads=[tn0, tn1, "cstb"], writes=["psb"])
                            else:
                                P.op("pe", f, reads=[tn0, tn1, "cstb"], writes=["psb"])
                        dst_fn(nblk)
                    return tail_

                pend = [None]

                def flush():
                    if pend[0]:
                        pend[0]()
                    pend[0] = None

                wi = load_w(O_IK, 72)
                for c in range(NT):
                    bank = c % 2
                    proj_tm(c, wi, 72, bank)
                    P.op("act", lambda: nc.scalar.activation(out=iwa[:, c, :], in_=ps[bank][:, 64:72], func=AF.Abs),
                         reads=[f"ps{bank}"], writes=[("iw", c)])
                    P.op("act", lambda: nc.scalar.activation(out=iws[:, c, :], in_=ps[bank][:, 64:72], func=AF.Sign),
                         reads=[f"ps{bank}"], writes=[("iws", c)])
                    t_ = epilogue(c, bank, 1, "idx_k_norm", None,
                                  lambda nblk, c=c: P.op("act", lambda: nc.scalar.copy(out=ikT2[:, c * 128:(c + 1) * 128],
                                                                                       in_=psb[:, 0:128]),
                                                         reads=["psb"], writes=[("ikT2", c)]), True)
                    if pend[0]:
                        pend[0]()
                    pend[0] = t_
                flush()
                wi = load_w(O_IQ, 512)
                for c in range(NT if sub >= 2 else 0):
                    bank = c % 2
                    proj_tm(c, wi, 512, bank)
                    t_ = epilogue(c, bank, 8, None, iwa[:, c, :],
                                  lambda nblk, c=c: P.op("act", lambda: nc.scalar.copy(out=iqT[:, :, c * 128:(c + 1) * 128],
                                                                                       in_=bview(psb[:, 0:512], 4)),
                                                         reads=["psb"], writes=[("iqT", c)]), False)
                    if pend[0]:
                        pend[0]()
                    pend[0] = t_
                flush()
                for half in range(2):
                    wi = load_w(O_Q + half * 512, 512)
                    for c in range(NT if sub >= 3 else 0):
                        bank = c % 2
                        proj_tm(c, wi, 512, bank)
                        t_ = epilogue(c, bank, 8, "q_norm", None,
                                      lambda nblk, c=c, half=half: P.op("act", lambda: nc.scalar.copy(
                                          out=qT[:, half * 4:half * 4 + 4, c * 128:(c + 1) * 128], in_=bview(psb[:, 0:512], 4)),
                                          reads=["psb"], writes=[("qT", c, half)]), False)
                        if pend[0]:
                            pend[0]()
                        pend[0] = t_
                    flush()
                wi = load_w(O_K, 512)
                for c in range(NT if sub >= 4 else 0):
                    bank = c % 2
                    proj_tm(c, wi, 512, bank)
                    if sub != 5:
                        P.op("act", lambda: nc.scalar.copy(out=vaug[:, c, :, 0:64], in_=bview(ps[bank][:, 256:512], 4)),
                             reads=[f"ps{bank}", "vaug"], writes=[("vaug", c)])
                    t_ = epilogue(c, bank, 4, "k_norm", None,
                                  lambda nblk, c=c: P.op("act", lambda: nc.scalar.copy(out=kT2[:, :, c * 128:(c + 1) * 128],
                                                                                       in_=bview(psb[:, 0:512], 4)),
                                                         reads=["psb"], writes=[("kT2", c)]), True)
                    if pend[0]:
                        pend[0]()
                    pend[0] = t_
                flush()
                P.barrier()

            if stage == 1:
                for nm, t_, shp in [("qT", qT, [128, 8, T]), ("kT2", kT2, [128, 4, T]), ("iqT", iqT, [128, 4, T]),
                                    ("ikT2", ikT2, [128, T])]:
                    o = dout(nm + "_dbg", shp, BF16)
                    P.dma("sp", "out", [(o, t_[:])])
                o = dout("vaug_dbg", [128, NT, 4, 66], BF16)
                P.dma("sp", "out", [(o, vaug[:])])
                o = dout("iwa_dbg", [128, NT, 8], F32)
                P.dma("sp", "out", [(o, iwa[:])])
                o = dout("iws_dbg", [128, NT, 8], F32)
                P.dma("sp", "out", [(o, iws[:])])
                P.barrier()
                return nc, dbg

            with contextlib.ExitStack() as st3:
                score = sb("score", [128, T], F32, st3)
                junk = sb("ajunk", [128, T], BF16, st3)
                maskb = [sb(f"maskb{i}", [128, T], BF16, st3) for i in range(2)]
                maskT = [sb(f"maskT{i}", [128, NT, 128], BF16, st3) for i in range(2)]
                relu = [sb(f"relu{i}", [128, 512], BF16, st3) for i in range(2)]
                diag = [sb(f"diag{i}", [128, 8, 128], BF16, st3) for i in range(2)]
                PT = [sb(f"PT{i}", [128, 512], BF16, st3) for i in range(3)]
                PTm = [sb(f"PTm{i}", [128, 512], BF16, st3) for i in range(3)]
                ytm = sb("ytm", [128, 1024], BF16, st3)
                hi = sb("b_hi", [128, 1], F32, st3)
                lo = sb("b_lo", [128, 1], F32, st3)
                w0 = sb("b_w0", [128, 1], F32, st3)
                wtab = sb("b_wtab", [128, NBIS], F32, st3)
                tt = sb("b_t", [128, 1], F32, st3)
                cnt = sb("b_cnt", [128, 1], F32, st3)
                uu = sb("b_u", [128, 1], F32, st3)
                thr = sb("b_thr", [128, 1], F32, st3)
                rcp = sb("b_rcp", [128, 8], F32, st3)
                pvc = [0]
                SB3 = [4, 5, 6]
                NQ = NT if sub >= 30 else max(0, sub - 10)

                def score_steps(qi):
                    nkeys = 128 * (qi + 1)
                    qs = slice(qi * 128, (qi + 1) * 128)
                    dgt = diag[qi % 2]
                    dn = f"diag{qi % 2}"
                    steps = []

                    def sd():
                        for h in range(8):
                            P.op("dve", lambda: nc.vector.tensor_scalar(out=dgt[:, h, :], in0=ident, scalar1=iws[:, qi, h:h + 1],
                                                                        scalar2=None, op0=ALU.mult),
                                 reads=["cstb"], writes=[(dn, h)])
                    steps.append(sd)
                    nkb = (nkeys + 511) // 512
                    items = [(kb, h) for kb in range(nkb) for h in range(8)]

                    def acc(kb, h):
                        kw = min(512, nkeys - kb * 512)
                        rb = h % 2
                        f = lambda: nc.tensor.matmul(ps[1][:, 0:kw], lhsT=dgt[:, h, :], rhs=relu[rb][:, 0:kw],
                                                     start=(h == 0), stop=(h == 7))
                        if h < 7:
                            P.quiet("pe", f, reads=[f"relu{rb}", (dn, h)], writes=["ps1"])
                        else:
                            P.op("pe", f, reads=[f"relu{rb}", (dn, h)], writes=["ps1"])
                            c0 = kb * 512
                            last = (kb == nkb - 1)
                            nd = kw - 128 if last else kw
                            if nd > 0:
                                P.op("dve", lambda: nc.vector.tensor_copy(out=score[:, c0:c0 + nd], in_=ps[1][:, 0:nd]),
                                     reads=["ps1"], writes=[("score", kb)])
                            if last:
                                P.op("dve", lambda: nc.vector.tensor_tensor(out=score[:, nkeys - 128:nkeys], in0=ps[1][:, nd:nd + 128],
                                                                            in1=cst[:, C_DM:C_DM + 128], op=ALU.mult),
                                     reads=["ps1", "cst"], writes=[("score", "d")])
                                P.op("dve", lambda: nc.vector.tensor_tensor(out=score[:, nkeys - 128:nkeys],
                                                                            in0=score[:, nkeys - 128:nkeys],
                                                                            in1=cst[:, C_NB:C_NB + 128], op=ALU.add),
                                     reads=[("score", "d"), "cst"], writes=[("score", "d")])

                    for idx, (kb, h) in enumerate(items):
                        def st_(idx=idx, kb=kb, h=h):
                            kw = min(512, nkeys - kb * 512)
                            hf, pr_ = h % 2, h // 2
                            rb = h % 2
                            P.op("pe", lambda: nc.tensor.matmul(ps[0][:, 0:kw], lhsT=iqT[64 * hf:64 * hf + 64, pr_, qs],
                                                                rhs=ikT2[64 * hf:64 * hf + 64, kb * 512:kb * 512 + kw],
                                                                start=True, stop=True),
                                 writes=["ps0"])
                            P.op("act", lambda: nc.scalar.activation(out=relu[rb][:, 0:kw], in_=ps[0][:, 0:kw], func=AF.Relu),
                                 reads=["ps0"], writes=[f"relu{rb}"])
                            if idx > 0:
                                acc(*items[idx - 1])
                        steps.append(st_)
                    steps.append(lambda: acc(*items[-1]))
                    return steps

                def search_steps(qi):
                    nkeys = 128 * (qi + 1)
                    nkb = (nkeys + 511) // 512
                    sres = [("score", kb) for kb in range(nkb)] + [("score", "d")]
                    mb = maskb[qi % 2]
                    steps = []

                    def s0():
                        P.op("dve", lambda: nc.vector.tensor_reduce(out=hi[:], in_=score[:, 0:nkeys], axis=AX.X, op=ALU.max),
                             reads=sres, writes=["b_hi"])
                        P.op("dve", lambda: nc.vector.tensor_reduce(out=lo[:], in_=score[:, 0:nkeys - 128], axis=AX.X, op=ALU.min),
                             reads=sres, writes=["b_lo"])
                        P.op("dve", lambda: nc.vector.tensor_tensor(out=w0[:], in0=hi[:], in1=lo[:], op=ALU.subtract),
                             reads=["b_hi", "b_lo"], writes=["b_w0"])
                        P.op("dve", lambda: nc.vector.tensor_scalar(out=wtab[:], in0=cst[:, C_BIS:C_BIS + NBIS], scalar1=w0[:, 0:1],
                                                                    scalar2=None, op0=ALU.mult),
                             reads=["b_w0", "cst"], writes=["b_wtab"])
                        P.op("dve", lambda: nc.vector.tensor_tensor(out=tt[:], in0=lo[:], in1=wtab[:, 0:1], op=ALU.add),
                             reads=["b_lo", "b_wtab"], writes=["b_t"])
                    steps.append(s0)
                    for it in range(NBIS):
                        def si(it=it):
                            P.op("dve", lambda: nc.vector.tensor_scalar(out=junk[:, 0:nkeys], in0=score[:, 0:nkeys], scalar1=tt[:, 0:1],
                                                                        scalar2=None, op0=ALU.is_ge, op1=ALU.add, accum_out=cnt[:]),
                                 reads=sres + ["b_t"], writes=["ajunk", "b_cnt"])
                            P.op("dve", lambda: nc.vector.tensor_scalar(out=uu[:], in0=cnt[:], scalar1=256.0, scalar2=-0.5,
                                                                        op0=ALU.is_ge, op1=ALU.add),
                                 reads=["b_cnt"], writes=["b_u"])
                            P.op("dve", lambda: nc.vector.scalar_tensor_tensor(out=tt[:], in0=uu[:], scalar=wtab[:, it:it + 1],
                                                                               in1=tt[:], op0=ALU.mult, op1=ALU.add),
                                 reads=["b_u", "b_wtab", "b_t"], writes=["b_t"])
                        steps.append(si)

                    def sf():
                        P.op("dve", lambda: nc.vector.scalar_tensor_tensor(out=thr[:], in0=wtab[:, NBIS - 1:NBIS], scalar=-0.5,
                                                                           in1=tt[:], op0=ALU.mult, op1=ALU.add),
                             reads=["b_wtab", "b_t"], writes=["b_thr"])
                        P.op("dve", lambda: nc.vector.tensor_scalar(out=mb[:, 0:nkeys], in0=score[:, 0:nkeys], scalar1=thr[:, 0:1],
                                                                    scalar2=None, op0=ALU.is_ge),
                             reads=sres + ["b_thr"], writes=[f"maskb{qi % 2}"])
                    steps.append(sf)
                    return steps

                def const_mask(qi):
                    nkeys = 128 * (qi + 1)
                    mb = maskb[qi % 2]
                    if qi == 1:
                        P.op("pool", lambda: nc.gpsimd.tensor_copy(out=mb[:, 0:128], in_=cstb[:, C_ONE:C_ONE + 128]),
                             reads=["cstb"], writes=[f"maskb{qi % 2}"])
                    P.op("pool", lambda: nc.gpsimd.tensor_copy(out=mb[:, nkeys - 128:nkeys], in_=cstb[:, C_DM:C_DM + 128]),
                         reads=["cstb"], writes=[f"maskb{qi % 2}"])

                def emit_maskT(qi):
                    nk = qi + 1
                    mb = maskb[qi % 2]
                    mT = maskT[qi % 2]
                    for k0 in range(0, nk, 8):
                        n = min(8, nk - k0)
                        for j in range(n):
                            f = lambda: nc.tensor.transpose(out=psb[:, j * 128:(j + 1) * 128],
                                                            in_=mb[:, (k0 + j) * 128:(k0 + j + 1) * 128], identity=ident)
                            if j < n - 1:
                                P.quiet("pe", f, reads=[f"maskb{qi % 2}", "cstb"], writes=["psb"])
                            else:
                                P.op("pe", f, reads=[f"maskb{qi % 2}", "cstb"], writes=["psb"])
                        P.op("act", lambda: nc.scalar.copy(out=mT[:, k0:k0 + n, :], in_=bview(psb[:, 0:n * 128], n)),
                             reads=["psb"], writes=[(f"maskT{qi % 2}", k0 // 8)])

                SBK = [4, 5, 6]
                LOOK = 2

                def attention_tile(qi, steps):
                    nk = qi + 1
                    qs = slice(qi * 128, (qi + 1) * 128)
                    mT = maskT[qi % 2]
                    seq = [(g, kj) for g in range(4) for kj in range(nk)]
                    nseq = len(seq)
                    info = {}
                    stq = list(steps)
                    per_i = (len(stq) + nseq - 1) // nseq if stq else 0

                    def front(i):
                        g, kj = seq[i]
                        ks = slice(kj * 128, (kj + 1) * 128)
                        n_ = pvc[0]
                        pvc[0] += 1
                        par = n_ % 3
                        sa, sb_ = SBK[(2 * n_) % 3], SBK[(2 * n_ + 1) % 3]
                        info[i] = par
                        P.op("pe", lambda: nc.tensor.matmul(bview(ps[sa][:, 0:256], 2), lhsT=kT2[0:64, g, ks],
                                                            rhs=qT[0:64, 2 * g:2 * g + 2, qs], start=True, stop=True),
                             writes=[f"ps{sa}"])
                        P.op("pe", lambda: nc.tensor.matmul(bview(ps[sb_][:, 0:256], 2), lhsT=kT2[64:128, g, ks],
                                                            rhs=qT[64:128, 2 * g:2 * g + 2, qs], start=True, stop=True),
                             writes=[f"ps{sb_}"])
                        P.op("act", lambda: nc.scalar.activation(out=PT[par][:, 0:256], in_=ps[sa][:, 0:256], func=AF.Exp,
                                                                 scale=0.125), reads=[f"ps{sa}"], writes=[("PT", par, 0)])
                        P.op("act", lambda: nc.scalar.activation(out=PT[par][:, 256:512], in_=ps[sb_][:, 0:256], func=AF.Exp,
                                                                 scale=0.125), reads=[f"ps{sb_}"], writes=[("PT", par, 1)])
                        me = "dve" if (n_ % 3 == 2) else "pool"
                        P.op(me, lambda: P.e[me].tensor_tensor(out=bview(PTm[par][:], 4), in0=bview(PT[par][:], 4),
                                                               in1=mT[:, kj, :].unsqueeze(1).to_broadcast([128, 4, 128]),
                                                               op=ALU.mult),
                             reads=[("PT", par, 0), ("PT", par, 1), (f"maskT{qi % 2}", kj // 8)], writes=[("PTm", par)])

                    def back(i):
                        g, kj = seq[i]
                        par = info[i]
                        ob = 2 + g % 2
                        for j in range(4):
                            hl = [0, 2, 1, 3][j]
                            f = lambda: nc.tensor.matmul(ps[ob][:, hl * 65:hl * 65 + 65], lhsT=PTm[par][:, j * 128:(j + 1) * 128],
                                                         rhs=vaug[:, kj, g, 0:65], start=(kj == 0 and j == 0),
                                                         stop=(kj == nk - 1 and j == 3), skip_group_check=True)
                            if j < 3:
                                P.quiet("pe", f, reads=[("PTm", par)], writes=[f"ps{ob}"])
                            else:
                                P.op("pe", f, reads=[("PTm", par)], writes=[f"ps{ob}"])
                        if kj == nk - 1:
                            ov = ps[ob][:, 0:260].rearrange("p (h d) -> p h d", h=4)
                            P.op("dve", lambda: nc.vector.reciprocal(out=rcp[:, 4 * (g % 2):4 * (g % 2) + 4], in_=ov[:, :, 64]),
                                 reads=[f"ps{ob}"], writes=[("b_rcp", g % 2)])
                            for hl in range(4):
                                hh = 4 * g + hl
                                P.op("act", lambda: nc.scalar.activation(out=ytm[:, hh * 64:(hh + 1) * 64],
                                                                         in_=ps[ob][:, hl * 65:hl * 65 + 64], func=AF.Copy,
                                                                         scale=rcp[:, 4 * (g % 2) + hl:4 * (g % 2) + hl + 1]),
                                     reads=[f"ps{ob}", ("b_rcp", g % 2)], writes=[("ytm", hh)])

                    for i in range(nseq + LOOK):
                        if i < nseq:
                            front(i)
                        if i >= LOOK:
                            back(i - LOOK)
                        for _ in range(per_i):
                            if stq:
                                stq.pop(0)()
                    while stq:
                        stq.pop(0)()

                if NQ > 0:
                    const_mask(0)
                    emit_maskT(0)
                for qi in range(NQ):
                    nxt = qi + 1
                    steps = []
                    if nxt < NQ:
                        if nxt >= 2:
                            steps = score_steps(nxt) + search_steps(nxt)
                        else:
                            const_mask(nxt)
                    attention_tile(qi, steps)
                    if nxt < NQ:
                        emit_maskT(nxt)
                    qs = slice(qi * 128, (qi + 1) * 128)
                    for j in range(8):
                        f = lambda: nc.tensor.transpose(out=psb[:, j * 128:(j + 1) * 128], in_=ytm[:, j * 128:(j + 1) * 128],
                                                        identity=ident)
                        if j < 7:
                            P.quiet("pe", f, reads=[("ytm", hh_) for hh_ in range(16)] + ["cstb"], writes=["psb"])
                        else:
                            P.op("pe", f, reads=[("ytm", hh_) for hh_ in range(16)] + ["cstb"], writes=["psb"])
                    P.op("act", lambda: nc.scalar.copy(out=yaT[:, :, qs], in_=bview(psb[:], 8)), reads=["psb"], writes=[("yaT", qi)])
                P.barrier()
            if stage == 2:
                o = dout("yaT_dbg", [128, 8, T], BF16)
                P.dma("sp", "out", [(o, yaT[:])])
                P.barrier()
                return nc, dbg
            P.dma("sp", "spill", [(ya_spill, yaT[:])])
            P.barrier()

        stB = contextlib.ExitStack()
        es.enter_context(stB)
        ysT = sb("ysT", [128, 16, T], BF16, stB)
        with contextlib.ExitStack() as sS:
            G8 = lambda t_, c_, g_: t_[:, c_, 8 * g_:8 * g_ + 8].unsqueeze(2).to_broadcast([128, 8, 64])
            wstS = sb("wstS", [128, 8, 256], F32, sS)
            selb = sb("selb", [128, 32, 128], BF16, sS)
            P.op("pool", lambda: nc.gpsimd.memset(selb[:], 0.0), writes=["selb"])
            for r3 in range(3):
                P.op("pool", lambda: nc.gpsimd.tensor_copy(
                    out=selb[32 * r3:32 * r3 + 32, :, :],
                    in_=cstb[32 * r3:32 * r3 + 32, C_ID + 32 * r3:C_ID + 32 * r3 + 32].unsqueeze(2).to_broadcast([32, 32, 128])),
                    reads=["cstb", "selb"], writes=["selb"])
            if sub == 101:
                P.barrier(); return nc, dbg
            for n_ in ("dt_bias", "a_log", "d_skip"):
                load_gain(n_, 32, sS)
            aneg = sb("aneg", [128, 32], F32, sS)
            P.op("act", lambda: nc.scalar.activation(out=aneg[:], in_=gb["a_log"][:], func=AF.Exp), reads=["g_a_log"], writes=["aneg"])
            P.op("dve", lambda: nc.vector.tensor_scalar(out=aneg[:], in0=aneg[:], scalar1=-1.0, scalar2=None, op0=ALU.mult),
                 reads=["aneg"], writes=["aneg"])
            if sub == 102:
                P.barrier(); return nc, dbg
            cwfm = sb("cwfm", [128, 24, 5], F32, sS)
            s0 = contextlib.ExitStack()
            cw5 = sb("cw5", [5, 3072], F32, s0)
            P.dma("sp", "cw5", [(cw5[0:4, :], convw_d), (cw5[4:5, :], gains["conv_b"])], writes=["cw5"])
            for t_ in range(24):
                f = lambda: nc.tensor.transpose(out=ps[0][:, t_ * 5:t_ * 5 + 5], in_=cw5[:, t_ * 128:(t_ + 1) * 128],
                                                identity=cst[0:5, C_ID:C_ID + 5])
                if t_ < 23:
                    P.quiet("pe", f, reads=["cw5", "cst"], writes=["ps0"])
                else:
                    P.op("pe", f, reads=["cw5", "cst"], writes=["ps0"])
            P.op("dve", lambda: nc.vector.tensor_copy(out=cwfm[:], in_=bview(ps[0][:, 0:120], 24)), reads=["ps0"], writes=["cwfm"])
            P.barrier()
            s0.close()
            if sub == 103:
                P.barrier(); return nc, dbg
            dt_all = sb("dt_all", [128, NT, 32], F32, sS)
            acs = sb("acs", [128, NT, 32], F32, sS)
            ea = sb("ea", [128, NT, 32], F32, sS)
            dtw = sb("dtw", [128, NT, 32], F32, sS)
            cdb = sb("cdb", [128, NT, 32], F32, sS)
            A3 = sb("A3", [128, NT, 128], BF16, sS)
            P.op("pool", lambda: nc.gpsimd.memset(A3[:], 0.0), writes=["A3z"])
            with contextlib.ExitStack() as s1:
                wdt_s = sb("wdt_s", [128, 8, 32], F32, s1)
                wdt = sb("wdt", [128, 8, 32], BF16, s1)
                P.dma("sp", "wdt", [(wdt_s[:], win_v[:, :, O_DT:O_DT + 32])], writes=["wdt_s"])
                P.op("pool", lambda: nc.gpsimd.tensor_copy(out=wdt[:], in_=wdt_s[:]), reads=["wdt_s"], writes=["wdt"])
                f32t = [sb(f"s1_{i}", [128, 32], F32, s1) for i in range(6)]
                a3 = sb("s1_a3", [128, 3, 32], F32, s1)
                Hb = sb("s1_Hb", [128, 128], BF16, s1)
                Mb = sb("s1_Mb", [128, 128], BF16, s1)
                r1 = sb("s1_r1", [128, 128], F32, s1)
                r2 = sb("s1_r2", [128, 128], F32, s1)
                ones_f = cst[:, C_ONE:C_ONE + 128]
                uinc = cst[:, C_CT:C_CT + 128]
                for c in range(NT):
                    cs_ = slice(c * 128, (c + 1) * 128)
                    xd, ax, ee, ll, rr, aa = f32t
                    for kt in range(8):
                        f = lambda: nc.tensor.matmul(ps[1][:, 0:32], lhsT=hT[:, kt, cs_], rhs=wdt[:, kt, :], start=(kt == 0), stop=(kt == 7))
                        if kt < 7:
                            P.quiet("pe", f, reads=["wdt"], writes=["ps1"])
                        else:
                            P.op("pe", f, reads=["wdt"], writes=["ps1"])
                    P.op("dve", lambda: nc.vector.tensor_tensor(out=xd[:], in0=ps[1][:, 0:32], in1=gb["dt_bias"][:], op=ALU.add),
                         reads=["ps1", "g_dt_bias"], writes=["s1_xd"])
                    P.op("act", lambda: nc.scalar.activation(out=ax[:], in_=xd[:], func=AF.Abs), reads=["s1_xd"], writes=["s1_ax"])
                    P.op("act", lambda: nc.scalar.activation(out=ee[:], in_=ax[:], func=AF.Exp, scale=-1.0), reads=["s1_ax"], writes=["s1_ee"])
                    P.op("act", lambda: nc.scalar.activation(out=ll[:], in_=ee[:], func=AF.Ln, bias=1.0), reads=["s1_ee"], writes=["s1_ll"])
                    P.op("dve", lambda: nc.vector.tensor_scalar(out=rr[:], in0=xd[:], scalar1=0.0, scalar2=None, op0=ALU.max),
                         reads=["s1_xd"], writes=["s1_rr"])
                    P.op("dve", lambda: nc.vector.tensor_tensor(out=dt_all[:, c, :], in0=rr[:], in1=ll[:], op=ALU.add),
                         reads=["s1_rr", "s1_ll"], writes=[("dt", c)])
                    P.op("dve", lambda: nc.vector.tensor_tensor(out=aa[:], in0=dt_all[:, c, :], in1=aneg[:], op=ALU.mult),
                         reads=[("dt", c), "aneg"], writes=["s1_aa"])
                    if sub == 104:
                        P.barrier(); return nc, dbg
                    P.op("dve", lambda: nc.vector.tensor_copy(out=a3[:], in_=aa[:].unsqueeze(1).to_broadcast([128, 3, 32])),
                         reads=["s1_aa"], writes=["s1_a3"])
                    if sub == 105:
                        P.barrier(); return nc, dbg
                    P.op("pe", lambda: nc.tensor.matmul(ps[2][:, 0:32], lhsT=uinc, rhs=aa[:], start=True, stop=True),
                         reads=["s1_aa", "cst"], writes=["ps2"])
                    P.op("pe", lambda: nc.tensor.matmul(ps[3][:, 0:32], lhsT=ones_f, rhs=aa[:], start=True, stop=True),
                         reads=["s1_aa", "cst"], writes=["ps3"])
                    P.op("pe", lambda: nc.tensor.matmul(ps[4][0:96, 0:128], lhsT=a3[:].rearrange("p a b -> p (a b)"), rhs=uinc,
                                                        start=True, stop=True),
                         reads=["s1_a3", "cst"], writes=["ps4"])
                    if sub == 106:
                        P.barrier(); return nc, dbg
                    P.op("dve", lambda: nc.vector.tensor_copy(out=acs[:, c, :], in_=ps[2][:, 0:32]), reads=["ps2"], writes=[("acs", c)])
                    if sub == 108:
                        P.barrier(); return nc, dbg
                    P.op("act", lambda: nc.scalar.activation(out=ea[:, c, :], in_=acs[:, c, :], func=AF.Exp), reads=[("acs", c)], writes=[("ea", c)])
                    P.op("dve", lambda: nc.vector.tensor_copy(out=rr[:], in_=ps[3][:, 0:32]), reads=["ps3"], writes=["s1_rr"])
                    P.op("act", lambda: nc.scalar.activation(out=cdb[:, c, :], in_=rr[:], func=AF.Exp), reads=["s1_rr"], writes=[("cdb", c)])
                    if sub == 109:
                        P.barrier(); return nc, dbg
                    P.op("dve", lambda: nc.vector.tensor_tensor(out=xd[:], in0=rr[:], in1=acs[:, c, :], op=ALU.subtract),
                         reads=["s1_rr", ("acs", c)], writes=["s1_xd"])
                    P.op("act", lambda: nc.scalar.activation(out=ee[:], in_=xd[:], func=AF.Exp), reads=["s1_xd"], writes=["s1_ee"])
                    P.op("dve", lambda: nc.vector.tensor_tensor(out=dtw[:, c, :], in0=dt_all[:, c, :], in1=ee[:], op=ALU.mult),
                         reads=[("dt", c), "s1_ee"], writes=[("dtw", c)])
                    if sub == 107:
                        P.barrier(); return nc, dbg
                    P.op("act", lambda: nc.scalar.copy(out=Hb[0:96, :], in_=ps[4][0:96, 0:128]), reads=["ps4"], writes=["s1_Hb"])
                    P.op("dve", lambda: nc.vector.tensor_tensor(out=r1[0:96, :], in0=ps[4][0:96, 0:128], in1=Hb[0:96, :], op=ALU.subtract),
                         reads=["ps4", "s1_Hb"], writes=["s1_r1"])
                    P.op("act", lambda: nc.scalar.copy(out=Mb[0:96, :], in_=r1[0:96, :]), reads=["s1_r1"], writes=["s1_Mb"])
                    P.op("dve", lambda: nc.vector.tensor_tensor(out=r2[0:96, :], in0=r1[0:96, :], in1=Mb[0:96, :], op=ALU.subtract),
                         reads=["s1_r1", "s1_Mb"], writes=["s1_r2"])
                    P.op("pool", lambda: nc.gpsimd.tensor_copy(out=A3[0:32, c, :], in_=Hb[0:32, :]), reads=["s1_Hb", "A3z"], writes=[("A3", c, 0)])
                    P.op("pool", lambda: nc.gpsimd.tensor_copy(out=A3[32:64, c, :], in_=Mb[32:64, :]), reads=["s1_Mb", "A3z"], writes=[("A3", c, 1)])
                    P.op("act", lambda: nc.scalar.copy(out=A3[64:96, c, :], in_=r2[64:96, :]), reads=["s1_r2", "A3z"], writes=[("A3", c, 2)])
                P.barrier()
            if stage == 3 and sub == 1:
                for nm, t_ in [("dt_all", dt_all), ("acs", acs), ("ea", ea), ("dtw", dtw), ("cdb", cdb)]:
                    o = dout(nm + "_dbg", [128, NT, 32], F32)
                    P.dma("sp", "out", [(o, t_[:])])
                o = dout("A3_dbg", [128, NT, 128], BF16)
                P.dma("sp", "out", [(o, A3[:])])
                o = dout("cwfm_dbg", [128, 24, 5], F32)
                P.dma("sp", "out", [(o, cwfm[:])])
                o = dout("selb_dbg", [128, 32, 128], BF16)
                P.dma("sp", "out", [(o, selb[:])])
                P.barrier()
                return nc, dbg

            xs_tm = sb("xs_tm", [128, NT, 512], BF16, sS)
            BT = sb("BT", [128, T], BF16, sS)
            CT = sb("CT", [128, T], BF16, sS)
            B_tm = sb("B_tm", [128, NT, 128], BF16, sS)
            rawb = sb("rawb", [128, T + 4], BF16, sS)
            xcf = [sb(f"xcf{i}", [128, 512], BF16, sS) for i in range(2)]
            dg = [sb(f"dg{i}", [128, 4, 128], BF16, sS) for i in range(2)]
            wch = [sb(f"wch{i}", [128, 8, 128], BF16, sS) for i in range(2)]
            wz = sb("wz", [128, 8, 512], BF16, sS)
            ssdg = sb("ssdg", [128, 512], F32, sS)
            hst = sb("hst", [128, 512], F32, sS)
            hstb = sb("hstb", [128, 512], BF16, sS)
            cbm = sb("cbm", [128, 128], F32, sS)
            seg = [sb(f"seg{i}", [128, 512], F32, sS) for i in range(2)]
            Ee = seg
            MT = [sb(f"MT{i}", [128, 512], BF16, sS) for i in range(4)]
            xdt = [sb(f"xdt{i}", [128, 512], BF16, sS) for i in range(2)]
            xw = [sb(f"xw{i}", [128, 512], BF16, sS) for i in range(2)]
            t1 = sb("t1", [128, 512], F32, sS)
            t1b = [t1, sb("t1b", [128, 512], F32, sS)]
            dsk = sb("dsk", [128, 8, 128], BF16, sS)
            epsb = sb("epsb", [128, 1], F32, sS)
            P.op("pool", lambda: nc.gpsimd.memset(epsb[:], EPS), writes=["epsb"])
            t3 = sb("t3", [128, 512], F32, sS)
            yv = t1
            sz = t3
            ynb = [sb(f"ynb{i}", [128, 512], BF16, sS) for i in range(2)]
            sjk = xcf[0]
            g1 = [sb(f"g1_{i}", [128, 2], F32, sS) for i in range(4)]
            P.op("pool", lambda: nc.gpsimd.memset(rawb[:, 0:4], 0.0), writes=["rawb_halo"])
            wctr2 = [0]
            for g in range(4 if sub >= 30 else 1):
                for hf in range(2):
                    c0 = O_Z + g * 512 + hf * 256
                    P.dma("sp", "wstS", [(wstS[:], win_v[:, :, c0:c0 + 256])], writes=["wstS"])
                    P.op("pool", lambda: nc.gpsimd.tensor_copy(out=wz[:, :, hf * 256:(hf + 1) * 256], in_=wstS[:]),
                         reads=["wstS"], writes=[("wz", hf)])
                P.dma("sp", "ssdg", [(ssdg[:], gains["ssd_norm"][:, g * 512:(g + 1) * 512].partition_broadcast(128))], writes=["ssdg"])
                chts = [(O_XBC + g * 512 + j * 128, 4 * g + j, "x", j) for j in range(4)]
                chts += [(O_XBC + 2048 + g * 128, 16 + g, "B", 0), (O_XBC + 2560 + g * 128, 20 + g, "C", 0)]
                for (c0, cti, kind, j) in chts:
                    wi = wctr2[0] % 2
                    wctr2[0] += 1
                    P.dma("sp", "wstS", [(wstS[:, :, 0:128], win_v[:, :, c0:c0 + 128])], writes=["wstS"])
                    P.op("pool", lambda: nc.gpsimd.tensor_copy(out=wch[wi][:], in_=wstS[:, :, 0:128]), reads=["wstS"], writes=[f"wch{wi}"])
                    for jj in range(4):
                        P.op("dve", lambda: nc.vector.tensor_scalar(out=dg[wi][:, jj, :], in0=ident, scalar1=cwfm[:, cti, jj:jj + 1],
                                                                    scalar2=None, op0=ALU.mult),
                             reads=["cstb", "cwfm"], writes=[(f"dg{wi}", jj)])
                    for tb in range(4):
                        bank = tb % 2
                        for kt in range(8):
                            f = lambda: nc.tensor.matmul(ps[bank][:], lhsT=wch[wi][:, kt, :], rhs=hT[:, kt, tb * 512:(tb + 1) * 512],
                                                         start=(kt == 0), stop=(kt == 7))
                            if kt < 7:
                                P.quiet("pe", f, reads=[f"wch{wi}"], writes=[f"ps{bank}"])
                            else:
                                P.op("pe", f, reads=[f"wch{wi}"], writes=[f"ps{bank}"])
                        P.op("act", lambda: nc.scalar.copy(out=rawb[:, 4 + tb * 512:4 + (tb + 1) * 512], in_=ps[bank][:]),
                             reads=[f"ps{bank}"], writes=[("rawb", tb)])
                    for tb in range(4):
                        bank = 2 + tb % 2
                        for jj in range(4):
                            f = lambda: nc.tensor.matmul(ps[bank][:], lhsT=dg[wi][:, jj, :],
                                                         rhs=rawb[:, 1 + tb * 512 + jj:1 + tb * 512 + jj + 512],
                                                         start=(jj == 0), stop=(jj == 3))
                            rd = [(f"dg{wi}", jj), ("rawb", tb), "rawb_halo"] + ([("rawb", tb - 1)] if tb > 0 else [])
                            if jj < 3:
                                P.quiet("pe", f, reads=rd, writes=[f"ps{bank}"])
                            else:
                                P.op("pe", f, reads=rd, writes=[f"ps{bank}"])
                        if kind == "x":
                            xb_ = tb % 2
                            P.op("act", lambda: nc.scalar.activation(out=xcf[xb_][:], in_=ps[bank][:], func=AF.Silu,
                                                                     bias=cwfm[:, cti, 4:5]),
                                 reads=[f"ps{bank}", "cwfm"], writes=[f"xcf{xb_}"])
                            for i4 in range(4):
                                f = lambda: nc.tensor.transpose(out=psb[:, i4 * 128:(i4 + 1) * 128], in_=xcf[xb_][:, i4 * 128:(i4 + 1) * 128],
                                                                identity=ident)
                                if i4 < 3:
                                    P.quiet("pe", f, reads=[f"xcf{xb_}", "cstb"], writes=["psb"])
                                else:
                                    P.op("pe", f, reads=[f"xcf{xb_}", "cstb"], writes=["psb"])
                            P.op("act", lambda: nc.scalar.copy(out=xs_tm[:, tb * 4:(tb + 1) * 4, j * 128:(j + 1) * 128],
                                                               in_=bview(psb[:, 0:512], 4)),
                                 reads=["psb"], writes=[("xs_tm", tb, j)])
                        else:
                            dstT = BT if kind == "B" else CT
                            P.op("act", lambda: nc.scalar.activation(out=dstT[:, tb * 512:(tb + 1) * 512], in_=ps[bank][:], func=AF.Silu,
                                                                     bias=cwfm[:, cti, 4:5]),
                                 reads=[f"ps{bank}", "cwfm"], writes=[(kind + "T", tb)])
                if sub == 202:
                    P.barrier(); return nc, dbg
                for k0 in range(0, NT, 8):
                    for jj in range(8):
                        cc = k0 + jj
                        f = lambda: nc.tensor.transpose(out=psb[:, jj * 128:(jj + 1) * 128], in_=BT[:, cc * 128:(cc + 1) * 128], identity=ident)
                        if jj < 7:
                            P.quiet("pe", f, reads=[("BT", cc // 4), "cstb"], writes=["psb"])
                        else:
                            P.op("pe", f, reads=[("BT", cc // 4), "cstb"], writes=["psb"])
                    P.op("act", lambda: nc.scalar.copy(out=B_tm[:, k0:k0 + 8, :], in_=bview(psb[:], 8)), reads=["psb"], writes=[("B_tm", k0 // 8)])
                for hl_ in range(8):
                    P.op("dve", lambda: nc.vector.tensor_scalar(out=dsk[:, hl_, :], in0=ident, scalar1=gb["d_skip"][:, 8 * g + hl_:8 * g + hl_ + 1],
                                                                scalar2=None, op0=ALU.mult),
                         reads=["cstb", "g_d_skip"], writes=["dsk"])
                P.op("pool", lambda: nc.gpsimd.memset(hst[:], 0.0), writes=["hst"])
                P.op("pool", lambda: nc.gpsimd.memset(hstb[:], 0.0), writes=["hstb"])
                if sub == 203:
                    P.barrier(); return nc, dbg
                xsr = lambda c_: [("xs_tm", c_ // 4, j_) for j_ in range(4)]
                def front(c):
                    cs_ = slice(c * 128, (c + 1) * 128)
                    pb = c % 2
                    P.op("pe", lambda: nc.tensor.matmul(ps[0][:, 0:128], lhsT=BT[:, cs_], rhs=CT[:, cs_], start=True, stop=True),
                         reads=[("BT", c // 4), ("CT", c // 4)], writes=["ps0"])
                    P.op("dve", lambda: nc.vector.tensor_tensor(out=cbm[:], in0=ps[0][:, 0:128], in1=cst[:, C_CT:C_CT + 128], op=ALU.mult),
                         reads=["ps0", "cst"], writes=["cbm"])
                    P.op("pool", lambda: nc.gpsimd.tensor_tensor(out=bview(xdt[pb][:], 8), in0=bview(xs_tm[:, c, :], 8), in1=G8(dt_all, c, g),
                                                                 op=ALU.mult), reads=xsr(c), writes=[f"xdt{pb}"])
                    P.op("pool", lambda: nc.gpsimd.tensor_tensor(out=bview(xw[pb][:], 8), in0=bview(xs_tm[:, c, :], 8), in1=G8(dtw, c, g),
                                                                 op=ALU.mult), reads=xsr(c), writes=[f"xw{pb}"])
                    for hb in range(2):
                        bcb = 1 + hb
                        for hh in range(4):
                            h = 8 * g + 4 * hb + hh
                            f = lambda: nc.tensor.matmul(ps[bcb][:, hh * 128:(hh + 1) * 128], lhsT=selb[:, h, :], rhs=A3[:, c, :],
                                                         start=True, stop=True, skip_group_check=True)
                            if hh < 3:
                                P.quiet("pe", f, reads=["selb"], writes=[f"ps{bcb}"])
                            else:
                                P.op("pe", f, reads=["selb"], writes=[f"ps{bcb}"])
                    for hb in range(2):
                        bcb = 1 + hb
                        for hh in range(4):
                            h = 8 * g + 4 * hb + hh
                            P.op("dve", lambda: nc.vector.tensor_scalar(out=seg[hb][:, hh * 128:(hh + 1) * 128],
                                                                        in0=ps[bcb][:, hh * 128:(hh + 1) * 128],
                                                                        scalar1=acs[:, c, h:h + 1], scalar2=0.0, op0=ALU.subtract, op1=ALU.min),
                                 reads=[f"ps{bcb}"], writes=[(f"seg{hb}", hh), f"Ee{hb}"])
                        P.op("act", lambda: nc.scalar.activation(out=Ee[hb][:], in_=seg[hb][:], func=AF.Exp),
                             reads=[(f"seg{hb}", hh_) for hh_ in range(4)], writes=[f"Ee{hb}"] + [(f"seg{hb}", hh_) for hh_ in range(4)])
                    for hb in range(2):
                        mi = 2 * pb + hb
                        P.op("dve", lambda: nc.vector.tensor_tensor(out=bview(MT[mi][:], 4), in0=bview(Ee[hb][:], 4),
                                                                    in1=cbm[:].unsqueeze(1).to_broadcast([128, 4, 128]), op=ALU.mult),
                             reads=[f"Ee{hb}", "cbm"], writes=[f"MT{mi}"])

                def back(c):
                    cs_ = slice(c * 128, (c + 1) * 128)
                    pb = c % 2
                    P.op("pe", lambda: nc.tensor.matmul(ps[5][:], lhsT=B_tm[:, c, :], rhs=xw[pb][:], start=True, stop=True),
                         reads=[("B_tm", c // 8), f"xw{pb}"], writes=["ps5"])
                    for hb in range(2):
                        mi = 2 * pb + hb
                        for hh in range(4):
                            hl = 4 * hb + hh
                            P.quiet("pe", lambda: nc.tensor.matmul(ps[3][:, hl * 64:(hl + 1) * 64], lhsT=MT[mi][:, hh * 128:(hh + 1) * 128],
                                                                   rhs=xdt[pb][:, hl * 64:(hl + 1) * 64], start=True, stop=False, skip_group_check=True),
                                    reads=[f"MT{mi}", f"xdt{pb}"], writes=["ps3"])
                            f = lambda: nc.tensor.matmul(ps[3][:, hl * 64:(hl + 1) * 64], lhsT=dsk[:, hl, :],
                                                         rhs=xs_tm[:, c, hl * 64:(hl + 1) * 64], start=False, stop=True, skip_group_check=True)
                            if hl < 7:
                                P.quiet("pe", f, reads=["dsk"] + xsr(c), writes=["ps3"])
                            else:
                                P.op("pe", f, reads=["dsk"] + xsr(c), writes=["ps3"])
                    for kt in range(8):
                        f = lambda: nc.tensor.matmul(ps[6][:], lhsT=hT[:, kt, cs_], rhs=wz[:, kt, :], start=(kt == 0), stop=(kt == 7))
                        if kt < 7:
                            P.quiet("pe", f, reads=[("wz", 0), ("wz", 1)], writes=["ps6"])
                        else:
                            P.op("pe", f, reads=[("wz", 0), ("wz", 1)], writes=["ps6"])
                    P.op("act", lambda: nc.scalar.activation(out=sz[:], in_=ps[6][:], func=AF.Silu), reads=["ps6"], writes=["t3"])
                    tb_ = t1b[pb]
                    tn = f"t1_{pb}"
                    if c > 0:
                        P.op("pe", lambda: nc.tensor.matmul(ps[4][:], lhsT=CT[:, cs_], rhs=hstb[:], start=True, stop=True),
                             reads=[("CT", c // 4), "hstb"], writes=["ps4"])
                        P.op("dve", lambda: nc.vector.tensor_tensor(out=bview(tb_[:], 8), in0=bview(ps[4][:], 8), in1=G8(ea, c, g), op=ALU.mult),
                             reads=["ps4"], writes=[tn])
                        P.op("dve", lambda: nc.vector.tensor_tensor(out=tb_[:], in0=ps[3][:], in1=tb_[:], op=ALU.add),
                             reads=["ps3", tn], writes=[tn])
                    else:
                        P.op("dve", lambda: nc.vector.tensor_copy(out=tb_[:], in_=ps[3][:]), reads=["ps3"], writes=[tn])
                    P.op("dve", lambda: nc.vector.tensor_tensor(out=bview(hst[:], 8), in0=bview(hst[:], 8), in1=G8(cdb, c, g), op=ALU.mult),
                         reads=["hst"], writes=["hst"])
                    P.op("dve", lambda: nc.vector.tensor_tensor(out=hst[:], in0=ps[5][:], in1=hst[:], op=ALU.add), reads=["ps5", "hst"], writes=["hst"])
                    P.op("act", lambda: nc.scalar.copy(out=hstb[:], in_=hst[:]), reads=["hst"], writes=["hstb"])
                    P.op("dve", lambda: nc.vector.tensor_tensor(out=tb_[:], in0=tb_[:], in1=sz[:], op=ALU.mult), reads=[tn, "t3"], writes=[tn])
                    P.op("act", lambda: nc.scalar.activation(out=sjk[:], in_=tb_[:], func=AF.Square, accum_out=g1[0][:, pb:pb + 1]),
                         reads=[tn], writes=["xcf0", ("g1_0", pb)])
                    P.op("act", lambda: nc.scalar.activation(out=g1[2][:, pb:pb + 1], in_=g1[0][:, pb:pb + 1], func=AF.Ln, scale=1.0 / 512, bias=epsb[:, 0:1]),
                         reads=[("g1_0", pb), "epsb"], writes=[("g1_2", pb)])
                    P.op("act", lambda: nc.scalar.activation(out=g1[3][:, pb:pb + 1], in_=g1[2][:, pb:pb + 1], func=AF.Exp, scale=-0.5),
                         reads=[("g1_2", pb)], writes=[("g1_3", pb)])

                def backB(c):
                    pb = c % 2
                    tb_ = t1b[pb]
                    tn = f"t1_{pb}"
                    P.op("dve", lambda: nc.vector.scalar_tensor_tensor(out=ynb[pb][:], in0=tb_[:], scalar=g1[3][:, pb:pb + 1], in1=ssdg[:], op0=ALU.mult, op1=ALU.mult),
                         reads=[tn, ("g1_3", pb), "ssdg"], writes=[f"ynb{pb}"])

                def tail(c):
                    cs_ = slice(c * 128, (c + 1) * 128)
                    pb = c % 2
                    for i4 in range(4):
                        f = lambda: nc.tensor.transpose(out=psb[:, i4 * 128:(i4 + 1) * 128], in_=ynb[pb][:, i4 * 128:(i4 + 1) * 128], identity=ident)
                        if i4 < 3:
                            P.quiet("pe", f, reads=[f"ynb{pb}", "cstb"], writes=["psb"])
                        else:
                            P.op("pe", f, reads=[f"ynb{pb}", "cstb"], writes=["psb"])
                    P.op("act", lambda: nc.scalar.copy(out=ysT[:, 4 * g:4 * g + 4, cs_], in_=bview(psb[:, 0:512], 4)), reads=["psb"], writes=[("ysT", g, c)])

                front(0)
                for c in range(NT):
                    if c + 1 < NT:
                        front(c + 1)
                    back(c)
                    if c >= 1:
                        backB(c - 1)
                        tail(c - 1)
                backB(NT - 1)
                tail(NT - 1)
            P.barrier()
        if stage == 3:
            o = dout("ysT_dbg", [128, 16, T], BF16)
            P.dma("sp", "out", [(o, ysT[:])])
            P.barrier()
            return nc, dbg

        stM = contextlib.ExitStack()
        es.enter_context(stM)
        mgT = sb("mgT", [128, 8, T], BF16, stM)
        with contextlib.ExitStack() as sM:
            yaT2 = sb("yaT2", [128, 8, T], BF16, sM)
            P.dma("sp", "ya_reload", [(yaT2[:], ya_spill)], writes=["yaT2"])
            wstM = sb("wstM", [128, 16, 128], F32, sM)
            wac = [sb(f"wac{i}", [128, 8, 128], BF16, sM) for i in range(2)]
            wbc = [sb(f"wbc{i}", [128, 16, 128], BF16, sM) for i in range(2)]
            wgac = [sb(f"wgac{i}", [128, 8, 128], BF16, sM) for i in range(2)]
            wgbc = [sb(f"wgbc{i}", [128, 8, 128], BF16, sM) for i in range(2)]
            sga = [sb(f"sga{i}", [128, 512], F32, sM) for i in range(2)]
            sgb = [sb(f"sgb{i}", [128, 512], F32, sM) for i in range(2)]
            wa_v = wa_d.rearrange("(kt p) n -> p kt n", p=128)
            wb_v = wb_d.rearrange("(kt p) n -> p kt n", p=128)
            it = [0]

            def load_merge_w(nt):
                ns = slice(nt * 128, (nt + 1) * 128)
                wp = nt % 2
                for (dst, src, nk_, nm) in [(wgac[wp], win_v[:, :, O_GA + nt * 128:O_GA + (nt + 1) * 128], 8, f"wgac{wp}"),
                                            (wgbc[wp], win_v[:, :, O_GB + nt * 128:O_GB + (nt + 1) * 128], 8, f"wgbc{wp}"),
                                            (wac[wp], wa_v[:, :, ns], 8, f"wac{wp}"), (wbc[wp], wb_v[:, :, ns], 16, f"wbc{wp}")]:
                    P.dma("sp", "wstM", [(wstM[:, 0:nk_, :], src)], writes=["wstM"])
                    P.op("dve", lambda: nc.vector.tensor_copy(out=dst[:], in_=wstM[:, 0:nk_, :]), reads=["wstM"], writes=[nm])

            load_merge_w(0)
            for nt in range(8):
                wp = nt % 2
                if nt + 1 < 8:
                    load_merge_w(nt + 1)
                for tb in range(4):
                    ts_ = slice(tb * 512, (tb + 1) * 512)
                    par = it[0] % 2
                    it[0] += 1
                    bA, bB, bGA = (0, 1, 2) if par == 0 else (4, 5, 6)
                    bGB = 3

                    def acc(bank, wt, nk_, rhsT, nm, rd):
                        for kt in range(nk_):
                            f = lambda: nc.tensor.matmul(ps[bank][:], lhsT=wt[:, kt, :], rhs=rhsT[:, kt, ts_], start=(kt == 0), stop=(kt == nk_ - 1))
                            if kt < nk_ - 1:
                                P.quiet("pe", f, reads=[nm] + rd, writes=[f"ps{bank}"])
                            else:
                                P.op("pe", f, reads=[nm] + rd, writes=[f"ps{bank}"])
                    acc(bGA, wgac[wp], 8, hT, f"wgac{wp}", [])
                    acc(bGB, wgbc[wp], 8, hT, f"wgbc{wp}", [])
                    acc(bA, wac[wp], 8, yaT2, f"wac{wp}", ["yaT2"])
                    acc(bB, wbc[wp], 16, ysT, f"wbc{wp}", [])
                    P.op("act", lambda: nc.scalar.activation(out=sga[par][:], in_=ps[bGA][:], func=AF.Sigmoid), reads=[f"ps{bGA}"], writes=[f"sga{par}"])
                    P.op("act", lambda: nc.scalar.activation(out=sgb[par][:], in_=ps[bGB][:], func=AF.Sigmoid), reads=[f"ps{bGB}"], writes=[f"sgb{par}"])
                    P.op("dve", lambda: nc.vector.tensor_tensor(out=sga[par][:], in0=ps[bA][:], in1=sga[par][:], op=ALU.mult),
                         reads=[f"ps{bA}", f"sga{par}"], writes=[f"sga{par}"])
                    P.op("dve", lambda: nc.vector.tensor_tensor(out=sgb[par][:], in0=ps[bB][:], in1=sgb[par][:], op=ALU.mult),
                         reads=[f"ps{bB}", f"sgb{par}"], writes=[f"sgb{par}"])
                    P.op("pool", lambda: nc.gpsimd.tensor_tensor(out=mgT[:, nt, ts_], in0=sga[par][:], in1=sgb[par][:], op=ALU.add),
                         reads=[f"sga{par}", f"sgb{par}"], writes=[("mgT", nt, tb)])
            P.barrier()
        if stage == 4:
            o = dout("mgT_dbg", [128, 8, T], BF16)
            P.dma("sp", "out", [(o, mgT[:])])
            P.barrier()
            return nc, dbg

        x1 = ysT[:].bitcast(F32)
        assert list(x1.shape) == [128, NT, D], x1.shape
        with contextlib.ExitStack() as sO:
            wstO = sb("wstO", [128, 8, 256], F32, sO)
            wo = sb("wo", [128, 8, D], BF16, sO)
            wo_v = wo_d.rearrange("(kt p) n -> p kt n", p=128)
            for q4 in range(4):
                P.dma("sp", "wstO", [(wstO[:], wo_v[:, :, q4 * 256:(q4 + 1) * 256])], writes=["wstO"])
                P.op("pool", lambda: nc.gpsimd.tensor_copy(out=wo[:, :, q4 * 256:(q4 + 1) * 256], in_=wstO[:]), reads=["wstO"], writes=[("wo", q4)])
            for c in range(NT):
                cs_ = slice(c * 128, (c + 1) * 128)
                P.dma("sp", f"x1ld{c}", [(x1[:, c, :], x_d[cs_, :])], writes=[("x1", c)])
                for hf in range(2):
                    bank = (2 * c + hf) % 4
                    for kt in range(8):
                        f = lambda: nc.tensor.matmul(ps[bank][:], lhsT=mgT[:, kt, cs_], rhs=wo[:, kt, hf * 512:(hf + 1) * 512],
                                                     start=(kt == 0), stop=(kt == 7))
                        rd = [("wo", 2 * hf), ("wo", 2 * hf + 1)]
                        if kt < 7:
                            P.quiet("pe", f, reads=rd, writes=[f"ps{bank}"])
                        else:
                            P.op("pe", f, reads=rd, writes=[f"ps{bank}"])
                    P.op("dve", lambda: nc.vector.tensor_tensor(out=x1[:, c, hf * 512:(hf + 1) * 512], in0=ps[bank][:],
                                                                in1=x1[:, c, hf * 512:(hf + 1) * 512], op=ALU.add),
                         reads=[f"ps{bank}", ("x1", c)], writes=[("x1", c)])
            P.barrier()
        stM.close()
        if stage == 5:
            o = dout("x1_dbg", [128, NT, D], F32)
            P.dma("sp", "out", [(o, x1)])
            P.barrier()
            return nc, dbg

        with contextlib.ExitStack() as sN:
            load_gain("ffn_norm", D, sN)

            def from_x1(c, bufs):
                return x1[:, c, :], ("x1", c)
            rmsnorm_T(from_x1, "ffn_norm", hT, sN, nbuf=0)
            P.barrier()

        with contextlib.ExitStack() as sE:
            selm = sb("selm", [128, 32, 128], BF16, sE)
            P.op("pool", lambda: nc.gpsimd.memset(selm[:], 0.0), writes=["selm"])
            for r3 in range(3):
                P.op("pool", lambda: nc.gpsimd.tensor_copy(
                    out=selm[32 * r3:32 * r3 + 32, :, :],
                    in_=cstb[32 * r3:32 * r3 + 32, C_ID + 32 * r3:C_ID + 32 * r3 + 32].unsqueeze(2).to_broadcast([32, 32, 128])),
                    reads=["cstb", "selm"], writes=["selm"])
            cT3 = sb("cT3", [128, T], BF16, sE)
            P.op("pool", lambda: nc.gpsimd.memset(cT3[:], 0.0), writes=["cT3z"])
            with contextlib.ExitStack() as sR:
                wr_s = sb("wr_s", [128, 8, 36], F32, sR)
                wr = sb("wr", [128, 8, 36], BF16, sR)
                P.dma("sp", "wr_s", [(wr_s[:, :, 0:4], wrg_d.rearrange("(kt p) n -> p kt n", p=128)),
                                     (wr_s[:, :, 4:36], wre_d.rearrange("(kt p) n -> p kt n", p=128))], writes=["wr_s"])
                P.op("pool", lambda: nc.gpsimd.tensor_copy(out=wr[:], in_=wr_s[:]), reads=["wr_s"], writes=["wr"])
                rb_ = sb("rbias", [128, 36], F32, sR)
                P.dma("sp", "rbias", [(rb_[:, 0:4], gains["b_route_group"].partition_broadcast(128)),
                                      (rb_[:, 4:36], gains["b_route_expert"].partition_broadcast(128))], writes=["rbias"])
                def mk_scratch(p):
                    S = {}
                    S["lg"] = sb(f"r_lg{p}", [128, 36], F32, sR)
                    S["c1"] = [sb(f"r_c{i}_{p}", [128, 1], F32, sR) for i in range(8)]
                    S["oh"] = sb(f"r_oh{p}", [128, 4], F32, sR)
                    S["ge"] = sb(f"r_ge{p}", [128, 4], F32, sR)
                    S["t48"] = sb(f"r_t48{p}", [128, 4, 8], F32, sR)
                    S["ein"] = sb(f"r_ein{p}", [128, 8], F32, sR)
                    S["top8"] = sb(f"r_top8{p}", [128, 8], F32, sR)
                    S["msel"] = sb(f"r_msel{p}", [128, 8], F32, sR)
                    S["wex"] = sb(f"r_wex{p}", [128, 8], F32, sR)
                    S["comb"] = sb(f"r_comb{p}", [128, 4, 8], F32, sR)
                    S["comb3"] = sb(f"r_comb3{p}", [128, 3, 32], F32, sR)
                    S["Hb"] = sb(f"r_Hb{p}", [128, 128], BF16, sR)
                    S["Mb"] = sb(f"r_Mb{p}", [128, 128], BF16, sR)
                    S["q1"] = sb(f"r_q1{p}", [128, 128], F32, sR)
                    S["q2"] = sb(f"r_q2{p}", [128, 128], F32, sR)
                    return S
                SC = [mk_scratch(0), mk_scratch(1)]

                def router_tile(c, p):
                    S = SC[p]
                    n = lambda x: f"{x}{p}"
                    bl, bt = (0, 1) if p == 0 else (2, 3)
                    lg, oh, ge, tmp48, ein, top8, msel, wex, comb, comb3 = (S[k] for k in ("lg", "oh", "ge", "t48", "ein", "top8", "msel", "wex", "comb", "comb3"))
                    Hb2, Mb2, q1, q2 = S["Hb"], S["Mb"], S["q1"], S["q2"]
                    mx, nmx, sme, gw, m21, den, rden, sc_ = S["c1"]
                    cs_ = slice(c * 128, (c + 1) * 128)
                    for kt in range(8):
                        f = lambda kt=kt: nc.tensor.matmul(ps[bl][:, 0:36], lhsT=hT[:, kt, cs_], rhs=wr[:, kt, :], start=(kt == 0), stop=(kt == 7))
                        if kt < 7:
                            P.quiet("pe", f, reads=["wr"], writes=[f"ps{bl}"])
                        else:
                            P.op("pe", f, reads=["wr"], writes=[f"ps{bl}"])
                    P.op("dve", lambda: nc.vector.tensor_tensor(out=lg[:], in0=ps[bl][:, 0:36], in1=rb_[:], op=ALU.add), reads=[f"ps{bl}", "rbias"], writes=[n("r_lg")])
                    P.op("dve", lambda: nc.vector.tensor_reduce(out=mx[:], in_=lg[:, 0:4], axis=AX.X, op=ALU.max), reads=[n("r_lg")], writes=[n("r_mx")])
                    P.op("dve", lambda: nc.vector.tensor_scalar(out=oh[:], in0=lg[:, 0:4], scalar1=mx[:, 0:1], scalar2=None, op0=ALU.is_ge),
                         reads=[n("r_lg"), n("r_mx")], writes=[n("r_oh")])
                    P.op("dve", lambda: nc.vector.tensor_scalar(out=nmx[:], in0=mx[:], scalar1=-1.0, scalar2=None, op0=ALU.mult), reads=[n("r_mx")], writes=[n("r_nmx")])
                    P.op("act", lambda: nc.scalar.activation(out=ge[:], in_=lg[:, 0:4], func=AF.Exp, bias=nmx[:, 0:1], accum_out=sme[:]),
                         reads=[n("r_lg"), n("r_nmx")], writes=[n("r_ge"), n("r_sme")])
                    P.op("dve", lambda: nc.vector.tensor_tensor(out=tmp48[:], in0=bview(lg[:, 4:36], 4), in1=oh[:].unsqueeze(2).to_broadcast([128, 4, 8]),
                                                                op=ALU.mult), reads=[n("r_lg"), n("r_oh")], writes=[n("r_t48")])
                    P.op("dve", lambda: nc.vector.tensor_reduce(out=ein[:], in_=tmp48[:].rearrange("p g e -> p e g"), axis=AX.X, op=ALU.add),
                         reads=[n("r_t48")], writes=[n("r_ein")])
                    P.op("dve", lambda: nc.vector.max(out=top8[:], in_=ein[:]), reads=[n("r_ein")], writes=[n("r_top8")])
                    P.op("dve", lambda: nc.vector.tensor_scalar(out=msel[:], in0=ein[:], scalar1=top8[:, 1:2], scalar2=None, op0=ALU.is_ge),
                         reads=[n("r_ein"), n("r_top8")], writes=[n("r_msel")])
                    P.op("dve", lambda: nc.vector.tensor_scalar(out=den[:], in0=top8[:, 0:1], scalar1=-1.0, scalar2=None, op0=ALU.mult),
                         reads=[n("r_top8")], writes=[n("r_nm1")])
                    P.op("act", lambda: nc.scalar.activation(out=wex[:], in_=ein[:], func=AF.Exp, bias=den[:, 0:1]), reads=[n("r_ein"), n("r_nm1")], writes=[n("r_wex")])
                    P.op("act", lambda: nc.scalar.activation(out=m21[:], in_=top8[:, 1:2], func=AF.Exp, bias=den[:, 0:1]), reads=[n("r_top8"), n("r_nm1")], writes=[n("r_m21")])
                    P.op("dve", lambda: nc.vector.scalar_tensor_tensor(out=rden[:], in0=m21[:], scalar=1.0, in1=sme[:], op0=ALU.add, op1=ALU.mult),
                         reads=[n("r_m21"), n("r_sme")], writes=[n("r_rden")])
                    P.op("dve", lambda: nc.vector.reciprocal(out=sc_[:], in_=rden[:]), reads=[n("r_rden")], writes=[n("r_sc")])
                    P.op("dve", lambda: nc.vector.scalar_tensor_tensor(out=wex[:], in0=wex[:], scalar=sc_[:, 0:1], in1=msel[:], op0=ALU.mult, op1=ALU.mult),
                         reads=[n("r_wex"), n("r_sc"), n("r_msel")], writes=[n("r_wex")])
                    P.op("dve", lambda: nc.vector.tensor_tensor(out=comb[:], in0=oh[:].unsqueeze(2).to_broadcast([128, 4, 8]),
                                                                in1=wex[:].unsqueeze(1).to_broadcast([128, 4, 8]), op=ALU.mult),
                         reads=[n("r_oh"), n("r_wex")], writes=[n("r_comb")])
                    P.op("dve", lambda: nc.vector.tensor_copy(out=comb3[:], in_=comb[:].rearrange("p g e -> p (g e)").unsqueeze(1).to_broadcast([128, 3, 32])),
                         reads=[n("r_comb")], writes=[n("r_comb3")])
                    P.op("pe", lambda: nc.tensor.transpose(out=ps[bt][0:96, 0:128], in_=comb3[:].rearrange("p a b -> p (a b)"),
                                                           identity=cst[:, C_ID:C_ID + 128]),
                         reads=[n("r_comb3"), "cst"], writes=[f"ps{bt}"])
                    P.op("act", lambda: nc.scalar.copy(out=Hb2[0:96, :], in_=ps[bt][0:96, 0:128]), reads=[f"ps{bt}"], writes=[n("r_Hb")])
                    P.op("dve", lambda: nc.vector.tensor_tensor(out=q1[0:96, :], in0=ps[bt][0:96, 0:128], in1=Hb2[0:96, :], op=ALU.subtract),
                         reads=[f"ps{bt}", n("r_Hb")], writes=[n("r_q1")])
                    P.op("act", lambda: nc.scalar.copy(out=Mb2[0:96, :], in_=q1[0:96, :]), reads=[n("r_q1")], writes=[n("r_Mb")])
                    P.op("dve", lambda: nc.vector.tensor_tensor(out=q2[0:96, :], in0=q1[0:96, :], in1=Mb2[0:96, :], op=ALU.subtract),
                         reads=[n("r_q1"), n("r_Mb")], writes=[n("r_q2")])
                    P.op("act", lambda: nc.scalar.copy(out=cT3[0:32, cs_], in_=Hb2[0:32, :]), reads=[n("r_Hb"), "cT3z"], writes=[("cT3", c, 0)])
                    P.op("act", lambda: nc.scalar.copy(out=cT3[32:64, cs_], in_=Mb2[32:64, :]), reads=[n("r_Mb"), "cT3z"], writes=[("cT3", c, 1)])
                    P.op("act", lambda: nc.scalar.copy(out=cT3[64:96, cs_], in_=q2[64:96, :]), reads=[n("r_q2"), "cT3z"], writes=[("cT3", c, 2)])

                for c0 in range(0, NT, 2):
                    recs = []
                    for p in range(2):
                        P.rec = []
                        router_tile(c0 + p, p)
                        recs.append(P.rec)
                        P.rec = None
                    for i in range(max(len(recs[0]), len(recs[1]))):
                        for p in range(2):
                            if i < len(recs[p]):
                                P.replay([recs[p][i]])
                P.barrier()
            if stage == 6:
                o = dout("cT3_dbg", [128, T], BF16)
                P.dma("sp", "out", [(o, cT3[:])])
                o = dout("h2T_dbg", [128, 8, T], BF16)
                P.dma("sp", "out", [(o, hT[:])])
                P.barrier()
                return nc, dbg

            NE = 32 if sub >= 30 else 2
            wstE = [sb(f"wstE{i}", [128, 8, 256], F32, sE) for i in range(2)]
            wgu = [sb(f"wgu{i}", [128, 8, 512], BF16, sE) for i in range(2)]
            wdn = [sb(f"wdn{i}", [128, 2, D], BF16, sE) for i in range(4)]
            actT = [sb(f"actT{i}", [128, 2, T], BF16, sE) for i in range(4)]
            sgs = [sb(f"sgs{i}", [128, 512], F32, sE) for i in range(2)]
            tms = [sb(f"tms{i}", [128, 512], F32, sE) for i in range(2)]
            stc = [0]
            itc = [0]
            def down_pair(e):
                for c in range(NT):
                    cs_ = slice(c * 128, (c + 1) * 128)
                    for hf in range(2):
                        bank = 5 + (2 * c + hf) % 2
                        n_ = 0
                        for ee in (e - 1, e):
                            for ft in range(2):
                                f = lambda: nc.tensor.matmul(ps[bank][:], lhsT=actT[ee % 4][:, ft, cs_], rhs=wdn[ee % 4][:, ft, hf * 512:(hf + 1) * 512],
                                                             start=(n_ == 0), stop=(n_ == 3))
                                rd = [(f"actT{ee % 4}", ft, c // 4), f"wdn{ee % 4}"]
                                if n_ < 3:
                                    P.quiet("pe", f, reads=rd, writes=[f"ps{bank}"])
                                else:
                                    P.op("pe", f, reads=rd, writes=[f"ps{bank}"])
                                n_ += 1
                        P.op("dve", lambda: nc.vector.tensor_tensor(out=x1[:, c, hf * 512:(hf + 1) * 512], in0=ps[bank][:],
                                                                    in1=x1[:, c, hf * 512:(hf + 1) * 512], op=ALU.add),
                             reads=[f"ps{bank}", ("x1", c)], writes=[("x1", c)])

            for e in range(NE):
                sl = e % 2
                dsl = e % 4
                asl = e % 4
                for (k_, src) in [(0, wg_d[e].rearrange("(kt p) n -> p kt n", p=128)), (1, wu_d[e].rearrange("(kt p) n -> p kt n", p=128))]:
                    si = stc[0] % 2
                    stc[0] += 1
                    P.dma("sp", f"wstE{si}", [(wstE[si][:], src)], writes=[f"wstE{si}"])
                    P.op("pool", lambda: nc.gpsimd.tensor_copy(out=wgu[sl][:, :, k_ * 256:(k_ + 1) * 256], in_=wstE[si][:]),
                         reads=[f"wstE{si}"], writes=[(f"wgu{sl}", k_)])
                si = stc[0] % 2
                stc[0] += 1
                P.dma("sp", f"wstE{si}", [(wstE[si][:].rearrange("p a b -> p (a b)").rearrange("p (f n) -> p f n", f=2),
                                           wd_d[e].rearrange("(ft p) n -> p ft n", p=128))],
                      writes=[f"wstE{si}"])
                P.op("pool", lambda: nc.gpsimd.tensor_copy(out=wdn[dsl][:].rearrange("p a b -> p (a b)"), in_=wstE[si][:].rearrange("p a b -> p (a b)")),
                     reads=[f"wstE{si}"], writes=[f"wdn{dsl}"])
                for tb in range(4):
                    ts_ = slice(tb * 512, (tb + 1) * 512)
                    P.op("pe", lambda: nc.tensor.matmul(ps[4][:], lhsT=selm[:, e, :], rhs=cT3[:, ts_], start=True, stop=True),
                         reads=["selm"], writes=["ps4"])
                    for ft in range(2):
                        par = itc[0] % 2
                        itc[0] += 1
                        bG, bU = (0, 1) if par == 0 else (2, 3)
                        for (bank, k_) in [(bG, 0), (bU, 1)]:
                            for kt in range(8):
                                f = lambda: nc.tensor.matmul(ps[bank][:], lhsT=wgu[sl][:, kt, k_ * 256 + ft * 128:k_ * 256 + (ft + 1) * 128],
                                                             rhs=hT[:, kt, ts_], start=(kt == 0), stop=(kt == 7))
                                if kt < 7:
                                    P.quiet("pe", f, reads=[(f"wgu{sl}", k_)], writes=[f"ps{bank}"])
                                else:
                                    P.op("pe", f, reads=[(f"wgu{sl}", k_)], writes=[f"ps{bank}"])
                        P.op("act", lambda: nc.scalar.activation(out=sgs[par][:], in_=ps[bG][:], func=AF.Silu), reads=[f"ps{bG}"], writes=[f"sgs{par}"])
                        P.op("dve", lambda: nc.vector.tensor_tensor(out=tms[par][:], in0=ps[bU][:], in1=sgs[par][:], op=ALU.mult),
                             reads=[f"ps{bU}", f"sgs{par}"], writes=[f"tms{par}"])
                        P.op("dve", lambda: nc.vector.tensor_tensor(out=actT[asl][:, ft, ts_], in0=ps[4][:], in1=tms[par][:], op=ALU.mult),
                             reads=["ps4", f"tms{par}"], writes=[(f"actT{asl}", ft, tb)])
                    if tb == 0 and e >= 2 and e % 2 == 0:
                        down_pair(e - 1)
            down_pair(NE - 1)
            for c in range(NT):
                P.dma("sp", "out", [(out_d[c * 128:(c + 1) * 128, :], x1[:, c, :])], reads=[("x1", c)])
            P.barrier()
    return nc, dbg


def host_consts():
    cst = np.zeros((128, CW), np.float32)
    cst[:, C_ID:C_ID + 128] = np.eye(128, dtype=np.float32)
    dm = np.ones((128, 128), np.float32)
    dm[0:64, 64:128] = 0.0
    cst[:, C_DM:C_DM + 128] = dm
    cst[:, C_NB:C_NB + 128] = (dm - 1.0) * 1e30
    cst[:, C_CT:C_CT + 128] = np.triu(np.ones((128, 128), np.float32))
    cst[:, C_ONE:C_ONE + 128] = 1.0
    for i in range(NBIS):
        cst[:, C_BIS + i] = 2.0 ** (-(i + 1))
    half = 8
    inv = 500000.0 ** (-np.arange(half, dtype=np.float64) * 2.0 / 16)
    ang = np.arange(T, dtype=np.float64)[:, None] * inv[None, :]
    cos, sin = np.cos(ang), np.sin(ang)
    rope = np.concatenate([cos, cos, -sin, sin], axis=1).astype(np.float32)
    return cst, rope


_CACHE = {}


def make_inmaps(inputs):
    cst, rope = host_consts()
    maps = []
    sq = lambda a: np.ascontiguousarray(np.asarray(a, np.float32)[0])
    shared = {
        "w_in": sq(inputs["w_in"]),
        "conv_w": sq(inputs["conv_w"]),
        "w_attn_branch": sq(inputs["w_attn_branch"]), "w_ssd_branch": sq(inputs["w_ssd_branch"]),
        "w_out": sq(inputs["w_out"]), "w_route_group": sq(inputs["w_route_group"]),
        "w_route_expert": sq(inputs["w_route_expert"]),
        "w_gate": sq(inputs["w_gate"]).reshape(32, 1024, 256), "w_up": sq(inputs["w_up"]).reshape(32, 1024, 256),
        "w_down": sq(inputs["w_down"]).reshape(32, 256, 1024),
        "cst": cst, "rope": rope,
    }
    for n in ["attn_norm", "q_norm", "k_norm", "idx_k_norm", "ffn_norm", "ssd_norm", "conv_b", "dt_bias", "a_log",
              "d_skip", "b_route_group", "b_route_expert"]:
        shared[n] = np.ascontiguousarray(np.asarray(inputs[n], np.float32).reshape(1, -1))
    x = np.asarray(inputs["x"], np.float32)
    for b in range(8):
        m = dict(shared)
        m["x"] = np.ascontiguousarray(x[b])
        maps.append(m)
    return maps


def kernel(**inputs):
    if "nc" not in _CACHE:
        _CACHE["nc"] = build()[0]
    nc = _CACHE["nc"]
    maps = make_inmaps(inputs)
    res = run_bass_kernel_spmd(nc, maps, core_ids=list(range(8)))
    return np.stack([np.asarray(r["out"], np.float32) for r in res.results], axis=0)
```
